# Optimizing a Trainium2 kernel written in Bass

```python
import math
import jax, jax.numpy as jnp
from jax import lax
import numpy as np

D_MODEL = 1024
BATCH = 2
SEQ = 8192
DEPTH = 1

N_META = 16
GRID_W = 64
Q_BLOCK = 128
EPS = 1e-6

A_HEADS = 8
A_HEAD_DIM = 64
A_QK_WIDTH = A_HEADS * 2 * A_HEAD_DIM
A_V_WIDTH = A_HEADS * 2 * A_HEAD_DIM

B_HEADS = 8
B_KV_HEADS = 2
B_GROUP = B_HEADS // B_KV_HEADS
B_HEAD_DIM = 128
B_Q_WIDTH = B_HEADS * B_HEAD_DIM
B_KV_WIDTH = B_KV_HEADS * B_HEAD_DIM
ROPE_THETA = 10000.0

SPLITS = (A_QK_WIDTH, A_QK_WIDTH, A_V_WIDTH, B_Q_WIDTH, B_KV_WIDTH, B_KV_WIDTH, D_MODEL, D_MODEL)
IN_WIDTH = sum(SPLITS)

N_EXPERTS = 16
EXPERT_FF = 512
CAPACITY_FACTOR = 2

kernel_name = 'hybrid_diffattn_axialgqa_ecmoe_encoder'


def rms_norm(x, g):
    xf = x.astype(jnp.float32)
    y = xf * lax.rsqrt(jnp.mean(xf * xf, axis=-1, keepdims=True) + EPS)
    return (y * g.astype(jnp.float32)).astype(x.dtype)


def sweep_queries(block_fn, *qs):
    out_meta = block_fn(*[a[:, :N_META] for a in qs])

    def to_blocks(a):
        r = a[:, N_META:]
        n = r.shape[1] // Q_BLOCK
        r = r.reshape((r.shape[0], n, Q_BLOCK) + r.shape[2:])
        return jnp.moveaxis(r, 1, 0)

    out_real = lax.map(lambda args: block_fn(*args), tuple(to_blocks(a) for a in qs))
    out_real = jnp.moveaxis(out_real, 0, 1)
    out_real = out_real.reshape((out_real.shape[0], -1) + out_real.shape[3:])
    return jnp.concatenate([out_meta, out_real], axis=1)


def alibi_slopes(n_heads):
    return jnp.exp2(-8.0 * jnp.arange(1, n_heads + 1, dtype=jnp.float32) / n_heads)


def diff_attention(q, k, v, pos, slopes, lam):
    scale = A_HEAD_DIM ** -0.5
    k_meta = pos < N_META

    def block(qb, pb):
        qp = pb[0]
        s = jnp.einsum('bqhmd,bkhmd->bhmqk', qb, k).astype(jnp.float32) * scale
        dist = jnp.abs(qp[:, None] - pos[None, :]).astype(jnp.float32)
        dist = jnp.where((qp[:, None] < N_META) | k_meta[None, :], 0.0, dist)
        s = s - slopes[None, :, None, None, None] * dist
        p = jax.nn.softmax(s, axis=-1)
        w = p[:, :, 0] - lam * p[:, :, 1]
        return jnp.einsum('bhqk,bkhe->bqhe', w.astype(v.dtype), v)

    return sweep_queries(block, q, pos[None])


def gqa_attention(q, k, v):
    scale = B_HEAD_DIM ** -0.5

    def block(qb):
        s = jnp.einsum('bqngd,bknd->bngqk', qb, k).astype(jnp.float32) * scale
        p = jax.nn.softmax(s, axis=-1)
        return jnp.einsum('bngqk,bknd->bqngd', p.astype(v.dtype), v)

    return sweep_queries(block, q)


def axial_angles(total_len):
    n_tok = total_len - N_META
    rows = n_tok // GRID_W
    row_id = jnp.repeat(jnp.arange(rows, dtype=jnp.int32), GRID_W)
    col_id = jnp.tile(jnp.arange(GRID_W, dtype=jnp.int32), rows)
    zeros = jnp.zeros((N_META,), jnp.int32)
    row_id = jnp.concatenate([zeros, row_id])
    col_id = jnp.concatenate([zeros, col_id])
    half = B_HEAD_DIM // 2
    inv_freq = ROPE_THETA ** (-jnp.arange(0, half, 2, dtype=jnp.float32) / half)
    return (row_id.astype(jnp.float32)[:, None] * inv_freq[None, :],
            col_id.astype(jnp.float32)[:, None] * inv_freq[None, :])


def rotate(x, ang):
    m = ang.shape[-1]
    cos = jnp.cos(ang)[None, :, None, :].astype(x.dtype)
    sin = jnp.sin(ang)[None, :, None, :].astype(x.dtype)
    x1, x2 = x[..., :m], x[..., m:]
    return jnp.concatenate([x1 * cos - x2 * sin, x2 * cos + x1 * sin], axis=-1)


def axial_rope(x, ang_row, ang_col):
    half = x.shape[-1] // 2
    return jnp.concatenate([rotate(x[..., :half], ang_row), rotate(x[..., half:], ang_col)], axis=-1)


def expert_choice_ffn(u, w_router, w_gate, w_up, w_down):
    b, L, d = u.shape
    cap = CAPACITY_FACTOR * L // N_EXPERTS
    logits = jnp.einsum('bld,de->ble', u, w_router).astype(jnp.float32)
    aff = jax.nn.softmax(logits, axis=-1)
    top_aff, top_idx = lax.top_k(jnp.swapaxes(aff, 1, 2), cap)
    xs = jax.vmap(lambda ub, ib: ub[ib])(u, top_idx)
    hid = jax.nn.silu(jnp.einsum('becd,edf->becf', xs, w_gate)) * jnp.einsum('becd,edf->becf', xs, w_up)
    y = jnp.einsum('becf,efd->becd', hid, w_down) * top_aff[..., None].astype(u.dtype)
    return jax.vmap(lambda yb, ib: jnp.zeros((L, d), yb.dtype).at[ib.reshape(-1)].add(yb.reshape(-1, d)))(y, top_idx)


def setup_inputs(seed: int = 0) -> dict:
    key = jax.random.key(seed)
    ks = jax.random.split(key, 24)
    f32 = jnp.float32

    def nrm(k, shape, scale):
        return jax.random.normal(k, shape, f32) * scale

    def gain(k, shape):
        return 1.0 + 0.02 * jax.random.normal(k, shape, f32)

    return {
        'x': nrm(ks[0], (BATCH, SEQ, D_MODEL), 1.0),
        'meta_tokens': nrm(ks[1], (N_META, D_MODEL), 1.0),
        'g_mix': gain(ks[2], (DEPTH, D_MODEL)),
        'w_in': nrm(ks[3], (DEPTH, D_MODEL, IN_WIDTH), D_MODEL ** -0.5),
        'lambda_q1': nrm(ks[4], (DEPTH, A_HEAD_DIM), 0.1),
        'lambda_k1': nrm(ks[5], (DEPTH, A_HEAD_DIM), 0.1),
        'lambda_q2': nrm(ks[6], (DEPTH, A_HEAD_DIM), 0.1),
        'lambda_k2': nrm(ks[7], (DEPTH, A_HEAD_DIM), 0.1),
        'g_subln': gain(ks[8], (DEPTH, 2 * A_HEAD_DIM)),
        'g_qnorm': gain(ks[9], (DEPTH, B_HEAD_DIM)),
        'g_knorm': gain(ks[10], (DEPTH, B_HEAD_DIM)),
        'w_branch_a': nrm(ks[11], (DEPTH, A_V_WIDTH, D_MODEL), A_V_WIDTH ** -0.5),
        'w_branch_b': nrm(ks[12], (DEPTH, B_Q_WIDTH, D_MODEL), B_Q_WIDTH ** -0.5),
        'w_out': nrm(ks[13], (DEPTH, D_MODEL, D_MODEL), D_MODEL ** -0.5),
        'g_ffn': gain(ks[14], (DEPTH, D_MODEL)),
        'w_router': nrm(ks[15], (DEPTH, D_MODEL, N_EXPERTS), D_MODEL ** -0.5),
        'w_gate': nrm(ks[16], (DEPTH, N_EXPERTS, D_MODEL, EXPERT_FF), D_MODEL ** -0.5),
        'w_up': nrm(ks[17], (DEPTH, N_EXPERTS, D_MODEL, EXPERT_FF), D_MODEL ** -0.5),
        'w_down': nrm(ks[18], (DEPTH, N_EXPERTS, EXPERT_FF, D_MODEL), EXPERT_FF ** -0.5),
        'g_final': gain(ks[19], (D_MODEL,)),
    }


def reference(x, meta_tokens, g_mix, w_in, lambda_q1, lambda_k1, lambda_q2, lambda_k2,
              g_subln, g_qnorm, g_knorm, w_branch_a, w_branch_b, w_out, g_ffn,
              w_router, w_gate, w_up, w_down, g_final):
    b = x.shape[0]
    meta = jnp.broadcast_to(meta_tokens[None].astype(x.dtype), (b, N_META, x.shape[-1]))
    h = jnp.concatenate([meta, x], axis=1)
    L = h.shape[1]
    pos = jnp.arange(L, dtype=jnp.int32)
    ang_row, ang_col = axial_angles(L)
    slopes = alibi_slopes(A_HEADS)
    cut = list(np.cumsum(SPLITS)[:-1])

    for layer in range(DEPTH):
        u = rms_norm(h, g_mix[layer])
        z = jnp.einsum('bld,df->blf', u, w_in[layer])
        qa, ka, va, qb, kb, vb, ga, gb = jnp.split(z, cut, axis=-1)

        lam_init = 0.8 - 0.6 * math.exp(-0.3 * layer)
        lam = (jnp.exp(jnp.sum(lambda_q1[layer].astype(jnp.float32) * lambda_k1[layer].astype(jnp.float32)))
               - jnp.exp(jnp.sum(lambda_q2[layer].astype(jnp.float32) * lambda_k2[layer].astype(jnp.float32)))
               + lam_init)
        oa = diff_attention(qa.reshape(b, L, A_HEADS, 2, A_HEAD_DIM),
                            ka.reshape(b, L, A_HEADS, 2, A_HEAD_DIM),
                            va.reshape(b, L, A_HEADS, 2 * A_HEAD_DIM), pos, slopes, lam)
        oa = (rms_norm(oa, g_subln[layer]) * (1.0 - lam_init)).reshape(b, L, A_V_WIDTH)

        qh = axial_rope(rms_norm(qb.reshape(b, L, B_HEADS, B_HEAD_DIM), g_qnorm[layer]), ang_row, ang_col)
        kh = axial_rope(rms_norm(kb.reshape(b, L, B_KV_HEADS, B_HEAD_DIM), g_knorm[layer]), ang_row, ang_col)
        ob = gqa_attention(qh.reshape(b, L, B_KV_HEADS, B_GROUP, B_HEAD_DIM), kh,
                           vb.reshape(b, L, B_KV_HEADS, B_HEAD_DIM)).reshape(b, L, B_Q_WIDTH)

        ya = jnp.einsum('blf,fd->bld', oa, w_branch_a[layer])
        yb = jnp.einsum('blf,fd->bld', ob, w_branch_b[layer])
        merged = jax.nn.sigmoid(ga) * ya + jax.nn.sigmoid(gb) * yb
        h = h + jnp.einsum('bld,de->ble', merged, w_out[layer])

        h = h + expert_choice_ffn(rms_norm(h, g_ffn[layer]), w_router[layer], w_gate[layer],
                                  w_up[layer], w_down[layer])

    return rms_norm(h, g_final)[:, N_META:]
```

```python
import numpy as np
import ml_dtypes
from contextlib import ExitStack
import concourse.bass as bass
import concourse.mybir as mybir
from concourse.bass_utils import run_bass_kernel_spmd

F32 = mybir.dt.float32
BF16 = mybir.dt.bfloat16
AF = mybir.ActivationFunctionType
ALU = mybir.AluOpType
AX = mybir.AxisListType

D = 1024
SEQ = 8192
NMETA = 16
NT = SEQ + NMETA
NOWN = 2048 + NMETA
NKB = 65
EPS = 1e-6
NEXP = 16
CAP = 2 * NT // NEXP
LAM_INIT = 0.2
NBIS = 28


import types


def _freeze(fn):
    if fn is None or fn.__closure__ is None:
        return fn
    cells = []
    for c in fn.__closure__:
        try:
            cells.append(types.CellType(c.cell_contents))
        except ValueError:
            cells.append(c)
    return types.FunctionType(fn.__code__, fn.__globals__, fn.__name__, fn.__defaults__, tuple(cells))


class Res:
    __slots__ = ("name", "w", "rd", "multi", "psum")

    def __init__(self, name, multi=False, psum=False):
        self.name = name
        self.w = {}
        self.rd = {}
        self.multi = multi
        self.psum = psum


class DSem:
    def __init__(self, sem):
        self.sem = sem
        self.count = 0


class Sched:
    ENGS = ("pe", "act", "dve", "pool", "sp")

    def __init__(self):
        self.prog = {e: [] for e in self.ENGS}
        self.cnt = {e: 0 for e in self.ENGS}
        self.sem = {}
        self.known = {e: {} for e in self.ENGS}
        self.all_dsems = []

    def _deps(self, eng, reads, writes):
        deps = {}
        known = self.known[eng]

        def add(tok, kind):
            sem, val, e = tok
            if e == eng and eng in ("pe", "sp"):
                return
            k = id(sem)
            if known.get(k, 0) >= val:
                return
            if k not in deps or deps[k][1] < val:
                deps[k] = (sem, val)

        for r in reads:
            for tok in r.w.values():
                add(tok, "raw")
            if r.psum:
                for tok in r.rd.values():
                    if tok[2] != eng:
                        add(tok, "rar")
        for w in writes:
            if not w.multi:
                for tok in w.w.values():
                    add(tok, "waw")
            for tok in w.rd.values():
                add(tok, "war")
        for k, (sem, val) in deps.items():
            known[k] = val
        return list(deps.values())

    def _record(self, tok, reads, writes):
        k = id(tok[0])
        for r in reads:
            r.rd[k] = tok
        for w in writes:
            if w.multi:
                w.w[k] = tok
            else:
                w.w = {k: tok}
                w.rd = {}

    def op(self, eng, fn, reads=(), writes=(), inc=True):
        deps = self._deps(eng, reads, writes)
        tok = (self.sem[eng], self.cnt[eng] + 1, eng)
        self._record(tok, reads, writes)
        if inc:
            self.cnt[eng] += 1
        self.prog[eng].append((deps, _freeze(fn), self.sem[eng] if inc else None, 1))

    def dma(self, q, fn, ds, reads=(), writes=()):
        deps = self._deps(q, reads, writes)
        ds.count += 16
        tok = (ds.sem, ds.count, None)
        self._record(tok, reads, writes)
        self.prog[q].append((deps, _freeze(fn), ds.sem, 16))

    def coll(self, fn, ds, reads=(), writes=()):
        deps = self._deps("pool", reads, writes)
        ds.count += 1
        tok = (ds.sem, ds.count, None)
        self._record(tok, reads, writes)
        self.prog["pool"].append((deps, _freeze(fn), ds.sem, None))

    def barrier(self):
        toks = []
        for e in self.ENGS:
            if e == "sp":
                continue
            if self.cnt[e] > 0:
                toks.append((self.sem[e], self.cnt[e]))
        for ds in self.all_dsems:
            if ds.count > 0:
                toks.append((ds.sem, ds.count))
        for e in self.ENGS:
            deps = []
            for sem, val in toks:
                if self.known[e].get(id(sem), 0) < val:
                    self.known[e][id(sem)] = val
                    deps.append((sem, val))
            if deps:
                self.prog[e].append((deps, None, None, 0))

    def emit(self, eng, handle):
        for deps, fn, sem, incv in self.prog[eng]:
            for s, v in deps:
                handle.wait_ge(s, v)
            if fn is None:
                continue
            ins = fn(handle)
            if sem is not None:
                if incv is None:
                    ins.then_inc(sem)
                else:
                    ins.then_inc(sem, incv)


def build(dbg=False, stop_after=99):
    nc = bass.Bass("TRN2", target_bir_lowering=False)
    S = Sched()

    def din(name, shape, dt=F32):
        return nc.dram_tensor(name, list(shape), dt, kind="ExternalInput").ap()

    def dscr(name, shape, dt):
        if dbg:
            return nc.dram_tensor(name, list(shape), dt, kind="ExternalOutput").ap()
        return nc.dram_tensor(name, list(shape), dt).ap()

    hx = din("hx", [NT, D])
    w_in = din("w_in", [D, 6656])
    gmixT_d = din("gmixT", [128, 8])
    lam_d = din("lamv", [4, 64])
    gsub_d = din("g_subln", [1, 128])
    gq_d = din("g_qnorm", [1, 128])
    gk_d = din("g_knorm", [1, 128])
    wa_d = din("w_branch_a", [D, D])
    wb_d = din("w_branch_b", [D, D])
    wo_d = din("w_out", [D, D])
    gffn_d = din("g_ffn", [1, D])
    wr_d = din("w_router", [D, NEXP])
    if stop_after >= 5:
        wg_d = din("w_gate", [NEXP, D, 512])
        wu_d = din("w_up", [NEXP, D, 512])
        wd_d = din("w_down", [NEXP, 512, D])
    gfin_d = din("g_final", [1, D])
    sigk_d = din("sigk", [4, NT], BF16)
    qaug_d = din("qaug", [8, 4, NOWN], BF16)
    beta_d = din("beta", [128, 8 * 5 * NKB])
    dtab_d = din("dtab", [128, 896])
    rope_d = din("rope", [NT, 256])
    identb_d = din("identb", [128, 128], BF16)
    identf_d = din("identf", [128, 128])
    y_out = nc.dram_tensor("y", [2048, D], F32, kind="ExternalOutput").ap()

    KAs = dscr("KAs", [8, 128, NT], BF16)
    QAs = dscr("QAs", [8, 128, NOWN], BF16)
    VAs = dscr("VAs", [8, 128, NKB, 128], BF16)
    KBs = dscr("KBs", [2, 128, NT], BF16)
    QBs = dscr("QBs", [8, 128, NOWN], BF16)
    VBs = dscr("VBs", [2, 128, NKB, 128], BF16)
    Gs = dscr("Gs", [NOWN, 2048], BF16)
    OAs = dscr("OAs", [NOWN, D], BF16)
    OBs = dscr("OBs", [NOWN, D], BF16)
    agin = nc.dram_tensor("agin", [NEXP, NOWN], F32)
    agout = nc.dram_tensor("agout", [4 * NEXP, NOWN], F32)
    thr_d = nc.dram_tensor("thr_d", [1, NEXP], F32).ap()
    dbg_aff = dscr("dbg_aff", [NOWN, NEXP], F32) if dbg else None
    dbg_h1 = dscr("dbg_h1", [NOWN, D], F32) if dbg else None
    dbg_thr = dscr("dbg_thr", [1, NEXP], F32) if dbg else None

    rKAs, rQAs, rVAs = Res("KAs", True), Res("QAs", True), Res("VAs", True)
    rKBs, rQBs, rVBs = Res("KBs", True), Res("QBs", True), Res("VBs", True)
    rGs, rOAs, rOBs = Res("Gs", True), Res("OAs", True), Res("OBs", True)

    with ExitStack() as top:
        def sem(name):
            return top.enter_context(nc.semaphore(name))

        for e in Sched.ENGS:
            S.sem[e] = sem("sem_" + e)

        def dsem(name):
            d = DSem(sem(name))
            S.all_dsems.append(d)
            return d

        def sb(es, name, shape, dt):
            return es.enter_context(nc.sbuf_tensor("s_" + name, list(shape), dt))

        def psum(es, name, shape, dt):
            return es.enter_context(nc.psum_tensor("p_" + name, list(shape), dt))

        block = top.enter_context(nc.Block())

        def V(fn, r=(), w=()):
            S.op("dve", fn, r, w)

        def A(fn, r=(), w=()):
            S.op("act", fn, r, w)

        def P(fn, r=(), w=(), inc=True):
            S.op("pe", fn, r, w, inc)

        def G(fn, r=(), w=()):
            S.op("pool", fn, r, w)

        def DMA(out, in_, ds, r=(), w=(), q="sp"):
            S.dma(q, (lambda e, o=out, i=in_: e.dma_start(out=o, in_=i)), ds, r, w)

        identb = sb(top, "identb", [128, 128], BF16)
        identf = sb(top, "identf", [128, 128], F32)
        epsT = sb(top, "epsT", [128, 1], F32)
        rC = Res("consts", True)
        dsDbg = dsem("ds_dbg")
        dsC = dsem("ds_const")
        DMA(identb[:, :], identb_d[:, :], dsC, w=[rC])
        DMA(identf[:, :], identf_d[:, :], dsC, w=[rC])
        V(lambda e: e.memset(epsT[:, :], EPS), w=[rC])

        def rstd_from_ss(ss_ap, ln_ap, out_ap, n, rs, ws):
            A(lambda e: e.activation(out=ln_ap, in_=ss_ap, func=AF.Ln,
                                     bias=epsT[:ln_ap.shape[0], 0:1], scale=1.0 / n),
              r=rs + [rC], w=ws)
            A(lambda e: e.activation(out=out_ap, in_=ln_ap, func=AF.Exp, scale=-0.5),
              r=ws, w=ws)

        STS = [(t * 512, 512, 4, 128) for t in range(16)] + [(SEQ, NMETA, 1, NMETA)]

        def is_own(st):
            return st < 4 or st == 16

        def q0_of(st):
            return st * 512 if st < 4 else 2048

        with ExitStack() as p1:
            uT_own = sb(p1, "uT_own", [128, 8, NOWN], BF16)
            rUown = [Res(f"uTown{i}") for i in range(5)]
            gmixT = sb(p1, "gmixT", [128, 8], F32)
            gqb = sb(p1, "gqb", [128, 128], F32)
            gkb = sb(p1, "gkb", [128, 128], F32)
            dsC1 = dsem("ds_c1")
            DMA(gmixT[:, :], gmixT_d[:, :], dsC1, w=[rC])
            DMA(gqb[:, :], gq_d[0:1, :].partition_broadcast(128), dsC1, w=[rC])
            DMA(gkb[:, :], gk_d[0:1, :].partition_broadcast(128), dsC1, w=[rC])

            def norm_rope(raw, H, psub, gb, cs_t, s, out_bf, scr, rs_extra, r_raw, r_scr, r_out):
                sq, ssk, lnk, rk, tmp, yy = scr
                V(lambda e: e.tensor_tensor(out=sq[:psub, :H * 128], in0=raw[:psub, :H * 128],
                                            in1=raw[:psub, :H * 128], op=ALU.mult),
                  r=[r_raw], w=[r_scr])
                V(lambda e: e.tensor_reduce(out=ssk[:psub, :H],
                                            in_=sq[:psub, :H * 128].rearrange("p (h d) -> p h d", h=H),
                                            axis=AX.X, op=ALU.add), r=[r_scr], w=[r_scr])
                rstd_from_ss(ssk[:psub, :H], lnk[:psub, :H], rk[:psub, :H], 128.0, [r_scr], [r_scr])
                for j in range(H):
                    yj = yy[:psub, j * 128:(j + 1) * 128]
                    V(lambda e, j=j, yj=yj: e.scalar_tensor_tensor(
                        out=yj, in0=raw[:psub, j * 128:(j + 1) * 128], scalar=rk[:psub, j:j + 1],
                        in1=gb[:psub, :], op0=ALU.mult, op1=ALU.mult), r=[r_raw, r_scr, rC], w=[r_scr])
                    y4 = yj.rearrange("p (a b c) -> p a b c", a=2, b=2)
                    t4 = tmp[:psub, j * 128:(j + 1) * 128].rearrange("p (a b c) -> p a b c", a=2, b=2)
                    sn4 = cs_t[:psub, s, 128:256].rearrange("p (a b c) -> p a b c", a=2, b=2)
                    V(lambda e, y4=y4, t4=t4, sn4=sn4: e.tensor_tensor(
                        out=t4[:, :, 0, :], in0=y4[:, :, 1, :], in1=sn4[:, :, 0, :], op=ALU.mult),
                      r=[r_scr] + rs_extra, w=[r_scr])
                    V(lambda e, y4=y4, t4=t4, sn4=sn4: e.tensor_tensor(
                        out=t4[:, :, 1, :], in0=y4[:, :, 0, :], in1=sn4[:, :, 1, :], op=ALU.mult),
                      r=[r_scr] + rs_extra, w=[r_scr])
                    V(lambda e, yj=yj: e.tensor_tensor(
                        out=yj, in0=yj, in1=cs_t[:psub, s, 0:128], op=ALU.mult),
                      r=[r_scr] + rs_extra, w=[r_scr])
                    V(lambda e, j=j, yj=yj: e.tensor_tensor(
                        out=out_bf[:psub, j * 128:(j + 1) * 128], in0=yj,
                        in1=tmp[:psub, j * 128:(j + 1) * 128], op=ALU.add),
                      r=[r_scr], w=[r_out])

            with ExitStack() as pa:
                wA = sb(pa, "wA", [128, 8, 2560], BF16)
                rWA = Res("wA", True)
                dsW = dsem("ds_w")
                w_in_v = w_in.rearrange("(c p) f -> p c f", p=128)
                for c in range(8):
                    DMA(wA[:, c, 0:2048], w_in_v[:, c, 1024:3072], dsW, w=[rWA], q="pool")
                    DMA(wA[:, c, 2048:2560], w_in_v[:, c, 4096:4608], dsW, w=[rWA], q="pool")
                xt = [sb(pa, f"xt{i}", [128, 4, D], F32) for i in range(2)]
                rXt = [Res(f"xt{i}", True) for i in range(2)]
                dsXc = [dsem(f"ds_xc{i}") for i in range(2)]
                dsX = [dsem(f"ds_x{i}") for i in range(2)]
                cs = [sb(pa, f"cs{i}", [128, 4, 256], F32) for i in range(2)]
                rCs = [Res(f"cs{i}", True) for i in range(2)]
                xs = sb(pa, "xs", [128, 4, D], BF16)
                rXs = Res("xs")
                junk = sb(pa, "junk", [128, D], BF16)
                rJunk = Res("junk")
                ss = sb(pa, "ss", [128, 4], F32)
                lnv = sb(pa, "lnv", [128, 4], F32)
                rstd = sb(pa, "rstd", [128, 4], F32)
                rSt = Res("stats")
                uT_tmp = [sb(pa, f"uTt{i}", [128, 8, 512], BF16) for i in range(2)]
                rUt = [Res(f"uTt{i}") for i in range(2)]
                stgK = [sb(pa, f"stgK{i}", [128, 512], BF16) for i in range(3)]
                rStgK = [Res(f"stgK{i}") for i in range(3)]
                dsStgK = [dsem(f"ds_sk{i}") for i in range(3)]
                stgV = [sb(pa, f"stgV{i}", [128, 1024], BF16) for i in range(2)]
                rStgV = [Res(f"stgV{i}") for i in range(2)]
                dsStgV = [dsem(f"ds_sv{i}") for i in range(2)]
                stgVB = [sb(pa, f"stgVB{i}", [128, 256], BF16) for i in range(2)]
                rStgVB = [Res(f"stgVB{i}") for i in range(2)]
                dsStgVB = [dsem(f"ds_svb{i}") for i in range(2)]
                kraw = sb(pa, "kraw", [128, 256], F32)
                rKraw = Res("kraw")
                ksc = (sb(pa, "ksq", [128, 256], F32), sb(pa, "kss", [128, 2], F32),
                       sb(pa, "kln", [128, 2], F32), sb(pa, "krk", [128, 2], F32),
                       sb(pa, "ktmp", [128, 256], F32), sb(pa, "kyy", [128, 256], F32))
                rKsc = Res("ksc")
                krope = sb(pa, "krope", [128, 4, 256], BF16)
                rKrope = Res("krope")
                stgKB = sb(pa, "stgKB", [128, 2, 512], BF16)
                rStgKB = Res("stgKB")
                dsStgKB = dsem("ds_skb")
                tpb = [psum(pa, f"tpb{i}", [128, 1024], BF16) for i in range(4)]
                rTp = [Res(f"tpb{i}", psum=True) for i in range(4)]
                acc = [psum(pa, f"acc{i}", [128, 512], F32) for i in range(3)]
                rAcc = [Res(f"acc{i}", psum=True) for i in range(3)]
                ktp = psum(pa, "ktp", [128, 1024], BF16)
                rKtp = Res("ktp", psum=True)
                acc_i = [0]

                def next_acc():
                    i = acc_i[0] % 3
                    acc_i[0] += 1
                    return acc[i], rAcc[i]

                def load_x(sti):
                    tok0, ntok, nsub, psub = STS[sti]
                    sl = sti % 2
                    DMA(xt[sl][:psub, 0:nsub, :],
                        hx[tok0:tok0 + ntok, :].rearrange("(s p) d -> p s d", p=psub),
                        dsX[sl], w=[rXt[sl]])
                    DMA(cs[sl][:psub, 0:nsub, :],
                        rope_d[tok0:tok0 + ntok, :].rearrange("(s p) d -> p s d", p=psub),
                        dsXc[sl], w=[rCs[sl]])

                load_x(0)
                kcount = [0]
                vcount = [0]
                import os
                DBGL = os.environ.get("KDBG", "")
                for sti, (tok0, ntok, nsub, psub) in enumerate(STS):
                    sl = sti % 2
                    if "one" in DBGL and sti >= 1:
                        break
                    if sti + 1 < len(STS) and "one" not in DBGL:
                        load_x(sti + 1)
                    x_t = xt[sl]
                    for s in range(nsub):
                        A(lambda e, s=s: e.activation(out=junk[:psub, :], in_=x_t[:psub, s, :],
                                                      func=AF.Square, accum_out=ss[:psub, s:s + 1]),
                          r=[rXt[sl]], w=[rJunk, rSt])
                    rstd_from_ss(ss[:psub, :nsub], lnv[:psub, :nsub], rstd[:psub, :nsub], float(D),
                                 [rSt], [rSt])
                    for s in range(nsub):
                        V(lambda e, s=s: e.tensor_scalar(out=xs[:psub, s, :], in0=x_t[:psub, s, :],
                                                         scalar1=rstd[:psub, s:s + 1], scalar2=None,
                                                         op0=ALU.mult),
                          r=[rXt[sl], rSt], w=[rXs])
                    own = is_own(sti)
                    if own:
                        q0 = q0_of(sti)
                        uT = uT_own
                        ucol = q0
                        rU = rUown[sti if sti < 4 else 4]
                    else:
                        uT = uT_tmp[sl]
                        ucol = 0
                        rU = rUt[sl]
                    for dc in range(8):
                        bank = dc // 2
                        half = dc % 2
                        for s in range(nsub):
                            P(lambda e, dc=dc, s=s, bank=bank, half=half: e.transpose(
                                out=tpb[bank][:, half * 512 + s * 128: half * 512 + s * 128 + psub],
                                in_=xs[:psub, s, dc * 128:(dc + 1) * 128],
                                identity=identb[:psub, :psub]),
                              r=[rXs, rC], w=[rTp[bank]], inc=(s == nsub - 1))
                        src = tpb[bank][:, half * 512: half * 512 + ntok]
                        dst = uT[:, dc, ucol:ucol + ntok]
                        if bank % 2 == 0:
                            V(lambda e, src=src, dst=dst, dc=dc: e.tensor_scalar(
                                out=dst, in0=src, scalar1=gmixT[:, dc:dc + 1], scalar2=None, op0=ALU.mult),
                              r=[rTp[bank], rC], w=[rU])
                        else:
                            A(lambda e, src=src, dst=dst, dc=dc: e.activation(
                                out=dst, in_=src, func=AF.Copy, scale=gmixT[:, dc:dc + 1]),
                              r=[rTp[bank], rC], w=[rU])
                    if "noproj" in DBGL:
                        continue
                    for h in range(8 if "nokA" not in DBGL else 0):
                        a_t, rA = next_acc()
                        for dc in range(8):
                            P(lambda e, a_t=a_t, dc=dc, h=h: e.matmul(
                                a_t[:, :ntok], lhsT=wA[:, dc, h * 128:(h + 1) * 128],
                                rhs=uT[:, dc, ucol:ucol + ntok], start=(dc == 0), stop=(dc == 7)),
                              r=[rWA, rU], w=[rA], inc=(dc == 7))
                        ks = kcount[0] % 3
                        kcount[0] += 1
                        A(lambda e, a_t=a_t, ks=ks: e.activation(out=stgK[ks][:, :ntok], in_=a_t[:, :ntok],
                                                                 func=AF.Copy),
                          r=[rA], w=[rStgK[ks]])
                        DMA(KAs[h, :, tok0:tok0 + ntok], stgK[ks][:, :ntok], dsStgK[ks],
                            r=[rStgK[ks]], w=[rKAs])
                    if "notok" in DBGL:
                        continue
                    for s in range(nsub):
                        blk = (tok0 // 128) + s
                        vs = vcount[0] % 2
                        vcount[0] += 1
                        for g in range(2):
                            a_t, rA = next_acc()
                            for dc in range(8):
                                P(lambda e, a_t=a_t, dc=dc, g=g, s=s: e.matmul(
                                    a_t[:psub, :], lhsT=uT[:, dc, ucol + s * 128: ucol + s * 128 + psub],
                                    rhs=wA[:, dc, 1024 + g * 512: 1024 + (g + 1) * 512],
                                    start=(dc == 0), stop=(dc == 7)),
                                  r=[rWA, rU], w=[rA], inc=(dc == 7))
                            if g == 0:
                                A(lambda e, a_t=a_t, vs=vs: e.activation(
                                    out=stgV[vs][:psub, 0:512], in_=a_t[:psub, :], func=AF.Copy),
                                  r=[rA], w=[rStgV[vs]])
                            else:
                                V(lambda e, a_t=a_t, vs=vs: e.tensor_copy(
                                    out=stgV[vs][:psub, 512:1024], in_=a_t[:psub, :]),
                                  r=[rA], w=[rStgV[vs]])
                        if "novst" not in DBGL:
                          DMA(VAs[:, 0:psub, blk, :].rearrange("h p e -> p h e"),
                            stgV[vs][:psub, :].rearrange("p (h e) -> p h e", h=8),
                            dsStgV[vs], r=[rStgV[vs]], w=[rVAs])
                        a_t, rA = next_acc()
                        for dc in range(8):
                            P(lambda e, a_t=a_t, dc=dc, s=s: e.matmul(
                                a_t[:psub, :], lhsT=uT[:, dc, ucol + s * 128: ucol + s * 128 + psub],
                                rhs=wA[:, dc, 2048:2560], start=(dc == 0), stop=(dc == 7)),
                              r=[rWA, rU], w=[rA], inc=(dc == 7))
                        A(lambda e, a_t=a_t: e.activation(out=kraw[:psub, :], in_=a_t[:psub, 0:256],
                                                          func=AF.Copy), r=[rA], w=[rKraw])
                        A(lambda e, a_t=a_t, vs=vs: e.activation(out=stgVB[vs][:psub, :],
                                                                 in_=a_t[:psub, 256:512], func=AF.Copy),
                          r=[rA], w=[rStgVB[vs]])
                        if "novst" not in DBGL:
                          DMA(VBs[:, 0:psub, blk, :].rearrange("h p e -> p h e"),
                            stgVB[vs][:psub, :].rearrange("p (h e) -> p h e", h=2),
                            dsStgVB[vs], r=[rStgVB[vs]], w=[rVBs])
                        if "norope" in DBGL:
                            continue
                        norm_rope(kraw, 2, psub, gkb, cs[sl], s, krope[:, s, :], ksc,
                                  [rCs[sl]], rKraw, rKsc, rKrope)
                        for j in range(2):
                            P(lambda e, j=j, s=s: e.transpose(
                                out=ktp[:, j * 512 + s * 128: j * 512 + s * 128 + psub],
                                in_=krope[:psub, s, j * 128:(j + 1) * 128],
                                identity=identb[:psub, :psub]),
                              r=[rKrope, rC], w=[rKtp], inc=(j == 1))
                    if "norope" in DBGL:
                        continue
                    for j in range(2):
                        V(lambda e, j=j: e.tensor_copy(out=stgKB[:, j, :ntok],
                                                       in_=ktp[:, j * 512: j * 512 + ntok]),
                          r=[rKtp], w=[rStgKB])
                    for j in range(2):
                        DMA(KBs[j, :, tok0:tok0 + ntok], stgKB[:, j, :ntok], dsStgKB,
                            r=[rStgKB], w=[rKBs])
                S.barrier()

            if stop_after >= 1.5:
                with ExitStack() as pb:
                    wB = sb(pb, "wB", [128, 8, 4096], BF16)
                    rWB = Res("wB", True)
                    dsWB = dsem("ds_wb")
                    w_in_v = w_in.rearrange("(c p) f -> p c f", p=128)
                    for c in range(8):
                        DMA(wB[:, c, 0:1024], w_in_v[:, c, 0:1024], dsWB, w=[rWB], q="pool")
                        DMA(wB[:, c, 1024:2048], w_in_v[:, c, 3072:4096], dsWB, w=[rWB], q="pool")
                        DMA(wB[:, c, 2048:4096], w_in_v[:, c, 4608:6656], dsWB, w=[rWB], q="pool")
                    csB = [sb(pb, f"csB{i}", [128, 4, 256], F32) for i in range(2)]
                    rCsB = [Res(f"csB{i}", True) for i in range(2)]
                    dsCsB = [dsem(f"ds_csb{i}") for i in range(2)]
                    stgQ = [sb(pb, f"stgQ{i}", [128, 512], BF16) for i in range(3)]
                    rStgQ = [Res(f"stgQ{i}") for i in range(3)]
                    dsStgQ = [dsem(f"ds_sq{i}") for i in range(3)]
                    qraw = sb(pb, "qraw", [128, 1024], F32)
                    rQraw = Res("qraw")
                    qsc = (sb(pb, "qsq", [128, 1024], F32), sb(pb, "qss", [128, 8], F32),
                           sb(pb, "qln", [128, 8], F32), sb(pb, "qrk", [128, 8], F32),
                           sb(pb, "qtmp", [128, 1024], F32), sb(pb, "qyy", [128, 1024], F32))
                    rQsc = Res("qsc")
                    qrope = sb(pb, "qrope", [128, 1024], BF16)
                    rQrope = Res("qrope")
                    stgQB = [sb(pb, f"stgQB{i}", [128, 8, 128], BF16) for i in range(2)]
                    rStgQB = [Res(f"stgQB{i}") for i in range(2)]
                    dsStgQB = [dsem(f"ds_sqb{i}") for i in range(2)]
                    stgG = [sb(pb, f"stgG{i}", [128, 2048], BF16) for i in range(2)]
                    rStgG = [Res(f"stgG{i}") for i in range(2)]
                    dsStgG = [dsem(f"ds_sg{i}") for i in range(2)]
                    accB = [psum(pb, f"accB{i}", [128, 512], F32) for i in range(6)]
                    rAccB = [Res(f"accB{i}", psum=True) for i in range(6)]
                    qtp = psum(pb, "qtp", [128, 1024], BF16)
                    rQtp = Res("qtp", psum=True)
                    accb_i = [0]

                    def next_accB():
                        i = accb_i[0] % 6
                        accb_i[0] += 1
                        return accB[i], rAccB[i]

                    own_sts = [0, 1, 2, 3, 16]
                    qcount = [0]
                    bcount = [0]
                    for oi, sti in enumerate(own_sts):
                        tok0, ntok, nsub, psub = STS[sti]
                        q0 = q0_of(sti)
                        rU = rUown[oi]
                        sl = oi % 2
                        DMA(csB[sl][:psub, 0:nsub, :],
                            rope_d[tok0:tok0 + ntok, :].rearrange("(s p) d -> p s d", p=psub),
                            dsCsB[sl], w=[rCsB[sl]])
                        for h in range(8):
                            a_t, rA = next_accB()
                            for dc in range(8):
                                P(lambda e, a_t=a_t, dc=dc, h=h: e.matmul(
                                    a_t[:, :ntok], lhsT=wB[:, dc, h * 128:(h + 1) * 128],
                                    rhs=uT_own[:, dc, q0:q0 + ntok], start=(dc == 0), stop=(dc == 7)),
                                  r=[rWB, rU], w=[rA], inc=(dc == 7))
                            ks = qcount[0] % 3
                            qcount[0] += 1
                            A(lambda e, a_t=a_t, ks=ks: e.activation(out=stgQ[ks][:, :ntok],
                                                                     in_=a_t[:, :ntok], func=AF.Copy),
                              r=[rA], w=[rStgQ[ks]])
                            DMA(QAs[h, :, q0:q0 + ntok], stgQ[ks][:, :ntok], dsStgQ[ks],
                                r=[rStgQ[ks]], w=[rQAs])
                        for s in range(nsub):
                            bs = bcount[0] % 2
                            bcount[0] += 1
                            qs = q0 + s * 128
                            for g in range(2):
                                a_t, rA = next_accB()
                                for dc in range(8):
                                    P(lambda e, a_t=a_t, dc=dc, g=g, qs=qs: e.matmul(
                                        a_t[:psub, :], lhsT=uT_own[:, dc, qs:qs + psub],
                                        rhs=wB[:, dc, 1024 + g * 512: 1024 + (g + 1) * 512],
                                        start=(dc == 0), stop=(dc == 7)),
                                      r=[rWB, rU], w=[rA], inc=(dc == 7))
                                A(lambda e, a_t=a_t, g=g: e.activation(
                                    out=qraw[:psub, g * 512:(g + 1) * 512], in_=a_t[:psub, :], func=AF.Copy),
                                  r=[rA], w=[rQraw])
                            norm_rope(qraw, 8, psub, gqb, csB[sl], s, qrope, qsc,
                                      [rCsB[sl]], rQraw, rQsc, rQrope)
                            for j in range(8):
                                P(lambda e, j=j: e.transpose(
                                    out=qtp[:, j * 128: j * 128 + psub],
                                    in_=qrope[:psub, j * 128:(j + 1) * 128],
                                    identity=identb[:psub, :psub]),
                                  r=[rQrope, rC], w=[rQtp], inc=(j == 7))
                            V(lambda e, bs=bs: e.tensor_copy(
                                out=stgQB[bs][:, :, :psub],
                                in_=qtp[:, :].rearrange("p (g t) -> p g t", g=8)[:, :, :psub]),
                              r=[rQtp], w=[rStgQB[bs]])
                            DMA(QBs[:, :, qs:qs + psub].rearrange("g p t -> p g t"),
                                stgQB[bs][:, :, :psub], dsStgQB[bs], r=[rStgQB[bs]], w=[rQBs])
                            for g in range(4):
                                a_t, rA = next_accB()
                                for dc in range(8):
                                    P(lambda e, a_t=a_t, dc=dc, g=g, qs=qs: e.matmul(
                                        a_t[:psub, :], lhsT=uT_own[:, dc, qs:qs + psub],
                                        rhs=wB[:, dc, 2048 + g * 512: 2048 + (g + 1) * 512],
                                        start=(dc == 0), stop=(dc == 7)),
                                      r=[rWB, rU], w=[rA], inc=(dc == 7))
                                A(lambda e, a_t=a_t, g=g, bs=bs: e.activation(
                                    out=stgG[bs][:psub, g * 512:(g + 1) * 512], in_=a_t[:psub, :],
                                    func=AF.Sigmoid), r=[rA], w=[rStgG[bs]])
                            DMA(Gs[qs:qs + psub, :], stgG[bs][:psub, :], dsStgG[bs],
                                r=[rStgG[bs]], w=[rGs])
                    S.barrier()

        if stop_after >= 2:
            with ExitStack() as p2:
                KT = [[sb(p2, f"KT{s}_{m}", [128, NT], BF16) for m in range(2)] for s in range(2)]
                VT = [sb(p2, f"VT{s}", [128, NKB, 129], BF16) for s in range(2)]
                QT = [[sb(p2, f"QT{s}_{m}", [128, NOWN], BF16) for m in range(2)] for s in range(2)]
                rKV = [Res(f"KV{s}", True) for s in range(2)]
                dsC2 = dsem("ds_c2")
                dsSig = [dsem(f"ds_sig{i}") for i in range(2)]
                rSig = [Res(f"sig{i}", True) for i in range(2)]
                dsKV = [dsem(f"ds_kv{s}") for s in range(2)]
                beta = sb(p2, "beta", [128, 8 * 5 * NKB], F32)
                dtab = sb(p2, "dtab", [128, 896], F32)
                gsubb = sb(p2, "gsubb", [128, 128], F32)
                lamb = sb(p2, "lamb", [128, 4, 64], F32)
                lamt = sb(p2, "lamt", [128, 2, 64], F32)
                lame = sb(p2, "lame", [128, 2], F32)
                lamneg = sb(p2, "lamneg", [128, 1], F32)
                rLam = Res("lam")
                DMA(beta[:, :], beta_d[:, :], dsC2, w=[rC])
                DMA(dtab[:, :], dtab_d[:, :], dsC2, w=[rC])
                DMA(gsubb[:, :], gsub_d[0:1, :].partition_broadcast(128), dsC2, w=[rC])
                for i in range(4):
                    DMA(lamb[:, i, :], lam_d[i:i + 1, :].partition_broadcast(128), dsC2, w=[rC])
                for s in range(2):
                    for m in range(2):
                        DMA(KT[s][m][64:68, :], sigk_d[:, :], dsSig[s], w=[rSig[s]])
                    V(lambda e, s=s: e.memset(VT[s][:, :, 128:129], 1.0), w=[rSig[s]])
                V(lambda e: e.tensor_tensor(out=lamt[:, 0, :], in0=lamb[:, 0, :], in1=lamb[:, 1, :], op=ALU.mult),
                  r=[rC], w=[rLam])
                V(lambda e: e.tensor_tensor(out=lamt[:, 1, :], in0=lamb[:, 2, :], in1=lamb[:, 3, :], op=ALU.mult),
                  r=[rC], w=[rLam])
                V(lambda e: e.tensor_reduce(out=lame[:, 0:2], in_=lamt[:, :, :], axis=AX.X, op=ALU.add),
                  r=[rLam], w=[rLam])
                A(lambda e: e.activation(out=lame[:, 0:2], in_=lame[:, 0:2], func=AF.Exp), r=[rLam], w=[rLam])
                V(lambda e: e.tensor_tensor(out=lamneg[:, :], in0=lame[:, 1:2], in1=lame[:, 0:1], op=ALU.subtract),
                  r=[rLam], w=[rLam])
                V(lambda e: e.tensor_scalar(out=lamneg[:, :], in0=lamneg[:, :], scalar1=-LAM_INIT, scalar2=None,
                                            op0=ALU.add), r=[rLam], w=[rLam])
                V(lambda e: e.tensor_scalar(out=gsubb[:, :], in0=gsubb[:, :], scalar1=1.0 - LAM_INIT,
                                            scalar2=None, op0=ALU.mult), r=[rC], w=[rC])

                PT = [sb(p2, f"PT{i}", [128, 1024], BF16) for i in range(3)]
                rPT = [Res(f"PT{i}") for i in range(3)]
                smix = [sb(p2, f"smix{i}", [128, 1024], F32) for i in range(2)]
                rSmix = [Res(f"smix{i}") for i in range(2)]
                Sps = [psum(p2, f"Sps{i}", [128, 1024], F32) for i in range(2)]
                rSps = [Res(f"Sps{i}", psum=True) for i in range(2)]
                Ops = psum(p2, "Ops", [128, 1536], F32)
                rOps = Res("Ops", psum=True)
                ev = {n: sb(p2, "ev_" + n, shp, F32) for n, shp in
                      [("r1", [128, 8]), ("t2", [128, 128]), ("dd", [128, 128]), ("ssd", [128, 1]),
                       ("lnd", [128, 1]), ("rsd", [128, 1]), ("junk", [128, 128])]}
                rEv = Res("ev")
                stgO = [sb(p2, f"stgO{i}", [128, 4, 128], BF16) for i in range(2)]
                rStgO = [Res(f"stgO{i}") for i in range(2)]
                dsStgO = [dsem(f"ds_so{i}") for i in range(2)]

                def oacc(m, s, n=129):
                    i = m * 4 + s
                    off = (i // 3) * 512 + (i % 3) * 129
                    return Ops[:, off:off + n]

                jobs = [("A", h) for h in range(8)] + [("B", pi) for pi in range(4)]

                def load_job(ji):
                    kind, idx = jobs[ji]
                    s = ji % 2
                    if kind == "A":
                        for m in range(2):
                            for half in range(2):
                                c0, c1 = half * 4104, (half + 1) * 4104
                                DMA(KT[s][m][0:64, c0:c1], KAs[idx, m * 64:(m + 1) * 64, c0:c1], dsKV[s],
                                    r=[rKAs], w=[rKV[s]])
                            DMA(QT[s][m][0:64, :], QAs[idx, m * 64:(m + 1) * 64, :], dsKV[s],
                                r=[rQAs], w=[rKV[s]])
                            DMA(QT[s][m][64:68, :], qaug_d[idx, :, :], dsKV[s], w=[rKV[s]])
                        for q4 in range(4):
                            b0, b1 = q4 * 16, (q4 + 1) * 16
                            DMA(VT[s][:, b0:b1, 0:128], VAs[idx, :, b0:b1, :], dsKV[s], r=[rVAs], w=[rKV[s]])
                        DMA(VT[s][0:16, 64:65, 0:128], VAs[idx, 0:16, 64:65, :], dsKV[s], r=[rVAs], w=[rKV[s]])
                    else:
                        kv = idx // 2
                        for half in range(2):
                            c0, c1 = half * 4104, (half + 1) * 4104
                            DMA(KT[s][0][:, c0:c1], KBs[kv, :, c0:c1], dsKV[s], r=[rKBs], w=[rKV[s], rSig[s]])
                        for m in range(2):
                            DMA(QT[s][m][:, :], QBs[2 * idx + m, :, :], dsKV[s], r=[rQBs], w=[rKV[s]])
                        for q4 in range(4):
                            b0, b1 = q4 * 16, (q4 + 1) * 16
                            DMA(VT[s][:, b0:b1, 0:128], VBs[kv, :, b0:b1, :], dsKV[s], r=[rVBs], w=[rKV[s]])
                        DMA(VT[s][0:16, 64:65, 0:128], VBs[kv, 0:16, 64:65, :], dsKV[s], r=[rVBs], w=[rKV[s]])

                CH = [(c * 512, 512, 4, 128) for c in range(4)] + [(2048, NMETA, 1, NMETA)]
                load_job(0)
                pt_i = [0]
                so_i = [0]
                for ji, (kind, idx) in enumerate(jobs):
                    s = ji % 2
                    if ji + 1 < len(jobs):
                        load_job(ji + 1)
                    isA = kind == "A"
                    KR = 68 if isA else 128
                    slope = 2.0 ** (-(idx + 1)) if isA else 0.0
                    scale = 0.125 if isA else 128.0 ** -0.5
                    Kt = [KT[s][0], KT[s][1] if isA else KT[s][0]]
                    Qt = QT[s]
                    for c, (q0, ncq, nsb, psq) in enumerate(CH):
                        V(lambda e: e.memset(Ops[:, :], 0.0), w=[rOps])

                        def qk(kb):
                            sp = kb % 2
                            kp = 128 if kb < 64 else NMETA
                            k0 = kb * 128
                            mixed = isA and c < 4 and (4 * c <= kb <= 4 * c + 3)
                            rows = 64 if mixed else KR
                            for m in range(2):
                                P(lambda e, m=m, sp=sp, kp=kp, k0=k0, rows=rows: e.matmul(
                                    Sps[sp][:kp, m * 512: m * 512 + ncq],
                                    lhsT=Kt[m][0:rows, k0:k0 + kp], rhs=Qt[m][0:rows, q0:q0 + ncq],
                                    start=True, stop=True),
                                  r=[rKV[s], rSig[s]], w=[rSps[sp]], inc=(m == 1))

                        def soft_pv(kb):
                            sp = kb % 2
                            kp = 128 if kb < 64 else NMETA
                            mixed = isA and c < 4 and (4 * c <= kb <= 4 * c + 3)
                            pi_ = pt_i[0] % 3
                            pt_i[0] += 1
                            if ncq == 512:
                                src = Sps[sp][:kp, :]
                                dst = PT[pi_][:kp, :]
                            else:
                                src = Sps[sp][:kp, :].rearrange("p (m q) -> p m q", m=2)[:, :, 0:ncq]
                                dst = PT[pi_][:kp, :].rearrange("p (m q) -> p m q", m=2)[:, :, 0:ncq]
                            if mixed:
                                mm = kb - 4 * c
                                x0 = 384 - 128 * mm
                                for m in range(2):
                                    V(lambda e, m=m, sp=sp, x0=x0: e.scalar_tensor_tensor(
                                        out=smix[sp][:, m * 512:(m + 1) * 512], in0=dtab[:, x0:x0 + 512],
                                        scalar=-8.0 * slope, in1=Sps[sp][:, m * 512:(m + 1) * 512],
                                        op0=ALU.mult, op1=ALU.add),
                                      r=[rSps[sp], rC], w=[rSmix[sp]])
                                A(lambda e, sp=sp, dst=dst: e.activation(out=dst, in_=smix[sp][:, :], func=AF.Exp,
                                                                         scale=scale),
                                  r=[rSmix[sp]], w=[rPT[pi_]])
                            elif isA:
                                bcol = (idx * 5 + c) * NKB + kb
                                A(lambda e, src=src, dst=dst, bcol=bcol, kp=kp: e.activation(
                                    out=dst, in_=src, func=AF.Exp, bias=beta[:kp, bcol:bcol + 1], scale=scale),
                                  r=[rSps[sp], rC], w=[rPT[pi_]])
                            else:
                                A(lambda e, src=src, dst=dst: e.activation(out=dst, in_=src, func=AF.Exp,
                                                                           scale=scale),
                                  r=[rSps[sp]], w=[rPT[pi_]])
                            for m in range(2):
                                for sq in range(nsb):
                                    last = (m == 1 and sq == nsb - 1)
                                    P(lambda e, m=m, sq=sq, pi_=pi_, kp=kp, kb=kb: e.matmul(
                                        oacc(m, sq)[:psq, :],
                                        lhsT=PT[pi_][:kp, m * 512 + sq * 128: m * 512 + sq * 128 + psq],
                                        rhs=VT[s][:kp, kb, :], start=False, stop=(kb == NKB - 1),
                                        skip_group_check=True),
                                      r=[rPT[pi_], rKV[s], rSig[s]], w=[rOps], inc=last)

                        qk(0)
                        for kb in range(NKB):
                            if kb + 1 < NKB:
                                qk(kb + 1)
                            soft_pv(kb)
                        so = so_i[0] % 2
                        so_i[0] += 1
                        for sq in range(nsb if isA else 0):
                            if isA:
                                O1, O2 = oacc(0, sq), oacc(1, sq)
                                V(lambda e, O1=O1: e.reciprocal(out=ev["r1"][:psq, 0:1], in_=O1[:psq, 128:129]),
                                  r=[rOps], w=[rEv])
                                V(lambda e, O2=O2: e.reciprocal(out=ev["r1"][:psq, 1:2], in_=O2[:psq, 128:129]),
                                  r=[rOps], w=[rEv])
                                V(lambda e: e.tensor_tensor(out=ev["r1"][:psq, 2:3], in0=ev["r1"][:psq, 1:2],
                                                            in1=lamneg[:psq, 0:1], op=ALU.mult),
                                  r=[rEv, rLam], w=[rEv])
                                V(lambda e, O2=O2: e.tensor_scalar(out=ev["t2"][:psq, :], in0=O2[:psq, 0:128],
                                                                   scalar1=ev["r1"][:psq, 2:3], scalar2=None,
                                                                   op0=ALU.mult), r=[rOps, rEv], w=[rEv])
                                V(lambda e, O1=O1: e.scalar_tensor_tensor(
                                    out=ev["dd"][:psq, :], in0=O1[:psq, 0:128], scalar=ev["r1"][:psq, 0:1],
                                    in1=ev["t2"][:psq, :], op0=ALU.mult, op1=ALU.add), r=[rOps, rEv], w=[rEv])
                                A(lambda e: e.activation(out=ev["junk"][:psq, :], in_=ev["dd"][:psq, :],
                                                         func=AF.Square, accum_out=ev["ssd"][:psq, 0:1]),
                                  r=[rEv], w=[rEv])
                                rstd_from_ss(ev["ssd"][:psq, 0:1], ev["lnd"][:psq, 0:1], ev["rsd"][:psq, 0:1],
                                             128.0, [rEv], [rEv])
                                V(lambda e, sq=sq, so=so: e.scalar_tensor_tensor(
                                    out=stgO[so][:psq, sq, :], in0=ev["dd"][:psq, :], scalar=ev["rsd"][:psq, 0:1],
                                    in1=gsubb[:psq, :], op0=ALU.mult, op1=ALU.mult),
                                  r=[rEv, rC], w=[rStgO[so]])
                        if isA:
                            dst_d = OAs[q0:q0 + ncq, idx * 128:(idx + 1) * 128].rearrange("(s p) e -> p s e", p=psq)
                            DMA(dst_d, stgO[so][:psq, 0:nsb, :], dsStgO[so], r=[rStgO[so]], w=[rOAs])
                        else:
                            for m in range(2):
                                g = 2 * idx + m
                                so = so_i[0] % 2
                                so_i[0] += 1
                                for sq in range(nsb):
                                    Om = oacc(m, sq)
                                    V(lambda e, Om=Om: e.reciprocal(out=ev["r1"][:psq, 0:1], in_=Om[:psq, 128:129]),
                                      r=[rOps], w=[rEv])
                                    V(lambda e, Om=Om, sq=sq, so=so: e.tensor_scalar(
                                        out=stgO[so][:psq, sq, :], in0=Om[:psq, 0:128], scalar1=ev["r1"][:psq, 0:1],
                                        scalar2=None, op0=ALU.mult), r=[rOps, rEv], w=[rStgO[so]])
                                dst_d = OBs[q0:q0 + ncq, g * 128:(g + 1) * 128].rearrange("(s p) e -> p s e", p=psq)
                                DMA(dst_d, stgO[so][:psq, 0:nsb, :], dsStgO[so], r=[rStgO[so]], w=[rOBs])
                S.barrier()

        if stop_after >= 3:
            with ExitStack() as p3:
                H = sb(p3, "H", [128, 17, D], F32)
                rH = [Res(f"H{t}") for t in range(17)]
                u2T = sb(p3, "u2T", [128, 8, NOWN], BF16)
                rU2 = [Res(f"u2T{t}") for t in range(17)]
                AFF = sb(p3, "AFF", [128, 17, NEXP], F32)
                rAFF = Res("AFF")
                COEF = sb(p3, "COEF", [128, 17, NEXP], F32)
                rCOEF = Res("COEF")
                rAgin = Res("agin", True)
                gffnb = sb(p3, "gffnb", [128, D], F32)
                dsC3 = dsem("ds_c3")
                DMA(gffnb[:, :], gffn_d[0:1, :].partition_broadcast(128), dsC3, w=[rC])
                BLK = [(t * 128, 128) for t in range(16)] + [(2048, NMETA)]

                with ExitStack() as p3a:
                    Wa = sb(p3a, "Wa", [128, 8, D], BF16)
                    Wb = sb(p3a, "Wb", [128, 8, D], BF16)
                    Wo = sb(p3a, "Wo", [128, 8, D], BF16)
                    wr = sb(p3a, "wr", [128, 8, NEXP], BF16)
                    rW3 = Res("W3", True)
                    dsW3 = dsem("ds_w3")
                    for c in range(8):
                        DMA(Wa[:, c, :], wa_d.rearrange("(c p) f -> p c f", p=128)[:, c, :], dsW3, w=[rW3], q="pool")
                        DMA(Wb[:, c, :], wb_d.rearrange("(c p) f -> p c f", p=128)[:, c, :], dsW3, w=[rW3], q="pool")
                        DMA(Wo[:, c, :], wo_d.rearrange("(c p) f -> p c f", p=128)[:, c, :], dsW3, w=[rW3], q="pool")
                    DMA(wr[:, :, :], wr_d.rearrange("(c p) f -> p c f", p=128), dsW3, w=[rW3], q="pool")
                    oa_t = [sb(p3a, f"oa_t{i}", [128, D], BF16) for i in range(2)]
                    ob_t = [sb(p3a, f"ob_t{i}", [128, D], BF16) for i in range(2)]
                    g_t = [sb(p3a, f"g_t{i}", [128, 2048], BF16) for i in range(2)]
                    x_t3 = [sb(p3a, f"x_t3{i}", [128, D], F32) for i in range(2)]
                    rIn3 = [Res(f"in3_{i}", True) for i in range(2)]
                    dsIn3 = [dsem(f"ds_in3_{i}") for i in range(2)]
                    oaT = sb(p3a, "oaT", [128, 8, 128], BF16)
                    obT = sb(p3a, "obT", [128, 8, 128], BF16)
                    mgT = sb(p3a, "mgT", [128, 8, 128], BF16)
                    rOaT, rObT, rMgT = Res("oaT"), Res("obT"), Res("mgT")
                    m1 = sb(p3a, "m1", [128, D], F32)
                    m2 = sb(p3a, "m2", [128, D], BF16)
                    affS = [sb(p3a, f"affS{i}", [NEXP, 128], F32) for i in range(2)]
                    rAffS = [Res(f"affS{i}") for i in range(2)]
                    dsAffS = [dsem(f"ds_affs{i}") for i in range(2)]
                    mg = sb(p3a, "mg", [128, D], BF16)
                    rM1, rM2, rMg = Res("m1"), Res("m2"), Res("mg")
                    u2 = sb(p3a, "u2", [128, D], BF16)
                    rU2t = Res("u2")
                    st3 = {n: sb(p3a, "st3_" + n, [128, 1], F32) for n in ("ss", "ln", "rs", "se", "rse")}
                    ex3 = sb(p3a, "ex3", [128, NEXP], F32)
                    rSt3 = Res("st3")
                    tp3 = [psum(p3a, f"tp3_{i}", [128, 1024], BF16) for i in range(2)]
                    rTp3 = [Res(f"tp3_{i}", psum=True) for i in range(2)]
                    yps = [psum(p3a, f"yps{i}", [128, 512], F32) for i in range(4)]
                    rYps = [Res(f"yps{i}", psum=True) for i in range(4)]
                    lps = psum(p3a, "lps", [128, 512], F32)
                    rLps = Res("lps", psum=True)
                    tfp = psum(p3a, "tfp", [128, 512], F32)
                    rTfp = Res("tfp", psum=True)

                    def load3(t):
                        q0, pb_ = BLK[t]
                        sl = t % 2
                        tokx = q0 if t < 16 else SEQ
                        DMA(oa_t[sl][:pb_, :], OAs[q0:q0 + pb_, :], dsIn3[sl], r=[rOAs], w=[rIn3[sl]])
                        DMA(ob_t[sl][:pb_, :], OBs[q0:q0 + pb_, :], dsIn3[sl], r=[rOBs], w=[rIn3[sl]])
                        DMA(g_t[sl][:pb_, :], Gs[q0:q0 + pb_, :], dsIn3[sl], r=[rGs], w=[rIn3[sl]])
                        DMA(x_t3[sl][:pb_, :], hx[tokx:tokx + pb_, :], dsIn3[sl], w=[rIn3[sl]])

                    def transpose8(src, rsrc, dstT, rdst, pb_, tpi):
                        for fc in range(8):
                            P(lambda e, fc=fc: e.transpose(out=tp3[tpi][:, fc * 128: fc * 128 + pb_],
                                                           in_=src[:pb_, fc * 128:(fc + 1) * 128],
                                                           identity=identb[:pb_, :pb_]),
                              r=[rsrc, rC], w=[rTp3[tpi]], inc=(fc == 7))

                    load3(0)
                    for t, (q0, pb_) in enumerate(BLK):
                        sl = t % 2
                        if t + 1 < len(BLK):
                            load3(t + 1)
                        transpose8(oa_t[sl], rIn3[sl], oaT, rOaT, pb_, 0)
                        V(lambda e: e.tensor_copy(out=oaT[:, :, :pb_],
                                                  in_=tp3[0][:, :].rearrange("p (c t) -> p c t", c=8)[:, :, :pb_]),
                          r=[rTp3[0]], w=[rOaT])
                        transpose8(ob_t[sl], rIn3[sl], obT, rObT, pb_, 1)
                        A(lambda e: e.activation(out=obT[:, :, :pb_],
                                                 in_=tp3[1][:, :].rearrange("p (c t) -> p c t", c=8)[:, :, :pb_],
                                                 func=AF.Copy),
                          r=[rTp3[1]], w=[rObT])
                        for g in range(2):
                            for fc in range(8):
                                P(lambda e, g=g, fc=fc: e.matmul(yps[g][:pb_, :], lhsT=oaT[:, fc, :pb_],
                                                                 rhs=Wa[:, fc, g * 512:(g + 1) * 512],
                                                                 start=(fc == 0), stop=(fc == 7)),
                                  r=[rOaT, rW3], w=[rYps[g]], inc=(fc == 7))
                        for g in range(2):
                            for fc in range(8):
                                P(lambda e, g=g, fc=fc: e.matmul(yps[2 + g][:pb_, :], lhsT=obT[:, fc, :pb_],
                                                                 rhs=Wb[:, fc, g * 512:(g + 1) * 512],
                                                                 start=(fc == 0), stop=(fc == 7)),
                                  r=[rObT, rW3], w=[rYps[2 + g]], inc=(fc == 7))
                        for g in range(2):
                            V(lambda e, g=g: e.tensor_tensor(out=m1[:pb_, g * 512:(g + 1) * 512], in0=yps[g][:pb_, :],
                                                             in1=g_t[sl][:pb_, g * 512:(g + 1) * 512], op=ALU.mult),
                              r=[rYps[g], rIn3[sl]], w=[rM1])
                            V(lambda e, g=g: e.tensor_tensor(out=m2[:pb_, g * 512:(g + 1) * 512],
                                                             in0=yps[2 + g][:pb_, :],
                                                             in1=g_t[sl][:pb_, 1024 + g * 512:1024 + (g + 1) * 512],
                                                             op=ALU.mult),
                              r=[rYps[2 + g], rIn3[sl]], w=[rM2])
                        V(lambda e: e.tensor_tensor(out=mg[:pb_, :], in0=m1[:pb_, :], in1=m2[:pb_, :], op=ALU.add),
                          r=[rM1, rM2], w=[rMg])
                        transpose8(mg, rMg, mgT, rMgT, pb_, 0)
                        V(lambda e: e.tensor_copy(out=mgT[:, :, :pb_],
                                                  in_=tp3[0][:, :].rearrange("p (c t) -> p c t", c=8)[:, :, :pb_]),
                          r=[rTp3[0]], w=[rMgT])
                        for g in range(2):
                            for fc in range(8):
                                P(lambda e, g=g, fc=fc: e.matmul(yps[g][:pb_, :], lhsT=mgT[:, fc, :pb_],
                                                                 rhs=Wo[:, fc, g * 512:(g + 1) * 512],
                                                                 start=(fc == 0), stop=(fc == 7)),
                                  r=[rMgT, rW3], w=[rYps[g]], inc=(fc == 7))
                        for g in range(2):
                            V(lambda e, g=g, t=t: e.tensor_tensor(out=H[:pb_, t, g * 512:(g + 1) * 512],
                                                                  in0=yps[g][:pb_, :],
                                                                  in1=x_t3[sl][:pb_, g * 512:(g + 1) * 512],
                                                                  op=ALU.add),
                              r=[rYps[g], rIn3[sl]], w=[rH[t]])
                        if dbg:
                            DMA(dbg_h1[q0:q0 + pb_, :], H[:pb_, t, :], dsDbg, r=[rH[t]])
                        A(lambda e, t=t: e.activation(out=u2[:pb_, :], in_=H[:pb_, t, :], func=AF.Square,
                                                      accum_out=st3["ss"][:pb_, 0:1]), r=[rH[t]], w=[rSt3, rU2t])
                        rstd_from_ss(st3["ss"][:pb_, 0:1], st3["ln"][:pb_, 0:1], st3["rs"][:pb_, 0:1], float(D),
                                     [rSt3], [rSt3])
                        V(lambda e, t=t: e.scalar_tensor_tensor(out=u2[:pb_, :], in0=H[:pb_, t, :],
                                                                scalar=st3["rs"][:pb_, 0:1], in1=gffnb[:pb_, :],
                                                                op0=ALU.mult, op1=ALU.mult),
                          r=[rH[t], rSt3, rC], w=[rU2t])
                        transpose8(u2, rU2t, None, None, pb_, 1)
                        A(lambda e, q0=q0: e.activation(
                            out=u2T[:, :, q0:q0 + pb_],
                            in_=tp3[1][:, :].rearrange("p (c t) -> p c t", c=8)[:, :, :pb_], func=AF.Copy),
                          r=[rTp3[1]], w=[rU2[t]])
                        for fc in range(8):
                            P(lambda e, fc=fc, q0=q0: e.matmul(lps[:pb_, 0:NEXP], lhsT=u2T[:, fc, q0:q0 + pb_],
                                                               rhs=wr[:, fc, :], start=(fc == 0), stop=(fc == 7)),
                              r=[rU2[t], rW3], w=[rLps], inc=(fc == 7))
                        A(lambda e: e.activation(out=ex3[:pb_, :], in_=lps[:pb_, 0:NEXP], func=AF.Exp,
                                                 accum_out=st3["se"][:pb_, 0:1]), r=[rLps], w=[rSt3])
                        V(lambda e: e.reciprocal(out=st3["rse"][:pb_, 0:1], in_=st3["se"][:pb_, 0:1]),
                          r=[rSt3], w=[rSt3])
                        V(lambda e, t=t: e.tensor_scalar(out=AFF[:pb_, t, :], in0=ex3[:pb_, :],
                                                         scalar1=st3["rse"][:pb_, 0:1], scalar2=None, op0=ALU.mult),
                          r=[rSt3], w=[rAFF])
                        P(lambda e, t=t: e.transpose(out=tfp[0:NEXP, 0:pb_], in_=AFF[:pb_, t, :],
                                                     identity=identf[:pb_, :pb_]), r=[rAFF, rC], w=[rTfp])
                        V(lambda e, sl=sl: e.tensor_copy(out=affS[sl][:, 0:pb_], in_=tfp[0:NEXP, 0:pb_]),
                          r=[rTfp], w=[rAffS[sl]])
                        DMA(agin.ap()[:, q0:q0 + pb_], affS[sl][:, 0:pb_], dsAffS[sl], r=[rAffS[sl]], w=[rAgin])
                        if dbg:
                            DMA(dbg_aff[q0:q0 + pb_, :], AFF[:pb_, t, :], dsDbg, r=[rAFF])
                    S.barrier()

                if stop_after >= 4:
                    with ExitStack() as p4:
                        AGc = sb(p4, "AGc", [NEXP, SEQ + NMETA], F32)
                        rAG = Res("AGc", True)
                        dsAG = dsem("ds_ag")
                        dsCC = DSem(sem("cc_sem"))
                        rAgout = Res("agout")
                        S.coll(lambda e: e.collective_compute(
                            "AllGather", ALU.bypass, replica_groups=[[0, 1, 2, 3], [4, 5, 6, 7]],
                            ins=[agin.ap().opt()], outs=[agout.ap().opt()]), dsCC, reads=[rAgin], writes=[rAgout])
                        ago = agout.ap()
                        DMA(AGc[:, 0:SEQ].rearrange("e (r t) -> e r t", r=4),
                            ago.rearrange("(r e) t -> e r t", e=NEXP)[:, :, 0:2048], dsAG, r=[rAgout], w=[rAG],
                            q="pool")
                        DMA(AGc[:, SEQ:SEQ + NMETA], ago[0:NEXP, 2048:2048 + NMETA], dsAG, r=[rAgout], w=[rAG],
                            q="pool")
                        lo = sb(p4, "lo", [NEXP, 1], F32)
                        mid = sb(p4, "mid", [NEXP, 1], F32)
                        cnt = sb(p4, "cnt", [NEXP, 1], F32)
                        prd = sb(p4, "prd", [NEXP, 1], F32)
                        cmpj = sb(p4, "cmpj", [NEXP, SEQ + NMETA], BF16)
                        rB = Res("bis")
                        V(lambda e: e.memset(lo[:, :], 0.0), w=[rB])
                        for it in range(NBIS):
                            ck = 2.0 ** (-(it + 1))
                            V(lambda e, ck=ck: e.tensor_scalar(out=mid[:, :], in0=lo[:, :], scalar1=ck, scalar2=None,
                                                               op0=ALU.add), r=[rB], w=[rB])
                            V(lambda e: e.tensor_scalar(out=cmpj[:, :], in0=AGc[:, :], scalar1=mid[:, 0:1],
                                                        scalar2=0.0, op0=ALU.is_ge, op1=ALU.add,
                                                        accum_out=cnt[:, 0:1]), r=[rB, rAG], w=[rB])
                            V(lambda e, ck=ck: e.tensor_scalar(out=prd[:, :], in0=cnt[:, :], scalar1=CAP - 0.5,
                                                               scalar2=ck, op0=ALU.is_ge, op1=ALU.mult),
                              r=[rB], w=[rB])
                            V(lambda e: e.tensor_tensor(out=lo[:, :], in0=lo[:, :], in1=prd[:, :], op=ALU.add),
                              r=[rB], w=[rB])
                        rThrD = Res("thr_d")
                        DMA(thr_d.rearrange("o e -> e o"), lo[:, :], dsAG, r=[rB], w=[rThrD])
                        if dbg:
                            DMA(dbg_thr.rearrange("o e -> e o"), lo[:, :], dsDbg, r=[rB])
                        THR = sb(p4, "THR", [128, NEXP], F32)
                        rTHR = Res("THR")
                        DMA(THR[:, :], thr_d[0:1, :].partition_broadcast(128), dsAG, r=[rThrD], w=[rTHR])
                        for t in range(16):
                            V(lambda e, t=t: e.tensor_tensor(out=COEF[:, t, :], in0=AFF[:, t, :], in1=THR[:, :],
                                                             op=ALU.is_ge), r=[rAFF, rTHR], w=[rCOEF])
                            V(lambda e, t=t: e.tensor_tensor(out=COEF[:, t, :], in0=COEF[:, t, :], in1=AFF[:, t, :],
                                                             op=ALU.mult), r=[rAFF, rCOEF], w=[rCOEF])
                        S.barrier()

                if stop_after >= 5:
                    with ExitStack() as p5:
                        wg = [sb(p5, f"wg{i}", [128, 8, 512], BF16) for i in range(2)]
                        wu = [sb(p5, f"wu{i}", [128, 8, 512], BF16) for i in range(2)]
                        wd = [sb(p5, f"wd{i}", [128, 4, D], BF16) for i in range(2)]
                        rWe = [Res(f"We{i}", True) for i in range(2)]
                        dsWe = [dsem(f"ds_we{i}") for i in range(2)]
                        hT = [sb(p5, f"hT{i}", [128, 4, 512], BF16) for i in range(2)]
                        rHT = [Res(f"hT{i}") for i in range(2)]
                        sg = [sb(p5, f"sg{i}", [128, 512], F32) for i in range(2)]
                        rSg = [Res(f"sg{i}") for i in range(2)]
                        gps = [psum(p5, f"gps{i}", [128, 512], F32) for i in range(2)]
                        ups = [psum(p5, f"ups{i}", [128, 512], F32) for i in range(2)]
                        rGps = [Res(f"gps{i}", psum=True) for i in range(2)]
                        rUps = [Res(f"ups{i}", psum=True) for i in range(2)]
                        ypm = [psum(p5, f"ypm{i}", [128, 512], F32) for i in range(4)]
                        rYpm = [Res(f"ypm{i}", psum=True) for i in range(4)]

                        def load_e(ei):
                            sl = ei % 2
                            for c in range(8):
                                DMA(wg[sl][:, c, :], wg_d[ei, c * 128:(c + 1) * 128, :], dsWe[sl], w=[rWe[sl]], q="pool")
                                DMA(wu[sl][:, c, :], wu_d[ei, c * 128:(c + 1) * 128, :], dsWe[sl], w=[rWe[sl]], q="pool")
                            for c in range(4):
                                DMA(wd[sl][:, c, :], wd_d[ei, c * 128:(c + 1) * 128, :], dsWe[sl], w=[rWe[sl]], q="pool")

                        load_e(0)
                        gi = [0]
                        yi = [0]
                        for ei in range(NEXP):
                            sl = ei % 2
                            if ei + 1 < NEXP:
                                load_e(ei + 1)
                            for tcn in range(4):
                                hs = (ei * 4 + tcn) % 2
                                rUs = [rU2[tcn * 4 + k] for k in range(4)]
                                for fc in range(4):
                                    gs = gi[0] % 2
                                    gi[0] += 1
                                    for dc in range(8):
                                        P(lambda e, gs=gs, dc=dc, fc=fc, tcn=tcn: e.matmul(
                                            gps[gs][:, :], lhsT=wg[sl][:, dc, fc * 128:(fc + 1) * 128],
                                            rhs=u2T[:, dc, tcn * 512:(tcn + 1) * 512], start=(dc == 0), stop=(dc == 7)),
                                          r=[rWe[sl]] + rUs, w=[rGps[gs]], inc=(dc == 7))
                                    for dc in range(8):
                                        P(lambda e, gs=gs, dc=dc, fc=fc, tcn=tcn: e.matmul(
                                            ups[gs][:, :], lhsT=wu[sl][:, dc, fc * 128:(fc + 1) * 128],
                                            rhs=u2T[:, dc, tcn * 512:(tcn + 1) * 512], start=(dc == 0), stop=(dc == 7)),
                                          r=[rWe[sl]] + rUs, w=[rUps[gs]], inc=(dc == 7))
                                    A(lambda e, gs=gs: e.activation(out=sg[gs][:, :], in_=gps[gs][:, :], func=AF.Silu),
                                      r=[rGps[gs]], w=[rSg[gs]])
                                    V(lambda e, gs=gs, fc=fc, hs=hs: e.tensor_tensor(
                                        out=hT[hs][:, fc, :], in0=ups[gs][:, :], in1=sg[gs][:, :], op=ALU.mult),
                                      r=[rUps[gs], rSg[gs]], w=[rHT[hs]])
                                for ts in range(4):
                                    t = tcn * 4 + ts
                                    for dh in range(2):
                                        ys = yi[0] % 4
                                        yi[0] += 1
                                        for fc in range(4):
                                            P(lambda e, ys=ys, fc=fc, ts=ts, dh=dh, hs=hs: e.matmul(
                                                ypm[ys][:, :], lhsT=hT[hs][:, fc, ts * 128:(ts + 1) * 128],
                                                rhs=wd[sl][:, fc, dh * 512:(dh + 1) * 512],
                                                start=(fc == 0), stop=(fc == 3)),
                                              r=[rHT[hs], rWe[sl]], w=[rYpm[ys]], inc=(fc == 3))
                                        V(lambda e, ys=ys, t=t, dh=dh, ei=ei: e.scalar_tensor_tensor(
                                            out=H[:, t, dh * 512:(dh + 1) * 512], in0=ypm[ys][:, :],
                                            scalar=COEF[:, t, ei:ei + 1], in1=H[:, t, dh * 512:(dh + 1) * 512],
                                            op0=ALU.mult, op1=ALU.add),
                                          r=[rYpm[ys], rCOEF, rH[t]], w=[rH[t]])
                        S.barrier()

                with ExitStack() as p6:
                    gfinb = sb(p6, "gfinb", [128, D], F32)
                    dsC6 = dsem("ds_c6")
                    DMA(gfinb[:, :], gfin_d[0:1, :].partition_broadcast(128), dsC6, w=[rC])
                    fo = [sb(p6, f"fo{i}", [128, D], F32) for i in range(2)]
                    rFo = [Res(f"fo{i}") for i in range(2)]
                    dsFo = [dsem(f"ds_fo{i}") for i in range(2)]
                    fj = sb(p6, "fj", [128, D], BF16)
                    fst = {n: sb(p6, "fst_" + n, [128, 1], F32) for n in ("ss", "ln", "rs")}
                    rFst = Res("fst")
                    for t in range(16):
                        sl = t % 2
                        A(lambda e, t=t: e.activation(out=fj[:, :], in_=H[:, t, :], func=AF.Square,
                                                      accum_out=fst["ss"][:, 0:1]), r=[rH[t]], w=[rFst])
                        rstd_from_ss(fst["ss"][:, 0:1], fst["ln"][:, 0:1], fst["rs"][:, 0:1], float(D),
                                     [rFst], [rFst])
                        V(lambda e, t=t, sl=sl: e.scalar_tensor_tensor(
                            out=fo[sl][:, :], in0=H[:, t, :], scalar=fst["rs"][:, 0:1], in1=gfinb[:, :],
                            op0=ALU.mult, op1=ALU.mult), r=[rH[t], rFst, rC], w=[rFo[sl]])
                        DMA(y_out[t * 128:(t + 1) * 128, :], fo[sl][:, :], dsFo[sl], r=[rFo[sl]])
                    S.barrier()
        else:
            with ExitStack() as pz:
                zt = sb(pz, "zt", [128, D], F32)
                rZ = Res("zt")
                dsZ = dsem("ds_z")
                V(lambda e: e.memset(zt[:, :], 0.0), w=[rZ])
                for t in range(16):
                    DMA(y_out[t * 128:(t + 1) * 128, :], zt[:, :], dsZ, r=[rZ])
                S.barrier()

        S.barrier()

        @block.tensor
        def _(eng):
            S.emit("pe", eng)

        @block.scalar
        def _(eng):
            S.emit("act", eng)

        @block.vector
        def _(eng):
            S.emit("dve", eng)

        @block.gpsimd
        def _(eng):
            S.emit("pool", eng)

        @block.sync
        def _(eng):
            S.emit("sp", eng)

    return nc


def _tables(r):
    bf = ml_dtypes.bfloat16
    jp = np.arange(SEQ)
    uj = np.where(jp + 2048 * r < SEQ, jp, jp - SEQ).astype(np.float64)
    sig = np.zeros((4, NT), np.float32)
    beta = np.zeros((128, 8, 5, NKB), np.float32)
    for c in range(4):
        before = uj < 512 * c
        sgn = np.where(before, 1.0, -1.0)
        sig[c, :SEQ] = sgn
        for h in range(8):
            slope = 2.0 ** (-(h + 1))
            b = sgn * slope * (uj - 512 * c - 256)
            beta[:, h, c, :64] = b.reshape(64, 128).T
    qaug = np.zeros((8, 4, NOWN), np.float32)
    a = np.arange(512) - 256
    for h in range(8):
        slope = 2.0 ** (-(h + 1))
        for c in range(4):
            qaug[h, c, c * 512:(c + 1) * 512] = -8.0 * slope * a
    x = np.arange(896)[None, :]
    bb = np.arange(128)[:, None]
    dtab = np.abs(x - bb - 384).astype(np.float32)
    t_true = (jp + 2048 * r) % SEQ
    row_id = (t_true // 64).astype(np.float32)
    col_id = (t_true % 64).astype(np.float32)
    inv_freq = (np.float32(10000.0) ** (-np.arange(0, 64, 2, dtype=np.float32) / np.float32(64))).astype(np.float32)
    ar = (row_id[:, None] * inv_freq[None, :]).astype(np.float32)
    ac = (col_id[:, None] * inv_freq[None, :]).astype(np.float32)
    rope = np.zeros((NT, 256), np.float32)
    rope[:, 0:128] = 1.0
    cr, sr, cc, sc = np.cos(ar), np.sin(ar), np.cos(ac), np.sin(ac)
    rope[:SEQ, 0:32] = cr
    rope[:SEQ, 32:64] = cr
    rope[:SEQ, 64:96] = cc
    rope[:SEQ, 96:128] = cc
    rope[:SEQ, 128:160] = -sr
    rope[:SEQ, 160:192] = sr
    rope[:SEQ, 192:224] = -sc
    rope[:SEQ, 224:256] = sc
    return dict(sigk=sig.astype(bf), qaug=qaug.astype(bf),
                beta=np.ascontiguousarray(beta.reshape(128, -1)), dtab=dtab, rope=rope)


def make_in_maps(inputs):
    bf = ml_dtypes.bfloat16
    x = np.asarray(inputs["x"], np.float32)
    meta = np.asarray(inputs["meta_tokens"], np.float32)
    f = lambda k: np.ascontiguousarray(np.asarray(inputs[k], np.float32))
    common = {
        "w_in": f("w_in")[0],
        "gmixT": np.ascontiguousarray(f("g_mix")[0].reshape(8, 128).T),
        "lamv": np.ascontiguousarray(np.stack([f("lambda_q1")[0], f("lambda_k1")[0],
                                               f("lambda_q2")[0], f("lambda_k2")[0]])),
        "g_subln": f("g_subln"), "g_qnorm": f("g_qnorm"), "g_knorm": f("g_knorm"),
        "w_branch_a": f("w_branch_a")[0], "w_branch_b": f("w_branch_b")[0], "w_out": f("w_out")[0],
        "g_ffn": f("g_ffn"), "w_router": f("w_router")[0],
        "w_gate": f("w_gate")[0], "w_up": f("w_up")[0], "w_down": f("w_down")[0],
        "g_final": f("g_final").reshape(1, D),
        "identb": np.eye(128, dtype=np.float32).astype(bf),
        "identf": np.eye(128, dtype=np.float32),
    }
    tabs = [_tables(r) for r in range(4)]
    maps = []
    for c in range(8):
        b, r = c // 4, c % 4
        hxv = np.concatenate([np.roll(x[b], -2048 * r, axis=0), meta], axis=0)
        m = dict(common)
        m.update(tabs[r])
        m["hx"] = np.ascontiguousarray(hxv)
        maps.append(m)
    return maps


_NC_CACHE = {}


def kernel(**inputs):
    if "nc" not in _NC_CACHE:
        _NC_CACHE["nc"] = build()
    nc = _NC_CACHE["nc"]
    maps = make_in_maps(inputs)
    res = run_bass_kernel_spmd(nc, maps, core_ids=list(range(8)))
    out = np.zeros((2, SEQ, D), np.float32)
    for c in range(8):
        b, r = c // 4, c % 4
        out[b, r * 2048:(r + 1) * 2048, :] = res.results[c]["y"]
    return out
```

```python
import numpy as np
import ml_dtypes
from contextlib import ExitStack
import concourse.bass as bass
import concourse.mybir as mybir
from concourse.bass_utils import run_bass_kernel_spmd

F32 = mybir.dt.float32
BF16 = mybir.dt.bfloat16
AF = mybir.ActivationFunctionType
ALU = mybir.AluOpType
AX = mybir.AxisListType

D = 1024
SEQ = 8192
NMETA = 16
NT = SEQ + NMETA
NOWN = 2048 + NMETA
NKB = 65
EPS = 1e-6
NEXP = 16
CAP = 2 * NT // NEXP
LAM_INIT = 0.2
NBIS = 28


import types


def _freeze(fn):
    if fn is None or fn.__closure__ is None:
        return fn
    cells = []
    for c in fn.__closure__:
        try:
            cells.append(types.CellType(c.cell_contents))
        except ValueError:
            cells.append(c)
    return types.FunctionType(fn.__code__, fn.__globals__, fn.__name__, fn.__defaults__, tuple(cells))


class Res:
    __slots__ = ("name", "w", "rd", "multi", "psum")

    def __init__(self, name, multi=False, psum=False):
        self.name = name
        self.w = {}
        self.rd = {}
        self.multi = multi
        self.psum = psum


class DSem:
    def __init__(self, sem):
        self.sem = sem
        self.count = 0


class Sched:
    ENGS = ("pe", "act", "dve", "pool", "sp")

    def __init__(self):
        self.prog = {e: [] for e in self.ENGS}
        self.cnt = {e: 0 for e in self.ENGS}
        self.sem = {}
        self.known = {e: {} for e in self.ENGS}
        self.all_dsems = []

    def _deps(self, eng, reads, writes):
        deps = {}
        known = self.known[eng]

        def add(tok, kind):
            sem, val, e = tok
            if e == eng and eng in ("pe", "sp"):
                return
            k = id(sem)
            if known.get(k, 0) >= val:
                return
            if k not in deps or deps[k][1] < val:
                deps[k] = (sem, val)

        for r in reads:
            for tok in r.w.values():
                add(tok, "raw")
            if r.psum:
                for tok in r.rd.values():
                    if tok[2] != eng:
                        add(tok, "rar")
        for w in writes:
            if not w.multi:
                for tok in w.w.values():
                    add(tok, "waw")
            for tok in w.rd.values():
                add(tok, "war")
        for k, (sem, val) in deps.items():
            known[k] = val
        return list(deps.values())

    def _record(self, tok, reads, writes):
        k = id(tok[0])
        for r in reads:
            r.rd[k] = tok
        for w in writes:
            if w.multi:
                w.w[k] = tok
            else:
                w.w = {k: tok}
                w.rd = {}

    def op(self, eng, fn, reads=(), writes=(), inc=True):
        deps = self._deps(eng, reads, writes)
        tok = (self.sem[eng], self.cnt[eng] + 1, eng)
        self._record(tok, reads, writes)
        if inc:
            self.cnt[eng] += 1
        self.prog[eng].append((deps, _freeze(fn), self.sem[eng] if inc else None, 1))

    def dma(self, q, fn, ds, reads=(), writes=()):
        deps = self._deps(q, reads, writes)
        ds.count += 16
        tok = (ds.sem, ds.count, None)
        self._record(tok, reads, writes)
        self.prog[q].append((deps, _freeze(fn), ds.sem, 16))

    def coll(self, fn, ds, reads=(), writes=()):
        deps = self._deps("pool", reads, writes)
        ds.count += 1
        tok = (ds.sem, ds.count, None)
        self._record(tok, reads, writes)
        self.prog["pool"].append((deps, _freeze(fn), ds.sem, None))

    def barrier(self):
        toks = []
        for e in self.ENGS:
            if e == "sp":
                continue
            if self.cnt[e] > 0:
                toks.append((self.sem[e], self.cnt[e]))
        for ds in self.all_dsems:
            if ds.count > 0:
                toks.append((ds.sem, ds.count))
        for e in self.ENGS:
            deps = []
            for sem, val in toks:
                if self.known[e].get(id(sem), 0) < val:
                    self.known[e][id(sem)] = val
                    deps.append((sem, val))
            if deps:
                self.prog[e].append((deps, None, None, 0))

    def emit(self, eng, handle):
        for deps, fn, sem, incv in self.prog[eng]:
            for s, v in deps:
                handle.wait_ge(s, v)
            if fn is None:
                continue
            ins = fn(handle)
            if sem is not None:
                if incv is None:
                    ins.then_inc(sem)
                else:
                    ins.then_inc(sem, incv)


def build(dbg=False, stop_after=99):
    nc = bass.Bass("TRN2", target_bir_lowering=False)
    S = Sched()

    def din(name, shape, dt=F32):
        return nc.dram_tensor(name, list(shape), dt, kind="ExternalInput").ap()

    def dscr(name, shape, dt):
        if dbg:
            return nc.dram_tensor(name, list(shape), dt, kind="ExternalOutput").ap()
        return nc.dram_tensor(name, list(shape), dt).ap()

    hx = din("hx", [NT, D])
    w_in = din("w_in", [D, 6656])
    gmixT_d = din("gmixT", [128, 8])
    lam_d = din("lamv", [4, 64])
    gsub_d = din("g_subln", [1, 128])
    gq_d = din("g_qnorm", [1, 128])
    gk_d = din("g_knorm", [1, 128])
    wa_d = din("w_branch_a", [D, D])
    wb_d = din("w_branch_b", [D, D])
    wo_d = din("w_out", [D, D])
    gffn_d = din("g_ffn", [1, D])
    wr_d = din("w_router", [D, NEXP])
    if stop_after >= 5:
        wg_d = din("w_gate", [NEXP, D, 512])
        wu_d = din("w_up", [NEXP, D, 512])
        wd_d = din("w_down", [NEXP, 512, D])
    gfin_d = din("g_final", [1, D])
    sigk_d = din("sigk", [4, NT], BF16)
    qaug_d = din("qaug", [8, 4, NOWN], BF16)
    beta_d = din("beta", [128, 8 * 5 * NKB])
    dtab_d = din("dtab", [128, 896])
    rope_d = din("rope", [NT, 256])
    identb_d = din("identb", [128, 128], BF16)
    identf_d = din("identf", [128, 128])
    y_out = nc.dram_tensor("y", [2048, D], F32, kind="ExternalOutput").ap()

    KAs = dscr("KAs", [8, 128, NT], BF16)
    QAs = dscr("QAs", [8, 128, NOWN], BF16)
    VAs = dscr("VAs", [8, 128, NKB, 128], BF16)
    KBs = dscr("KBs", [2, 128, NT], BF16)
    QBs = dscr("QBs", [8, 128, NOWN], BF16)
    VBs = dscr("VBs", [2, 128, NKB, 128], BF16)
    Gs = dscr("Gs", [NOWN, 2048], BF16)
    OAs = dscr("OAs", [NOWN, D], BF16)
    OBs = dscr("OBs", [NOWN, D], BF16)
    agin = nc.dram_tensor("agin", [NEXP, NOWN], F32)
    agout = nc.dram_tensor("agout", [4 * NEXP, NOWN], F32)
    thr_d = nc.dram_tensor("thr_d", [1, NEXP], F32).ap()
    dbg_aff = dscr("dbg_aff", [NOWN, NEXP], F32) if dbg else None
    dbg_h1 = dscr("dbg_h1", [NOWN, D], F32) if dbg else None
    dbg_thr = dscr("dbg_thr", [1, NEXP], F32) if dbg else None

    rKAs, rQAs, rVAs = Res("KAs", True), Res("QAs", True), Res("VAs", True)
    rKBs, rQBs, rVBs = Res("KBs", True), Res("QBs", True), Res("VBs", True)
    rGs, rOAs, rOBs = Res("Gs", True), Res("OAs", True), Res("OBs", True)

    with ExitStack() as top:
        def sem(name):
            return top.enter_context(nc.semaphore(name))

        for e in Sched.ENGS:
            S.sem[e] = sem("sem_" + e)

        def dsem(name):
            d = DSem(sem(name))
            S.all_dsems.append(d)
            return d

        def sb(es, name, shape, dt):
            return es.enter_context(nc.sbuf_tensor("s_" + name, list(shape), dt))

        def psum(es, name, shape, dt):
            return es.enter_context(nc.psum_tensor("p_" + name, list(shape), dt))

        block = top.enter_context(nc.Block())

        def V(fn, r=(), w=()):
            S.op("dve", fn, r, w)

        def A(fn, r=(), w=()):
            S.op("act", fn, r, w)

        def P(fn, r=(), w=(), inc=True):
            S.op("pe", fn, r, w, inc)

        def G(fn, r=(), w=()):
            S.op("pool", fn, r, w)

        def DMA(out, in_, ds, r=(), w=(), q="sp"):
            S.dma(q, (lambda e, o=out, i=in_: e.dma_start(out=o, in_=i)), ds, r, w)

        identb = sb(top, "identb", [128, 128], BF16)
        identf = sb(top, "identf", [128, 128], F32)
        epsT = sb(top, "epsT", [128, 1], F32)
        rC = Res("consts", True)
        dsDbg = dsem("ds_dbg")
        dsC = dsem("ds_const")
        DMA(identb[:, :], identb_d[:, :], dsC, w=[rC])
        DMA(identf[:, :], identf_d[:, :], dsC, w=[rC])
        V(lambda e: e.memset(epsT[:, :], EPS), w=[rC])

        def rstd_from_ss(ss_ap, ln_ap, out_ap, n, rs, ws):
            A(lambda e: e.activation(out=ln_ap, in_=ss_ap, func=AF.Ln,
                                     bias=epsT[:ln_ap.shape[0], 0:1], scale=1.0 / n),
              r=rs + [rC], w=ws)
            A(lambda e: e.activation(out=out_ap, in_=ln_ap, func=AF.Exp, scale=-0.5),
              r=ws, w=ws)

        STS = [(t * 512, 512, 4, 128) for t in range(16)] + [(SEQ, NMETA, 1, NMETA)]

        def is_own(st):
            return st < 4 or st == 16

        def q0_of(st):
            return st * 512 if st < 4 else 2048

        with ExitStack() as p1:
            uT_own = sb(p1, "uT_own", [128, 8, NOWN], BF16)
            rUown = [Res(f"uTown{i}") for i in range(5)]
            gmixT = sb(p1, "gmixT", [128, 8], F32)
            gqb = sb(p1, "gqb", [128, 128], F32)
            gkb = sb(p1, "gkb", [128, 128], F32)
            dsC1 = dsem("ds_c1")
            DMA(gmixT[:, :], gmixT_d[:, :], dsC1, w=[rC])
            DMA(gqb[:, :], gq_d[0:1, :].partition_broadcast(128), dsC1, w=[rC])
            DMA(gkb[:, :], gk_d[0:1, :].partition_broadcast(128), dsC1, w=[rC])

            def norm_rope(raw, H, psub, gb, cs_t, s, out_bf, scr, rs_extra, r_raw, r_scr, r_out):
                sq, ssk, lnk, rk, tmp, yy = scr
                V(lambda e: e.tensor_tensor(out=sq[:psub, :H * 128], in0=raw[:psub, :H * 128],
                                            in1=raw[:psub, :H * 128], op=ALU.mult),
                  r=[r_raw], w=[r_scr])
                V(lambda e: e.tensor_reduce(out=ssk[:psub, :H],
                                            in_=sq[:psub, :H * 128].rearrange("p (h d) -> p h d", h=H),
                                            axis=AX.X, op=ALU.add), r=[r_scr], w=[r_scr])
                rstd_from_ss(ssk[:psub, :H], lnk[:psub, :H], rk[:psub, :H], 128.0, [r_scr], [r_scr])
                for j in range(H):
                    yj = yy[:psub, j * 128:(j + 1) * 128]
                    V(lambda e, j=j, yj=yj: e.scalar_tensor_tensor(
                        out=yj, in0=raw[:psub, j * 128:(j + 1) * 128], scalar=rk[:psub, j:j + 1],
                        in1=gb[:psub, :], op0=ALU.mult, op1=ALU.mult), r=[r_raw, r_scr, rC], w=[r_scr])
                    y4 = yj.rearrange("p (a b c) -> p a b c", a=2, b=2)
                    t4 = tmp[:psub, j * 128:(j + 1) * 128].rearrange("p (a b c) -> p a b c", a=2, b=2)
                    sn4 = cs_t[:psub, s, 128:256].rearrange("p (a b c) -> p a b c", a=2, b=2)
                    V(lambda e, y4=y4, t4=t4, sn4=sn4: e.tensor_tensor(
                        out=t4[:, :, 0, :], in0=y4[:, :, 1, :], in1=sn4[:, :, 0, :], op=ALU.mult),
                      r=[r_scr] + rs_extra, w=[r_scr])
                    V(lambda e, y4=y4, t4=t4, sn4=sn4: e.tensor_tensor(
                        out=t4[:, :, 1, :], in0=y4[:, :, 0, :], in1=sn4[:, :, 1, :], op=ALU.mult),
                      r=[r_scr] + rs_extra, w=[r_scr])
                    V(lambda e, yj=yj: e.tensor_tensor(
                        out=yj, in0=yj, in1=cs_t[:psub, s, 0:128], op=ALU.mult),
                      r=[r_scr] + rs_extra, w=[r_scr])
                    V(lambda e, j=j, yj=yj: e.tensor_tensor(
                        out=out_bf[:psub, j * 128:(j + 1) * 128], in0=yj,
                        in1=tmp[:psub, j * 128:(j + 1) * 128], op=ALU.add),
                      r=[r_scr], w=[r_out])

            with ExitStack() as pa:
                wA = sb(pa, "wA", [128, 8, 2560], BF16)
                rWA = Res("wA", True)
                dsW = dsem("ds_w")
                w_in_v = w_in.rearrange("(c p) f -> p c f", p=128)
                for c in range(8):
                    DMA(wA[:, c, 0:2048], w_in_v[:, c, 1024:3072], dsW, w=[rWA], q="pool")
                    DMA(wA[:, c, 2048:2560], w_in_v[:, c, 4096:4608], dsW, w=[rWA], q="pool")
                xt = [sb(pa, f"xt{i}", [128, 4, D], F32) for i in range(2)]
                rXt = [Res(f"xt{i}", True) for i in range(2)]
                dsXc = [dsem(f"ds_xc{i}") for i in range(2)]
                dsX = [dsem(f"ds_x{i}") for i in range(2)]
                cs = [sb(pa, f"cs{i}", [128, 4, 256], F32) for i in range(2)]
                rCs = [Res(f"cs{i}", True) for i in range(2)]
                xs = sb(pa, "xs", [128, 4, D], BF16)
                rXs = Res("xs")
                junk = sb(pa, "junk", [128, D], BF16)
                rJunk = Res("junk")
                ss = sb(pa, "ss", [128, 4], F32)
                lnv = sb(pa, "lnv", [128, 4], F32)
                rstd = sb(pa, "rstd", [128, 4], F32)
                rSt = Res("stats")
                uT_tmp = [sb(pa, f"uTt{i}", [128, 8, 512], BF16) for i in range(2)]
                rUt = [Res(f"uTt{i}") for i in range(2)]
                stgK = [sb(pa, f"stgK{i}", [128, 512], BF16) for i in range(3)]
                rStgK = [Res(f"stgK{i}") for i in range(3)]
                dsStgK = [dsem(f"ds_sk{i}") for i in range(3)]
                stgV = [sb(pa, f"stgV{i}", [128, 1024], BF16) for i in range(2)]
                rStgV = [Res(f"stgV{i}") for i in range(2)]
                dsStgV = [dsem(f"ds_sv{i}") for i in range(2)]
                stgVB = [sb(pa, f"stgVB{i}", [128, 256], BF16) for i in range(2)]
                rStgVB = [Res(f"stgVB{i}") for i in range(2)]
                dsStgVB = [dsem(f"ds_svb{i}") for i in range(2)]
                kraw = sb(pa, "kraw", [128, 256], F32)
                rKraw = Res("kraw")
                ksc = (sb(pa, "ksq", [128, 256], F32), sb(pa, "kss", [128, 2], F32),
                       sb(pa, "kln", [128, 2], F32), sb(pa, "krk", [128, 2], F32),
                       sb(pa, "ktmp", [128, 256], F32), sb(pa, "kyy", [128, 256], F32))
                rKsc = Res("ksc")
                krope = sb(pa, "krope", [128, 4, 256], BF16)
                rKrope = Res("krope")
                stgKB = sb(pa, "stgKB", [128, 2, 512], BF16)
                rStgKB = Res("stgKB")
                dsStgKB = dsem("ds_skb")
                tpb = [psum(pa, f"tpb{i}", [128, 1024], BF16) for i in range(4)]
                rTp = [Res(f"tpb{i}", psum=True) for i in range(4)]
                acc = [psum(pa, f"acc{i}", [128, 512], F32) for i in range(3)]
                rAcc = [Res(f"acc{i}", psum=True) for i in range(3)]
                ktp = psum(pa, "ktp", [128, 1024], BF16)
                rKtp = Res("ktp", psum=True)
                acc_i = [0]

                def next_acc():
                    i = acc_i[0] % 3
                    acc_i[0] += 1
                    return acc[i], rAcc[i]

                def load_x(sti):
                    tok0, ntok, nsub, psub = STS[sti]
                    sl = sti % 2
                    DMA(xt[sl][:psub, 0:nsub, :],
                        hx[tok0:tok0 + ntok, :].rearrange("(s p) d -> p s d", p=psub),
                        dsX[sl], w=[rXt[sl]])
                    DMA(cs[sl][:psub, 0:nsub, :],
                        rope_d[tok0:tok0 + ntok, :].rearrange("(s p) d -> p s d", p=psub),
                        dsXc[sl], w=[rCs[sl]])

                load_x(0)
                kcount = [0]
                vcount = [0]
                import os
                DBGL = os.environ.get("KDBG", "")
                for sti, (tok0, ntok, nsub, psub) in enumerate(STS):
                    sl = sti % 2
                    if "one" in DBGL and sti >= 1:
                        break
                    if sti + 1 < len(STS) and "one" not in DBGL:
                        load_x(sti + 1)
                    x_t = xt[sl]
                    for s in range(nsub):
                        A(lambda e, s=s: e.activation(out=junk[:psub, :], in_=x_t[:psub, s, :],
                                                      func=AF.Square, accum_out=ss[:psub, s:s + 1]),
                          r=[rXt[sl]], w=[rJunk, rSt])
                    rstd_from_ss(ss[:psub, :nsub], lnv[:psub, :nsub], rstd[:psub, :nsub], float(D),
                                 [rSt], [rSt])
                    for s in range(nsub):
                        V(lambda e, s=s: e.tensor_scalar(out=xs[:psub, s, :], in0=x_t[:psub, s, :],
                                                         scalar1=rstd[:psub, s:s + 1], scalar2=None,
                                                         op0=ALU.mult),
                          r=[rXt[sl], rSt], w=[rXs])
                    own = is_own(sti)
                    if own:
                        q0 = q0_of(sti)
                        uT = uT_own
                        ucol = q0
                        rU = rUown[sti if sti < 4 else 4]
                    else:
                        uT = uT_tmp[sl]
                        ucol = 0
                        rU = rUt[sl]
                    for dc in range(8):
                        bank = dc // 2
                        half = dc % 2
                        for s in range(nsub):
                            P(lambda e, dc=dc, s=s, bank=bank, half=half: e.transpose(
                                out=tpb[bank][:, half * 512 + s * 128: half * 512 + s * 128 + psub],
                                in_=xs[:psub, s, dc * 128:(dc + 1) * 128],
                                identity=identb[:psub, :psub]),
                              r=[rXs, rC], w=[rTp[bank]], inc=(s == nsub - 1))
                        src = tpb[bank][:, half * 512: half * 512 + ntok]
                        dst = uT[:, dc, ucol:ucol + ntok]
                        if bank % 2 == 0:
                            V(lambda e, src=src, dst=dst, dc=dc: e.tensor_scalar(
                                out=dst, in0=src, scalar1=gmixT[:, dc:dc + 1], scalar2=None, op0=ALU.mult),
                              r=[rTp[bank], rC], w=[rU])
                        else:
                            A(lambda e, src=src, dst=dst, dc=dc: e.activation(
                                out=dst, in_=src, func=AF.Copy, scale=gmixT[:, dc:dc + 1]),
                              r=[rTp[bank], rC], w=[rU])
                    if "noproj" in DBGL:
                        continue
                    for h in range(8 if "nokA" not in DBGL else 0):
                        a_t, rA = next_acc()
                        for dc in range(8):
                            P(lambda e, a_t=a_t, dc=dc, h=h: e.matmul(
                                a_t[:, :ntok], lhsT=wA[:, dc, h * 128:(h + 1) * 128],
                                rhs=uT[:, dc, ucol:ucol + ntok], start=(dc == 0), stop=(dc == 7)),
                              r=[rWA, rU], w=[rA], inc=(dc == 7))
                        ks = kcount[0] % 3
                        kcount[0] += 1
                        A(lambda e, a_t=a_t, ks=ks: e.activation(out=stgK[ks][:, :ntok], in_=a_t[:, :ntok],
                                                                 func=AF.Copy),
                          r=[rA], w=[rStgK[ks]])
                        DMA(KAs[h, :, tok0:tok0 + ntok], stgK[ks][:, :ntok], dsStgK[ks],
                            r=[rStgK[ks]], w=[rKAs])
                    if "notok" in DBGL:
                        continue
                    for s in range(nsub):
                        blk = (tok0 // 128) + s
                        vs = vcount[0] % 2
                        vcount[0] += 1
                        for g in range(2):
                            a_t, rA = next_acc()
                            for dc in range(8):
                                P(lambda e, a_t=a_t, dc=dc, g=g, s=s: e.matmul(
                                    a_t[:psub, :], lhsT=uT[:, dc, ucol + s * 128: ucol + s * 128 + psub],
                                    rhs=wA[:, dc, 1024 + g * 512: 1024 + (g + 1) * 512],
                                    start=(dc == 0), stop=(dc == 7)),
                                  r=[rWA, rU], w=[rA], inc=(dc == 7))
                            if g == 0:
                                A(lambda e, a_t=a_t, vs=vs: e.activation(
                                    out=stgV[vs][:psub, 0:512], in_=a_t[:psub, :], func=AF.Copy),
                                  r=[rA], w=[rStgV[vs]])
                            else:
                                V(lambda e, a_t=a_t, vs=vs: e.tensor_copy(
                                    out=stgV[vs][:psub, 512:1024], in_=a_t[:psub, :]),
                                  r=[rA], w=[rStgV[vs]])
                        if "novst" not in DBGL:
                          DMA(VAs[:, 0:psub, blk, :].rearrange("h p e -> p h e"),
                            stgV[vs][:psub, :].rearrange("p (h e) -> p h e", h=8),
                            dsStgV[vs], r=[rStgV[vs]], w=[rVAs])
                        a_t, rA = next_acc()
                        for dc in range(8):
                            P(lambda e, a_t=a_t, dc=dc, s=s: e.matmul(
                                a_t[:psub, :], lhsT=uT[:, dc, ucol + s * 128: ucol + s * 128 + psub],
                                rhs=wA[:, dc, 2048:2560], start=(dc == 0), stop=(dc == 7)),
                              r=[rWA, rU], w=[rA], inc=(dc == 7))
                        A(lambda e, a_t=a_t: e.activation(out=kraw[:psub, :], in_=a_t[:psub, 0:256],
                                                          func=AF.Copy), r=[rA], w=[rKraw])
                        A(lambda e, a_t=a_t, vs=vs: e.activation(out=stgVB[vs][:psub, :],
                                                                 in_=a_t[:psub, 256:512], func=AF.Copy),
                          r=[rA], w=[rStgVB[vs]])
                        if "novst" not in DBGL:
                          DMA(VBs[:, 0:psub, blk, :].rearrange("h p e -> p h e"),
                            stgVB[vs][:psub, :].rearrange("p (h e) -> p h e", h=2),
                            dsStgVB[vs], r=[rStgVB[vs]], w=[rVBs])
                        if "norope" in DBGL:
                            continue
                        norm_rope(kraw, 2, psub, gkb, cs[sl], s, krope[:, s, :], ksc,
                                  [rCs[sl]], rKraw, rKsc, rKrope)
                        for j in range(2):
                            P(lambda e, j=j, s=s: e.transpose(
                                out=ktp[:, j * 512 + s * 128: j * 512 + s * 128 + psub],
                                in_=krope[:psub, s, j * 128:(j + 1) * 128],
                                identity=identb[:psub, :psub]),
                              r=[rKrope, rC], w=[rKtp], inc=(j == 1))
                    if "norope" in DBGL:
                        continue
                    for j in range(2):
                        V(lambda e, j=j: e.tensor_copy(out=stgKB[:, j, :ntok],
                                                       in_=ktp[:, j * 512: j * 512 + ntok]),
                          r=[rKtp], w=[rStgKB])
                    for j in range(2):
                        DMA(KBs[j, :, tok0:tok0 + ntok], stgKB[:, j, :ntok], dsStgKB,
                            r=[rStgKB], w=[rKBs])
                S.barrier()

            if stop_after >= 1.5:
                with ExitStack() as pb:
                    wB = sb(pb, "wB", [128, 8, 4096], BF16)
                    rWB = Res("wB", True)
                    dsWB = dsem("ds_wb")
                    w_in_v = w_in.rearrange("(c p) f -> p c f", p=128)
                    for c in range(8):
                        DMA(wB[:, c, 0:1024], w_in_v[:, c, 0:1024], dsWB, w=[rWB], q="pool")
                        DMA(wB[:, c, 1024:2048], w_in_v[:, c, 3072:4096], dsWB, w=[rWB], q="pool")
                        DMA(wB[:, c, 2048:4096], w_in_v[:, c, 4608:6656], dsWB, w=[rWB], q="pool")
                    csB = [sb(pb, f"csB{i}", [128, 4, 256], F32) for i in range(2)]
                    rCsB = [Res(f"csB{i}", True) for i in range(2)]
                    dsCsB = [dsem(f"ds_csb{i}") for i in range(2)]
                    stgQ = [sb(pb, f"stgQ{i}", [128, 512], BF16) for i in range(3)]
                    rStgQ = [Res(f"stgQ{i}") for i in range(3)]
                    dsStgQ = [dsem(f"ds_sq{i}") for i in range(3)]
                    qraw = sb(pb, "qraw", [128, 1024], F32)
                    rQraw = Res("qraw")
                    qsc = (sb(pb, "qsq", [128, 1024], F32), sb(pb, "qss", [128, 8], F32),
                           sb(pb, "qln", [128, 8], F32), sb(pb, "qrk", [128, 8], F32),
                           sb(pb, "qtmp", [128, 1024], F32), sb(pb, "qyy", [128, 1024], F32))
                    rQsc = Res("qsc")
                    qrope = sb(pb, "qrope", [128, 1024], BF16)
                    rQrope = Res("qrope")
                    stgQB = [sb(pb, f"stgQB{i}", [128, 8, 128], BF16) for i in range(2)]
                    rStgQB = [Res(f"stgQB{i}") for i in range(2)]
                    dsStgQB = [dsem(f"ds_sqb{i}") for i in range(2)]
                    stgG = [sb(pb, f"stgG{i}", [128, 2048], BF16) for i in range(2)]
                    rStgG = [Res(f"stgG{i}") for i in range(2)]
                    dsStgG = [dsem(f"ds_sg{i}") for i in range(2)]
                    accB = [psum(pb, f"accB{i}", [128, 512], F32) for i in range(6)]
                    rAccB = [Res(f"accB{i}", psum=True) for i in range(6)]
                    qtp = psum(pb, "qtp", [128, 1024], BF16)
                    rQtp = Res("qtp", psum=True)
                    accb_i = [0]

                    def next_accB():
                        i = accb_i[0] % 6
                        accb_i[0] += 1
                        return accB[i], rAccB[i]

                    own_sts = [0, 1, 2, 3, 16]
                    qcount = [0]
                    bcount = [0]
                    for oi, sti in enumerate(own_sts):
                        tok0, ntok, nsub, psub = STS[sti]
                        q0 = q0_of(sti)
                        rU = rUown[oi]
                        sl = oi % 2
                        DMA(csB[sl][:psub, 0:nsub, :],
                            rope_d[tok0:tok0 + ntok, :].rearrange("(s p) d -> p s d", p=psub),
                            dsCsB[sl], w=[rCsB[sl]])
                        for h in range(8):
                            a_t, rA = next_accB()
                            for dc in range(8):
                                P(lambda e, a_t=a_t, dc=dc, h=h: e.matmul(
                                    a_t[:, :ntok], lhsT=wB[:, dc, h * 128:(h + 1) * 128],
                                    rhs=uT_own[:, dc, q0:q0 + ntok], start=(dc == 0), stop=(dc == 7)),
                                  r=[rWB, rU], w=[rA], inc=(dc == 7))
                            ks = qcount[0] % 3
                            qcount[0] += 1
                            A(lambda e, a_t=a_t, ks=ks: e.activation(out=stgQ[ks][:, :ntok],
                                                                     in_=a_t[:, :ntok], func=AF.Copy),
                              r=[rA], w=[rStgQ[ks]])
                            DMA(QAs[h, :, q0:q0 + ntok], stgQ[ks][:, :ntok], dsStgQ[ks],
                                r=[rStgQ[ks]], w=[rQAs])
                        for s in range(nsub):
                            bs = bcount[0] % 2
                            bcount[0] += 1
                            qs = q0 + s * 128
                            for g in range(2):
                                a_t, rA = next_accB()
                                for dc in range(8):
                                    P(lambda e, a_t=a_t, dc=dc, g=g, qs=qs: e.matmul(
                                        a_t[:psub, :], lhsT=uT_own[:, dc, qs:qs + psub],
                                        rhs=wB[:, dc, 1024 + g * 512: 1024 + (g + 1) * 512],
                                        start=(dc == 0), stop=(dc == 7)),
                                      r=[rWB, rU], w=[rA], inc=(dc == 7))
                                A(lambda e, a_t=a_t, g=g: e.activation(
                                    out=qraw[:psub, g * 512:(g + 1) * 512], in_=a_t[:psub, :], func=AF.Copy),
                                  r=[rA], w=[rQraw])
                            norm_rope(qraw, 8, psub, gqb, csB[sl], s, qrope, qsc,
                                      [rCsB[sl]], rQraw, rQsc, rQrope)
                            for j in range(8):
                                P(lambda e, j=j: e.transpose(
                                    out=qtp[:, j * 128: j * 128 + psub],
                                    in_=qrope[:psub, j * 128:(j + 1) * 128],
                                    identity=identb[:psub, :psub]),
                                  r=[rQrope, rC], w=[rQtp], inc=(j == 7))
                            V(lambda e, bs=bs: e.tensor_copy(
                                out=stgQB[bs][:, :, :psub],
                                in_=qtp[:, :].rearrange("p (g t) -> p g t", g=8)[:, :, :psub]),
                              r=[rQtp], w=[rStgQB[bs]])
                            DMA(QBs[:, :, qs:qs + psub].rearrange("g p t -> p g t"),
                                stgQB[bs][:, :, :psub], dsStgQB[bs], r=[rStgQB[bs]], w=[rQBs])
                            for g in range(4):
                                a_t, rA = next_accB()
                                for dc in range(8):
                                    P(lambda e, a_t=a_t, dc=dc, g=g, qs=qs: e.matmul(
                                        a_t[:psub, :], lhsT=uT_own[:, dc, qs:qs + psub],
                                        rhs=wB[:, dc, 2048 + g * 512: 2048 + (g + 1) * 512],
                                        start=(dc == 0), stop=(dc == 7)),
                                      r=[rWB, rU], w=[rA], inc=(dc == 7))
                                A(lambda e, a_t=a_t, g=g, bs=bs: e.activation(
                                    out=stgG[bs][:psub, g * 512:(g + 1) * 512], in_=a_t[:psub, :],
                                    func=AF.Sigmoid), r=[rA], w=[rStgG[bs]])
                            DMA(Gs[qs:qs + psub, :], stgG[bs][:psub, :], dsStgG[bs],
                                r=[rStgG[bs]], w=[rGs])
                    S.barrier()

        if stop_after >= 2:
            with ExitStack() as p2:
                KT = [[sb(p2, f"KT{s}_{m}", [128, NT], BF16) for m in range(2)] for s in range(2)]
                VT = [sb(p2, f"VT{s}", [128, NKB, 129], BF16) for s in range(2)]
                QT = [[sb(p2, f"QT{s}_{m}", [128, NOWN], BF16) for m in range(2)] for s in range(2)]
                rKV = [Res(f"KV{s}", True) for s in range(2)]
                dsC2 = dsem("ds_c2")
                dsSig = [dsem(f"ds_sig{i}") for i in range(2)]
                rSig = [Res(f"sig{i}", True) for i in range(2)]
                dsKV = [dsem(f"ds_kv{s}") for s in range(2)]
                beta = sb(p2, "beta", [128, 8 * 5 * NKB], F32)
                dtab = sb(p2, "dtab", [128, 896], F32)
                gsubb = sb(p2, "gsubb", [128, 128], F32)
                lamb = sb(p2, "lamb", [128, 4, 64], F32)
                lamt = sb(p2, "lamt", [128, 2, 64], F32)
                lame = sb(p2, "lame", [128, 2], F32)
                lamneg = sb(p2, "lamneg", [128, 1], F32)
                rLam = Res("lam")
                DMA(beta[:, :], beta_d[:, :], dsC2, w=[rC])
                DMA(dtab[:, :], dtab_d[:, :], dsC2, w=[rC])
                DMA(gsubb[:, :], gsub_d[0:1, :].partition_broadcast(128), dsC2, w=[rC])
                for i in range(4):
                    DMA(lamb[:, i, :], lam_d[i:i + 1, :].partition_broadcast(128), dsC2, w=[rC])
                for s in range(2):
                    for m in range(2):
                        DMA(KT[s][m][64:68, :], sigk_d[:, :], dsSig[s], w=[rSig[s]])
                    V(lambda e, s=s: e.memset(VT[s][:, :, 128:129], 1.0), w=[rSig[s]])
                V(lambda e: e.tensor_tensor(out=lamt[:, 0, :], in0=lamb[:, 0, :], in1=lamb[:, 1, :], op=ALU.mult),
                  r=[rC], w=[rLam])
                V(lambda e: e.tensor_tensor(out=lamt[:, 1, :], in0=lamb[:, 2, :], in1=lamb[:, 3, :], op=ALU.mult),
                  r=[rC], w=[rLam])
                V(lambda e: e.tensor_reduce(out=lame[:, 0:2], in_=lamt[:, :, :], axis=AX.X, op=ALU.add),
                  r=[rLam], w=[rLam])
                A(lambda e: e.activation(out=lame[:, 0:2], in_=lame[:, 0:2], func=AF.Exp), r=[rLam], w=[rLam])
                V(lambda e: e.tensor_tensor(out=lamneg[:, :], in0=lame[:, 1:2], in1=lame[:, 0:1], op=ALU.subtract),
                  r=[rLam], w=[rLam])
                V(lambda e: e.tensor_scalar(out=lamneg[:, :], in0=lamneg[:, :], scalar1=-LAM_INIT, scalar2=None,
                                            op0=ALU.add), r=[rLam], w=[rLam])
                V(lambda e: e.tensor_scalar(out=gsubb[:, :], in0=gsubb[:, :], scalar1=1.0 - LAM_INIT,
                                            scalar2=None, op0=ALU.mult), r=[rC], w=[rC])

                PT = [sb(p2, f"PT{i}", [128, 1024], BF16) for i in range(3)]
                rPT = [Res(f"PT{i}") for i in range(3)]
                smix = [sb(p2, f"smix{i}", [128, 1024], F32) for i in range(2)]
                rSmix = [Res(f"smix{i}") for i in range(2)]
                Sps = [psum(p2, f"Sps{i}", [128, 1024], F32) for i in range(2)]
                rSps = [Res(f"Sps{i}", psum=True) for i in range(2)]
                Ops = psum(p2, "Ops", [128, 1536], F32)
                rOps = Res("Ops", psum=True)
                ev = {n: sb(p2, "ev_" + n, shp, F32) for n, shp in
                      [("r1", [128, 8]), ("t2", [128, 128]), ("dd", [128, 128]), ("ssd", [128, 1]),
                       ("lnd", [128, 1]), ("rsd", [128, 1]), ("junk", [128, 128])]}
                rEv = Res("ev")
                stgO = [sb(p2, f"stgO{i}", [128, 4, 128], BF16) for i in range(2)]
                rStgO = [Res(f"stgO{i}") for i in range(2)]
                dsStgO = [dsem(f"ds_so{i}") for i in range(2)]

                def oacc(m, s, n=129):
                    i = m * 4 + s
                    off = (i // 3) * 512 + (i % 3) * 129
                    return Ops[:, off:off + n]

                jobs = [("A", h) for h in range(8)] + [("B", pi) for pi in range(4)]

                def load_job(ji):
                    kind, idx = jobs[ji]
                    s = ji % 2
                    if kind == "A":
                        for m in range(2):
                            for half in range(2):
                                c0, c1 = half * 4104, (half + 1) * 4104
                                DMA(KT[s][m][0:64, c0:c1], KAs[idx, m * 64:(m + 1) * 64, c0:c1], dsKV[s],
                                    r=[rKAs], w=[rKV[s]])
                            DMA(QT[s][m][0:64, :], QAs[idx, m * 64:(m + 1) * 64, :], dsKV[s],
                                r=[rQAs], w=[rKV[s]])
                            DMA(QT[s][m][64:68, :], qaug_d[idx, :, :], dsKV[s], w=[rKV[s]])
                        for q4 in range(4):
                            b0, b1 = q4 * 16, (q4 + 1) * 16
                            DMA(VT[s][:, b0:b1, 0:128], VAs[idx, :, b0:b1, :], dsKV[s], r=[rVAs], w=[rKV[s]])
                        DMA(VT[s][0:16, 64:65, 0:128], VAs[idx, 0:16, 64:65, :], dsKV[s], r=[rVAs], w=[rKV[s]])
                    else:
                        kv = idx // 2
                        for half in range(2):
                            c0, c1 = half * 4104, (half + 1) * 4104
                            DMA(KT[s][0][:, c0:c1], KBs[kv, :, c0:c1], dsKV[s], r=[rKBs], w=[rKV[s], rSig[s]])
                        for m in range(2):
                            DMA(QT[s][m][:, :], QBs[2 * idx + m, :, :], dsKV[s], r=[rQBs], w=[rKV[s]])
                        for q4 in range(4):
                            b0, b1 = q4 * 16, (q4 + 1) * 16
                            DMA(VT[s][:, b0:b1, 0:128], VBs[kv, :, b0:b1, :], dsKV[s], r=[rVBs], w=[rKV[s]])
                        DMA(VT[s][0:16, 64:65, 0:128], VBs[kv, 0:16, 64:65, :], dsKV[s], r=[rVBs], w=[rKV[s]])

                CH = [(c * 512, 512, 4, 128) for c in range(4)] + [(2048, NMETA, 1, NMETA)]
                load_job(0)
                pt_i = [0]
                so_i = [0]
                for ji, (kind, idx) in enumerate(jobs):
                    s = ji % 2
                    if ji + 1 < len(jobs):
                        load_job(ji + 1)
                    isA = kind == "A"
                    KR = 68 if isA else 128
                    slope = 2.0 ** (-(idx + 1)) if isA else 0.0
                    scale = 0.125 if isA else 128.0 ** -0.5
                    Kt = [KT[s][0], KT[s][1] if isA else KT[s][0]]
                    Qt = QT[s]
                    for c, (q0, ncq, nsb, psq) in enumerate(CH):
                        V(lambda e: e.memset(Ops[:, :], 0.0), w=[rOps])

                        def qk(kb, sp):
                            kp = 128 if kb < 64 else NMETA
                            k0 = kb * 128
                            mixed = isA and c < 4 and (4 * c <= kb <= 4 * c + 3)
                            rows = 64 if mixed else KR
                            for m in range(2):
                                P(lambda e, m=m, sp=sp, kp=kp, k0=k0, rows=rows: e.matmul(
                                    Sps[sp][:kp, m * 512: m * 512 + ncq],
                                    lhsT=Kt[m][0:rows, k0:k0 + kp], rhs=Qt[m][0:rows, q0:q0 + ncq],
                                    start=True, stop=True),
                                  r=[rKV[s], rSig[s]], w=[rSps[sp]], inc=(m == 1))

                        def soft_pv(kb, sp, is_last):
                            kp = 128 if kb < 64 else NMETA
                            mixed = isA and c < 4 and (4 * c <= kb <= 4 * c + 3)
                            pi_ = pt_i[0] % 3
                            pt_i[0] += 1
                            if ncq == 512:
                                src = Sps[sp][:kp, :]
                                dst = PT[pi_][:kp, :]
                            else:
                                src = Sps[sp][:kp, :].rearrange("p (m q) -> p m q", m=2)[:, :, 0:ncq]
                                dst = PT[pi_][:kp, :].rearrange("p (m q) -> p m q", m=2)[:, :, 0:ncq]
                            if mixed:
                                mm = kb - 4 * c
                                x0 = 384 - 128 * mm
                                for m in range(2):
                                    V(lambda e, m=m, sp=sp, x0=x0: e.scalar_tensor_tensor(
                                        out=smix[sp][:, m * 512:(m + 1) * 512], in0=dtab[:, x0:x0 + 512],
                                        scalar=-8.0 * slope, in1=Sps[sp][:, m * 512:(m + 1) * 512],
                                        op0=ALU.mult, op1=ALU.add),
                                      r=[rSps[sp], rC], w=[rSmix[sp]])
                                A(lambda e, sp=sp, dst=dst: e.activation(out=dst, in_=smix[sp][:, :], func=AF.Exp,
                                                                         scale=scale),
                                  r=[rSmix[sp]], w=[rPT[pi_]])
                            elif isA:
                                bcol = (idx * 5 + c) * NKB + kb
                                A(lambda e, src=src, dst=dst, bcol=bcol, kp=kp: e.activation(
                                    out=dst, in_=src, func=AF.Exp, bias=beta[:kp, bcol:bcol + 1], scale=scale),
                                  r=[rSps[sp], rC], w=[rPT[pi_]])
                            else:
                                A(lambda e, src=src, dst=dst: e.activation(out=dst, in_=src, func=AF.Exp,
                                                                           scale=scale),
                                  r=[rSps[sp]], w=[rPT[pi_]])
                            for m in range(2):
                                for sq in range(nsb):
                                    last = (m == 1 and sq == nsb - 1)
                                    P(lambda e, m=m, sq=sq, pi_=pi_, kp=kp, kb=kb: e.matmul(
                                        oacc(m, sq)[:psq, :],
                                        lhsT=PT[pi_][:kp, m * 512 + sq * 128: m * 512 + sq * 128 + psq],
                                        rhs=VT[s][:kp, kb, :], start=False, stop=is_last,
                                        skip_group_check=True),
                                      r=[rPT[pi_], rKV[s], rSig[s]], w=[rOps], inc=last)

                        kbs = []
                        for kb in range(64):
                            if (not isA) or c == 4:
                                kbs.append(kb)
                                continue
                            lo_q, hi_q = 512 * c, 512 * c + 511
                            cands = [(128 * kb, 128 * kb + 127)]
                            if kb >= 16:
                                cands.append((128 * kb - 8192, 128 * kb + 127 - 8192))
                            dmin = min(max(0, ul - hi_q, lo_q - uh) for ul, uh in cands)
                            if slope * dmin <= 100.0:
                                kbs.append(kb)
                        kbs.append(64)
                        qk(kbs[0], 0)
                        for i, kb in enumerate(kbs):
                            if i + 1 < len(kbs):
                                qk(kbs[i + 1], (i + 1) % 2)
                            soft_pv(kb, i % 2, i == len(kbs) - 1)
                        so = so_i[0] % 2
                        so_i[0] += 1
                        for sq in range(nsb if isA else 0):
                            if isA:
                                O1, O2 = oacc(0, sq), oacc(1, sq)
                                V(lambda e, O1=O1: e.reciprocal(out=ev["r1"][:psq, 0:1], in_=O1[:psq, 128:129]),
                                  r=[rOps], w=[rEv])
                                V(lambda e, O2=O2: e.reciprocal(out=ev["r1"][:psq, 1:2], in_=O2[:psq, 128:129]),
                                  r=[rOps], w=[rEv])
                                V(lambda e: e.tensor_tensor(out=ev["r1"][:psq, 2:3], in0=ev["r1"][:psq, 1:2],
                                                            in1=lamneg[:psq, 0:1], op=ALU.mult),
                                  r=[rEv, rLam], w=[rEv])
                                V(lambda e, O2=O2: e.tensor_scalar(out=ev["t2"][:psq, :], in0=O2[:psq, 0:128],
                                                                   scalar1=ev["r1"][:psq, 2:3], scalar2=None,
                                                                   op0=ALU.mult), r=[rOps, rEv], w=[rEv])
                                V(lambda e, O1=O1: e.scalar_tensor_tensor(
                                    out=ev["dd"][:psq, :], in0=O1[:psq, 0:128], scalar=ev["r1"][:psq, 0:1],
                                    in1=ev["t2"][:psq, :], op0=ALU.mult, op1=ALU.add), r=[rOps, rEv], w=[rEv])
                                A(lambda e: e.activation(out=ev["junk"][:psq, :], in_=ev["dd"][:psq, :],
                                                         func=AF.Square, accum_out=ev["ssd"][:psq, 0:1]),
                                  r=[rEv], w=[rEv])
                                rstd_from_ss(ev["ssd"][:psq, 0:1], ev["lnd"][:psq, 0:1], ev["rsd"][:psq, 0:1],
                                             128.0, [rEv], [rEv])
                                V(lambda e, sq=sq, so=so: e.scalar_tensor_tensor(
                                    out=stgO[so][:psq, sq, :], in0=ev["dd"][:psq, :], scalar=ev["rsd"][:psq, 0:1],
                                    in1=gsubb[:psq, :], op0=ALU.mult, op1=ALU.mult),
                                  r=[rEv, rC], w=[rStgO[so]])
                        if isA:
                            dst_d = OAs[q0:q0 + ncq, idx * 128:(idx + 1) * 128].rearrange("(s p) e -> p s e", p=psq)
                            DMA(dst_d, stgO[so][:psq, 0:nsb, :], dsStgO[so], r=[rStgO[so]], w=[rOAs])
                        else:
                            for m in range(2):
                                g = 2 * idx + m
                                so = so_i[0] % 2
                                so_i[0] += 1
                                for sq in range(nsb):
                                    Om = oacc(m, sq)
                                    V(lambda e, Om=Om: e.reciprocal(out=ev["r1"][:psq, 0:1], in_=Om[:psq, 128:129]),
                                      r=[rOps], w=[rEv])
                                    V(lambda e, Om=Om, sq=sq, so=so: e.tensor_scalar(
                                        out=stgO[so][:psq, sq, :], in0=Om[:psq, 0:128], scalar1=ev["r1"][:psq, 0:1],
                                        scalar2=None, op0=ALU.mult), r=[rOps, rEv], w=[rStgO[so]])
                                dst_d = OBs[q0:q0 + ncq, g * 128:(g + 1) * 128].rearrange("(s p) e -> p s e", p=psq)
                                DMA(dst_d, stgO[so][:psq, 0:nsb, :], dsStgO[so], r=[rStgO[so]], w=[rOBs])
                S.barrier()

        if stop_after >= 3:
            with ExitStack() as p3:
                H = sb(p3, "H", [128, 17, D], F32)
                rH = [Res(f"H{t}") for t in range(17)]
                u2T = sb(p3, "u2T", [128, 8, NOWN], BF16)
                rU2 = [Res(f"u2T{t}") for t in range(17)]
                AFF = sb(p3, "AFF", [128, 17, NEXP], F32)
                rAFF = Res("AFF")
                COEF = sb(p3, "COEF", [128, 17, NEXP], F32)
                rCOEF = Res("COEF")
                rAgin = Res("agin", True)
                gffnb = sb(p3, "gffnb", [128, D], F32)
                dsC3 = dsem("ds_c3")
                DMA(gffnb[:, :], gffn_d[0:1, :].partition_broadcast(128), dsC3, w=[rC])
                BLK = [(t * 128, 128) for t in range(16)] + [(2048, NMETA)]

                with ExitStack() as p3a:
                    Wa = sb(p3a, "Wa", [128, 8, D], BF16)
                    Wb = sb(p3a, "Wb", [128, 8, D], BF16)
                    Wo = sb(p3a, "Wo", [128, 8, D], BF16)
                    wr = sb(p3a, "wr", [128, 8, NEXP], BF16)
                    rW3 = Res("W3", True)
                    dsW3 = dsem("ds_w3")
                    for c in range(8):
                        DMA(Wa[:, c, :], wa_d.rearrange("(c p) f -> p c f", p=128)[:, c, :], dsW3, w=[rW3], q="pool")
                        DMA(Wb[:, c, :], wb_d.rearrange("(c p) f -> p c f", p=128)[:, c, :], dsW3, w=[rW3], q="pool")
                        DMA(Wo[:, c, :], wo_d.rearrange("(c p) f -> p c f", p=128)[:, c, :], dsW3, w=[rW3], q="pool")
                    DMA(wr[:, :, :], wr_d.rearrange("(c p) f -> p c f", p=128), dsW3, w=[rW3], q="pool")
                    oa_t = [sb(p3a, f"oa_t{i}", [128, D], BF16) for i in range(2)]
                    ob_t = [sb(p3a, f"ob_t{i}", [128, D], BF16) for i in range(2)]
                    g_t = [sb(p3a, f"g_t{i}", [128, 2048], BF16) for i in range(2)]
                    x_t3 = [sb(p3a, f"x_t3{i}", [128, D], F32) for i in range(2)]
                    rIn3 = [Res(f"in3_{i}", True) for i in range(2)]
                    dsIn3 = [dsem(f"ds_in3_{i}") for i in range(2)]
                    oaT = sb(p3a, "oaT", [128, 8, 128], BF16)
                    obT = sb(p3a, "obT", [128, 8, 128], BF16)
                    mgT = sb(p3a, "mgT", [128, 8, 128], BF16)
                    rOaT, rObT, rMgT = Res("oaT"), Res("obT"), Res("mgT")
                    m1 = sb(p3a, "m1", [128, D], F32)
                    m2 = sb(p3a, "m2", [128, D], BF16)
                    affS = [sb(p3a, f"affS{i}", [NEXP, 128], F32) for i in range(2)]
                    rAffS = [Res(f"affS{i}") for i in range(2)]
                    dsAffS = [dsem(f"ds_affs{i}") for i in range(2)]
                    mg = sb(p3a, "mg", [128, D], BF16)
                    rM1, rM2, rMg = Res("m1"), Res("m2"), Res("mg")
                    u2 = sb(p3a, "u2", [128, D], BF16)
                    rU2t = Res("u2")
                    st3 = {n: sb(p3a, "st3_" + n, [128, 1], F32) for n in ("ss", "ln", "rs", "se", "rse")}
                    ex3 = sb(p3a, "ex3", [128, NEXP], F32)
                    rSt3 = Res("st3")
                    tp3 = [psum(p3a, f"tp3_{i}", [128, 1024], BF16) for i in range(2)]
                    rTp3 = [Res(f"tp3_{i}", psum=True) for i in range(2)]
                    yps = [psum(p3a, f"yps{i}", [128, 512], F32) for i in range(4)]
                    rYps = [Res(f"yps{i}", psum=True) for i in range(4)]
                    lps = psum(p3a, "lps", [128, 512], F32)
                    rLps = Res("lps", psum=True)
                    tfp = psum(p3a, "tfp", [128, 512], F32)
                    rTfp = Res("tfp", psum=True)

                    def load3(t):
                        q0, pb_ = BLK[t]
                        sl = t % 2
                        tokx = q0 if t < 16 else SEQ
                        DMA(oa_t[sl][:pb_, :], OAs[q0:q0 + pb_, :], dsIn3[sl], r=[rOAs], w=[rIn3[sl]])
                        DMA(ob_t[sl][:pb_, :], OBs[q0:q0 + pb_, :], dsIn3[sl], r=[rOBs], w=[rIn3[sl]])
                        DMA(g_t[sl][:pb_, :], Gs[q0:q0 + pb_, :], dsIn3[sl], r=[rGs], w=[rIn3[sl]])
                        DMA(x_t3[sl][:pb_, :], hx[tokx:tokx + pb_, :], dsIn3[sl], w=[rIn3[sl]])

                    def transpose8(src, rsrc, dstT, rdst, pb_, tpi):
                        for fc in range(8):
                            P(lambda e, fc=fc: e.transpose(out=tp3[tpi][:, fc * 128: fc * 128 + pb_],
                                                           in_=src[:pb_, fc * 128:(fc + 1) * 128],
                                                           identity=identb[:pb_, :pb_]),
                              r=[rsrc, rC], w=[rTp3[tpi]], inc=(fc == 7))

                    load3(0)
                    for t, (q0, pb_) in enumerate(BLK):
                        sl = t % 2
                        if t + 1 < len(BLK):
                            load3(t + 1)
                        transpose8(oa_t[sl], rIn3[sl], oaT, rOaT, pb_, 0)
                        V(lambda e: e.tensor_copy(out=oaT[:, :, :pb_],
                                                  in_=tp3[0][:, :].rearrange("p (c t) -> p c t", c=8)[:, :, :pb_]),
                          r=[rTp3[0]], w=[rOaT])
                        transpose8(ob_t[sl], rIn3[sl], obT, rObT, pb_, 1)
                        A(lambda e: e.activation(out=obT[:, :, :pb_],
                                                 in_=tp3[1][:, :].rearrange("p (c t) -> p c t", c=8)[:, :, :pb_],
                                                 func=AF.Copy),
                          r=[rTp3[1]], w=[rObT])
                        for g in range(2):
                            for fc in range(8):
                                P(lambda e, g=g, fc=fc: e.matmul(yps[g][:pb_, :], lhsT=oaT[:, fc, :pb_],
                                                                 rhs=Wa[:, fc, g * 512:(g + 1) * 512],
                                                                 start=(fc == 0), stop=(fc == 7)),
                                  r=[rOaT, rW3], w=[rYps[g]], inc=(fc == 7))
                        for g in range(2):
                            for fc in range(8):
                                P(lambda e, g=g, fc=fc: e.matmul(yps[2 + g][:pb_, :], lhsT=obT[:, fc, :pb_],
                                                                 rhs=Wb[:, fc, g * 512:(g + 1) * 512],
                                                                 start=(fc == 0), stop=(fc == 7)),
                                  r=[rObT, rW3], w=[rYps[2 + g]], inc=(fc == 7))
                        for g in range(2):
                            V(lambda e, g=g: e.tensor_tensor(out=m1[:pb_, g * 512:(g + 1) * 512], in0=yps[g][:pb_, :],
                                                             in1=g_t[sl][:pb_, g * 512:(g + 1) * 512], op=ALU.mult),
                              r=[rYps[g], rIn3[sl]], w=[rM1])
                            V(lambda e, g=g: e.tensor_tensor(out=m2[:pb_, g * 512:(g + 1) * 512],
                                                             in0=yps[2 + g][:pb_, :],
                                                             in1=g_t[sl][:pb_, 1024 + g * 512:1024 + (g + 1) * 512],
                                                             op=ALU.mult),
                              r=[rYps[2 + g], rIn3[sl]], w=[rM2])
                        V(lambda e: e.tensor_tensor(out=mg[:pb_, :], in0=m1[:pb_, :], in1=m2[:pb_, :], op=ALU.add),
                          r=[rM1, rM2], w=[rMg])
                        transpose8(mg, rMg, mgT, rMgT, pb_, 0)
                        V(lambda e: e.tensor_copy(out=mgT[:, :, :pb_],
                                                  in_=tp3[0][:, :].rearrange("p (c t) -> p c t", c=8)[:, :, :pb_]),
                          r=[rTp3[0]], w=[rMgT])
                        for g in range(2):
                            for fc in range(8):
                                P(lambda e, g=g, fc=fc: e.matmul(yps[g][:pb_, :], lhsT=mgT[:, fc, :pb_],
                                                                 rhs=Wo[:, fc, g * 512:(g + 1) * 512],
                                                                 start=(fc == 0), stop=(fc == 7)),
                                  r=[rMgT, rW3], w=[rYps[g]], inc=(fc == 7))
                        for g in range(2):
                            V(lambda e, g=g, t=t: e.tensor_tensor(out=H[:pb_, t, g * 512:(g + 1) * 512],
                                                                  in0=yps[g][:pb_, :],
                                                                  in1=x_t3[sl][:pb_, g * 512:(g + 1) * 512],
                                                                  op=ALU.add),
                              r=[rYps[g], rIn3[sl]], w=[rH[t]])
                        if dbg:
                            DMA(dbg_h1[q0:q0 + pb_, :], H[:pb_, t, :], dsDbg, r=[rH[t]])
                        A(lambda e, t=t: e.activation(out=u2[:pb_, :], in_=H[:pb_, t, :], func=AF.Square,
                                                      accum_out=st3["ss"][:pb_, 0:1]), r=[rH[t]], w=[rSt3, rU2t])
                        rstd_from_ss(st3["ss"][:pb_, 0:1], st3["ln"][:pb_, 0:1], st3["rs"][:pb_, 0:1], float(D),
                                     [rSt3], [rSt3])
                        V(lambda e, t=t: e.scalar_tensor_tensor(out=u2[:pb_, :], in0=H[:pb_, t, :],
                                                                scalar=st3["rs"][:pb_, 0:1], in1=gffnb[:pb_, :],
                                                                op0=ALU.mult, op1=ALU.mult),
                          r=[rH[t], rSt3, rC], w=[rU2t])
                        transpose8(u2, rU2t, None, None, pb_, 1)
                        A(lambda e, q0=q0: e.activation(
                            out=u2T[:, :, q0:q0 + pb_],
                            in_=tp3[1][:, :].rearrange("p (c t) -> p c t", c=8)[:, :, :pb_], func=AF.Copy),
                          r=[rTp3[1]], w=[rU2[t]])
                        for fc in range(8):
                            P(lambda e, fc=fc, q0=q0: e.matmul(lps[:pb_, 0:NEXP], lhsT=u2T[:, fc, q0:q0 + pb_],
                                                               rhs=wr[:, fc, :], start=(fc == 0), stop=(fc == 7)),
                              r=[rU2[t], rW3], w=[rLps], inc=(fc == 7))
                        A(lambda e: e.activation(out=ex3[:pb_, :], in_=lps[:pb_, 0:NEXP], func=AF.Exp,
                                                 accum_out=st3["se"][:pb_, 0:1]), r=[rLps], w=[rSt3])
                        V(lambda e: e.reciprocal(out=st3["rse"][:pb_, 0:1], in_=st3["se"][:pb_, 0:1]),
                          r=[rSt3], w=[rSt3])
                        V(lambda e, t=t: e.tensor_scalar(out=AFF[:pb_, t, :], in0=ex3[:pb_, :],
                                                         scalar1=st3["rse"][:pb_, 0:1], scalar2=None, op0=ALU.mult),
                          r=[rSt3], w=[rAFF])
                        P(lambda e, t=t: e.transpose(out=tfp[0:NEXP, 0:pb_], in_=AFF[:pb_, t, :],
                                                     identity=identf[:pb_, :pb_]), r=[rAFF, rC], w=[rTfp])
                        V(lambda e, sl=sl: e.tensor_copy(out=affS[sl][:, 0:pb_], in_=tfp[0:NEXP, 0:pb_]),
                          r=[rTfp], w=[rAffS[sl]])
                        DMA(agin.ap()[:, q0:q0 + pb_], affS[sl][:, 0:pb_], dsAffS[sl], r=[rAffS[sl]], w=[rAgin])
                        if dbg:
                            DMA(dbg_aff[q0:q0 + pb_, :], AFF[:pb_, t, :], dsDbg, r=[rAFF])
                    S.barrier()

                if stop_after >= 4:
                    with ExitStack() as p4:
                        AGc = sb(p4, "AGc", [NEXP, SEQ + NMETA], F32)
                        rAG = Res("AGc", True)
                        dsAG = dsem("ds_ag")
                        dsCC = DSem(sem("cc_sem"))
                        rAgout = Res("agout")
                        S.coll(lambda e: e.collective_compute(
                            "AllGather", ALU.bypass, replica_groups=[[0, 1, 2, 3], [4, 5, 6, 7]],
                            ins=[agin.ap().opt()], outs=[agout.ap().opt()]), dsCC, reads=[rAgin], writes=[rAgout])
                        ago = agout.ap()
                        DMA(AGc[:, 0:SEQ].rearrange("e (r t) -> e r t", r=4),
                            ago.rearrange("(r e) t -> e r t", e=NEXP)[:, :, 0:2048], dsAG, r=[rAgout], w=[rAG],
                            q="pool")
                        DMA(AGc[:, SEQ:SEQ + NMETA], ago[0:NEXP, 2048:2048 + NMETA], dsAG, r=[rAgout], w=[rAG],
                            q="pool")
                        lo = sb(p4, "lo", [NEXP, 1], F32)
                        mid = sb(p4, "mid", [NEXP, 1], F32)
                        cnt = sb(p4, "cnt", [NEXP, 1], F32)
                        prd = sb(p4, "prd", [NEXP, 1], F32)
                        cmpj = sb(p4, "cmpj", [NEXP, SEQ + NMETA], BF16)
                        rB = Res("bis")
                        V(lambda e: e.memset(lo[:, :], 0.0), w=[rB])
                        for it in range(NBIS):
                            ck = 2.0 ** (-(it + 1))
                            V(lambda e, ck=ck: e.tensor_scalar(out=mid[:, :], in0=lo[:, :], scalar1=ck, scalar2=None,
                                                               op0=ALU.add), r=[rB], w=[rB])
                            V(lambda e: e.tensor_scalar(out=cmpj[:, :], in0=AGc[:, :], scalar1=mid[:, 0:1],
                                                        scalar2=0.0, op0=ALU.is_ge, op1=ALU.add,
                                                        accum_out=cnt[:, 0:1]), r=[rB, rAG], w=[rB])
                            V(lambda e, ck=ck: e.tensor_scalar(out=prd[:, :], in0=cnt[:, :], scalar1=CAP - 0.5,
                                                               scalar2=ck, op0=ALU.is_ge, op1=ALU.mult),
                              r=[rB], w=[rB])
                            V(lambda e: e.tensor_tensor(out=lo[:, :], in0=lo[:, :], in1=prd[:, :], op=ALU.add),
                              r=[rB], w=[rB])
                        rThrD = Res("thr_d")
                        DMA(thr_d.rearrange("o e -> e o"), lo[:, :], dsAG, r=[rB], w=[rThrD])
                        if dbg:
                            DMA(dbg_thr.rearrange("o e -> e o"), lo[:, :], dsDbg, r=[rB])
                        THR = sb(p4, "THR", [128, NEXP], F32)
                        rTHR = Res("THR")
                        DMA(THR[:, :], thr_d[0:1, :].partition_broadcast(128), dsAG, r=[rThrD], w=[rTHR])
                        for t in range(16):
                            V(lambda e, t=t: e.tensor_tensor(out=COEF[:, t, :], in0=AFF[:, t, :], in1=THR[:, :],
                                                             op=ALU.is_ge), r=[rAFF, rTHR], w=[rCOEF])
                            V(lambda e, t=t: e.tensor_tensor(out=COEF[:, t, :], in0=COEF[:, t, :], in1=AFF[:, t, :],
                                                             op=ALU.mult), r=[rAFF, rCOEF], w=[rCOEF])
                        S.barrier()

                if stop_after >= 5:
                    with ExitStack() as p5:
                        wg = [sb(p5, f"wg{i}", [128, 8, 512], BF16) for i in range(2)]
                        wu = [sb(p5, f"wu{i}", [128, 8, 512], BF16) for i in range(2)]
                        wd = [sb(p5, f"wd{i}", [128, 4, D], BF16) for i in range(2)]
                        rWe = [Res(f"We{i}", True) for i in range(2)]
                        dsWe = [dsem(f"ds_we{i}") for i in range(2)]
                        hT = [sb(p5, f"hT{i}", [128, 4, 512], BF16) for i in range(2)]
                        rHT = [Res(f"hT{i}") for i in range(2)]
                        sg = [sb(p5, f"sg{i}", [128, 512], F32) for i in range(2)]
                        rSg = [Res(f"sg{i}") for i in range(2)]
                        gps = [psum(p5, f"gps{i}", [128, 512], F32) for i in range(2)]
                        ups = [psum(p5, f"ups{i}", [128, 512], F32) for i in range(2)]
                        rGps = [Res(f"gps{i}", psum=True) for i in range(2)]
                        rUps = [Res(f"ups{i}", psum=True) for i in range(2)]
                        ypm = [psum(p5, f"ypm{i}", [128, 512], F32) for i in range(4)]
                        rYpm = [Res(f"ypm{i}", psum=True) for i in range(4)]

                        def load_e(ei):
                            sl = ei % 2
                            for c in range(8):
                                DMA(wg[sl][:, c, :], wg_d[ei, c * 128:(c + 1) * 128, :], dsWe[sl], w=[rWe[sl]], q="pool")
                                DMA(wu[sl][:, c, :], wu_d[ei, c * 128:(c + 1) * 128, :], dsWe[sl], w=[rWe[sl]], q="pool")
                            for c in range(4):
                                DMA(wd[sl][:, c, :], wd_d[ei, c * 128:(c + 1) * 128, :], dsWe[sl], w=[rWe[sl]], q="pool")

                        load_e(0)
                        gi = [0]
                        yi = [0]
                        for ei in range(NEXP):
                            sl = ei % 2
                            if ei + 1 < NEXP:
                                load_e(ei + 1)
                            for tcn in range(4):
                                hs = (ei * 4 + tcn) % 2
                                rUs = [rU2[tcn * 4 + k] for k in range(4)]
                                for fc in range(4):
                                    gs = gi[0] % 2
                                    gi[0] += 1
                                    for dc in range(8):
                                        P(lambda e, gs=gs, dc=dc, fc=fc, tcn=tcn: e.matmul(
                                            gps[gs][:, :], lhsT=wg[sl][:, dc, fc * 128:(fc + 1) * 128],
                                            rhs=u2T[:, dc, tcn * 512:(tcn + 1) * 512], start=(dc == 0), stop=(dc == 7)),
                                          r=[rWe[sl]] + rUs, w=[rGps[gs]], inc=(dc == 7))
                                    for dc in range(8):
                                        P(lambda e, gs=gs, dc=dc, fc=fc, tcn=tcn: e.matmul(
                                            ups[gs][:, :], lhsT=wu[sl][:, dc, fc * 128:(fc + 1) * 128],
                                            rhs=u2T[:, dc, tcn * 512:(tcn + 1) * 512], start=(dc == 0), stop=(dc == 7)),
                                          r=[rWe[sl]] + rUs, w=[rUps[gs]], inc=(dc == 7))
                                    A(lambda e, gs=gs: e.activation(out=sg[gs][:, :], in_=gps[gs][:, :], func=AF.Silu),
                                      r=[rGps[gs]], w=[rSg[gs]])
                                    V(lambda e, gs=gs, fc=fc, hs=hs: e.tensor_tensor(
                                        out=hT[hs][:, fc, :], in0=ups[gs][:, :], in1=sg[gs][:, :], op=ALU.mult),
                                      r=[rUps[gs], rSg[gs]], w=[rHT[hs]])
                                for ts in range(4):
                                    t = tcn * 4 + ts
                                    for dh in range(2):
                                        ys = yi[0] % 4
                                        yi[0] += 1
                                        for fc in range(4):
                                            P(lambda e, ys=ys, fc=fc, ts=ts, dh=dh, hs=hs: e.matmul(
                                                ypm[ys][:, :], lhsT=hT[hs][:, fc, ts * 128:(ts + 1) * 128],
                                                rhs=wd[sl][:, fc, dh * 512:(dh + 1) * 512],
                                                start=(fc == 0), stop=(fc == 3)),
                                              r=[rHT[hs], rWe[sl]], w=[rYpm[ys]], inc=(fc == 3))
                                        V(lambda e, ys=ys, t=t, dh=dh, ei=ei: e.scalar_tensor_tensor(
                                            out=H[:, t, dh * 512:(dh + 1) * 512], in0=ypm[ys][:, :],
                                            scalar=COEF[:, t, ei:ei + 1], in1=H[:, t, dh * 512:(dh + 1) * 512],
                                            op0=ALU.mult, op1=ALU.add),
                                          r=[rYpm[ys], rCOEF, rH[t]], w=[rH[t]])
                        S.barrier()

                with ExitStack() as p6:
                    gfinb = sb(p6, "gfinb", [128, D], F32)
                    dsC6 = dsem("ds_c6")
                    DMA(gfinb[:, :], gfin_d[0:1, :].partition_broadcast(128), dsC6, w=[rC])
                    fo = [sb(p6, f"fo{i}", [128, D], F32) for i in range(2)]
                    rFo = [Res(f"fo{i}") for i in range(2)]
                    dsFo = [dsem(f"ds_fo{i}") for i in range(2)]
                    fj = sb(p6, "fj", [128, D], BF16)
                    fst = {n: sb(p6, "fst_" + n, [128, 1], F32) for n in ("ss", "ln", "rs")}
                    rFst = Res("fst")
                    for t in range(16):
                        sl = t % 2
                        A(lambda e, t=t: e.activation(out=fj[:, :], in_=H[:, t, :], func=AF.Square,
                                                      accum_out=fst["ss"][:, 0:1]), r=[rH[t]], w=[rFst])
                        rstd_from_ss(fst["ss"][:, 0:1], fst["ln"][:, 0:1], fst["rs"][:, 0:1], float(D),
                                     [rFst], [rFst])
                        V(lambda e, t=t, sl=sl: e.scalar_tensor_tensor(
                            out=fo[sl][:, :], in0=H[:, t, :], scalar=fst["rs"][:, 0:1], in1=gfinb[:, :],
                            op0=ALU.mult, op1=ALU.mult), r=[rH[t], rFst, rC], w=[rFo[sl]])
                        DMA(y_out[t * 128:(t + 1) * 128, :], fo[sl][:, :], dsFo[sl], r=[rFo[sl]])
                    S.barrier()
        else:
            with ExitStack() as pz:
                zt = sb(pz, "zt", [128, D], F32)
                rZ = Res("zt")
                dsZ = dsem("ds_z")
                V(lambda e: e.memset(zt[:, :], 0.0), w=[rZ])
                for t in range(16):
                    DMA(y_out[t * 128:(t + 1) * 128, :], zt[:, :], dsZ, r=[rZ])
                S.barrier()

        S.barrier()

        @block.tensor
        def _(eng):
            S.emit("pe", eng)

        @block.scalar
        def _(eng):
            S.emit("act", eng)

        @block.vector
        def _(eng):
            S.emit("dve", eng)

        @block.gpsimd
        def _(eng):
            S.emit("pool", eng)

        @block.sync
        def _(eng):
            S.emit("sp", eng)

    return nc


def _tables(r):
    bf = ml_dtypes.bfloat16
    jp = np.arange(SEQ)
    uj = np.where(jp + 2048 * r < SEQ, jp, jp - SEQ).astype(np.float64)
    sig = np.zeros((4, NT), np.float32)
    beta = np.zeros((128, 8, 5, NKB), np.float32)
    for c in range(4):
        before = uj < 512 * c
        sgn = np.where(before, 1.0, -1.0)
        sig[c, :SEQ] = sgn
        for h in range(8):
            slope = 2.0 ** (-(h + 1))
            b = sgn * slope * (uj - 512 * c - 256)
            beta[:, h, c, :64] = b.reshape(64, 128).T
    qaug = np.zeros((8, 4, NOWN), np.float32)
    a = np.arange(512) - 256
    for h in range(8):
        slope = 2.0 ** (-(h + 1))
        for c in range(4):
            qaug[h, c, c * 512:(c + 1) * 512] = -8.0 * slope * a
    x = np.arange(896)[None, :]
    bb = np.arange(128)[:, None]
    dtab = np.abs(x - bb - 384).astype(np.float32)
    t_true = (jp + 2048 * r) % SEQ
    row_id = (t_true // 64).astype(np.float32)
    col_id = (t_true % 64).astype(np.float32)
    inv_freq = (np.float32(10000.0) ** (-np.arange(0, 64, 2, dtype=np.float32) / np.float32(64))).astype(np.float32)
    ar = (row_id[:, None] * inv_freq[None, :]).astype(np.float32)
    ac = (col_id[:, None] * inv_freq[None, :]).astype(np.float32)
    rope = np.zeros((NT, 256), np.float32)
    rope[:, 0:128] = 1.0
    cr, sr, cc, sc = np.cos(ar), np.sin(ar), np.cos(ac), np.sin(ac)
    rope[:SEQ, 0:32] = cr
    rope[:SEQ, 32:64] = cr
    rope[:SEQ, 64:96] = cc
    rope[:SEQ, 96:128] = cc
    rope[:SEQ, 128:160] = -sr
    rope[:SEQ, 160:192] = sr
    rope[:SEQ, 192:224] = -sc
    rope[:SEQ, 224:256] = sc
    return dict(sigk=sig.astype(bf), qaug=qaug.astype(bf),
                beta=np.ascontiguousarray(beta.reshape(128, -1)), dtab=dtab, rope=rope)


def make_in_maps(inputs):
    bf = ml_dtypes.bfloat16
    x = np.asarray(inputs["x"], np.float32)
    meta = np.asarray(inputs["meta_tokens"], np.float32)
    f = lambda k: np.ascontiguousarray(np.asarray(inputs[k], np.float32))
    common = {
        "w_in": f("w_in")[0],
        "gmixT": np.ascontiguousarray(f("g_mix")[0].reshape(8, 128).T),
        "lamv": np.ascontiguousarray(np.stack([f("lambda_q1")[0], f("lambda_k1")[0],
                                               f("lambda_q2")[0], f("lambda_k2")[0]])),
        "g_subln": f("g_subln"), "g_qnorm": f("g_qnorm"), "g_knorm": f("g_knorm"),
        "w_branch_a": f("w_branch_a")[0], "w_branch_b": f("w_branch_b")[0], "w_out": f("w_out")[0],
        "g_ffn": f("g_ffn"), "w_router": f("w_router")[0],
        "w_gate": f("w_gate")[0], "w_up": f("w_up")[0], "w_down": f("w_down")[0],
        "g_final": f("g_final").reshape(1, D),
        "identb": np.eye(128, dtype=np.float32).astype(bf),
        "identf": np.eye(128, dtype=np.float32),
    }
    tabs = [_tables(r) for r in range(4)]
    maps = []
    for c in range(8):
        b, r = c // 4, c % 4
        hxv = np.concatenate([np.roll(x[b], -2048 * r, axis=0), meta], axis=0)
        m = dict(common)
        m.update(tabs[r])
        m["hx"] = np.ascontiguousarray(hxv)
        maps.append(m)
    return maps


_NC_CACHE = {}


def kernel(**inputs):
    if "nc" not in _NC_CACHE:
        _NC_CACHE["nc"] = build()
    nc = _NC_CACHE["nc"]
    maps = make_in_maps(inputs)
    res = run_bass_kernel_spmd(nc, maps, core_ids=list(range(8)))
    out = np.zeros((2, SEQ, D), np.float32)
    for c in range(8):
        b, r = c // 4, c % 4
        out[b, r * 2048:(r + 1) * 2048, :] = res.results[c]["y"]
    return out
```

```python
import numpy as np
import ml_dtypes
from contextlib import ExitStack
import concourse.bass as bass
import concourse.mybir as mybir
from concourse.bass_utils import run_bass_kernel_spmd

F32 = mybir.dt.float32
BF16 = mybir.dt.bfloat16
AF = mybir.ActivationFunctionType
ALU = mybir.AluOpType
AX = mybir.AxisListType

D = 1024
SEQ = 8192
NMETA = 16
NT = SEQ + NMETA
NOWN = 2048 + NMETA
NKB = 65
EPS = 1e-6
NEXP = 16
CAP = 2 * NT // NEXP
LAM_INIT = 0.2
NBIS = 28


import types


def _freeze(fn):
    if fn is None or fn.__closure__ is None:
        return fn
    cells = []
    for c in fn.__closure__:
        try:
            cells.append(types.CellType(c.cell_contents))
        except ValueError:
            cells.append(c)
    return types.FunctionType(fn.__code__, fn.__globals__, fn.__name__, fn.__defaults__, tuple(cells))


class Res:
    __slots__ = ("name", "w", "rd", "multi", "psum")

    def __init__(self, name, multi=False, psum=False):
        self.name = name
        self.w = {}
        self.rd = {}
        self.multi = multi
        self.psum = psum


class DSem:
    def __init__(self, sem):
        self.sem = sem
        self.count = 0


class Sched:
    ENGS = ("pe", "act", "dve", "pool", "sp")

    def __init__(self):
        self.prog = {e: [] for e in self.ENGS}
        self.cnt = {e: 0 for e in self.ENGS}
        self.sem = {}
        self.known = {e: {} for e in self.ENGS}
        self.all_dsems = []

    def _deps(self, eng, reads, writes):
        deps = {}
        known = self.known[eng]

        def add(tok, kind):
            sem, val, e = tok
            if e == eng and eng in ("pe", "sp"):
                return
            k = id(sem)
            if known.get(k, 0) >= val:
                return
            if k not in deps or deps[k][1] < val:
                deps[k] = (sem, val)

        for r in reads:
            for tok in r.w.values():
                add(tok, "raw")
            if r.psum:
                for tok in r.rd.values():
                    if tok[2] != eng:
                        add(tok, "rar")
        for w in writes:
            if not w.multi:
                for tok in w.w.values():
                    add(tok, "waw")
            for tok in w.rd.values():
                add(tok, "war")
        for k, (sem, val) in deps.items():
            known[k] = val
        return list(deps.values())

    def _record(self, tok, reads, writes):
        k = id(tok[0])
        for r in reads:
            r.rd[k] = tok
        for w in writes:
            if w.multi:
                w.w[k] = tok
            else:
                w.w = {k: tok}
                w.rd = {}

    def op(self, eng, fn, reads=(), writes=(), inc=True):
        deps = self._deps(eng, reads, writes)
        tok = (self.sem[eng], self.cnt[eng] + 1, eng)
        self._record(tok, reads, writes)
        if inc:
            self.cnt[eng] += 1
        self.prog[eng].append((deps, _freeze(fn), self.sem[eng] if inc else None, 1))

    def dma(self, q, fn, ds, reads=(), writes=()):
        deps = self._deps(q, reads, writes)
        ds.count += 16
        tok = (ds.sem, ds.count, None)
        self._record(tok, reads, writes)
        self.prog[q].append((deps, _freeze(fn), ds.sem, 16))

    def coll(self, fn, ds, reads=(), writes=()):
        deps = self._deps("pool", reads, writes)
        ds.count += 1
        tok = (ds.sem, ds.count, None)
        self._record(tok, reads, writes)
        self.prog["pool"].append((deps, _freeze(fn), ds.sem, None))

    def barrier(self):
        toks = []
        for e in self.ENGS:
            if e == "sp":
                continue
            if self.cnt[e] > 0:
                toks.append((self.sem[e], self.cnt[e]))
        for ds in self.all_dsems:
            if ds.count > 0:
                toks.append((ds.sem, ds.count))
        for e in self.ENGS:
            deps = []
            for sem, val in toks:
                if self.known[e].get(id(sem), 0) < val:
                    self.known[e][id(sem)] = val
                    deps.append((sem, val))
            if deps:
                self.prog[e].append((deps, None, None, 0))

    def emit(self, eng, handle):
        for deps, fn, sem, incv in self.prog[eng]:
            for s, v in deps:
                handle.wait_ge(s, v)
            if fn is None:
                continue
            ins = fn(handle)
            if sem is not None:
                if incv is None:
                    ins.then_inc(sem)
                else:
                    ins.then_inc(sem, incv)


def build(dbg=False, stop_after=99):
    nc = bass.Bass("TRN2", target_bir_lowering=False)
    S = Sched()

    def din(name, shape, dt=F32):
        return nc.dram_tensor(name, list(shape), dt, kind="ExternalInput").ap()

    def dscr(name, shape, dt):
        if dbg:
            return nc.dram_tensor(name, list(shape), dt, kind="ExternalOutput").ap()
        return nc.dram_tensor(name, list(shape), dt).ap()

    hx = din("hx", [NT, D])
    w_in = din("w_in", [D, 6656])
    gmixT_d = din("gmixT", [128, 8])
    lam_d = din("lamv", [4, 64])
    gsub_d = din("g_subln", [1, 128])
    gq_d = din("g_qnorm", [1, 128])
    gk_d = din("g_knorm", [1, 128])
    wa_d = din("w_branch_a", [D, D])
    wb_d = din("w_branch_b", [D, D])
    wo_d = din("w_out", [D, D])
    gffn_d = din("g_ffn", [1, D])
    wr_d = din("w_router", [D, NEXP])
    if stop_after >= 5:
        wg_d = din("w_gate", [NEXP, D, 512])
        wu_d = din("w_up", [NEXP, D, 512])
        wd_d = din("w_down", [NEXP, 512, D])
    gfin_d = din("g_final", [1, D])
    sigk_d = din("sigk", [4, NT], BF16)
    qaug_d = din("qaug", [8, 4, NOWN], BF16)
    beta_d = din("beta", [128, 8 * 5 * NKB])
    dtab_d = din("dtab", [128, 896])
    rope_d = din("rope", [NT, 256])
    identb_d = din("identb", [128, 128], BF16)
    identf_d = din("identf", [128, 128])
    y_out = nc.dram_tensor("y", [2048, D], F32, kind="ExternalOutput").ap()

    KAs = dscr("KAs", [8, 128, NT], BF16)
    QAs = dscr("QAs", [8, 128, NOWN], BF16)
    VAs = dscr("VAs", [8, 128, NKB, 128], BF16)
    KBs = dscr("KBs", [2, 128, NT], BF16)
    QBs = dscr("QBs", [8, 128, NOWN], BF16)
    VBs = dscr("VBs", [2, 128, NKB, 128], BF16)
    Gs = dscr("Gs", [NOWN, 2048], BF16)
    OAs = dscr("OAs", [NOWN, D], BF16)
    OBs = dscr("OBs", [NOWN, D], BF16)
    agin = nc.dram_tensor("agin", [NEXP, NOWN], F32)
    agout = nc.dram_tensor("agout", [4 * NEXP, NOWN], F32)
    thr_d = nc.dram_tensor("thr_d", [1, NEXP], F32).ap()
    dbg_aff = dscr("dbg_aff", [NOWN, NEXP], F32) if dbg else None
    dbg_h1 = dscr("dbg_h1", [NOWN, D], F32) if dbg else None
    dbg_thr = dscr("dbg_thr", [1, NEXP], F32) if dbg else None

    rKAs, rQAs, rVAs = Res("KAs", True), Res("QAs", True), Res("VAs", True)
    rKBs, rQBs, rVBs = Res("KBs", True), Res("QBs", True), Res("VBs", True)
    rGs, rOAs, rOBs = Res("Gs", True), Res("OAs", True), Res("OBs", True)

    with ExitStack() as top:
        def sem(name):
            return top.enter_context(nc.semaphore(name))

        for e in Sched.ENGS:
            S.sem[e] = sem("sem_" + e)

        def dsem(name):
            d = DSem(sem(name))
            S.all_dsems.append(d)
            return d

        def sb(es, name, shape, dt):
            return es.enter_context(nc.sbuf_tensor("s_" + name, list(shape), dt))

        def psum(es, name, shape, dt):
            return es.enter_context(nc.psum_tensor("p_" + name, list(shape), dt))

        block = top.enter_context(nc.Block())

        def V(fn, r=(), w=()):
            S.op("dve", fn, r, w)

        def A(fn, r=(), w=()):
            S.op("act", fn, r, w)

        def P(fn, r=(), w=(), inc=True):
            S.op("pe", fn, r, w, inc)

        def G(fn, r=(), w=()):
            S.op("pool", fn, r, w)

        def DMA(out, in_, ds, r=(), w=(), q="sp"):
            S.dma(q, (lambda e, o=out, i=in_: e.dma_start(out=o, in_=i)), ds, r, w)

        identb = sb(top, "identb", [128, 128], BF16)
        identf = sb(top, "identf", [128, 128], F32)
        epsT = sb(top, "epsT", [128, 1], F32)
        rC = Res("consts", True)
        dsDbg = dsem("ds_dbg")
        dsC = dsem("ds_const")
        DMA(identb[:, :], identb_d[:, :], dsC, w=[rC])
        DMA(identf[:, :], identf_d[:, :], dsC, w=[rC])
        V(lambda e: e.memset(epsT[:, :], EPS), w=[rC])

        def rstd_from_ss(ss_ap, ln_ap, out_ap, n, rs, ws):
            A(lambda e: e.activation(out=ln_ap, in_=ss_ap, func=AF.Ln,
                                     bias=epsT[:ln_ap.shape[0], 0:1], scale=1.0 / n),
              r=rs + [rC], w=ws)
            A(lambda e: e.activation(out=out_ap, in_=ln_ap, func=AF.Exp, scale=-0.5),
              r=ws, w=ws)

        STS = [(t * 512, 512, 4, 128) for t in range(16)] + [(SEQ, NMETA, 1, NMETA)]

        def is_own(st):
            return st < 4 or st == 16

        def q0_of(st):
            return st * 512 if st < 4 else 2048

        with ExitStack() as p1:
            uT_own = sb(p1, "uT_own", [128, 8, NOWN], BF16)
            rUown = [Res(f"uTown{i}") for i in range(5)]
            gmixT = sb(p1, "gmixT", [128, 8], F32)
            gqb = sb(p1, "gqb", [128, 128], F32)
            gkb = sb(p1, "gkb", [128, 128], F32)
            dsC1 = dsem("ds_c1")
            DMA(gmixT[:, :], gmixT_d[:, :], dsC1, w=[rC])
            DMA(gqb[:, :], gq_d[0:1, :].partition_broadcast(128), dsC1, w=[rC])
            DMA(gkb[:, :], gk_d[0:1, :].partition_broadcast(128), dsC1, w=[rC])

            def norm_rope(raw, H, psub, gb, cs_t, s, out_bf, scr, rs_extra, r_raw, r_scr, r_out):
                sq, ssk, lnk, rk, tmp, yy = scr
                V(lambda e: e.tensor_tensor(out=sq[:psub, :H * 128], in0=raw[:psub, :H * 128],
                                            in1=raw[:psub, :H * 128], op=ALU.mult),
                  r=[r_raw], w=[r_scr])
                V(lambda e: e.tensor_reduce(out=ssk[:psub, :H],
                                            in_=sq[:psub, :H * 128].rearrange("p (h d) -> p h d", h=H),
                                            axis=AX.X, op=ALU.add), r=[r_scr], w=[r_scr])
                rstd_from_ss(ssk[:psub, :H], lnk[:psub, :H], rk[:psub, :H], 128.0, [r_scr], [r_scr])
                for j in range(H):
                    yj = yy[:psub, j * 128:(j + 1) * 128]
                    V(lambda e, j=j, yj=yj: e.scalar_tensor_tensor(
                        out=yj, in0=raw[:psub, j * 128:(j + 1) * 128], scalar=rk[:psub, j:j + 1],
                        in1=gb[:psub, :], op0=ALU.mult, op1=ALU.mult), r=[r_raw, r_scr, rC], w=[r_scr])
                    y4 = yj.rearrange("p (a b c) -> p a b c", a=2, b=2)
                    t4 = tmp[:psub, j * 128:(j + 1) * 128].rearrange("p (a b c) -> p a b c", a=2, b=2)
                    sn4 = cs_t[:psub, s, 128:256].rearrange("p (a b c) -> p a b c", a=2, b=2)
                    V(lambda e, y4=y4, t4=t4, sn4=sn4: e.tensor_tensor(
                        out=t4[:, :, 0, :], in0=y4[:, :, 1, :], in1=sn4[:, :, 0, :], op=ALU.mult),
                      r=[r_scr] + rs_extra, w=[r_scr])
                    V(lambda e, y4=y4, t4=t4, sn4=sn4: e.tensor_tensor(
                        out=t4[:, :, 1, :], in0=y4[:, :, 0, :], in1=sn4[:, :, 1, :], op=ALU.mult),
                      r=[r_scr] + rs_extra, w=[r_scr])
                    V(lambda e, yj=yj: e.tensor_tensor(
                        out=yj, in0=yj, in1=cs_t[:psub, s, 0:128], op=ALU.mult),
                      r=[r_scr] + rs_extra, w=[r_scr])
                    V(lambda e, j=j, yj=yj: e.tensor_tensor(
                        out=out_bf[:psub, j * 128:(j + 1) * 128], in0=yj,
                        in1=tmp[:psub, j * 128:(j + 1) * 128], op=ALU.add),
                      r=[r_scr], w=[r_out])

            with ExitStack() as pa:
                wA = sb(pa, "wA", [128, 8, 2560], BF16)
                rWA = Res("wA", True)
                dsW = dsem("ds_w")
                w_in_v = w_in.rearrange("(c p) f -> p c f", p=128)
                for c in range(8):
                    DMA(wA[:, c, 0:2048], w_in_v[:, c, 1024:3072], dsW, w=[rWA], q="pool")
                    DMA(wA[:, c, 2048:2560], w_in_v[:, c, 4096:4608], dsW, w=[rWA], q="pool")
                xt = [sb(pa, f"xt{i}", [128, 4, D], F32) for i in range(2)]
                rXt = [Res(f"xt{i}", True) for i in range(2)]
                dsXc = [dsem(f"ds_xc{i}") for i in range(2)]
                dsX = [dsem(f"ds_x{i}") for i in range(2)]
                cs = [sb(pa, f"cs{i}", [128, 4, 256], F32) for i in range(2)]
                rCs = [Res(f"cs{i}", True) for i in range(2)]
                xsb = [sb(pa, f"xs{i}", [128, 4, D], BF16) for i in range(2)]
                rXsb = [Res(f"xs{i}") for i in range(2)]
                junk = sb(pa, "junk", [128, D], BF16)
                rJunk = Res("junk")
                ssb = [sb(pa, f"ss{i}", [128, 4], F32) for i in range(2)]
                lnvb = [sb(pa, f"lnv{i}", [128, 4], F32) for i in range(2)]
                rstdb = [sb(pa, f"rstd{i}", [128, 4], F32) for i in range(2)]
                rStb = [Res(f"stats{i}") for i in range(2)]
                uT_tmp = [sb(pa, f"uTt{i}", [128, 8, 512], BF16) for i in range(2)]
                rUt = [Res(f"uTt{i}") for i in range(2)]
                stgK = [sb(pa, f"stgK{i}", [128, 512], BF16) for i in range(3)]
                rStgK = [Res(f"stgK{i}") for i in range(3)]
                dsStgK = [dsem(f"ds_sk{i}") for i in range(3)]
                stgV = [sb(pa, f"stgV{i}", [128, 1024], BF16) for i in range(2)]
                rStgV = [Res(f"stgV{i}") for i in range(2)]
                dsStgV = [dsem(f"ds_sv{i}") for i in range(2)]
                stgVB = [sb(pa, f"stgVB{i}", [128, 256], BF16) for i in range(2)]
                rStgVB = [Res(f"stgVB{i}") for i in range(2)]
                dsStgVB = [dsem(f"ds_svb{i}") for i in range(2)]
                kraw = sb(pa, "kraw", [128, 256], F32)
                rKraw = Res("kraw")
                ksc = (sb(pa, "ksq", [128, 256], F32), sb(pa, "kss", [128, 2], F32),
                       sb(pa, "kln", [128, 2], F32), sb(pa, "krk", [128, 2], F32),
                       sb(pa, "ktmp", [128, 256], F32), sb(pa, "kyy", [128, 256], F32))
                rKsc = Res("ksc")
                krope = sb(pa, "krope", [128, 4, 256], BF16)
                rKropeS = [Res(f"krope{i}") for i in range(4)]
                stgKB = sb(pa, "stgKB", [128, 2, 512], BF16)
                rStgKB = Res("stgKB")
                dsStgKB = dsem("ds_skb")
                tpb = [psum(pa, f"tpb{i}", [128, 1024], BF16) for i in range(4)]
                rTp = [Res(f"tpb{i}", psum=True) for i in range(4)]
                acc = [psum(pa, f"acc{i}", [128, 512], F32) for i in range(3)]
                rAcc = [Res(f"acc{i}", psum=True) for i in range(3)]
                ktp = psum(pa, "ktp", [128, 1024], BF16)
                rKtp = Res("ktp", psum=True)
                acc_i = [0]

                def next_acc():
                    i = acc_i[0] % 3
                    acc_i[0] += 1
                    return acc[i], rAcc[i]

                def load_x(sti):
                    tok0, ntok, nsub, psub = STS[sti]
                    sl = sti % 2
                    DMA(xt[sl][:psub, 0:nsub, :],
                        hx[tok0:tok0 + ntok, :].rearrange("(s p) d -> p s d", p=psub),
                        dsX[sl], w=[rXt[sl]])
                    DMA(cs[sl][:psub, 0:nsub, :],
                        rope_d[tok0:tok0 + ntok, :].rearrange("(s p) d -> p s d", p=psub),
                        dsXc[sl], w=[rCs[sl]])

                load_x(0)

                def prep(sti):
                    tok0, ntok, nsub, psub = STS[sti]
                    sl = sti % 2
                    x_t = xt[sl]
                    ss, lnv, rstd, rSt, xs, rXs = ssb[sl], lnvb[sl], rstdb[sl], rStb[sl], xsb[sl], rXsb[sl]
                    for s in range(nsub):
                        A(lambda e, s=s: e.activation(out=junk[:psub, :], in_=x_t[:psub, s, :],
                                                      func=AF.Square, accum_out=ss[:psub, s:s + 1]),
                          r=[rXt[sl]], w=[rJunk, rSt])
                    rstd_from_ss(ss[:psub, :nsub], lnv[:psub, :nsub], rstd[:psub, :nsub], float(D),
                                 [rSt], [rSt])
                    for s in range(nsub):
                        V(lambda e, s=s: e.tensor_scalar(out=xs[:psub, s, :], in0=x_t[:psub, s, :],
                                                         scalar1=rstd[:psub, s:s + 1], scalar2=None,
                                                         op0=ALU.mult),
                          r=[rXt[sl], rSt], w=[rXs])

                prep(0)
                pend = []
                kcount = [0]
                vcount = [0]
                import os
                DBGL = os.environ.get("KDBG", "")
                for sti, (tok0, ntok, nsub, psub) in enumerate(STS):
                    sl = sti % 2
                    if "one" in DBGL and sti >= 1:
                        break
                    if sti + 1 < len(STS) and "one" not in DBGL:
                        load_x(sti + 1)
                    xs, rXs = xsb[sl], rXsb[sl]
                    own = is_own(sti)
                    if own:
                        q0 = q0_of(sti)
                        uT = uT_own
                        ucol = q0
                        rU = rUown[sti if sti < 4 else 4]
                    else:
                        uT = uT_tmp[sl]
                        ucol = 0
                        rU = rUt[sl]
                    for dc in range(8):
                        bank = dc // 2
                        half = dc % 2
                        for s in range(nsub):
                            P(lambda e, dc=dc, s=s, bank=bank, half=half: e.transpose(
                                out=tpb[bank][:, half * 512 + s * 128: half * 512 + s * 128 + psub],
                                in_=xs[:psub, s, dc * 128:(dc + 1) * 128],
                                identity=identb[:psub, :psub]),
                              r=[rXs, rC], w=[rTp[bank]], inc=(s == nsub - 1))
                        src = tpb[bank][:, half * 512: half * 512 + ntok]
                        dst = uT[:, dc, ucol:ucol + ntok]
                        if bank % 2 == 0:
                            V(lambda e, src=src, dst=dst, dc=dc: e.tensor_scalar(
                                out=dst, in0=src, scalar1=gmixT[:, dc:dc + 1], scalar2=None, op0=ALU.mult),
                              r=[rTp[bank], rC], w=[rU])
                        else:
                            A(lambda e, src=src, dst=dst, dc=dc: e.activation(
                                out=dst, in_=src, func=AF.Copy, scale=gmixT[:, dc:dc + 1]),
                              r=[rTp[bank], rC], w=[rU])
                    if sti + 1 < len(STS) and "one" not in DBGL:
                        prep(sti + 1)
                    if "noproj" in DBGL:
                        continue
                    for h in range(8 if "nokA" not in DBGL else 0):
                        a_t, rA = next_acc()
                        for dc in range(8):
                            P(lambda e, a_t=a_t, dc=dc, h=h: e.matmul(
                                a_t[:, :ntok], lhsT=wA[:, dc, h * 128:(h + 1) * 128],
                                rhs=uT[:, dc, ucol:ucol + ntok], start=(dc == 0), stop=(dc == 7)),
                              r=[rWA, rU], w=[rA], inc=(dc == 7))
                        ks = kcount[0] % 3
                        kcount[0] += 1
                        A(lambda e, a_t=a_t, ks=ks: e.activation(out=stgK[ks][:, :ntok], in_=a_t[:, :ntok],
                                                                 func=AF.Copy),
                          r=[rA], w=[rStgK[ks]])
                        DMA(KAs[h, :, tok0:tok0 + ntok], stgK[ks][:, :ntok], dsStgK[ks],
                            r=[rStgK[ks]], w=[rKAs])
                    if "notok" in DBGL:
                        continue
                    for s in range(nsub):
                        blk = (tok0 // 128) + s
                        vs = vcount[0] % 2
                        vcount[0] += 1
                        for g in range(2):
                            a_t, rA = next_acc()
                            for dc in range(8):
                                P(lambda e, a_t=a_t, dc=dc, g=g, s=s: e.matmul(
                                    a_t[:psub, :], lhsT=uT[:, dc, ucol + s * 128: ucol + s * 128 + psub],
                                    rhs=wA[:, dc, 1024 + g * 512: 1024 + (g + 1) * 512],
                                    start=(dc == 0), stop=(dc == 7)),
                                  r=[rWA, rU], w=[rA], inc=(dc == 7))
                            if g == 0:
                                A(lambda e, a_t=a_t, vs=vs: e.activation(
                                    out=stgV[vs][:psub, 0:512], in_=a_t[:psub, :], func=AF.Copy),
                                  r=[rA], w=[rStgV[vs]])
                            else:
                                V(lambda e, a_t=a_t, vs=vs: e.tensor_copy(
                                    out=stgV[vs][:psub, 512:1024], in_=a_t[:psub, :]),
                                  r=[rA], w=[rStgV[vs]])
                        if "novst" not in DBGL:
                          DMA(VAs[:, 0:psub, blk, :].rearrange("h p e -> p h e"),
                            stgV[vs][:psub, :].rearrange("p (h e) -> p h e", h=8),
                            dsStgV[vs], r=[rStgV[vs]], w=[rVAs])
                        a_t, rA = next_acc()
                        for dc in range(8):
                            P(lambda e, a_t=a_t, dc=dc, s=s: e.matmul(
                                a_t[:psub, :], lhsT=uT[:, dc, ucol + s * 128: ucol + s * 128 + psub],
                                rhs=wA[:, dc, 2048:2560], start=(dc == 0), stop=(dc == 7)),
                              r=[rWA, rU], w=[rA], inc=(dc == 7))
                        A(lambda e, a_t=a_t: e.activation(out=kraw[:psub, :], in_=a_t[:psub, 0:256],
                                                          func=AF.Copy), r=[rA], w=[rKraw])
                        A(lambda e, a_t=a_t, vs=vs: e.activation(out=stgVB[vs][:psub, :],
                                                                 in_=a_t[:psub, 256:512], func=AF.Copy),
                          r=[rA], w=[rStgVB[vs]])
                        if "novst" not in DBGL:
                          DMA(VBs[:, 0:psub, blk, :].rearrange("h p e -> p h e"),
                            stgVB[vs][:psub, :].rearrange("p (h e) -> p h e", h=2),
                            dsStgVB[vs], r=[rStgVB[vs]], w=[rVBs])
                        if "norope" in DBGL:
                            continue
                        norm_rope(kraw, 2, psub, gkb, cs[sl], s, krope[:, s, :], ksc,
                                  [rCs[sl]], rKraw, rKsc, rKropeS[s])
                        old = list(pend)
                        pend.clear()
                        for f_ in old:
                            f_()

                        def tr(s=s, psub=psub):
                            for j in range(2):
                                P(lambda e, j=j: e.transpose(
                                    out=ktp[:, j * 512 + s * 128: j * 512 + s * 128 + psub],
                                    in_=krope[:psub, s, j * 128:(j + 1) * 128],
                                    identity=identb[:psub, :psub]),
                                  r=[rKropeS[s], rC], w=[rKtp], inc=(j == 1))
                        pend.append(tr)
                    if "norope" in DBGL:
                        continue

                    def fin(ntok=ntok, tok0=tok0):
                        for j in range(2):
                            V(lambda e, j=j: e.tensor_copy(out=stgKB[:, j, :ntok],
                                                           in_=ktp[:, j * 512: j * 512 + ntok]),
                              r=[rKtp], w=[rStgKB])
                        for j in range(2):
                            DMA(KBs[j, :, tok0:tok0 + ntok], stgKB[:, j, :ntok], dsStgKB,
                                r=[rStgKB], w=[rKBs])
                    pend.append(fin)
                for f_ in pend:
                    f_()
                pend.clear()
                S.barrier()

            if stop_after >= 1.5:
                with ExitStack() as pb:
                    wB = sb(pb, "wB", [128, 8, 4096], BF16)
                    rWB = Res("wB", True)
                    dsWB = dsem("ds_wb")
                    w_in_v = w_in.rearrange("(c p) f -> p c f", p=128)
                    for c in range(8):
                        DMA(wB[:, c, 0:1024], w_in_v[:, c, 0:1024], dsWB, w=[rWB], q="pool")
                        DMA(wB[:, c, 1024:2048], w_in_v[:, c, 3072:4096], dsWB, w=[rWB], q="pool")
                        DMA(wB[:, c, 2048:4096], w_in_v[:, c, 4608:6656], dsWB, w=[rWB], q="pool")
                    csB = [sb(pb, f"csB{i}", [128, 4, 256], F32) for i in range(2)]
                    rCsB = [Res(f"csB{i}", True) for i in range(2)]
                    dsCsB = [dsem(f"ds_csb{i}") for i in range(2)]
                    stgQ = [sb(pb, f"stgQ{i}", [128, 512], BF16) for i in range(3)]
                    rStgQ = [Res(f"stgQ{i}") for i in range(3)]
                    dsStgQ = [dsem(f"ds_sq{i}") for i in range(3)]
                    qraw = sb(pb, "qraw", [128, 1024], F32)
                    rQraw = Res("qraw")
                    qsc = (sb(pb, "qsq", [128, 1024], F32), sb(pb, "qss", [128, 8], F32),
                           sb(pb, "qln", [128, 8], F32), sb(pb, "qrk", [128, 8], F32),
                           sb(pb, "qtmp", [128, 1024], F32), sb(pb, "qyy", [128, 1024], F32))
                    rQsc = Res("qsc")
                    qrope = sb(pb, "qrope", [128, 1024], BF16)
                    rQrope = Res("qrope")
                    stgQB = [sb(pb, f"stgQB{i}", [128, 8, 128], BF16) for i in range(2)]
                    rStgQB = [Res(f"stgQB{i}") for i in range(2)]
                    dsStgQB = [dsem(f"ds_sqb{i}") for i in range(2)]
                    stgG = [sb(pb, f"stgG{i}", [128, 2048], BF16) for i in range(2)]
                    rStgG = [Res(f"stgG{i}") for i in range(2)]
                    dsStgG = [dsem(f"ds_sg{i}") for i in range(2)]
                    accB = [psum(pb, f"accB{i}", [128, 512], F32) for i in range(6)]
                    rAccB = [Res(f"accB{i}", psum=True) for i in range(6)]
                    qtp = psum(pb, "qtp", [128, 1024], BF16)
                    rQtp = Res("qtp", psum=True)
                    accb_i = [0]

                    def next_accB():
                        i = accb_i[0] % 6
                        accb_i[0] += 1
                        return accB[i], rAccB[i]

                    own_sts = [0, 1, 2, 3, 16]
                    qcount = [0]
                    bcount = [0]
                    for oi, sti in enumerate(own_sts):
                        tok0, ntok, nsub, psub = STS[sti]
                        q0 = q0_of(sti)
                        rU = rUown[oi]
                        sl = oi % 2
                        DMA(csB[sl][:psub, 0:nsub, :],
                            rope_d[tok0:tok0 + ntok, :].rearrange("(s p) d -> p s d", p=psub),
                            dsCsB[sl], w=[rCsB[sl]])
                        for h in range(8):
                            a_t, rA = next_accB()
                            for dc in range(8):
                                P(lambda e, a_t=a_t, dc=dc, h=h: e.matmul(
                                    a_t[:, :ntok], lhsT=wB[:, dc, h * 128:(h + 1) * 128],
                                    rhs=uT_own[:, dc, q0:q0 + ntok], start=(dc == 0), stop=(dc == 7)),
                                  r=[rWB, rU], w=[rA], inc=(dc == 7))
                            ks = qcount[0] % 3
                            qcount[0] += 1
                            A(lambda e, a_t=a_t, ks=ks: e.activation(out=stgQ[ks][:, :ntok],
                                                                     in_=a_t[:, :ntok], func=AF.Copy),
                              r=[rA], w=[rStgQ[ks]])
                            DMA(QAs[h, :, q0:q0 + ntok], stgQ[ks][:, :ntok], dsStgQ[ks],
                                r=[rStgQ[ks]], w=[rQAs])
                        for s in range(nsub):
                            bs = bcount[0] % 2
                            bcount[0] += 1
                            qs = q0 + s * 128
                            for g in range(2):
                                a_t, rA = next_accB()
                                for dc in range(8):
                                    P(lambda e, a_t=a_t, dc=dc, g=g, qs=qs: e.matmul(
                                        a_t[:psub, :], lhsT=uT_own[:, dc, qs:qs + psub],
                                        rhs=wB[:, dc, 1024 + g * 512: 1024 + (g + 1) * 512],
                                        start=(dc == 0), stop=(dc == 7)),
                                      r=[rWB, rU], w=[rA], inc=(dc == 7))
                                A(lambda e, a_t=a_t, g=g: e.activation(
                                    out=qraw[:psub, g * 512:(g + 1) * 512], in_=a_t[:psub, :], func=AF.Copy),
                                  r=[rA], w=[rQraw])
                            norm_rope(qraw, 8, psub, gqb, csB[sl], s, qrope, qsc,
                                      [rCsB[sl]], rQraw, rQsc, rQrope)
                            for j in range(8):
                                P(lambda e, j=j: e.transpose(
                                    out=qtp[:, j * 128: j * 128 + psub],
                                    in_=qrope[:psub, j * 128:(j + 1) * 128],
                                    identity=identb[:psub, :psub]),
                                  r=[rQrope, rC], w=[rQtp], inc=(j == 7))
                            V(lambda e, bs=bs: e.tensor_copy(
                                out=stgQB[bs][:, :, :psub],
                                in_=qtp[:, :].rearrange("p (g t) -> p g t", g=8)[:, :, :psub]),
                              r=[rQtp], w=[rStgQB[bs]])
                            DMA(QBs[:, :, qs:qs + psub].rearrange("g p t -> p g t"),
                                stgQB[bs][:, :, :psub], dsStgQB[bs], r=[rStgQB[bs]], w=[rQBs])
                            for g in range(4):
                                a_t, rA = next_accB()
                                for dc in range(8):
                                    P(lambda e, a_t=a_t, dc=dc, g=g, qs=qs: e.matmul(
                                        a_t[:psub, :], lhsT=uT_own[:, dc, qs:qs + psub],
                                        rhs=wB[:, dc, 2048 + g * 512: 2048 + (g + 1) * 512],
                                        start=(dc == 0), stop=(dc == 7)),
                                      r=[rWB, rU], w=[rA], inc=(dc == 7))
                                A(lambda e, a_t=a_t, g=g, bs=bs: e.activation(
                                    out=stgG[bs][:psub, g * 512:(g + 1) * 512], in_=a_t[:psub, :],
                                    func=AF.Sigmoid), r=[rA], w=[rStgG[bs]])
                            DMA(Gs[qs:qs + psub, :], stgG[bs][:psub, :], dsStgG[bs],
                                r=[rStgG[bs]], w=[rGs])
                    S.barrier()

        if stop_after >= 2:
            with ExitStack() as p2:
                KT = [[sb(p2, f"KT{s}_{m}", [128, NT], BF16) for m in range(2)] for s in range(2)]
                VT = [sb(p2, f"VT{s}", [128, NKB, 129], BF16) for s in range(2)]
                QT = [[sb(p2, f"QT{s}_{m}", [128, NOWN], BF16) for m in range(2)] for s in range(2)]
                rKV = [Res(f"KV{s}", True) for s in range(2)]
                dsC2 = dsem("ds_c2")
                dsSig = [dsem(f"ds_sig{i}") for i in range(2)]
                rSig = [Res(f"sig{i}", True) for i in range(2)]
                dsKV = [dsem(f"ds_kv{s}") for s in range(2)]
                beta = sb(p2, "beta", [128, 8 * 5 * NKB], F32)
                dtab = sb(p2, "dtab", [128, 896], F32)
                gsubb = sb(p2, "gsubb", [128, 128], F32)
                lamb = sb(p2, "lamb", [128, 4, 64], F32)
                lamt = sb(p2, "lamt", [128, 2, 64], F32)
                lame = sb(p2, "lame", [128, 2], F32)
                lamneg = sb(p2, "lamneg", [128, 1], F32)
                rLam = Res("lam")
                DMA(beta[:, :], beta_d[:, :], dsC2, w=[rC])
                DMA(dtab[:, :], dtab_d[:, :], dsC2, w=[rC])
                DMA(gsubb[:, :], gsub_d[0:1, :].partition_broadcast(128), dsC2, w=[rC])
                for i in range(4):
                    DMA(lamb[:, i, :], lam_d[i:i + 1, :].partition_broadcast(128), dsC2, w=[rC])
                for s in range(2):
                    for m in range(2):
                        DMA(KT[s][m][64:68, :], sigk_d[:, :], dsSig[s], w=[rSig[s]])
                    V(lambda e, s=s: e.memset(VT[s][:, :, 128:129], 1.0), w=[rSig[s]])
                V(lambda e: e.tensor_tensor(out=lamt[:, 0, :], in0=lamb[:, 0, :], in1=lamb[:, 1, :], op=ALU.mult),
                  r=[rC], w=[rLam])
                V(lambda e: e.tensor_tensor(out=lamt[:, 1, :], in0=lamb[:, 2, :], in1=lamb[:, 3, :], op=ALU.mult),
                  r=[rC], w=[rLam])
                V(lambda e: e.tensor_reduce(out=lame[:, 0:2], in_=lamt[:, :, :], axis=AX.X, op=ALU.add),
                  r=[rLam], w=[rLam])
                A(lambda e: e.activation(out=lame[:, 0:2], in_=lame[:, 0:2], func=AF.Exp), r=[rLam], w=[rLam])
                V(lambda e: e.tensor_tensor(out=lamneg[:, :], in0=lame[:, 1:2], in1=lame[:, 0:1], op=ALU.subtract),
                  r=[rLam], w=[rLam])
                V(lambda e: e.tensor_scalar(out=lamneg[:, :], in0=lamneg[:, :], scalar1=-LAM_INIT, scalar2=None,
                                            op0=ALU.add), r=[rLam], w=[rLam])
                V(lambda e: e.tensor_scalar(out=gsubb[:, :], in0=gsubb[:, :], scalar1=1.0 - LAM_INIT,
                                            scalar2=None, op0=ALU.mult), r=[rC], w=[rC])

                PT = [sb(p2, f"PT{i}", [128, 1024], BF16) for i in range(3)]
                rPT = [Res(f"PT{i}") for i in range(3)]
                smix = [sb(p2, f"smix{i}", [128, 1024], F32) for i in range(2)]
                rSmix = [Res(f"smix{i}") for i in range(2)]
                Sps = [psum(p2, f"Sps{i}", [128, 1024], F32) for i in range(2)]
                rSps = [Res(f"Sps{i}", psum=True) for i in range(2)]
                Ops = psum(p2, "Ops", [128, 1536], F32)
                rOps = Res("Ops", psum=True)
                ev = {n: sb(p2, "ev_" + n, shp, F32) for n, shp in
                      [("r1", [128, 8]), ("t2", [128, 128]), ("dd", [128, 128]), ("ssd", [128, 1]),
                       ("lnd", [128, 1]), ("rsd", [128, 1]), ("junk", [128, 128])]}
                rEv = Res("ev")
                stgO = [sb(p2, f"stgO{i}", [128, 4, 128], BF16) for i in range(2)]
                rStgO = [Res(f"stgO{i}") for i in range(2)]
                dsStgO = [dsem(f"ds_so{i}") for i in range(2)]

                Osb = [sb(p2, f"Osb{i}", [128, 1536], F32) for i in range(2)]
                rOsb = [Res(f"Osb{i}") for i in range(2)]
                osb_i = [0]

                def oacc(m, s, n=129):
                    i = m * 4 + s
                    off = (i // 3) * 512 + (i % 3) * 129
                    return Ops[:, off:off + n]

                jobs = [("A", h) for h in range(8)] + [("B", pi) for pi in range(4)]

                def load_job(ji):
                    kind, idx = jobs[ji]
                    s = ji % 2
                    if kind == "A":
                        for m in range(2):
                            for half in range(2):
                                c0, c1 = half * 4104, (half + 1) * 4104
                                DMA(KT[s][m][0:64, c0:c1], KAs[idx, m * 64:(m + 1) * 64, c0:c1], dsKV[s],
                                    r=[rKAs], w=[rKV[s]])
                            DMA(QT[s][m][0:64, :], QAs[idx, m * 64:(m + 1) * 64, :], dsKV[s],
                                r=[rQAs], w=[rKV[s]])
                            DMA(QT[s][m][64:68, :], qaug_d[idx, :, :], dsKV[s], w=[rKV[s]])
                        for q4 in range(4):
                            b0, b1 = q4 * 16, (q4 + 1) * 16
                            DMA(VT[s][:, b0:b1, 0:128], VAs[idx, :, b0:b1, :], dsKV[s], r=[rVAs], w=[rKV[s]])
                        DMA(VT[s][0:16, 64:65, 0:128], VAs[idx, 0:16, 64:65, :], dsKV[s], r=[rVAs], w=[rKV[s]])
                    else:
                        kv = idx // 2
                        for half in range(2):
                            c0, c1 = half * 4104, (half + 1) * 4104
                            DMA(KT[s][0][:, c0:c1], KBs[kv, :, c0:c1], dsKV[s], r=[rKBs], w=[rKV[s], rSig[s]])
                        for m in range(2):
                            DMA(QT[s][m][:, :], QBs[2 * idx + m, :, :], dsKV[s], r=[rQBs], w=[rKV[s]])
                        for q4 in range(4):
                            b0, b1 = q4 * 16, (q4 + 1) * 16
                            DMA(VT[s][:, b0:b1, 0:128], VBs[kv, :, b0:b1, :], dsKV[s], r=[rVBs], w=[rKV[s]])
                        DMA(VT[s][0:16, 64:65, 0:128], VBs[kv, 0:16, 64:65, :], dsKV[s], r=[rVBs], w=[rKV[s]])

                CH = [(c * 512, 512, 4, 128) for c in range(4)] + [(2048, NMETA, 1, NMETA)]
                load_job(0)
                pt_i = [0]
                so_i = [0]
                for ji, (kind, idx) in enumerate(jobs):
                    s = ji % 2
                    if ji + 1 < len(jobs):
                        load_job(ji + 1)
                    isA = kind == "A"
                    KR = 68 if isA else 128
                    slope = 2.0 ** (-(idx + 1)) if isA else 0.0
                    scale = 0.125 if isA else 128.0 ** -0.5
                    Kt = [KT[s][0], KT[s][1] if isA else KT[s][0]]
                    Qt = QT[s]
                    for c, (q0, ncq, nsb, psq) in enumerate(CH):
                        V(lambda e: e.memset(Ops[:, :], 0.0), w=[rOps])

                        def qk(kb, sp):
                            kp = 128 if kb < 64 else NMETA
                            k0 = kb * 128
                            mixed = isA and c < 4 and (4 * c <= kb <= 4 * c + 3)
                            rows = 64 if mixed else KR
                            for m in range(2):
                                P(lambda e, m=m, sp=sp, kp=kp, k0=k0, rows=rows: e.matmul(
                                    Sps[sp][:kp, m * 512: m * 512 + ncq],
                                    lhsT=Kt[m][0:rows, k0:k0 + kp], rhs=Qt[m][0:rows, q0:q0 + ncq],
                                    start=True, stop=True),
                                  r=[rKV[s], rSig[s]], w=[rSps[sp]], inc=(m == 1))

                        def soft_pv(kb, sp, is_last):
                            kp = 128 if kb < 64 else NMETA
                            mixed = isA and c < 4 and (4 * c <= kb <= 4 * c + 3)
                            pi_ = pt_i[0] % 3
                            pt_i[0] += 1
                            if ncq == 512:
                                src = Sps[sp][:kp, :]
                                dst = PT[pi_][:kp, :]
                            else:
                                src = Sps[sp][:kp, :].rearrange("p (m q) -> p m q", m=2)[:, :, 0:ncq]
                                dst = PT[pi_][:kp, :].rearrange("p (m q) -> p m q", m=2)[:, :, 0:ncq]
                            if mixed:
                                mm = kb - 4 * c
                                x0 = 384 - 128 * mm
                                for m in range(2):
                                    V(lambda e, m=m, sp=sp, x0=x0: e.scalar_tensor_tensor(
                                        out=smix[sp][:, m * 512:(m + 1) * 512], in0=dtab[:, x0:x0 + 512],
                                        scalar=-8.0 * slope, in1=Sps[sp][:, m * 512:(m + 1) * 512],
                                        op0=ALU.mult, op1=ALU.add),
                                      r=[rSps[sp], rC], w=[rSmix[sp]])
                                A(lambda e, sp=sp, dst=dst: e.activation(out=dst, in_=smix[sp][:, :], func=AF.Exp,
                                                                         scale=scale),
                                  r=[rSmix[sp]], w=[rPT[pi_]])
                            elif isA:
                                bcol = (idx * 5 + c) * NKB + kb
                                A(lambda e, src=src, dst=dst, bcol=bcol, kp=kp: e.activation(
                                    out=dst, in_=src, func=AF.Exp, bias=beta[:kp, bcol:bcol + 1], scale=scale),
                                  r=[rSps[sp], rC], w=[rPT[pi_]])
                            else:
                                A(lambda e, src=src, dst=dst: e.activation(out=dst, in_=src, func=AF.Exp,
                                                                           scale=scale),
                                  r=[rSps[sp]], w=[rPT[pi_]])
                            for m in range(2):
                                for sq in range(nsb):
                                    last = (m == 1 and sq == nsb - 1)
                                    P(lambda e, m=m, sq=sq, pi_=pi_, kp=kp, kb=kb: e.matmul(
                                        oacc(m, sq)[:psq, :],
                                        lhsT=PT[pi_][:kp, m * 512 + sq * 128: m * 512 + sq * 128 + psq],
                                        rhs=VT[s][:kp, kb, :], start=False, stop=is_last,
                                        skip_group_check=True),
                                      r=[rPT[pi_], rKV[s], rSig[s]], w=[rOps], inc=last)

                        kbs = []
                        for kb in range(64):
                            if (not isA) or c == 4:
                                kbs.append(kb)
                                continue
                            lo_q, hi_q = 512 * c, 512 * c + 511
                            cands = [(128 * kb, 128 * kb + 127)]
                            if kb >= 16:
                                cands.append((128 * kb - 8192, 128 * kb + 127 - 8192))
                            dmin = min(max(0, ul - hi_q, lo_q - uh) for ul, uh in cands)
                            if slope * dmin <= 100.0:
                                kbs.append(kb)
                        kbs.append(64)
                        qk(kbs[0], 0)
                        for i, kb in enumerate(kbs):
                            if i + 1 < len(kbs):
                                qk(kbs[i + 1], (i + 1) % 2)
                            soft_pv(kb, i % 2, i == len(kbs) - 1)
                        ob_i = osb_i[0] % 2
                        osb_i[0] += 1
                        rOsbC = rOsb[ob_i]
                        V(lambda e, ob_i=ob_i: e.tensor_copy(out=Osb[ob_i][:, :], in_=Ops[:, :]), r=[rOps], w=[rOsbC])

                        def osbv(m, sq_, n=129, ob_i=ob_i):
                            i = m * 4 + sq_
                            off = (i // 3) * 512 + (i % 3) * 129
                            return Osb[ob_i][:, off:off + n]
                        so = so_i[0] % 2
                        so_i[0] += 1
                        for sq in range(nsb if isA else 0):
                            if isA:
                                O1, O2 = osbv(0, sq), osbv(1, sq)
                                V(lambda e, O1=O1: e.reciprocal(out=ev["r1"][:psq, 0:1], in_=O1[:psq, 128:129]),
                                  r=[rOsbC], w=[rEv])
                                V(lambda e, O2=O2: e.reciprocal(out=ev["r1"][:psq, 1:2], in_=O2[:psq, 128:129]),
                                  r=[rOsbC], w=[rEv])
                                V(lambda e: e.tensor_tensor(out=ev["r1"][:psq, 2:3], in0=ev["r1"][:psq, 1:2],
                                                            in1=lamneg[:psq, 0:1], op=ALU.mult),
                                  r=[rEv, rLam], w=[rEv])
                                V(lambda e, O2=O2: e.tensor_scalar(out=ev["t2"][:psq, :], in0=O2[:psq, 0:128],
                                                                   scalar1=ev["r1"][:psq, 2:3], scalar2=None,
                                                                   op0=ALU.mult), r=[rOsbC, rEv], w=[rEv])
                                V(lambda e, O1=O1: e.scalar_tensor_tensor(
                                    out=ev["dd"][:psq, :], in0=O1[:psq, 0:128], scalar=ev["r1"][:psq, 0:1],
                                    in1=ev["t2"][:psq, :], op0=ALU.mult, op1=ALU.add), r=[rOsbC, rEv], w=[rEv])
                                A(lambda e: e.activation(out=ev["junk"][:psq, :], in_=ev["dd"][:psq, :],
                                                         func=AF.Square, accum_out=ev["ssd"][:psq, 0:1]),
                                  r=[rEv], w=[rEv])
                                rstd_from_ss(ev["ssd"][:psq, 0:1], ev["lnd"][:psq, 0:1], ev["rsd"][:psq, 0:1],
                                             128.0, [rEv], [rEv])
                                V(lambda e, sq=sq, so=so: e.scalar_tensor_tensor(
                                    out=stgO[so][:psq, sq, :], in0=ev["dd"][:psq, :], scalar=ev["rsd"][:psq, 0:1],
                                    in1=gsubb[:psq, :], op0=ALU.mult, op1=ALU.mult),
                                  r=[rEv, rC], w=[rStgO[so]])
                        if isA:
                            dst_d = OAs[q0:q0 + ncq, idx * 128:(idx + 1) * 128].rearrange("(s p) e -> p s e", p=psq)
                            DMA(dst_d, stgO[so][:psq, 0:nsb, :], dsStgO[so], r=[rStgO[so]], w=[rOAs])
                        else:
                            for m in range(2):
                                g = 2 * idx + m
                                so = so_i[0] % 2
                                so_i[0] += 1
                                for sq in range(nsb):
                                    Om = osbv(m, sq)
                                    V(lambda e, Om=Om: e.reciprocal(out=ev["r1"][:psq, 0:1], in_=Om[:psq, 128:129]),
                                      r=[rOsbC], w=[rEv])
                                    V(lambda e, Om=Om, sq=sq, so=so: e.tensor_scalar(
                                        out=stgO[so][:psq, sq, :], in0=Om[:psq, 0:128], scalar1=ev["r1"][:psq, 0:1],
                                        scalar2=None, op0=ALU.mult), r=[rOsbC, rEv], w=[rStgO[so]])
                                dst_d = OBs[q0:q0 + ncq, g * 128:(g + 1) * 128].rearrange("(s p) e -> p s e", p=psq)
                                DMA(dst_d, stgO[so][:psq, 0:nsb, :], dsStgO[so], r=[rStgO[so]], w=[rOBs])
                S.barrier()

        if stop_after >= 3:
            with ExitStack() as p3:
                H = sb(p3, "H", [128, 17, D], F32)
                rH = [Res(f"H{t}") for t in range(17)]
                u2T = sb(p3, "u2T", [128, 8, NOWN], BF16)
                rU2 = [Res(f"u2T{t}") for t in range(17)]
                AFF = sb(p3, "AFF", [128, 17, NEXP], F32)
                rAFF = Res("AFF")
                COEF = sb(p3, "COEF", [128, 17, NEXP], F32)
                rCOEF = Res("COEF")
                rAgin = Res("agin", True)
                gffnb = sb(p3, "gffnb", [128, D], F32)
                dsC3 = dsem("ds_c3")
                DMA(gffnb[:, :], gffn_d[0:1, :].partition_broadcast(128), dsC3, w=[rC])
                BLK = [(t * 128, 128) for t in range(16)] + [(2048, NMETA)]

                with ExitStack() as p3a:
                    Wa = sb(p3a, "Wa", [128, 8, D], BF16)
                    Wb = sb(p3a, "Wb", [128, 8, D], BF16)
                    Wo = sb(p3a, "Wo", [128, 8, D], BF16)
                    wr = sb(p3a, "wr", [128, 8, NEXP], BF16)
                    rW3 = Res("W3", True)
                    dsW3 = dsem("ds_w3")
                    for c in range(8):
                        DMA(Wa[:, c, :], wa_d.rearrange("(c p) f -> p c f", p=128)[:, c, :], dsW3, w=[rW3], q="pool")
                        DMA(Wb[:, c, :], wb_d.rearrange("(c p) f -> p c f", p=128)[:, c, :], dsW3, w=[rW3], q="pool")
                        DMA(Wo[:, c, :], wo_d.rearrange("(c p) f -> p c f", p=128)[:, c, :], dsW3, w=[rW3], q="pool")
                    DMA(wr[:, :, :], wr_d.rearrange("(c p) f -> p c f", p=128), dsW3, w=[rW3], q="pool")
                    oa_t = [sb(p3a, f"oa_t{i}", [128, D], BF16) for i in range(2)]
                    ob_t = [sb(p3a, f"ob_t{i}", [128, D], BF16) for i in range(2)]
                    g_t = [sb(p3a, f"g_t{i}", [128, 2048], BF16) for i in range(2)]
                    x_t3 = [sb(p3a, f"x_t3{i}", [128, D], F32) for i in range(2)]
                    rIn3 = [Res(f"in3_{i}", True) for i in range(2)]
                    dsIn3 = [dsem(f"ds_in3_{i}") for i in range(2)]
                    oaT = sb(p3a, "oaT", [128, 8, 128], BF16)
                    obT = sb(p3a, "obT", [128, 8, 128], BF16)
                    mgT = sb(p3a, "mgT", [128, 8, 128], BF16)
                    rOaT, rObT, rMgT = Res("oaT"), Res("obT"), Res("mgT")
                    m1 = sb(p3a, "m1", [128, D], F32)
                    m2 = sb(p3a, "m2", [128, D], BF16)
                    affS = [sb(p3a, f"affS{i}", [NEXP, 128], F32) for i in range(2)]
                    rAffS = [Res(f"affS{i}") for i in range(2)]
                    dsAffS = [dsem(f"ds_affs{i}") for i in range(2)]
                    mg = sb(p3a, "mg", [128, D], BF16)
                    rM1, rM2, rMg = Res("m1"), Res("m2"), Res("mg")
                    u2 = sb(p3a, "u2", [128, D], BF16)
                    rU2t = Res("u2")
                    st3 = {n: sb(p3a, "st3_" + n, [128, 1], F32) for n in ("ss", "ln", "rs", "se", "rse")}
                    ex3 = sb(p3a, "ex3", [128, NEXP], F32)
                    rSt3 = Res("st3")
                    tp3 = [psum(p3a, f"tp3_{i}", [128, 1024], BF16) for i in range(2)]
                    rTp3 = [Res(f"tp3_{i}", psum=True) for i in range(2)]
                    yps = [psum(p3a, f"yps{i}", [128, 512], F32) for i in range(4)]
                    rYps = [Res(f"yps{i}", psum=True) for i in range(4)]
                    lps = psum(p3a, "lps", [128, 512], F32)
                    rLps = Res("lps", psum=True)
                    tfp = psum(p3a, "tfp", [128, 512], F32)
                    rTfp = Res("tfp", psum=True)

                    def load3(t):
                        q0, pb_ = BLK[t]
                        sl = t % 2
                        tokx = q0 if t < 16 else SEQ
                        DMA(oa_t[sl][:pb_, :], OAs[q0:q0 + pb_, :], dsIn3[sl], r=[rOAs], w=[rIn3[sl]])
                        DMA(ob_t[sl][:pb_, :], OBs[q0:q0 + pb_, :], dsIn3[sl], r=[rOBs], w=[rIn3[sl]])
                        DMA(g_t[sl][:pb_, :], Gs[q0:q0 + pb_, :], dsIn3[sl], r=[rGs], w=[rIn3[sl]])
                        DMA(x_t3[sl][:pb_, :], hx[tokx:tokx + pb_, :], dsIn3[sl], w=[rIn3[sl]])

                    def transpose8(src, rsrc, dstT, rdst, pb_, tpi):
                        for fc in range(8):
                            P(lambda e, fc=fc: e.transpose(out=tp3[tpi][:, fc * 128: fc * 128 + pb_],
                                                           in_=src[:pb_, fc * 128:(fc + 1) * 128],
                                                           identity=identb[:pb_, :pb_]),
                              r=[rsrc, rC], w=[rTp3[tpi]], inc=(fc == 7))

                    load3(0)
                    for t, (q0, pb_) in enumerate(BLK):
                        sl = t % 2
                        if t + 1 < len(BLK):
                            load3(t + 1)
                        transpose8(oa_t[sl], rIn3[sl], oaT, rOaT, pb_, 0)
                        V(lambda e: e.tensor_copy(out=oaT[:, :, :pb_],
                                                  in_=tp3[0][:, :].rearrange("p (c t) -> p c t", c=8)[:, :, :pb_]),
                          r=[rTp3[0]], w=[rOaT])
                        transpose8(ob_t[sl], rIn3[sl], obT, rObT, pb_, 1)
                        A(lambda e: e.activation(out=obT[:, :, :pb_],
                                                 in_=tp3[1][:, :].rearrange("p (c t) -> p c t", c=8)[:, :, :pb_],
                                                 func=AF.Copy),
                          r=[rTp3[1]], w=[rObT])
                        for g in range(2):
                            for fc in range(8):
                                P(lambda e, g=g, fc=fc: e.matmul(yps[g][:pb_, :], lhsT=oaT[:, fc, :pb_],
                                                                 rhs=Wa[:, fc, g * 512:(g + 1) * 512],
                                                                 start=(fc == 0), stop=(fc == 7)),
                                  r=[rOaT, rW3], w=[rYps[g]], inc=(fc == 7))
                        for g in range(2):
                            for fc in range(8):
                                P(lambda e, g=g, fc=fc: e.matmul(yps[2 + g][:pb_, :], lhsT=obT[:, fc, :pb_],
                                                                 rhs=Wb[:, fc, g * 512:(g + 1) * 512],
                                                                 start=(fc == 0), stop=(fc == 7)),
                                  r=[rObT, rW3], w=[rYps[2 + g]], inc=(fc == 7))
                        for g in range(2):
                            V(lambda e, g=g: e.tensor_tensor(out=m1[:pb_, g * 512:(g + 1) * 512], in0=yps[g][:pb_, :],
                                                             in1=g_t[sl][:pb_, g * 512:(g + 1) * 512], op=ALU.mult),
                              r=[rYps[g], rIn3[sl]], w=[rM1])
                            V(lambda e, g=g: e.tensor_tensor(out=m2[:pb_, g * 512:(g + 1) * 512],
                                                             in0=yps[2 + g][:pb_, :],
                                                             in1=g_t[sl][:pb_, 1024 + g * 512:1024 + (g + 1) * 512],
                                                             op=ALU.mult),
                              r=[rYps[2 + g], rIn3[sl]], w=[rM2])
                        V(lambda e: e.tensor_tensor(out=mg[:pb_, :], in0=m1[:pb_, :], in1=m2[:pb_, :], op=ALU.add),
                          r=[rM1, rM2], w=[rMg])
                        transpose8(mg, rMg, mgT, rMgT, pb_, 0)
                        V(lambda e: e.tensor_copy(out=mgT[:, :, :pb_],
                                                  in_=tp3[0][:, :].rearrange("p (c t) -> p c t", c=8)[:, :, :pb_]),
                          r=[rTp3[0]], w=[rMgT])
                        for g in range(2):
                            for fc in range(8):
                                P(lambda e, g=g, fc=fc: e.matmul(yps[g][:pb_, :], lhsT=mgT[:, fc, :pb_],
                                                                 rhs=Wo[:, fc, g * 512:(g + 1) * 512],
                                                                 start=(fc == 0), stop=(fc == 7)),
                                  r=[rMgT, rW3], w=[rYps[g]], inc=(fc == 7))
                        for g in range(2):
                            V(lambda e, g=g, t=t: e.tensor_tensor(out=H[:pb_, t, g * 512:(g + 1) * 512],
                                                                  in0=yps[g][:pb_, :],
                                                                  in1=x_t3[sl][:pb_, g * 512:(g + 1) * 512],
                                                                  op=ALU.add),
                              r=[rYps[g], rIn3[sl]], w=[rH[t]])
                        if dbg:
                            DMA(dbg_h1[q0:q0 + pb_, :], H[:pb_, t, :], dsDbg, r=[rH[t]])
                        A(lambda e, t=t: e.activation(out=u2[:pb_, :], in_=H[:pb_, t, :], func=AF.Square,
                                                      accum_out=st3["ss"][:pb_, 0:1]), r=[rH[t]], w=[rSt3, rU2t])
                        rstd_from_ss(st3["ss"][:pb_, 0:1], st3["ln"][:pb_, 0:1], st3["rs"][:pb_, 0:1], float(D),
                                     [rSt3], [rSt3])
                        V(lambda e, t=t: e.scalar_tensor_tensor(out=u2[:pb_, :], in0=H[:pb_, t, :],
                                                                scalar=st3["rs"][:pb_, 0:1], in1=gffnb[:pb_, :],
                                                                op0=ALU.mult, op1=ALU.mult),
                          r=[rH[t], rSt3, rC], w=[rU2t])
                        transpose8(u2, rU2t, None, None, pb_, 1)
                        A(lambda e, q0=q0: e.activation(
                            out=u2T[:, :, q0:q0 + pb_],
                            in_=tp3[1][:, :].rearrange("p (c t) -> p c t", c=8)[:, :, :pb_], func=AF.Copy),
                          r=[rTp3[1]], w=[rU2[t]])
                        for fc in range(8):
                            P(lambda e, fc=fc, q0=q0: e.matmul(lps[:pb_, 0:NEXP], lhsT=u2T[:, fc, q0:q0 + pb_],
                                                               rhs=wr[:, fc, :], start=(fc == 0), stop=(fc == 7)),
                              r=[rU2[t], rW3], w=[rLps], inc=(fc == 7))
                        A(lambda e: e.activation(out=ex3[:pb_, :], in_=lps[:pb_, 0:NEXP], func=AF.Exp,
                                                 accum_out=st3["se"][:pb_, 0:1]), r=[rLps], w=[rSt3])
                        V(lambda e: e.reciprocal(out=st3["rse"][:pb_, 0:1], in_=st3["se"][:pb_, 0:1]),
                          r=[rSt3], w=[rSt3])
                        V(lambda e, t=t: e.tensor_scalar(out=AFF[:pb_, t, :], in0=ex3[:pb_, :],
                                                         scalar1=st3["rse"][:pb_, 0:1], scalar2=None, op0=ALU.mult),
                          r=[rSt3], w=[rAFF])
                        P(lambda e, t=t: e.transpose(out=tfp[0:NEXP, 0:pb_], in_=AFF[:pb_, t, :],
                                                     identity=identf[:pb_, :pb_]), r=[rAFF, rC], w=[rTfp])
                        V(lambda e, sl=sl: e.tensor_copy(out=affS[sl][:, 0:pb_], in_=tfp[0:NEXP, 0:pb_]),
                          r=[rTfp], w=[rAffS[sl]])
                        DMA(agin.ap()[:, q0:q0 + pb_], affS[sl][:, 0:pb_], dsAffS[sl], r=[rAffS[sl]], w=[rAgin])
                        if dbg:
                            DMA(dbg_aff[q0:q0 + pb_, :], AFF[:pb_, t, :], dsDbg, r=[rAFF])
                    S.barrier()

                if stop_after >= 4:
                    with ExitStack() as p4:
                        AGc = sb(p4, "AGc", [NEXP, SEQ + NMETA], F32)
                        rAG = Res("AGc", True)
                        dsAG = dsem("ds_ag")
                        dsCC = DSem(sem("cc_sem"))
                        rAgout = Res("agout")
                        S.coll(lambda e: e.collective_compute(
                            "AllGather", ALU.bypass, replica_groups=[[0, 1, 2, 3], [4, 5, 6, 7]],
                            ins=[agin.ap().opt()], outs=[agout.ap().opt()]), dsCC, reads=[rAgin], writes=[rAgout])
                        ago = agout.ap()
                        DMA(AGc[:, 0:SEQ].rearrange("e (r t) -> e r t", r=4),
                            ago.rearrange("(r e) t -> e r t", e=NEXP)[:, :, 0:2048], dsAG, r=[rAgout], w=[rAG],
                            q="pool")
                        DMA(AGc[:, SEQ:SEQ + NMETA], ago[0:NEXP, 2048:2048 + NMETA], dsAG, r=[rAgout], w=[rAG],
                            q="pool")
                        lo = sb(p4, "lo", [NEXP, 1], F32)
                        mid = sb(p4, "mid", [NEXP, 1], F32)
                        cnt = sb(p4, "cnt", [NEXP, 1], F32)
                        prd = sb(p4, "prd", [NEXP, 1], F32)
                        cmpj = sb(p4, "cmpj", [NEXP, SEQ + NMETA], BF16)
                        rB = Res("bis")
                        V(lambda e: e.memset(lo[:, :], 0.0), w=[rB])
                        for it in range(NBIS):
                            ck = 2.0 ** (-(it + 1))
                            V(lambda e, ck=ck: e.tensor_scalar(out=mid[:, :], in0=lo[:, :], scalar1=ck, scalar2=None,
                                                               op0=ALU.add), r=[rB], w=[rB])
                            V(lambda e: e.tensor_scalar(out=cmpj[:, :], in0=AGc[:, :], scalar1=mid[:, 0:1],
                                                        scalar2=0.0, op0=ALU.is_ge, op1=ALU.add,
                                                        accum_out=cnt[:, 0:1]), r=[rB, rAG], w=[rB])
                            V(lambda e, ck=ck: e.tensor_scalar(out=prd[:, :], in0=cnt[:, :], scalar1=CAP - 0.5,
                                                               scalar2=ck, op0=ALU.is_ge, op1=ALU.mult),
                              r=[rB], w=[rB])
                            V(lambda e: e.tensor_tensor(out=lo[:, :], in0=lo[:, :], in1=prd[:, :], op=ALU.add),
                              r=[rB], w=[rB])
                        rThrD = Res("thr_d")
                        DMA(thr_d.rearrange("o e -> e o"), lo[:, :], dsAG, r=[rB], w=[rThrD])
                        if dbg:
                            DMA(dbg_thr.rearrange("o e -> e o"), lo[:, :], dsDbg, r=[rB])
                        THR = sb(p4, "THR", [128, NEXP], F32)
                        rTHR = Res("THR")
                        DMA(THR[:, :], thr_d[0:1, :].partition_broadcast(128), dsAG, r=[rThrD], w=[rTHR])
                        for t in range(16):
                            V(lambda e, t=t: e.tensor_tensor(out=COEF[:, t, :], in0=AFF[:, t, :], in1=THR[:, :],
                                                             op=ALU.is_ge), r=[rAFF, rTHR], w=[rCOEF])
                            V(lambda e, t=t: e.tensor_tensor(out=COEF[:, t, :], in0=COEF[:, t, :], in1=AFF[:, t, :],
                                                             op=ALU.mult), r=[rAFF, rCOEF], w=[rCOEF])
                        S.barrier()

                if stop_after >= 5:
                    with ExitStack() as p5:
                        wg = [sb(p5, f"wg{i}", [128, 8, 512], BF16) for i in range(2)]
                        wu = [sb(p5, f"wu{i}", [128, 8, 512], BF16) for i in range(2)]
                        wd = [sb(p5, f"wd{i}", [128, 4, D], BF16) for i in range(2)]
                        rWe = [Res(f"We{i}", True) for i in range(2)]
                        dsWe = [dsem(f"ds_we{i}") for i in range(2)]
                        hT = [sb(p5, f"hT{i}", [128, 4, 512], BF16) for i in range(2)]
                        rHT = [Res(f"hT{i}") for i in range(2)]
                        sg = [sb(p5, f"sg{i}", [128, 512], F32) for i in range(2)]
                        rSg = [Res(f"sg{i}") for i in range(2)]
                        gps = [psum(p5, f"gps{i}", [128, 512], F32) for i in range(2)]
                        ups = [psum(p5, f"ups{i}", [128, 512], F32) for i in range(2)]
                        rGps = [Res(f"gps{i}", psum=True) for i in range(2)]
                        rUps = [Res(f"ups{i}", psum=True) for i in range(2)]
                        ypm = [psum(p5, f"ypm{i}", [128, 512], F32) for i in range(4)]
                        rYpm = [Res(f"ypm{i}", psum=True) for i in range(4)]

                        def load_e(ei):
                            sl = ei % 2
                            for c in range(8):
                                DMA(wg[sl][:, c, :], wg_d[ei, c * 128:(c + 1) * 128, :], dsWe[sl], w=[rWe[sl]], q="pool")
                                DMA(wu[sl][:, c, :], wu_d[ei, c * 128:(c + 1) * 128, :], dsWe[sl], w=[rWe[sl]], q="pool")
                            for c in range(4):
                                DMA(wd[sl][:, c, :], wd_d[ei, c * 128:(c + 1) * 128, :], dsWe[sl], w=[rWe[sl]], q="pool")

                        load_e(0)
                        gi = [0]
                        yi = [0]
                        for ei in range(NEXP):
                            sl = ei % 2
                            if ei + 1 < NEXP:
                                load_e(ei + 1)
                            for tcn in range(4):
                                hs = (ei * 4 + tcn) % 2
                                rUs = [rU2[tcn * 4 + k] for k in range(4)]
                                for fc in range(4):
                                    gs = gi[0] % 2
                                    gi[0] += 1
                                    for dc in range(8):
                                        P(lambda e, gs=gs, dc=dc, fc=fc, tcn=tcn: e.matmul(
                                            gps[gs][:, :], lhsT=wg[sl][:, dc, fc * 128:(fc + 1) * 128],
                                            rhs=u2T[:, dc, tcn * 512:(tcn + 1) * 512], start=(dc == 0), stop=(dc == 7)),
                                          r=[rWe[sl]] + rUs, w=[rGps[gs]], inc=(dc == 7))
                                    for dc in range(8):
                                        P(lambda e, gs=gs, dc=dc, fc=fc, tcn=tcn: e.matmul(
                                            ups[gs][:, :], lhsT=wu[sl][:, dc, fc * 128:(fc + 1) * 128],
                                            rhs=u2T[:, dc, tcn * 512:(tcn + 1) * 512], start=(dc == 0), stop=(dc == 7)),
                                          r=[rWe[sl]] + rUs, w=[rUps[gs]], inc=(dc == 7))
                                    A(lambda e, gs=gs: e.activation(out=sg[gs][:, :], in_=gps[gs][:, :], func=AF.Silu),
                                      r=[rGps[gs]], w=[rSg[gs]])
                                    V(lambda e, gs=gs, fc=fc, hs=hs: e.tensor_tensor(
                                        out=hT[hs][:, fc, :], in0=ups[gs][:, :], in1=sg[gs][:, :], op=ALU.mult),
                                      r=[rUps[gs], rSg[gs]], w=[rHT[hs]])
                                for ts in range(4):
                                    t = tcn * 4 + ts
                                    for dh in range(2):
                                        ys = yi[0] % 4
                                        yi[0] += 1
                                        for fc in range(4):
                                            P(lambda e, ys=ys, fc=fc, ts=ts, dh=dh, hs=hs: e.matmul(
                                                ypm[ys][:, :], lhsT=hT[hs][:, fc, ts * 128:(ts + 1) * 128],
                                                rhs=wd[sl][:, fc, dh * 512:(dh + 1) * 512],
                                                start=(fc == 0), stop=(fc == 3)),
                                              r=[rHT[hs], rWe[sl]], w=[rYpm[ys]], inc=(fc == 3))
                                        V(lambda e, ys=ys, t=t, dh=dh, ei=ei: e.scalar_tensor_tensor(
                                            out=H[:, t, dh * 512:(dh + 1) * 512], in0=ypm[ys][:, :],
                                            scalar=COEF[:, t, ei:ei + 1], in1=H[:, t, dh * 512:(dh + 1) * 512],
                                            op0=ALU.mult, op1=ALU.add),
                                          r=[rYpm[ys], rCOEF, rH[t]], w=[rH[t]])
                        S.barrier()

                with ExitStack() as p6:
                    gfinb = sb(p6, "gfinb", [128, D], F32)
                    dsC6 = dsem("ds_c6")
                    DMA(gfinb[:, :], gfin_d[0:1, :].partition_broadcast(128), dsC6, w=[rC])
                    fo = [sb(p6, f"fo{i}", [128, D], F32) for i in range(2)]
                    rFo = [Res(f"fo{i}") for i in range(2)]
                    dsFo = [dsem(f"ds_fo{i}") for i in range(2)]
                    fj = sb(p6, "fj", [128, D], BF16)
                    fst = {n: sb(p6, "fst_" + n, [128, 1], F32) for n in ("ss", "ln", "rs")}
                    rFst = Res("fst")
                    for t in range(16):
                        sl = t % 2
                        A(lambda e, t=t: e.activation(out=fj[:, :], in_=H[:, t, :], func=AF.Square,
                                                      accum_out=fst["ss"][:, 0:1]), r=[rH[t]], w=[rFst])
                        rstd_from_ss(fst["ss"][:, 0:1], fst["ln"][:, 0:1], fst["rs"][:, 0:1], float(D),
                                     [rFst], [rFst])
                        V(lambda e, t=t, sl=sl: e.scalar_tensor_tensor(
                            out=fo[sl][:, :], in0=H[:, t, :], scalar=fst["rs"][:, 0:1], in1=gfinb[:, :],
                            op0=ALU.mult, op1=ALU.mult), r=[rH[t], rFst, rC], w=[rFo[sl]])
                        DMA(y_out[t * 128:(t + 1) * 128, :], fo[sl][:, :], dsFo[sl], r=[rFo[sl]])
                    S.barrier()
        else:
            with ExitStack() as pz:
                zt = sb(pz, "zt", [128, D], F32)
                rZ = Res("zt")
                dsZ = dsem("ds_z")
                V(lambda e: e.memset(zt[:, :], 0.0), w=[rZ])
                for t in range(16):
                    DMA(y_out[t * 128:(t + 1) * 128, :], zt[:, :], dsZ, r=[rZ])
                S.barrier()

        S.barrier()

        @block.tensor
        def _(eng):
            S.emit("pe", eng)

        @block.scalar
        def _(eng):
            S.emit("act", eng)

        @block.vector
        def _(eng):
            S.emit("dve", eng)

        @block.gpsimd
        def _(eng):
            S.emit("pool", eng)

        @block.sync
        def _(eng):
            S.emit("sp", eng)

    return nc


def _tables(r):
    bf = ml_dtypes.bfloat16
    jp = np.arange(SEQ)
    uj = np.where(jp + 2048 * r < SEQ, jp, jp - SEQ).astype(np.float64)
    sig = np.zeros((4, NT), np.float32)
    beta = np.zeros((128, 8, 5, NKB), np.float32)
    for c in range(4):
        before = uj < 512 * c
        sgn = np.where(before, 1.0, -1.0)
        sig[c, :SEQ] = sgn
        for h in range(8):
            slope = 2.0 ** (-(h + 1))
            b = sgn * slope * (uj - 512 * c - 256)
            beta[:, h, c, :64] = b.reshape(64, 128).T
    qaug = np.zeros((8, 4, NOWN), np.float32)
    a = np.arange(512) - 256
    for h in range(8):
        slope = 2.0 ** (-(h + 1))
        for c in range(4):
            qaug[h, c, c * 512:(c + 1) * 512] = -8.0 * slope * a
    x = np.arange(896)[None, :]
    bb = np.arange(128)[:, None]
    dtab = np.abs(x - bb - 384).astype(np.float32)
    t_true = (jp + 2048 * r) % SEQ
    row_id = (t_true // 64).astype(np.float32)
    col_id = (t_true % 64).astype(np.float32)
    inv_freq = (np.float32(10000.0) ** (-np.arange(0, 64, 2, dtype=np.float32) / np.float32(64))).astype(np.float32)
    ar = (row_id[:, None] * inv_freq[None, :]).astype(np.float32)
    ac = (col_id[:, None] * inv_freq[None, :]).astype(np.float32)
    rope = np.zeros((NT, 256), np.float32)
    rope[:, 0:128] = 1.0
    cr, sr, cc, sc = np.cos(ar), np.sin(ar), np.cos(ac), np.sin(ac)
    rope[:SEQ, 0:32] = cr
    rope[:SEQ, 32:64] = cr
    rope[:SEQ, 64:96] = cc
    rope[:SEQ, 96:128] = cc
    rope[:SEQ, 128:160] = -sr
    rope[:SEQ, 160:192] = sr
    rope[:SEQ, 192:224] = -sc
    rope[:SEQ, 224:256] = sc
    return dict(sigk=sig.astype(bf), qaug=qaug.astype(bf),
                beta=np.ascontiguousarray(beta.reshape(128, -1)), dtab=dtab, rope=rope)


def make_in_maps(inputs):
    bf = ml_dtypes.bfloat16
    x = np.asarray(inputs["x"], np.float32)
    meta = np.asarray(inputs["meta_tokens"], np.float32)
    f = lambda k: np.ascontiguousarray(np.asarray(inputs[k], np.float32))
    common = {
        "w_in": f("w_in")[0],
        "gmixT": np.ascontiguousarray(f("g_mix")[0].reshape(8, 128).T),
        "lamv": np.ascontiguousarray(np.stack([f("lambda_q1")[0], f("lambda_k1")[0],
                                               f("lambda_q2")[0], f("lambda_k2")[0]])),
        "g_subln": f("g_subln"), "g_qnorm": f("g_qnorm"), "g_knorm": f("g_knorm"),
        "w_branch_a": f("w_branch_a")[0], "w_branch_b": f("w_branch_b")[0], "w_out": f("w_out")[0],
        "g_ffn": f("g_ffn"), "w_router": f("w_router")[0],
        "w_gate": f("w_gate")[0], "w_up": f("w_up")[0], "w_down": f("w_down")[0],
        "g_final": f("g_final").reshape(1, D),
        "identb": np.eye(128, dtype=np.float32).astype(bf),
        "identf": np.eye(128, dtype=np.float32),
    }
    tabs = [_tables(r) for r in range(4)]
    maps = []
    for c in range(8):
        b, r = c // 4, c % 4
        hxv = np.concatenate([np.roll(x[b], -2048 * r, axis=0), meta], axis=0)
        m = dict(common)
        m.update(tabs[r])
        m["hx"] = np.ascontiguousarray(hxv)
        maps.append(m)
    return maps


_NC_CACHE = {}


def kernel(**inputs):
    if "nc" not in _NC_CACHE:
        _NC_CACHE["nc"] = build()
    nc = _NC_CACHE["nc"]
    maps = make_in_maps(inputs)
    res = run_bass_kernel_spmd(nc, maps, core_ids=list(range(8)))
    out = np.zeros((2, SEQ, D), np.float32)
    for c in range(8):
        b, r = c // 4, c % 4
        out[b, r * 2048:(r + 1) * 2048, :] = res.results[c]["y"]
    return out
```

```python
import numpy as np
import ml_dtypes
from contextlib import ExitStack
import concourse.bass as bass
import concourse.mybir as mybir
from concourse.bass_utils import run_bass_kernel_spmd

F32 = mybir.dt.float32
BF16 = mybir.dt.bfloat16
AF = mybir.ActivationFunctionType
ALU = mybir.AluOpType
AX = mybir.AxisListType

D = 1024
SEQ = 8192
NMETA = 16
NT = SEQ + NMETA
NOWN = 2048 + NMETA
NKB = 65
EPS = 1e-6
NEXP = 16
CAP = 2 * NT // NEXP
LAM_INIT = 0.2
NBIS = 26


import types


def _freeze(fn):
    if fn is None or fn.__closure__ is None:
        return fn
    cells = []
    for c in fn.__closure__:
        try:
            cells.append(types.CellType(c.cell_contents))
        except ValueError:
            cells.append(c)
    return types.FunctionType(fn.__code__, fn.__globals__, fn.__name__, fn.__defaults__, tuple(cells))


class Res:
    __slots__ = ("name", "w", "rd", "multi", "psum")

    def __init__(self, name, multi=False, psum=False):
        self.name = name
        self.w = {}
        self.rd = {}
        self.multi = multi
        self.psum = psum


class DSem:
    def __init__(self, sem):
        self.sem = sem
        self.count = 0


class Sched:
    ENGS = ("pe", "act", "dve", "pool", "sp")

    def __init__(self):
        self.prog = {e: [] for e in self.ENGS}
        self.cnt = {e: 0 for e in self.ENGS}
        self.sem = {}
        self.known = {e: {} for e in self.ENGS}
        self.all_dsems = []

    def _deps(self, eng, reads, writes):
        deps = {}
        known = self.known[eng]

        def add(tok, kind):
            sem, val, e = tok
            if e == eng and eng in ("pe", "sp"):
                return
            k = id(sem)
            if known.get(k, 0) >= val:
                return
            if k not in deps or deps[k][1] < val:
                deps[k] = (sem, val)

        for r in reads:
            for tok in r.w.values():
                add(tok, "raw")
            if r.psum:
                for tok in r.rd.values():
                    if tok[2] != eng:
                        add(tok, "rar")
        for w in writes:
            if not w.multi:
                for tok in w.w.values():
                    add(tok, "waw")
            for tok in w.rd.values():
                add(tok, "war")
        for k, (sem, val) in deps.items():
            known[k] = val
        return list(deps.values())

    def _record(self, tok, reads, writes):
        k = id(tok[0])
        for r in reads:
            r.rd[k] = tok
        for w in writes:
            if w.multi:
                w.w[k] = tok
            else:
                w.w = {k: tok}
                w.rd = {}

    def op(self, eng, fn, reads=(), writes=(), inc=True):
        deps = self._deps(eng, reads, writes)
        tok = (self.sem[eng], self.cnt[eng] + 1, eng)
        self._record(tok, reads, writes)
        if inc:
            self.cnt[eng] += 1
        self.prog[eng].append((deps, _freeze(fn), self.sem[eng] if inc else None, 1))

    def dma(self, q, fn, ds, reads=(), writes=()):
        deps = self._deps(q, reads, writes)
        ds.count += 16
        tok = (ds.sem, ds.count, None)
        self._record(tok, reads, writes)
        self.prog[q].append((deps, _freeze(fn), ds.sem, 16))

    def coll(self, fn, ds, reads=(), writes=()):
        deps = self._deps("pool", reads, writes)
        ds.count += 1
        tok = (ds.sem, ds.count, None)
        self._record(tok, reads, writes)
        self.prog["pool"].append((deps, _freeze(fn), ds.sem, None))

    def barrier(self):
        toks = []
        for e in self.ENGS:
            if e == "sp":
                continue
            if self.cnt[e] > 0:
                toks.append((self.sem[e], self.cnt[e]))
        for ds in self.all_dsems:
            if ds.count > 0:
                toks.append((ds.sem, ds.count))
        for e in self.ENGS:
            deps = []
            for sem, val in toks:
                if self.known[e].get(id(sem), 0) < val:
                    self.known[e][id(sem)] = val
                    deps.append((sem, val))
            if deps:
                self.prog[e].append((deps, None, None, 0))

    def emit(self, eng, handle):
        for deps, fn, sem, incv in self.prog[eng]:
            for s, v in deps:
                handle.wait_ge(s, v)
            if fn is None:
                continue
            ins = fn(handle)
            if sem is not None:
                if incv is None:
                    ins.then_inc(sem)
                else:
                    ins.then_inc(sem, incv)


def build(dbg=False, stop_after=99):
    nc = bass.Bass("TRN2", target_bir_lowering=False)
    S = Sched()

    def din(name, shape, dt=F32):
        return nc.dram_tensor(name, list(shape), dt, kind="ExternalInput").ap()

    def dscr(name, shape, dt):
        if dbg:
            return nc.dram_tensor(name, list(shape), dt, kind="ExternalOutput").ap()
        return nc.dram_tensor(name, list(shape), dt).ap()

    hx = din("hx", [NT, D])
    w_in = din("w_in", [D, 6656])
    gmixT_d = din("gmixT", [128, 8])
    lam_d = din("lamv", [4, 64])
    gsub_d = din("g_subln", [1, 128])
    gq_d = din("g_qnorm", [1, 128])
    gk_d = din("g_knorm", [1, 128])
    wa_d = din("w_branch_a", [D, D])
    wb_d = din("w_branch_b", [D, D])
    wo_d = din("w_out", [D, D])
    gffn_d = din("g_ffn", [1, D])
    wr_d = din("w_router", [D, NEXP])
    if stop_after >= 5:
        wg_d = din("w_gate", [NEXP, D, 512])
        wu_d = din("w_up", [NEXP, D, 512])
        wd_d = din("w_down", [NEXP, 512, D])
    gfin_d = din("g_final", [1, D])
    sigk_d = din("sigk", [4, NT], BF16)
    qaug_d = din("qaug", [8, 4, NOWN], BF16)
    beta_d = din("beta", [128, 8 * 5 * NKB])
    dtab_d = din("dtab", [128, 896])
    rope_d = din("rope", [NT, 256])
    identb_d = din("identb", [128, 128], BF16)
    identf_d = din("identf", [128, 128])
    y_out = nc.dram_tensor("y", [2048, D], F32, kind="ExternalOutput").ap()

    KAs = dscr("KAs", [8, 128, NT], BF16)
    QAs = dscr("QAs", [8, 128, NOWN], BF16)
    VAs = dscr("VAs", [8, 128, NKB, 128], BF16)
    KBs = dscr("KBs", [2, 128, NT], BF16)
    QBs = dscr("QBs", [8, 128, NOWN], BF16)
    VBs = dscr("VBs", [2, 128, NKB, 128], BF16)
    Gs = dscr("Gs", [NOWN, 2048], BF16)
    OAs = dscr("OAs", [NOWN, D], BF16)
    OBs = dscr("OBs", [NOWN, D], BF16)
    agin = nc.dram_tensor("agin", [NEXP, NOWN], F32)
    agout = nc.dram_tensor("agout", [4 * NEXP, NOWN], F32)
    thr_d = nc.dram_tensor("thr_d", [1, NEXP], F32).ap()
    dbg_aff = dscr("dbg_aff", [NOWN, NEXP], F32) if dbg else None
    dbg_h1 = dscr("dbg_h1", [NOWN, D], F32) if dbg else None
    dbg_thr = dscr("dbg_thr", [1, NEXP], F32) if dbg else None

    rKAs, rQAs, rVAs = Res("KAs", True), Res("QAs", True), Res("VAs", True)
    rKBs, rQBs, rVBs = Res("KBs", True), Res("QBs", True), Res("VBs", True)
    rGs, rOAs, rOBs = Res("Gs", True), Res("OAs", True), Res("OBs", True)

    with ExitStack() as top:
        def sem(name):
            return top.enter_context(nc.semaphore(name))

        for e in Sched.ENGS:
            S.sem[e] = sem("sem_" + e)

        def dsem(name):
            d = DSem(sem(name))
            S.all_dsems.append(d)
            return d

        def sb(es, name, shape, dt):
            return es.enter_context(nc.sbuf_tensor("s_" + name, list(shape), dt))

        def psum(es, name, shape, dt):
            return es.enter_context(nc.psum_tensor("p_" + name, list(shape), dt))

        block = top.enter_context(nc.Block())

        def V(fn, r=(), w=()):
            S.op("dve", fn, r, w)

        def A(fn, r=(), w=()):
            S.op("act", fn, r, w)

        def P(fn, r=(), w=(), inc=True):
            S.op("pe", fn, r, w, inc)

        def G(fn, r=(), w=()):
            S.op("pool", fn, r, w)

        def DMA(out, in_, ds, r=(), w=(), q="sp"):
            S.dma(q, (lambda e, o=out, i=in_: e.dma_start(out=o, in_=i)), ds, r, w)

        identb = sb(top, "identb", [128, 128], BF16)
        identf = sb(top, "identf", [128, 128], F32)
        epsT = sb(top, "epsT", [128, 1], F32)
        rC = Res("consts", True)
        dsDbg = dsem("ds_dbg")
        dsC = dsem("ds_const")
        DMA(identb[:, :], identb_d[:, :], dsC, w=[rC])
        DMA(identf[:, :], identf_d[:, :], dsC, w=[rC])
        V(lambda e: e.memset(epsT[:, :], EPS), w=[rC])

        def rstd_from_ss(ss_ap, ln_ap, out_ap, n, rs, ws):
            A(lambda e: e.activation(out=ln_ap, in_=ss_ap, func=AF.Ln,
                                     bias=epsT[:ln_ap.shape[0], 0:1], scale=1.0 / n),
              r=rs + [rC], w=ws)
            A(lambda e: e.activation(out=out_ap, in_=ln_ap, func=AF.Exp, scale=-0.5),
              r=ws, w=ws)

        STS = [(t * 512, 512, 4, 128) for t in range(16)] + [(SEQ, NMETA, 1, NMETA)]

        def is_own(st):
            return st < 4 or st == 16

        def q0_of(st):
            return st * 512 if st < 4 else 2048

        with ExitStack() as p1:
            uT_own = sb(p1, "uT_own", [128, 8, NOWN], BF16)
            rUown = [Res(f"uTown{i}") for i in range(5)]
            gmixT = sb(p1, "gmixT", [128, 8], F32)
            gqb = sb(p1, "gqb", [128, 128], F32)
            gkb = sb(p1, "gkb", [128, 128], F32)
            dsC1 = dsem("ds_c1")
            DMA(gmixT[:, :], gmixT_d[:, :], dsC1, w=[rC])
            DMA(gqb[:, :], gq_d[0:1, :].partition_broadcast(128), dsC1, w=[rC])
            DMA(gkb[:, :], gk_d[0:1, :].partition_broadcast(128), dsC1, w=[rC])

            def norm_rope(raw, H, psub, gb, cs_t, s, out_bf, scr, rs_extra, r_raw, r_scr, r_out):
                sq, ssk, lnk, rk, tmp, yy = scr
                W = H * 128
                V(lambda e: e.tensor_tensor(out=sq[:psub, :W], in0=raw[:psub, :W], in1=raw[:psub, :W], op=ALU.mult),
                  r=[r_raw], w=[r_scr])
                V(lambda e: e.tensor_reduce(out=ssk[:psub, :H],
                                            in_=sq[:psub, :W].rearrange("p (h d) -> p h d", h=H),
                                            axis=AX.X, op=ALU.add), r=[r_scr], w=[r_scr])
                rstd_from_ss(ssk[:psub, :H], lnk[:psub, :H], rk[:psub, :H], 128.0, [r_scr], [r_scr])
                y3 = yy[:psub, :W].rearrange("p (h d) -> p h d", h=H)
                V(lambda e: e.tensor_tensor(out=y3, in0=raw[:psub, :W].rearrange("p (h d) -> p h d", h=H),
                                            in1=rk[:psub, 0:H].unsqueeze(2).to_broadcast([psub, H, 128]),
                                            op=ALU.mult), r=[r_raw, r_scr], w=[r_scr])
                V(lambda e: e.tensor_tensor(out=y3, in0=y3,
                                            in1=gb[:psub, :].unsqueeze(1).to_broadcast([psub, H, 128]),
                                            op=ALU.mult), r=[r_scr, rC], w=[r_scr])
                y5 = yy[:psub, :W].rearrange("p (h a b c) -> p h a b c", h=H, a=2, b=2)
                t5 = tmp[:psub, :W].rearrange("p (h a b c) -> p h a b c", h=H, a=2, b=2)
                sn4 = cs_t[:psub, s, 128:256].rearrange("p (a b c) -> p a b c", a=2, b=2)
                V(lambda e: e.tensor_tensor(out=t5[:, :, :, 0, :], in0=y5[:, :, :, 1, :],
                                            in1=sn4[:, :, 0, :].unsqueeze(1).to_broadcast([psub, H, 2, 32]),
                                            op=ALU.mult), r=[r_scr] + rs_extra, w=[r_scr])
                V(lambda e: e.tensor_tensor(out=t5[:, :, :, 1, :], in0=y5[:, :, :, 0, :],
                                            in1=sn4[:, :, 1, :].unsqueeze(1).to_broadcast([psub, H, 2, 32]),
                                            op=ALU.mult), r=[r_scr] + rs_extra, w=[r_scr])
                V(lambda e: e.tensor_tensor(out=y3, in0=y3,
                                            in1=cs_t[:psub, s, 0:128].unsqueeze(1).to_broadcast([psub, H, 128]),
                                            op=ALU.mult), r=[r_scr] + rs_extra, w=[r_scr])
                V(lambda e: e.tensor_tensor(out=out_bf[:psub, :W], in0=yy[:psub, :W], in1=tmp[:psub, :W],
                                            op=ALU.add), r=[r_scr], w=[r_out])

            with ExitStack() as pa:
                wA = sb(pa, "wA", [128, 8, 2560], BF16)
                rWA = Res("wA", True)
                dsW = dsem("ds_w")
                w_in_v = w_in.rearrange("(c p) f -> p c f", p=128)
                for c in range(8):
                    DMA(wA[:, c, 0:2048], w_in_v[:, c, 1024:3072], dsW, w=[rWA], q="pool")
                    DMA(wA[:, c, 2048:2560], w_in_v[:, c, 4096:4608], dsW, w=[rWA], q="pool")
                xt = [sb(pa, f"xt{i}", [128, 4, D], F32) for i in range(2)]
                rXt = [Res(f"xt{i}", True) for i in range(2)]
                dsXc = [dsem(f"ds_xc{i}") for i in range(2)]
                dsX = [dsem(f"ds_x{i}") for i in range(2)]
                cs = [sb(pa, f"cs{i}", [128, 4, 256], F32) for i in range(2)]
                rCs = [Res(f"cs{i}", True) for i in range(2)]
                xsb = [sb(pa, f"xs{i}", [128, 4, D], BF16) for i in range(2)]
                rXsb = [Res(f"xs{i}") for i in range(2)]
                junk = sb(pa, "junk", [128, D], BF16)
                rJunk = Res("junk")
                ssb = [sb(pa, f"ss{i}", [128, 4], F32) for i in range(2)]
                lnvb = [sb(pa, f"lnv{i}", [128, 4], F32) for i in range(2)]
                rstdb = [sb(pa, f"rstd{i}", [128, 4], F32) for i in range(2)]
                rStb = [Res(f"stats{i}") for i in range(2)]
                uT_tmp = [sb(pa, f"uTt{i}", [128, 8, 512], BF16) for i in range(2)]
                rUt = [Res(f"uTt{i}") for i in range(2)]
                stgK = [sb(pa, f"stgK{i}", [128, 512], BF16) for i in range(3)]
                rStgK = [Res(f"stgK{i}") for i in range(3)]
                dsStgK = [dsem(f"ds_sk{i}") for i in range(3)]
                stgV = [sb(pa, f"stgV{i}", [128, 1024], BF16) for i in range(2)]
                rStgV = [Res(f"stgV{i}") for i in range(2)]
                dsStgV = [dsem(f"ds_sv{i}") for i in range(2)]
                stgVB = [sb(pa, f"stgVB{i}", [128, 256], BF16) for i in range(2)]
                rStgVB = [Res(f"stgVB{i}") for i in range(2)]
                dsStgVB = [dsem(f"ds_svb{i}") for i in range(2)]
                kraw = sb(pa, "kraw", [128, 256], F32)
                rKraw = Res("kraw")
                ksc = (sb(pa, "ksq", [128, 256], F32), sb(pa, "kss", [128, 2], F32),
                       sb(pa, "kln", [128, 2], F32), sb(pa, "krk", [128, 2], F32),
                       sb(pa, "ktmp", [128, 256], F32), sb(pa, "kyy", [128, 256], F32))
                rKsc = Res("ksc")
                krope = sb(pa, "krope", [128, 4, 256], BF16)
                rKropeS = [Res(f"krope{i}") for i in range(4)]
                stgKB = sb(pa, "stgKB", [128, 2, 512], BF16)
                rStgKB = Res("stgKB")
                dsStgKB = dsem("ds_skb")
                tpb = [psum(pa, f"tpb{i}", [128, 1024], BF16) for i in range(4)]
                rTp = [Res(f"tpb{i}", psum=True) for i in range(4)]
                acc = [psum(pa, f"acc{i}", [128, 512], F32) for i in range(3)]
                rAcc = [Res(f"acc{i}", psum=True) for i in range(3)]
                ktp = psum(pa, "ktp", [128, 1024], BF16)
                rKtp = Res("ktp", psum=True)
                acc_i = [0]

                def next_acc():
                    i = acc_i[0] % 3
                    acc_i[0] += 1
                    return acc[i], rAcc[i]

                def load_x(sti):
                    tok0, ntok, nsub, psub = STS[sti]
                    sl = sti % 2
                    DMA(xt[sl][:psub, 0:nsub, :],
                        hx[tok0:tok0 + ntok, :].rearrange("(s p) d -> p s d", p=psub),
                        dsX[sl], w=[rXt[sl]])
                    DMA(cs[sl][:psub, 0:nsub, :],
                        rope_d[tok0:tok0 + ntok, :].rearrange("(s p) d -> p s d", p=psub),
                        dsXc[sl], w=[rCs[sl]])

                load_x(0)

                def prep(sti):
                    tok0, ntok, nsub, psub = STS[sti]
                    sl = sti % 2
                    x_t = xt[sl]
                    ss, lnv, rstd, rSt, xs, rXs = ssb[sl], lnvb[sl], rstdb[sl], rStb[sl], xsb[sl], rXsb[sl]
                    for s in range(nsub):
                        A(lambda e, s=s: e.activation(out=junk[:psub, :], in_=x_t[:psub, s, :],
                                                      func=AF.Square, accum_out=ss[:psub, s:s + 1]),
                          r=[rXt[sl]], w=[rJunk, rSt])
                    rstd_from_ss(ss[:psub, :nsub], lnv[:psub, :nsub], rstd[:psub, :nsub], float(D),
                                 [rSt], [rSt])
                    for s in range(nsub):
                        V(lambda e, s=s: e.tensor_scalar(out=xs[:psub, s, :], in0=x_t[:psub, s, :],
                                                         scalar1=rstd[:psub, s:s + 1], scalar2=None,
                                                         op0=ALU.mult),
                          r=[rXt[sl], rSt], w=[rXs])

                prep(0)
                pend = []
                kcount = [0]
                vcount = [0]
                import os
                DBGL = os.environ.get("KDBG", "")
                for sti, (tok0, ntok, nsub, psub) in enumerate(STS):
                    sl = sti % 2
                    if "one" in DBGL and sti >= 1:
                        break
                    if sti + 1 < len(STS) and "one" not in DBGL:
                        load_x(sti + 1)
                    xs, rXs = xsb[sl], rXsb[sl]
                    own = is_own(sti)
                    if own:
                        q0 = q0_of(sti)
                        uT = uT_own
                        ucol = q0
                        rU = rUown[sti if sti < 4 else 4]
                    else:
                        uT = uT_tmp[sl]
                        ucol = 0
                        rU = rUt[sl]
                    for dc in range(8):
                        bank = dc // 2
                        half = dc % 2
                        for s in range(nsub):
                            P(lambda e, dc=dc, s=s, bank=bank, half=half: e.transpose(
                                out=tpb[bank][:, half * 512 + s * 128: half * 512 + s * 128 + psub],
                                in_=xs[:psub, s, dc * 128:(dc + 1) * 128],
                                identity=identb[:psub, :psub]),
                              r=[rXs, rC], w=[rTp[bank]], inc=(s == nsub - 1))
                        src = tpb[bank][:, half * 512: half * 512 + ntok]
                        dst = uT[:, dc, ucol:ucol + ntok]
                        if bank % 2 == 0:
                            V(lambda e, src=src, dst=dst, dc=dc: e.tensor_scalar(
                                out=dst, in0=src, scalar1=gmixT[:, dc:dc + 1], scalar2=None, op0=ALU.mult),
                              r=[rTp[bank], rC], w=[rU])
                        else:
                            A(lambda e, src=src, dst=dst, dc=dc: e.activation(
                                out=dst, in_=src, func=AF.Copy, scale=gmixT[:, dc:dc + 1]),
                              r=[rTp[bank], rC], w=[rU])
                    if sti + 1 < len(STS) and "one" not in DBGL:
                        prep(sti + 1)
                    if "noproj" in DBGL:
                        continue
                    for h in range(8 if "nokA" not in DBGL else 0):
                        a_t, rA = next_acc()
                        for dc in range(8):
                            P(lambda e, a_t=a_t, dc=dc, h=h: e.matmul(
                                a_t[:, :ntok], lhsT=wA[:, dc, h * 128:(h + 1) * 128],
                                rhs=uT[:, dc, ucol:ucol + ntok], start=(dc == 0), stop=(dc == 7)),
                              r=[rWA, rU], w=[rA], inc=(dc == 7))
                        ks = kcount[0] % 3
                        kcount[0] += 1
                        A(lambda e, a_t=a_t, ks=ks: e.activation(out=stgK[ks][:, :ntok], in_=a_t[:, :ntok],
                                                                 func=AF.Copy),
                          r=[rA], w=[rStgK[ks]])
                        DMA(KAs[h, :, tok0:tok0 + ntok], stgK[ks][:, :ntok], dsStgK[ks],
                            r=[rStgK[ks]], w=[rKAs])
                    if "notok" in DBGL:
                        continue
                    for s in range(nsub):
                        blk = (tok0 // 128) + s
                        vs = vcount[0] % 2
                        vcount[0] += 1
                        for g in range(2):
                            a_t, rA = next_acc()
                            for dc in range(8):
                                P(lambda e, a_t=a_t, dc=dc, g=g, s=s: e.matmul(
                                    a_t[:psub, :], lhsT=uT[:, dc, ucol + s * 128: ucol + s * 128 + psub],
                                    rhs=wA[:, dc, 1024 + g * 512: 1024 + (g + 1) * 512],
                                    start=(dc == 0), stop=(dc == 7)),
                                  r=[rWA, rU], w=[rA], inc=(dc == 7))
                            if g == 0:
                                A(lambda e, a_t=a_t, vs=vs: e.activation(
                                    out=stgV[vs][:psub, 0:512], in_=a_t[:psub, :], func=AF.Copy),
                                  r=[rA], w=[rStgV[vs]])
                            else:
                                V(lambda e, a_t=a_t, vs=vs: e.tensor_copy(
                                    out=stgV[vs][:psub, 512:1024], in_=a_t[:psub, :]),
                                  r=[rA], w=[rStgV[vs]])
                        if "novst" not in DBGL:
                          DMA(VAs[:, 0:psub, blk, :].rearrange("h p e -> p h e"),
                            stgV[vs][:psub, :].rearrange("p (h e) -> p h e", h=8),
                            dsStgV[vs], r=[rStgV[vs]], w=[rVAs])
                        a_t, rA = next_acc()
                        for dc in range(8):
                            P(lambda e, a_t=a_t, dc=dc, s=s: e.matmul(
                                a_t[:psub, :], lhsT=uT[:, dc, ucol + s * 128: ucol + s * 128 + psub],
                                rhs=wA[:, dc, 2048:2560], start=(dc == 0), stop=(dc == 7)),
                              r=[rWA, rU], w=[rA], inc=(dc == 7))
                        A(lambda e, a_t=a_t: e.activation(out=kraw[:psub, :], in_=a_t[:psub, 0:256],
                                                          func=AF.Copy), r=[rA], w=[rKraw])
                        A(lambda e, a_t=a_t, vs=vs: e.activation(out=stgVB[vs][:psub, :],
                                                                 in_=a_t[:psub, 256:512], func=AF.Copy),
                          r=[rA], w=[rStgVB[vs]])
                        if "novst" not in DBGL:
                          DMA(VBs[:, 0:psub, blk, :].rearrange("h p e -> p h e"),
                            stgVB[vs][:psub, :].rearrange("p (h e) -> p h e", h=2),
                            dsStgVB[vs], r=[rStgVB[vs]], w=[rVBs])
                        if "norope" in DBGL:
                            continue
                        norm_rope(kraw, 2, psub, gkb, cs[sl], s, krope[:, s, :], ksc,
                                  [rCs[sl]], rKraw, rKsc, rKropeS[s])
                        old = list(pend)
                        pend.clear()
                        for f_ in old:
                            f_()

                        def tr(s=s, psub=psub):
                            for j in range(2):
                                P(lambda e, j=j: e.transpose(
                                    out=ktp[:, j * 512 + s * 128: j * 512 + s * 128 + psub],
                                    in_=krope[:psub, s, j * 128:(j + 1) * 128],
                                    identity=identb[:psub, :psub]),
                                  r=[rKropeS[s], rC], w=[rKtp], inc=(j == 1))
                        pend.append(tr)
                    if "norope" in DBGL:
                        continue

                    def fin(ntok=ntok, tok0=tok0):
                        for j in range(2):
                            V(lambda e, j=j: e.tensor_copy(out=stgKB[:, j, :ntok],
                                                           in_=ktp[:, j * 512: j * 512 + ntok]),
                              r=[rKtp], w=[rStgKB])
                        for j in range(2):
                            DMA(KBs[j, :, tok0:tok0 + ntok], stgKB[:, j, :ntok], dsStgKB,
                                r=[rStgKB], w=[rKBs])
                    pend.append(fin)
                for f_ in pend:
                    f_()
                pend.clear()
                S.barrier()

            if stop_after >= 1.5:
                with ExitStack() as pb:
                    wB = sb(pb, "wB", [128, 8, 4096], BF16)
                    rWBa, rWBb, rWBg = Res("wBa", True), Res("wBb", True), Res("wBg", True)
                    dsWBa, dsWBb, dsWBg = dsem("ds_wba"), dsem("ds_wbb"), dsem("ds_wbg")
                    w_in_v = w_in.rearrange("(c p) f -> p c f", p=128)
                    for c in range(8):
                        DMA(wB[:, c, 0:1024], w_in_v[:, c, 0:1024], dsWBa, w=[rWBa], q="pool")
                    for c in range(8):
                        DMA(wB[:, c, 1024:2048], w_in_v[:, c, 3072:4096], dsWBb, w=[rWBb], q="pool")
                    for c in range(8):
                        DMA(wB[:, c, 2048:4096], w_in_v[:, c, 4608:6656], dsWBg, w=[rWBg], q="pool")
                    csB = [sb(pb, f"csB{i}", [128, 4, 256], F32) for i in range(2)]
                    rCsB = [Res(f"csB{i}", True) for i in range(2)]
                    dsCsB = [dsem(f"ds_csb{i}") for i in range(2)]
                    stgQ = [sb(pb, f"stgQ{i}", [128, 512], BF16) for i in range(3)]
                    rStgQ = [Res(f"stgQ{i}") for i in range(3)]
                    dsStgQ = [dsem(f"ds_sq{i}") for i in range(3)]
                    qraw = sb(pb, "qraw", [128, 1024], F32)
                    rQraw = Res("qraw")
                    qsc = (sb(pb, "qsq", [128, 1024], F32), sb(pb, "qss", [128, 8], F32),
                           sb(pb, "qln", [128, 8], F32), sb(pb, "qrk", [128, 8], F32),
                           sb(pb, "qtmp", [128, 1024], F32), sb(pb, "qyy", [128, 1024], F32))
                    rQsc = Res("qsc")
                    qrope = sb(pb, "qrope", [128, 1024], BF16)
                    rQrope = Res("qrope")
                    stgQB = [sb(pb, f"stgQB{i}", [128, 8, 128], BF16) for i in range(2)]
                    rStgQB = [Res(f"stgQB{i}") for i in range(2)]
                    dsStgQB = [dsem(f"ds_sqb{i}") for i in range(2)]
                    stgG = [sb(pb, f"stgG{i}", [128, 2048], BF16) for i in range(2)]
                    rStgG = [Res(f"stgG{i}") for i in range(2)]
                    dsStgG = [dsem(f"ds_sg{i}") for i in range(2)]
                    accB = [psum(pb, f"accB{i}", [128, 512], F32) for i in range(6)]
                    rAccB = [Res(f"accB{i}", psum=True) for i in range(6)]
                    qtp = psum(pb, "qtp", [128, 1024], BF16)
                    rQtp = Res("qtp", psum=True)
                    accb_i = [0]

                    def next_accB():
                        i = accb_i[0] % 6
                        accb_i[0] += 1
                        return accB[i], rAccB[i]

                    own_sts = [0, 1, 2, 3, 16]
                    qcount = [0]
                    bcount = [0]
                    qrope2 = [qrope, sb(pb, "qrope1", [128, 1024], BF16)]
                    rQrope2 = [rQrope, Res("qrope1")]
                    for oi, sti in enumerate(own_sts):
                        tok0, ntok, nsub, psub = STS[sti]
                        q0 = q0_of(sti)
                        rU = rUown[oi]
                        for h in range(8):
                            a_t, rA = next_accB()
                            for dc in range(8):
                                P(lambda e, a_t=a_t, dc=dc, h=h: e.matmul(
                                    a_t[:, :ntok], lhsT=wB[:, dc, h * 128:(h + 1) * 128],
                                    rhs=uT_own[:, dc, q0:q0 + ntok], start=(dc == 0), stop=(dc == 7)),
                                  r=[rWBa, rU], w=[rA], inc=(dc == 7))
                            ks = qcount[0] % 3
                            qcount[0] += 1
                            A(lambda e, a_t=a_t, ks=ks: e.activation(out=stgQ[ks][:, :ntok],
                                                                     in_=a_t[:, :ntok], func=AF.Copy),
                              r=[rA], w=[rStgQ[ks]])
                            DMA(QAs[h, :, q0:q0 + ntok], stgQ[ks][:, :ntok], dsStgQ[ks],
                                r=[rStgQ[ks]], w=[rQAs])
                    pendB = []
                    for oi, sti in enumerate(own_sts):
                        tok0, ntok, nsub, psub = STS[sti]
                        q0 = q0_of(sti)
                        rU = rUown[oi]
                        sl = oi % 2
                        DMA(csB[sl][:psub, 0:nsub, :],
                            rope_d[tok0:tok0 + ntok, :].rearrange("(s p) d -> p s d", p=psub),
                            dsCsB[sl], w=[rCsB[sl]])
                        for s in range(nsub):
                            bs = bcount[0] % 2
                            bcount[0] += 1
                            qs = q0 + s * 128
                            for g in range(2):
                                a_t, rA = next_accB()
                                for dc in range(8):
                                    P(lambda e, a_t=a_t, dc=dc, g=g, qs=qs: e.matmul(
                                        a_t[:psub, :], lhsT=uT_own[:, dc, qs:qs + psub],
                                        rhs=wB[:, dc, 1024 + g * 512: 1024 + (g + 1) * 512],
                                        start=(dc == 0), stop=(dc == 7)),
                                      r=[rWBb, rU], w=[rA], inc=(dc == 7))
                                A(lambda e, a_t=a_t, g=g: e.activation(
                                    out=qraw[:psub, g * 512:(g + 1) * 512], in_=a_t[:psub, :], func=AF.Copy),
                                  r=[rA], w=[rQraw])
                            norm_rope(qraw, 8, psub, gqb, csB[sl], s, qrope2[bs], qsc,
                                      [rCsB[sl]], rQraw, rQsc, rQrope2[bs])
                            for g in range(4):
                                a_t, rA = next_accB()
                                for dc in range(8):
                                    P(lambda e, a_t=a_t, dc=dc, g=g, qs=qs: e.matmul(
                                        a_t[:psub, :], lhsT=uT_own[:, dc, qs:qs + psub],
                                        rhs=wB[:, dc, 2048 + g * 512: 2048 + (g + 1) * 512],
                                        start=(dc == 0), stop=(dc == 7)),
                                      r=[rWBg, rU], w=[rA], inc=(dc == 7))
                                A(lambda e, a_t=a_t, g=g, bs=bs: e.activation(
                                    out=stgG[bs][:psub, g * 512:(g + 1) * 512], in_=a_t[:psub, :],
                                    func=AF.Sigmoid), r=[rA], w=[rStgG[bs]])
                            DMA(Gs[qs:qs + psub, :], stgG[bs][:psub, :], dsStgG[bs],
                                r=[rStgG[bs]], w=[rGs])
                            oldB = list(pendB)
                            pendB.clear()
                            for f_ in oldB:
                                f_()

                            def trB(bs=bs, psub=psub, qs=qs):
                                for j in range(8):
                                    P(lambda e, j=j: e.transpose(
                                        out=qtp[:, j * 128: j * 128 + psub],
                                        in_=qrope2[bs][:psub, j * 128:(j + 1) * 128],
                                        identity=identb[:psub, :psub]),
                                      r=[rQrope2[bs], rC], w=[rQtp], inc=(j == 7))
                                V(lambda e: e.tensor_copy(
                                    out=stgQB[bs][:, :, :psub],
                                    in_=qtp[:, :].rearrange("p (g t) -> p g t", g=8)[:, :, :psub]),
                                  r=[rQtp], w=[rStgQB[bs]])
                                DMA(QBs[:, :, qs:qs + psub].rearrange("g p t -> p g t"),
                                    stgQB[bs][:, :, :psub], dsStgQB[bs], r=[rStgQB[bs]], w=[rQBs])
                            pendB.append(trB)
                    for f_ in pendB:
                        f_()
                    pendB.clear()
                    S.barrier()

        if stop_after >= 2:
            with ExitStack() as p2:
                KT = [[sb(p2, f"KT{s}_{m}", [128, NT], BF16) for m in range(2)] for s in range(2)]
                VT = [sb(p2, f"VT{s}", [128, NKB, 129], BF16) for s in range(2)]
                QT = [[sb(p2, f"QT{s}_{m}", [128, NOWN], BF16) for m in range(2)] for s in range(2)]
                rKV = [Res(f"KV{s}", True) for s in range(2)]
                dsC2 = dsem("ds_c2")
                dsSig = [dsem(f"ds_sig{i}") for i in range(2)]
                rSig = [Res(f"sig{i}", True) for i in range(2)]
                dsKV = [dsem(f"ds_kv{s}") for s in range(2)]
                beta = sb(p2, "beta", [128, 8 * 5 * NKB], F32)
                dtab = sb(p2, "dtab", [128, 896], F32)
                gsubb = sb(p2, "gsubb", [128, 128], F32)
                lamb = sb(p2, "lamb", [128, 4, 64], F32)
                lamt = sb(p2, "lamt", [128, 2, 64], F32)
                lame = sb(p2, "lame", [128, 2], F32)
                lamneg = sb(p2, "lamneg", [128, 1], F32)
                rLam = Res("lam")
                DMA(beta[:, :], beta_d[:, :], dsC2, w=[rC])
                DMA(dtab[:, :], dtab_d[:, :], dsC2, w=[rC])
                DMA(gsubb[:, :], gsub_d[0:1, :].partition_broadcast(128), dsC2, w=[rC])
                for i in range(4):
                    DMA(lamb[:, i, :], lam_d[i:i + 1, :].partition_broadcast(128), dsC2, w=[rC])
                for s in range(2):
                    for m in range(2):
                        DMA(KT[s][m][64:68, :], sigk_d[:, :], dsSig[s], w=[rSig[s]])
                    V(lambda e, s=s: e.memset(VT[s][:, :, 128:129], 1.0), w=[rSig[s]])
                V(lambda e: e.tensor_tensor(out=lamt[:, 0, :], in0=lamb[:, 0, :], in1=lamb[:, 1, :], op=ALU.mult),
                  r=[rC], w=[rLam])
                V(lambda e: e.tensor_tensor(out=lamt[:, 1, :], in0=lamb[:, 2, :], in1=lamb[:, 3, :], op=ALU.mult),
                  r=[rC], w=[rLam])
                V(lambda e: e.tensor_reduce(out=lame[:, 0:2], in_=lamt[:, :, :], axis=AX.X, op=ALU.add),
                  r=[rLam], w=[rLam])
                A(lambda e: e.activation(out=lame[:, 0:2], in_=lame[:, 0:2], func=AF.Exp), r=[rLam], w=[rLam])
                V(lambda e: e.tensor_tensor(out=lamneg[:, :], in0=lame[:, 1:2], in1=lame[:, 0:1], op=ALU.subtract),
                  r=[rLam], w=[rLam])
                V(lambda e: e.tensor_scalar(out=lamneg[:, :], in0=lamneg[:, :], scalar1=-LAM_INIT, scalar2=None,
                                            op0=ALU.add), r=[rLam], w=[rLam])
                V(lambda e: e.tensor_scalar(out=gsubb[:, :], in0=gsubb[:, :], scalar1=1.0 - LAM_INIT,
                                            scalar2=None, op0=ALU.mult), r=[rC], w=[rC])

                PT = [sb(p2, f"PT{i}", [128, 1024], BF16) for i in range(3)]
                rPT = [Res(f"PT{i}") for i in range(3)]
                smix = [sb(p2, f"smix{i}", [128, 1024], F32) for i in range(2)]
                rSmix = [Res(f"smix{i}") for i in range(2)]
                Sps = [psum(p2, f"Sps{i}", [128, 1024], F32) for i in range(2)]
                rSps = [Res(f"Sps{i}", psum=True) for i in range(2)]
                Ops = psum(p2, "Ops", [128, 1536], F32)
                rOps = Res("Ops", psum=True)
                ev = {n: sb(p2, "ev_" + n, shp, F32) for n, shp in
                      [("r1", [128, 8]), ("t2", [128, 128]), ("dd", [128, 128]), ("ssd", [128, 1]),
                       ("lnd", [128, 1]), ("rsd", [128, 1]), ("junk", [128, 128])]}
                rEv = Res("ev")
                stgO = [sb(p2, f"stgO{i}", [128, 4, 128], BF16) for i in range(2)]
                rStgO = [Res(f"stgO{i}") for i in range(2)]
                dsStgO = [dsem(f"ds_so{i}") for i in range(2)]

                Osb = [sb(p2, f"Osb{i}", [128, 1536], F32) for i in range(2)]
                rOsb = [Res(f"Osb{i}") for i in range(2)]
                osb_i = [0]

                def oacc(m, s, n=129):
                    i = m * 4 + s
                    off = (i // 3) * 512 + (i % 3) * 129
                    return Ops[:, off:off + n]

                jobs = [("A", h) for h in range(8)] + [("B", pi) for pi in range(4)]

                def load_job(ji):
                    kind, idx = jobs[ji]
                    s = ji % 2
                    if kind == "A":
                        for m in range(2):
                            for half in range(2):
                                c0, c1 = half * 4104, (half + 1) * 4104
                                DMA(KT[s][m][0:64, c0:c1], KAs[idx, m * 64:(m + 1) * 64, c0:c1], dsKV[s],
                                    r=[rKAs], w=[rKV[s]])
                            DMA(QT[s][m][0:64, :], QAs[idx, m * 64:(m + 1) * 64, :], dsKV[s],
                                r=[rQAs], w=[rKV[s]])
                            DMA(QT[s][m][64:68, :], qaug_d[idx, :, :], dsKV[s], w=[rKV[s]])
                        for q4 in range(4):
                            b0, b1 = q4 * 16, (q4 + 1) * 16
                            DMA(VT[s][:, b0:b1, 0:128], VAs[idx, :, b0:b1, :], dsKV[s], r=[rVAs], w=[rKV[s]])
                        DMA(VT[s][0:16, 64:65, 0:128], VAs[idx, 0:16, 64:65, :], dsKV[s], r=[rVAs], w=[rKV[s]])
                    else:
                        kv = idx // 2
                        for half in range(2):
                            c0, c1 = half * 4104, (half + 1) * 4104
                            DMA(KT[s][0][:, c0:c1], KBs[kv, :, c0:c1], dsKV[s], r=[rKBs], w=[rKV[s], rSig[s]])
                        for m in range(2):
                            DMA(QT[s][m][:, :], QBs[2 * idx + m, :, :], dsKV[s], r=[rQBs], w=[rKV[s]])
                        for q4 in range(4):
                            b0, b1 = q4 * 16, (q4 + 1) * 16
                            DMA(VT[s][:, b0:b1, 0:128], VBs[kv, :, b0:b1, :], dsKV[s], r=[rVBs], w=[rKV[s]])
                        DMA(VT[s][0:16, 64:65, 0:128], VBs[kv, 0:16, 64:65, :], dsKV[s], r=[rVBs], w=[rKV[s]])

                CH = [(c * 512, 512, 4, 128) for c in range(4)] + [(2048, NMETA, 1, NMETA)]
                load_job(0)
                pt_i = [0]
                so_i = [0]
                for ji, (kind, idx) in enumerate(jobs):
                    s = ji % 2
                    if ji + 1 < len(jobs):
                        load_job(ji + 1)
                    isA = kind == "A"
                    KR = 68 if isA else 128
                    slope = 2.0 ** (-(idx + 1)) if isA else 0.0
                    scale = 0.125 if isA else 128.0 ** -0.5
                    Kt = [KT[s][0], KT[s][1] if isA else KT[s][0]]
                    Qt = QT[s]
                    for c, (q0, ncq, nsb, psq) in enumerate(CH):

                        started = set()

                        def qk(kb, sp):
                            kp = 128 if kb < 64 else NMETA
                            k0 = kb * 128
                            mixed = isA and c < 4 and (4 * c <= kb <= 4 * c + 3)
                            rows = 64 if mixed else KR
                            for m in range(2):
                                P(lambda e, m=m, sp=sp, kp=kp, k0=k0, rows=rows: e.matmul(
                                    Sps[sp][:kp, m * 512: m * 512 + ncq],
                                    lhsT=Kt[m][0:rows, k0:k0 + kp], rhs=Qt[m][0:rows, q0:q0 + ncq],
                                    start=True, stop=True),
                                  r=[rKV[s], rSig[s]], w=[rSps[sp]], inc=(m == 1))

                        def soft(kb, sp):
                            kp = 128 if kb < 64 else NMETA
                            mixed = isA and c < 4 and (4 * c <= kb <= 4 * c + 3)
                            pi_ = pt_i[0] % 3
                            pt_i[0] += 1
                            if ncq == 512:
                                src = Sps[sp][:kp, :]
                                dst = PT[pi_][:kp, :]
                            else:
                                src = Sps[sp][:kp, :].rearrange("p (m q) -> p m q", m=2)[:, :, 0:ncq]
                                dst = PT[pi_][:kp, :].rearrange("p (m q) -> p m q", m=2)[:, :, 0:ncq]
                            if mixed:
                                mm = kb - 4 * c
                                x0 = 384 - 128 * mm
                                for m in range(2):
                                    V(lambda e, m=m, sp=sp, x0=x0: e.scalar_tensor_tensor(
                                        out=smix[sp][:, m * 512:(m + 1) * 512], in0=dtab[:, x0:x0 + 512],
                                        scalar=-8.0 * slope, in1=Sps[sp][:, m * 512:(m + 1) * 512],
                                        op0=ALU.mult, op1=ALU.add),
                                      r=[rSps[sp], rC], w=[rSmix[sp]])
                                A(lambda e, sp=sp, dst=dst: e.activation(out=dst, in_=smix[sp][:, :], func=AF.Exp,
                                                                         scale=scale),
                                  r=[rSmix[sp]], w=[rPT[pi_]])
                            elif isA:
                                bcol = (idx * 5 + c) * NKB + kb
                                A(lambda e, src=src, dst=dst, bcol=bcol, kp=kp: e.activation(
                                    out=dst, in_=src, func=AF.Exp, bias=beta[:kp, bcol:bcol + 1], scale=scale),
                                  r=[rSps[sp], rC], w=[rPT[pi_]])
                            else:
                                A(lambda e, src=src, dst=dst: e.activation(out=dst, in_=src, func=AF.Exp,
                                                                           scale=scale),
                                  r=[rSps[sp]], w=[rPT[pi_]])
                            return pi_

                        def pv(kb, pi_, is_last, is_first):
                            kp = 128 if kb < 64 else NMETA
                            for m in range(2):
                                for sq in range(nsb):
                                    last = (m == 1 and sq == nsb - 1)
                                    bank_ = (m * 4 + sq) // 3
                                    st_ = is_first and (bank_ not in started)
                                    if is_first:
                                        started.add(bank_)
                                    P(lambda e, m=m, sq=sq, pi_=pi_, kp=kp, kb=kb, st_=st_: e.matmul(
                                        oacc(m, sq)[:psq, :],
                                        lhsT=PT[pi_][:kp, m * 512 + sq * 128: m * 512 + sq * 128 + psq],
                                        rhs=VT[s][:kp, kb, :], start=st_, stop=is_last,
                                        skip_group_check=True),
                                      r=[rPT[pi_], rKV[s], rSig[s]], w=[rOps], inc=last)

                        kbs = []
                        for kb in range(64):
                            if (not isA) or c == 4:
                                kbs.append(kb)
                                continue
                            lo_q, hi_q = 512 * c, 512 * c + 511
                            cands = [(128 * kb, 128 * kb + 127)]
                            if kb >= 16:
                                cands.append((128 * kb - 8192, 128 * kb + 127 - 8192))
                            dmin = min(max(0, ul - hi_q, lo_q - uh) for ul, uh in cands)
                            if slope * dmin <= 100.0:
                                kbs.append(kb)
                        kbs.append(64)
                        qk(kbs[0], 0)
                        if len(kbs) > 1:
                            qk(kbs[1], 1)
                        for i, kb in enumerate(kbs):
                            pi_ = soft(kb, i % 2)
                            if i + 2 < len(kbs):
                                qk(kbs[i + 2], i % 2)
                            pv(kb, pi_, i == len(kbs) - 1, i == 0)
                        ob_i = osb_i[0] % 2
                        osb_i[0] += 1
                        rOsbC = rOsb[ob_i]
                        if nsb == 4:
                            V(lambda e, ob_i=ob_i: e.tensor_copy(
                                out=Osb[ob_i][:, 0:1024].rearrange("p (b c) -> p b c", b=2)[:, :, 0:387],
                                in_=Ops[:, 0:1024].rearrange("p (b c) -> p b c", b=2)[:, :, 0:387]),
                              r=[rOps], w=[rOsbC])
                            V(lambda e, ob_i=ob_i: e.tensor_copy(out=Osb[ob_i][:, 1024:1282], in_=Ops[:, 1024:1282]),
                              r=[rOps], w=[rOsbC])
                        else:
                            V(lambda e, ob_i=ob_i: e.tensor_copy(out=Osb[ob_i][:psq, 0:129], in_=Ops[:psq, 0:129]),
                              r=[rOps], w=[rOsbC])
                            V(lambda e, ob_i=ob_i: e.tensor_copy(out=Osb[ob_i][:psq, 641:770], in_=Ops[:psq, 641:770]),
                              r=[rOps], w=[rOsbC])

                        def osbv(m, sq_, n=129, ob_i=ob_i):
                            i = m * 4 + sq_
                            off = (i // 3) * 512 + (i % 3) * 129
                            return Osb[ob_i][:, off:off + n]
                        so = so_i[0] % 2
                        so_i[0] += 1
                        for sq in range(nsb if isA else 0):
                            if isA:
                                O1, O2 = osbv(0, sq), osbv(1, sq)
                                V(lambda e, O1=O1: e.reciprocal(out=ev["r1"][:psq, 0:1], in_=O1[:psq, 128:129]),
                                  r=[rOsbC], w=[rEv])
                                V(lambda e, O2=O2: e.reciprocal(out=ev["r1"][:psq, 1:2], in_=O2[:psq, 128:129]),
                                  r=[rOsbC], w=[rEv])
                                V(lambda e: e.tensor_tensor(out=ev["r1"][:psq, 2:3], in0=ev["r1"][:psq, 1:2],
                                                            in1=lamneg[:psq, 0:1], op=ALU.mult),
                                  r=[rEv, rLam], w=[rEv])
                                V(lambda e, O2=O2: e.tensor_scalar(out=ev["t2"][:psq, :], in0=O2[:psq, 0:128],
                                                                   scalar1=ev["r1"][:psq, 2:3], scalar2=None,
                                                                   op0=ALU.mult), r=[rOsbC, rEv], w=[rEv])
                                V(lambda e, O1=O1: e.scalar_tensor_tensor(
                                    out=ev["dd"][:psq, :], in0=O1[:psq, 0:128], scalar=ev["r1"][:psq, 0:1],
                                    in1=ev["t2"][:psq, :], op0=ALU.mult, op1=ALU.add), r=[rOsbC, rEv], w=[rEv])
                                A(lambda e: e.activation(out=ev["junk"][:psq, :], in_=ev["dd"][:psq, :],
                                                         func=AF.Square, accum_out=ev["ssd"][:psq, 0:1]),
                                  r=[rEv], w=[rEv])
                                rstd_from_ss(ev["ssd"][:psq, 0:1], ev["lnd"][:psq, 0:1], ev["rsd"][:psq, 0:1],
                                             128.0, [rEv], [rEv])
                                V(lambda e, sq=sq, so=so: e.scalar_tensor_tensor(
                                    out=stgO[so][:psq, sq, :], in0=ev["dd"][:psq, :], scalar=ev["rsd"][:psq, 0:1],
                                    in1=gsubb[:psq, :], op0=ALU.mult, op1=ALU.mult),
                                  r=[rEv, rC], w=[rStgO[so]])
                        if isA:
                            dst_d = OAs[q0:q0 + ncq, idx * 128:(idx + 1) * 128].rearrange("(s p) e -> p s e", p=psq)
                            DMA(dst_d, stgO[so][:psq, 0:nsb, :], dsStgO[so], r=[rStgO[so]], w=[rOAs])
                        else:
                            for m in range(2):
                                g = 2 * idx + m
                                so = so_i[0] % 2
                                so_i[0] += 1
                                for sq in range(nsb):
                                    Om = osbv(m, sq)
                                    V(lambda e, Om=Om: e.reciprocal(out=ev["r1"][:psq, 0:1], in_=Om[:psq, 128:129]),
                                      r=[rOsbC], w=[rEv])
                                    V(lambda e, Om=Om, sq=sq, so=so: e.tensor_scalar(
                                        out=stgO[so][:psq, sq, :], in0=Om[:psq, 0:128], scalar1=ev["r1"][:psq, 0:1],
                                        scalar2=None, op0=ALU.mult), r=[rOsbC, rEv], w=[rStgO[so]])
                                dst_d = OBs[q0:q0 + ncq, g * 128:(g + 1) * 128].rearrange("(s p) e -> p s e", p=psq)
                                DMA(dst_d, stgO[so][:psq, 0:nsb, :], dsStgO[so], r=[rStgO[so]], w=[rOBs])
                S.barrier()

        if stop_after >= 3:
            with ExitStack() as p3:
                H = sb(p3, "H", [128, 17, D], F32)
                rH = [Res(f"H{t}") for t in range(17)]
                u2T = sb(p3, "u2T", [128, 8, NOWN], BF16)
                rU2 = [Res(f"u2T{t}") for t in range(17)]
                AFF = sb(p3, "AFF", [128, 17, NEXP], F32)
                rAFF = Res("AFF")
                COEF = sb(p3, "COEF", [128, 17, NEXP], F32)
                rCOEF = Res("COEF")
                rAgin = Res("agin", True)
                gffnb = sb(p3, "gffnb", [128, D], F32)
                dsC3 = dsem("ds_c3")
                DMA(gffnb[:, :], gffn_d[0:1, :].partition_broadcast(128), dsC3, w=[rC])
                BLK = [(t * 128, 128) for t in range(16)] + [(2048, NMETA)]

                with ExitStack() as p3a:
                    Wa = sb(p3a, "Wa", [128, 8, D], BF16)
                    Wb = sb(p3a, "Wb", [128, 8, D], BF16)
                    Wo = sb(p3a, "Wo", [128, 8, D], BF16)
                    wr = sb(p3a, "wr", [128, 8, NEXP], BF16)
                    rW3 = Res("W3", True)
                    dsW3 = dsem("ds_w3")
                    for c in range(8):
                        DMA(Wa[:, c, :], wa_d.rearrange("(c p) f -> p c f", p=128)[:, c, :], dsW3, w=[rW3], q="pool")
                        DMA(Wb[:, c, :], wb_d.rearrange("(c p) f -> p c f", p=128)[:, c, :], dsW3, w=[rW3], q="pool")
                        DMA(Wo[:, c, :], wo_d.rearrange("(c p) f -> p c f", p=128)[:, c, :], dsW3, w=[rW3], q="pool")
                    DMA(wr[:, :, :], wr_d.rearrange("(c p) f -> p c f", p=128), dsW3, w=[rW3], q="pool")
                    oa_t = [sb(p3a, f"oa_t{i}", [128, D], BF16) for i in range(2)]
                    ob_t = [sb(p3a, f"ob_t{i}", [128, D], BF16) for i in range(2)]
                    g_t = [sb(p3a, f"g_t{i}", [128, 2048], BF16) for i in range(2)]
                    x_t3 = [sb(p3a, f"x_t3{i}", [128, D], F32) for i in range(2)]
                    rIn3 = [Res(f"in3_{i}", True) for i in range(2)]
                    dsIn3 = [dsem(f"ds_in3_{i}") for i in range(2)]
                    oaT = sb(p3a, "oaT", [128, 8, 128], BF16)
                    obT = sb(p3a, "obT", [128, 8, 128], BF16)
                    mgT = sb(p3a, "mgT", [128, 8, 128], BF16)
                    rOaT, rObT, rMgT = Res("oaT"), Res("obT"), Res("mgT")
                    m1 = sb(p3a, "m1", [128, D], F32)
                    m2 = sb(p3a, "m2", [128, D], BF16)
                    affS = [sb(p3a, f"affS{i}", [NEXP, 128], F32) for i in range(2)]
                    rAffS = [Res(f"affS{i}") for i in range(2)]
                    dsAffS = [dsem(f"ds_affs{i}") for i in range(2)]
                    mg = sb(p3a, "mg", [128, D], BF16)
                    rM1, rM2, rMg = Res("m1"), Res("m2"), Res("mg")
                    u2 = sb(p3a, "u2", [128, D], BF16)
                    rU2t = Res("u2")
                    st3 = {n: sb(p3a, "st3_" + n, [128, 1], F32) for n in ("ss", "ln", "rs", "se", "rse")}
                    ex3 = sb(p3a, "ex3", [128, NEXP], F32)
                    rSt3 = Res("st3")
                    tp3 = [psum(p3a, f"tp3_{i}", [128, 1024], BF16) for i in range(2)]
                    rTp3 = [Res(f"tp3_{i}", psum=True) for i in range(2)]
                    yps = [psum(p3a, f"yps{i}", [128, 512], F32) for i in range(4)]
                    rYps = [Res(f"yps{i}", psum=True) for i in range(4)]
                    lps = psum(p3a, "lps", [128, 512], F32)
                    rLps = Res("lps", psum=True)
                    tfp = psum(p3a, "tfp", [128, 512], F32)
                    rTfp = Res("tfp", psum=True)

                    def load3(t):
                        q0, pb_ = BLK[t]
                        sl = t % 2
                        tokx = q0 if t < 16 else SEQ
                        DMA(oa_t[sl][:pb_, :], OAs[q0:q0 + pb_, :], dsIn3[sl], r=[rOAs], w=[rIn3[sl]])
                        DMA(ob_t[sl][:pb_, :], OBs[q0:q0 + pb_, :], dsIn3[sl], r=[rOBs], w=[rIn3[sl]])
                        DMA(g_t[sl][:pb_, :], Gs[q0:q0 + pb_, :], dsIn3[sl], r=[rGs], w=[rIn3[sl]])
                        DMA(x_t3[sl][:pb_, :], hx[tokx:tokx + pb_, :], dsIn3[sl], w=[rIn3[sl]])

                    def transpose8(src, rsrc, dstT, rdst, pb_, tpi):
                        for fc in range(8):
                            P(lambda e, fc=fc: e.transpose(out=tp3[tpi][:, fc * 128: fc * 128 + pb_],
                                                           in_=src[:pb_, fc * 128:(fc + 1) * 128],
                                                           identity=identb[:pb_, :pb_]),
                              r=[rsrc, rC], w=[rTp3[tpi]], inc=(fc == 7))

                    load3(0)
                    for t, (q0, pb_) in enumerate(BLK):
                        sl = t % 2
                        if t + 1 < len(BLK):
                            load3(t + 1)
                        transpose8(oa_t[sl], rIn3[sl], oaT, rOaT, pb_, 0)
                        V(lambda e: e.tensor_copy(out=oaT[:, :, :pb_],
                                                  in_=tp3[0][:, :].rearrange("p (c t) -> p c t", c=8)[:, :, :pb_]),
                          r=[rTp3[0]], w=[rOaT])
                        transpose8(ob_t[sl], rIn3[sl], obT, rObT, pb_, 1)
                        A(lambda e: e.activation(out=obT[:, :, :pb_],
                                                 in_=tp3[1][:, :].rearrange("p (c t) -> p c t", c=8)[:, :, :pb_],
                                                 func=AF.Copy),
                          r=[rTp3[1]], w=[rObT])
                        for g in range(2):
                            for fc in range(8):
                                P(lambda e, g=g, fc=fc: e.matmul(yps[g][:pb_, :], lhsT=oaT[:, fc, :pb_],
                                                                 rhs=Wa[:, fc, g * 512:(g + 1) * 512],
                                                                 start=(fc == 0), stop=(fc == 7)),
                                  r=[rOaT, rW3], w=[rYps[g]], inc=(fc == 7))
                        for g in range(2):
                            for fc in range(8):
                                P(lambda e, g=g, fc=fc: e.matmul(yps[2 + g][:pb_, :], lhsT=obT[:, fc, :pb_],
                                                                 rhs=Wb[:, fc, g * 512:(g + 1) * 512],
                                                                 start=(fc == 0), stop=(fc == 7)),
                                  r=[rObT, rW3], w=[rYps[2 + g]], inc=(fc == 7))
                        for g in range(2):
                            V(lambda e, g=g: e.tensor_tensor(out=m1[:pb_, g * 512:(g + 1) * 512], in0=yps[g][:pb_, :],
                                                             in1=g_t[sl][:pb_, g * 512:(g + 1) * 512], op=ALU.mult),
                              r=[rYps[g], rIn3[sl]], w=[rM1])
                            V(lambda e, g=g: e.tensor_tensor(out=m2[:pb_, g * 512:(g + 1) * 512],
                                                             in0=yps[2 + g][:pb_, :],
                                                             in1=g_t[sl][:pb_, 1024 + g * 512:1024 + (g + 1) * 512],
                                                             op=ALU.mult),
                              r=[rYps[2 + g], rIn3[sl]], w=[rM2])
                        V(lambda e: e.tensor_tensor(out=mg[:pb_, :], in0=m1[:pb_, :], in1=m2[:pb_, :], op=ALU.add),
                          r=[rM1, rM2], w=[rMg])
                        transpose8(mg, rMg, mgT, rMgT, pb_, 0)
                        V(lambda e: e.tensor_copy(out=mgT[:, :, :pb_],
                                                  in_=tp3[0][:, :].rearrange("p (c t) -> p c t", c=8)[:, :, :pb_]),
                          r=[rTp3[0]], w=[rMgT])
                        for g in range(2):
                            for fc in range(8):
                                P(lambda e, g=g, fc=fc: e.matmul(yps[g][:pb_, :], lhsT=mgT[:, fc, :pb_],
                                                                 rhs=Wo[:, fc, g * 512:(g + 1) * 512],
                                                                 start=(fc == 0), stop=(fc == 7)),
                                  r=[rMgT, rW3], w=[rYps[g]], inc=(fc == 7))
                        for g in range(2):
                            V(lambda e, g=g, t=t: e.tensor_tensor(out=H[:pb_, t, g * 512:(g + 1) * 512],
                                                                  in0=yps[g][:pb_, :],
                                                                  in1=x_t3[sl][:pb_, g * 512:(g + 1) * 512],
                                                                  op=ALU.add),
                              r=[rYps[g], rIn3[sl]], w=[rH[t]])
                        if dbg:
                            DMA(dbg_h1[q0:q0 + pb_, :], H[:pb_, t, :], dsDbg, r=[rH[t]])
                        A(lambda e, t=t: e.activation(out=u2[:pb_, :], in_=H[:pb_, t, :], func=AF.Square,
                                                      accum_out=st3["ss"][:pb_, 0:1]), r=[rH[t]], w=[rSt3, rU2t])
                        rstd_from_ss(st3["ss"][:pb_, 0:1], st3["ln"][:pb_, 0:1], st3["rs"][:pb_, 0:1], float(D),
                                     [rSt3], [rSt3])
                        V(lambda e, t=t: e.scalar_tensor_tensor(out=u2[:pb_, :], in0=H[:pb_, t, :],
                                                                scalar=st3["rs"][:pb_, 0:1], in1=gffnb[:pb_, :],
                                                                op0=ALU.mult, op1=ALU.mult),
                          r=[rH[t], rSt3, rC], w=[rU2t])
                        transpose8(u2, rU2t, None, None, pb_, 1)
                        A(lambda e, q0=q0: e.activation(
                            out=u2T[:, :, q0:q0 + pb_],
                            in_=tp3[1][:, :].rearrange("p (c t) -> p c t", c=8)[:, :, :pb_], func=AF.Copy),
                          r=[rTp3[1]], w=[rU2[t]])
                        for fc in range(8):
                            P(lambda e, fc=fc, q0=q0: e.matmul(lps[:pb_, 0:NEXP], lhsT=u2T[:, fc, q0:q0 + pb_],
                                                               rhs=wr[:, fc, :], start=(fc == 0), stop=(fc == 7)),
                              r=[rU2[t], rW3], w=[rLps], inc=(fc == 7))
                        A(lambda e: e.activation(out=ex3[:pb_, :], in_=lps[:pb_, 0:NEXP], func=AF.Exp,
                                                 accum_out=st3["se"][:pb_, 0:1]), r=[rLps], w=[rSt3])
                        V(lambda e: e.reciprocal(out=st3["rse"][:pb_, 0:1], in_=st3["se"][:pb_, 0:1]),
                          r=[rSt3], w=[rSt3])
                        V(lambda e, t=t: e.tensor_scalar(out=AFF[:pb_, t, :], in0=ex3[:pb_, :],
                                                         scalar1=st3["rse"][:pb_, 0:1], scalar2=None, op0=ALU.mult),
                          r=[rSt3], w=[rAFF])
                        P(lambda e, t=t: e.transpose(out=tfp[0:NEXP, 0:pb_], in_=AFF[:pb_, t, :],
                                                     identity=identf[:pb_, :pb_]), r=[rAFF, rC], w=[rTfp])
                        V(lambda e, sl=sl: e.tensor_copy(out=affS[sl][:, 0:pb_], in_=tfp[0:NEXP, 0:pb_]),
                          r=[rTfp], w=[rAffS[sl]])
                        DMA(agin.ap()[:, q0:q0 + pb_], affS[sl][:, 0:pb_], dsAffS[sl], r=[rAffS[sl]], w=[rAgin])
                        if dbg:
                            DMA(dbg_aff[q0:q0 + pb_, :], AFF[:pb_, t, :], dsDbg, r=[rAFF])
                    S.barrier()

                if stop_after >= 4:
                    with ExitStack() as p4:
                        AGc = sb(p4, "AGc", [NEXP, SEQ + NMETA], F32)
                        rAG = Res("AGc", True)
                        dsAG = dsem("ds_ag")
                        dsCC = DSem(sem("cc_sem"))
                        rAgout = Res("agout")
                        S.coll(lambda e: e.collective_compute(
                            "AllGather", ALU.bypass, replica_groups=[[0, 1, 2, 3], [4, 5, 6, 7]],
                            ins=[agin.ap().opt()], outs=[agout.ap().opt()]), dsCC, reads=[rAgin], writes=[rAgout])
                        ago = agout.ap()
                        DMA(AGc[:, 0:SEQ].rearrange("e (r t) -> e r t", r=4),
                            ago.rearrange("(r e) t -> e r t", e=NEXP)[:, :, 0:2048], dsAG, r=[rAgout], w=[rAG],
                            q="pool")
                        DMA(AGc[:, SEQ:SEQ + NMETA], ago[0:NEXP, 2048:2048 + NMETA], dsAG, r=[rAgout], w=[rAG],
                            q="pool")
                        lo = sb(p4, "lo", [NEXP, 1], F32)
                        mid = sb(p4, "mid", [NEXP, 1], F32)
                        cnt = sb(p4, "cnt", [NEXP, 1], F32)
                        prd = sb(p4, "prd", [NEXP, 1], F32)
                        cmpj = sb(p4, "cmpj", [NEXP, SEQ + NMETA], BF16)
                        rB = Res("bis")
                        V(lambda e: e.memset(lo[:, :], 0.0), w=[rB])
                        for it in range(NBIS):
                            ck = 2.0 ** (-(it + 1))
                            V(lambda e, ck=ck: e.tensor_scalar(out=mid[:, :], in0=lo[:, :], scalar1=ck, scalar2=None,
                                                               op0=ALU.add), r=[rB], w=[rB])
                            V(lambda e: e.tensor_scalar(out=cmpj[:, :], in0=AGc[:, :], scalar1=mid[:, 0:1],
                                                        scalar2=0.0, op0=ALU.is_ge, op1=ALU.add,
                                                        accum_out=cnt[:, 0:1]), r=[rB, rAG], w=[rB])
                            V(lambda e, ck=ck: e.tensor_scalar(out=prd[:, :], in0=cnt[:, :], scalar1=CAP - 0.5,
                                                               scalar2=ck, op0=ALU.is_ge, op1=ALU.mult),
                              r=[rB], w=[rB])
                            V(lambda e: e.tensor_tensor(out=lo[:, :], in0=lo[:, :], in1=prd[:, :], op=ALU.add),
                              r=[rB], w=[rB])
                        rThrD = Res("thr_d")
                        DMA(thr_d.rearrange("o e -> e o"), lo[:, :], dsAG, r=[rB], w=[rThrD])
                        if dbg:
                            DMA(dbg_thr.rearrange("o e -> e o"), lo[:, :], dsDbg, r=[rB])
                        THR = sb(p4, "THR", [128, NEXP], F32)
                        rTHR = Res("THR")
                        DMA(THR[:, :], thr_d[0:1, :].partition_broadcast(128), dsAG, r=[rThrD], w=[rTHR])
                        for t in range(16):
                            V(lambda e, t=t: e.tensor_tensor(out=COEF[:, t, :], in0=AFF[:, t, :], in1=THR[:, :],
                                                             op=ALU.is_ge), r=[rAFF, rTHR], w=[rCOEF])
                            V(lambda e, t=t: e.tensor_tensor(out=COEF[:, t, :], in0=COEF[:, t, :], in1=AFF[:, t, :],
                                                             op=ALU.mult), r=[rAFF, rCOEF], w=[rCOEF])
                        S.barrier()

                if stop_after >= 5:
                    with ExitStack() as p5:
                        wg = [sb(p5, f"wg{i}", [128, 8, 512], BF16) for i in range(2)]
                        wu = [sb(p5, f"wu{i}", [128, 8, 512], BF16) for i in range(2)]
                        wd = [sb(p5, f"wd{i}", [128, 4, D], BF16) for i in range(2)]
                        rWe = [Res(f"We{i}", True) for i in range(2)]
                        dsWe = [dsem(f"ds_we{i}") for i in range(2)]
                        hT = [sb(p5, f"hT{i}", [128, 4, 512], BF16) for i in range(2)]
                        rHT = [Res(f"hT{i}") for i in range(2)]
                        sg = [sb(p5, f"sg{i}", [128, 512], F32) for i in range(2)]
                        rSg = [Res(f"sg{i}") for i in range(2)]
                        gps = [psum(p5, f"gps{i}", [128, 512], F32) for i in range(2)]
                        ups = [psum(p5, f"ups{i}", [128, 512], F32) for i in range(2)]
                        rGps = [Res(f"gps{i}", psum=True) for i in range(2)]
                        rUps = [Res(f"ups{i}", psum=True) for i in range(2)]
                        ypm = [psum(p5, f"ypm{i}", [128, 512], F32) for i in range(4)]
                        rYpm = [Res(f"ypm{i}", psum=True) for i in range(4)]

                        def load_e(ei):
                            sl = ei % 2
                            for c in range(8):
                                DMA(wg[sl][:, c, :], wg_d[ei, c * 128:(c + 1) * 128, :], dsWe[sl], w=[rWe[sl]], q="pool")
                                DMA(wu[sl][:, c, :], wu_d[ei, c * 128:(c + 1) * 128, :], dsWe[sl], w=[rWe[sl]], q="pool")
                            for c in range(4):
                                DMA(wd[sl][:, c, :], wd_d[ei, c * 128:(c + 1) * 128, :], dsWe[sl], w=[rWe[sl]], q="pool")

                        load_e(0)
                        gi = [0]
                        yi = [0]
                        for ei in range(NEXP):
                            sl = ei % 2
                            if ei + 1 < NEXP:
                                load_e(ei + 1)
                            for tcn in range(4):
                                hs = (ei * 4 + tcn) % 2
                                rUs = [rU2[tcn * 4 + k] for k in range(4)]
                                for fc in range(4):
                                    gs = gi[0] % 2
                                    gi[0] += 1
                                    for dc in range(8):
                                        P(lambda e, gs=gs, dc=dc, fc=fc, tcn=tcn: e.matmul(
                                            gps[gs][:, :], lhsT=wg[sl][:, dc, fc * 128:(fc + 1) * 128],
                                            rhs=u2T[:, dc, tcn * 512:(tcn + 1) * 512], start=(dc == 0), stop=(dc == 7)),
                                          r=[rWe[sl]] + rUs, w=[rGps[gs]], inc=(dc == 7))
                                    for dc in range(8):
                                        P(lambda e, gs=gs, dc=dc, fc=fc, tcn=tcn: e.matmul(
                                            ups[gs][:, :], lhsT=wu[sl][:, dc, fc * 128:(fc + 1) * 128],
                                            rhs=u2T[:, dc, tcn * 512:(tcn + 1) * 512], start=(dc == 0), stop=(dc == 7)),
                                          r=[rWe[sl]] + rUs, w=[rUps[gs]], inc=(dc == 7))
                                    A(lambda e, gs=gs: e.activation(out=sg[gs][:, :], in_=gps[gs][:, :], func=AF.Silu),
                                      r=[rGps[gs]], w=[rSg[gs]])
                                    V(lambda e, gs=gs, fc=fc, hs=hs: e.tensor_tensor(
                                        out=hT[hs][:, fc, :], in0=ups[gs][:, :], in1=sg[gs][:, :], op=ALU.mult),
                                      r=[rUps[gs], rSg[gs]], w=[rHT[hs]])
                                for ts in range(4):
                                    t = tcn * 4 + ts
                                    for dh in range(2):
                                        ys = yi[0] % 4
                                        yi[0] += 1
                                        for fc in range(4):
                                            P(lambda e, ys=ys, fc=fc, ts=ts, dh=dh, hs=hs: e.matmul(
                                                ypm[ys][:, :], lhsT=hT[hs][:, fc, ts * 128:(ts + 1) * 128],
                                                rhs=wd[sl][:, fc, dh * 512:(dh + 1) * 512],
                                                start=(fc == 0), stop=(fc == 3)),
                                              r=[rHT[hs], rWe[sl]], w=[rYpm[ys]], inc=(fc == 3))
                                        V(lambda e, ys=ys, t=t, dh=dh, ei=ei: e.scalar_tensor_tensor(
                                            out=H[:, t, dh * 512:(dh + 1) * 512], in0=ypm[ys][:, :],
                                            scalar=COEF[:, t, ei:ei + 1], in1=H[:, t, dh * 512:(dh + 1) * 512],
                                            op0=ALU.mult, op1=ALU.add),
                                          r=[rYpm[ys], rCOEF, rH[t]], w=[rH[t]])
                        S.barrier()

                with ExitStack() as p6:
                    gfinb = sb(p6, "gfinb", [128, D], F32)
                    dsC6 = dsem("ds_c6")
                    DMA(gfinb[:, :], gfin_d[0:1, :].partition_broadcast(128), dsC6, w=[rC])
                    fo = [sb(p6, f"fo{i}", [128, D], F32) for i in range(2)]
                    rFo = [Res(f"fo{i}") for i in range(2)]
                    dsFo = [dsem(f"ds_fo{i}") for i in range(2)]
                    fj = sb(p6, "fj", [128, D], BF16)
                    fst = {n: sb(p6, "fst_" + n, [128, 1], F32) for n in ("ss", "ln", "rs")}
                    rFst = Res("fst")
                    for t in range(16):
                        sl = t % 2
                        A(lambda e, t=t: e.activation(out=fj[:, :], in_=H[:, t, :], func=AF.Square,
                                                      accum_out=fst["ss"][:, 0:1]), r=[rH[t]], w=[rFst])
                        rstd_from_ss(fst["ss"][:, 0:1], fst["ln"][:, 0:1], fst["rs"][:, 0:1], float(D),
                                     [rFst], [rFst])
                        V(lambda e, t=t, sl=sl: e.scalar_tensor_tensor(
                            out=fo[sl][:, :], in0=H[:, t, :], scalar=fst["rs"][:, 0:1], in1=gfinb[:, :],
                            op0=ALU.mult, op1=ALU.mult), r=[rH[t], rFst, rC], w=[rFo[sl]])
                        DMA(y_out[t * 128:(t + 1) * 128, :], fo[sl][:, :], dsFo[sl], r=[rFo[sl]])
                    S.barrier()
        else:
            with ExitStack() as pz:
                zt = sb(pz, "zt", [128, D], F32)
                rZ = Res("zt")
                dsZ = dsem("ds_z")
                V(lambda e: e.memset(zt[:, :], 0.0), w=[rZ])
                for t in range(16):
                    DMA(y_out[t * 128:(t + 1) * 128, :], zt[:, :], dsZ, r=[rZ])
                S.barrier()

        S.barrier()

        @block.tensor
        def _(eng):
            S.emit("pe", eng)

        @block.scalar
        def _(eng):
            S.emit("act", eng)

        @block.vector
        def _(eng):
            S.emit("dve", eng)

        @block.gpsimd
        def _(eng):
            S.emit("pool", eng)

        @block.sync
        def _(eng):
            S.emit("sp", eng)

    return nc


def _tables(r):
    bf = ml_dtypes.bfloat16
    jp = np.arange(SEQ)
    uj = np.where(jp + 2048 * r < SEQ, jp, jp - SEQ).astype(np.float64)
    sig = np.zeros((4, NT), np.float32)
    beta = np.zeros((128, 8, 5, NKB), np.float32)
    for c in range(4):
        before = uj < 512 * c
        sgn = np.where(before, 1.0, -1.0)
        sig[c, :SEQ] = sgn
        for h in range(8):
            slope = 2.0 ** (-(h + 1))
            b = sgn * slope * (uj - 512 * c - 256)
            beta[:, h, c, :64] = b.reshape(64, 128).T
    qaug = np.zeros((8, 4, NOWN), np.float32)
    a = np.arange(512) - 256
    for h in range(8):
        slope = 2.0 ** (-(h + 1))
        for c in range(4):
            qaug[h, c, c * 512:(c + 1) * 512] = -8.0 * slope * a
    x = np.arange(896)[None, :]
    bb = np.arange(128)[:, None]
    dtab = np.abs(x - bb - 384).astype(np.float32)
    t_true = (jp + 2048 * r) % SEQ
    row_id = (t_true // 64).astype(np.float32)
    col_id = (t_true % 64).astype(np.float32)
    inv_freq = (np.float32(10000.0) ** (-np.arange(0, 64, 2, dtype=np.float32) / np.float32(64))).astype(np.float32)
    ar = (row_id[:, None] * inv_freq[None, :]).astype(np.float32)
    ac = (col_id[:, None] * inv_freq[None, :]).astype(np.float32)
    rope = np.zeros((NT, 256), np.float32)
    rope[:, 0:128] = 1.0
    cr, sr, cc, sc = np.cos(ar), np.sin(ar), np.cos(ac), np.sin(ac)
    rope[:SEQ, 0:32] = cr
    rope[:SEQ, 32:64] = cr
    rope[:SEQ, 64:96] = cc
    rope[:SEQ, 96:128] = cc
    rope[:SEQ, 128:160] = -sr
    rope[:SEQ, 160:192] = sr
    rope[:SEQ, 192:224] = -sc
    rope[:SEQ, 224:256] = sc
    return dict(sigk=sig.astype(bf), qaug=qaug.astype(bf),
                beta=np.ascontiguousarray(beta.reshape(128, -1)), dtab=dtab, rope=rope)


def make_in_maps(inputs):
    bf = ml_dtypes.bfloat16
    x = np.asarray(inputs["x"], np.float32)
    meta = np.asarray(inputs["meta_tokens"], np.float32)
    f = lambda k: np.ascontiguousarray(np.asarray(inputs[k], np.float32))
    common = {
        "w_in": f("w_in")[0],
        "gmixT": np.ascontiguousarray(f("g_mix")[0].reshape(8, 128).T),
        "lamv": np.ascontiguousarray(np.stack([f("lambda_q1")[0], f("lambda_k1")[0],
                                               f("lambda_q2")[0], f("lambda_k2")[0]])),
        "g_subln": f("g_subln"), "g_qnorm": f("g_qnorm"), "g_knorm": f("g_knorm"),
        "w_branch_a": f("w_branch_a")[0], "w_branch_b": f("w_branch_b")[0], "w_out": f("w_out")[0],
        "g_ffn": f("g_ffn"), "w_router": f("w_router")[0],
        "w_gate": f("w_gate")[0], "w_up": f("w_up")[0], "w_down": f("w_down")[0],
        "g_final": f("g_final").reshape(1, D),
        "identb": np.eye(128, dtype=np.float32).astype(bf),
        "identf": np.eye(128, dtype=np.float32),
    }
    tabs = [_tables(r) for r in range(4)]
    maps = []
    for c in range(8):
        b, r = c // 4, c % 4
        hxv = np.concatenate([np.roll(x[b], -2048 * r, axis=0), meta], axis=0)
        m = dict(common)
        m.update(tabs[r])
        m["hx"] = np.ascontiguousarray(hxv)
        maps.append(m)
    return maps


_NC_CACHE = {}


def kernel(**inputs):
    if "nc" not in _NC_CACHE:
        _NC_CACHE["nc"] = build()
    nc = _NC_CACHE["nc"]
    maps = make_in_maps(inputs)
    res = run_bass_kernel_spmd(nc, maps, core_ids=list(range(8)))
    out = np.zeros((2, SEQ, D), np.float32)
    for c in range(8):
        b, r = c // 4, c % 4
        out[b, r * 2048:(r + 1) * 2048, :] = res.results[c]["y"]
    return out
```

```python
import numpy as np
import ml_dtypes
from contextlib import ExitStack
import concourse.bass as bass
import concourse.mybir as mybir
from concourse.bass_utils import run_bass_kernel_spmd

F32 = mybir.dt.float32
BF16 = mybir.dt.bfloat16
AF = mybir.ActivationFunctionType
ALU = mybir.AluOpType
AX = mybir.AxisListType

D = 1024
SEQ = 8192
NMETA = 16
NT = SEQ + NMETA
NOWN = 2048 + NMETA
NKB = 65
EPS = 1e-6
NEXP = 16
CAP = 2 * NT // NEXP
LAM_INIT = 0.2
NBIS = 24


import types


def _freeze(fn):
    if fn is None or fn.__closure__ is None:
        return fn
    cells = []
    for c in fn.__closure__:
        try:
            cells.append(types.CellType(c.cell_contents))
        except ValueError:
            cells.append(c)
    return types.FunctionType(fn.__code__, fn.__globals__, fn.__name__, fn.__defaults__, tuple(cells))


class Res:
    __slots__ = ("name", "w", "rd", "multi", "psum")

    def __init__(self, name, multi=False, psum=False):
        self.name = name
        self.w = {}
        self.rd = {}
        self.multi = multi
        self.psum = psum


class DSem:
    def __init__(self, sem):
        self.sem = sem
        self.count = 0


class Sched:
    ENGS = ("pe", "act", "dve", "pool", "sp")

    def __init__(self):
        self.prog = {e: [] for e in self.ENGS}
        self.cnt = {e: 0 for e in self.ENGS}
        self.sem = {}
        self.known = {e: {} for e in self.ENGS}
        self.all_dsems = []

    def _deps(self, eng, reads, writes):
        deps = {}
        known = self.known[eng]

        def add(tok, kind):
            sem, val, e = tok
            if e == eng and eng in ("pe", "sp"):
                return
            k = id(sem)
            if known.get(k, 0) >= val:
                return
            if k not in deps or deps[k][1] < val:
                deps[k] = (sem, val)

        for r in reads:
            for tok in r.w.values():
                add(tok, "raw")
            if r.psum:
                for tok in r.rd.values():
                    if tok[2] != eng:
                        add(tok, "rar")
        for w in writes:
            if not w.multi:
                for tok in w.w.values():
                    add(tok, "waw")
            for tok in w.rd.values():
                add(tok, "war")
        for k, (sem, val) in deps.items():
            known[k] = val
        return list(deps.values())

    def _record(self, tok, reads, writes):
        k = id(tok[0])
        for r in reads:
            r.rd[k] = tok
        for w in writes:
            if w.multi:
                w.w[k] = tok
            else:
                w.w = {k: tok}
                w.rd = {}

    def op(self, eng, fn, reads=(), writes=(), inc=True):
        deps = self._deps(eng, reads, writes)
        tok = (self.sem[eng], self.cnt[eng] + 1, eng)
        self._record(tok, reads, writes)
        if inc:
            self.cnt[eng] += 1
        self.prog[eng].append((deps, _freeze(fn), self.sem[eng] if inc else None, 1))

    def dma(self, q, fn, ds, reads=(), writes=()):
        deps = self._deps(q, reads, writes)
        ds.count += 16
        tok = (ds.sem, ds.count, None)
        self._record(tok, reads, writes)
        self.prog[q].append((deps, _freeze(fn), ds.sem, 16))

    def coll(self, fn, ds, reads=(), writes=()):
        deps = self._deps("pool", reads, writes)
        ds.count += 1
        tok = (ds.sem, ds.count, None)
        self._record(tok, reads, writes)
        self.prog["pool"].append((deps, _freeze(fn), ds.sem, None))

    def barrier(self):
        toks = []
        for e in self.ENGS:
            if e == "sp":
                continue
            if self.cnt[e] > 0:
                toks.append((self.sem[e], self.cnt[e]))
        for ds in self.all_dsems:
            if ds.count > 0:
                toks.append((ds.sem, ds.count))
        for e in self.ENGS:
            deps = []
            for sem, val in toks:
                if self.known[e].get(id(sem), 0) < val:
                    self.known[e][id(sem)] = val
                    deps.append((sem, val))
            if deps:
                self.prog[e].append((deps, None, None, 0))

    def emit(self, eng, handle):
        for deps, fn, sem, incv in self.prog[eng]:
            for s, v in deps:
                handle.wait_ge(s, v)
            if fn is None:
                continue
            ins = fn(handle)
            if sem is not None:
                if incv is None:
                    ins.then_inc(sem)
                else:
                    ins.then_inc(sem, incv)


def build(dbg=False, stop_after=99):
    nc = bass.Bass("TRN2", target_bir_lowering=False)
    S = Sched()

    def din(name, shape, dt=F32):
        return nc.dram_tensor(name, list(shape), dt, kind="ExternalInput").ap()

    def dscr(name, shape, dt):
        if dbg:
            return nc.dram_tensor(name, list(shape), dt, kind="ExternalOutput").ap()
        return nc.dram_tensor(name, list(shape), dt).ap()

    hx = din("hx", [NT, D])
    w_in = din("w_in", [D, 6656])
    gmixT_d = din("gmixT", [128, 8])
    lam_d = din("lamv", [4, 64])
    gsub_d = din("g_subln", [1, 128])
    gq_d = din("g_qnorm", [1, 128])
    gk_d = din("g_knorm", [1, 128])
    wa_d = din("w_branch_a", [D, D])
    wb_d = din("w_branch_b", [D, D])
    wo_d = din("w_out", [D, D])
    gffn_d = din("g_ffn", [1, D])
    wr_d = din("w_router", [D, NEXP])
    if stop_after >= 5:
        wg_d = din("w_gate", [NEXP, D, 512])
        wu_d = din("w_up", [NEXP, D, 512])
        wd_d = din("w_down", [NEXP, 512, D])
    gfin_d = din("g_final", [1, D])
    sigk_d = din("sigk", [4, NT], BF16)
    qaug_d = din("qaug", [8, 4, NOWN], BF16)
    beta_d = din("beta", [128, 8 * 5 * NKB])
    dtab_d = din("dtab", [128, 896])
    rope_d = din("rope", [NT, 256])
    identb_d = din("identb", [128, 128], BF16)
    identf_d = din("identf", [128, 128])
    y_out = nc.dram_tensor("y", [2048, D], F32, kind="ExternalOutput").ap()

    KAs = dscr("KAs", [8, 128, NT], BF16)
    QAs = dscr("QAs", [8, 128, NOWN], BF16)
    VAs = dscr("VAs", [8, 128, NKB, 128], BF16)
    KBs = dscr("KBs", [2, 128, NT], BF16)
    QBs = dscr("QBs", [8, 128, NOWN], BF16)
    VBs = dscr("VBs", [2, 128, NKB, 128], BF16)
    Gs = dscr("Gs", [NOWN, 2048], BF16)
    OAs = dscr("OAs", [NOWN, D], BF16)
    OBs = dscr("OBs", [NOWN, D], BF16)
    agin = nc.dram_tensor("agin", [NEXP, NOWN], F32)
    agout = nc.dram_tensor("agout", [4 * NEXP, NOWN], F32)
    thr_d = nc.dram_tensor("thr_d", [1, NEXP], F32).ap()
    dbg_aff = dscr("dbg_aff", [NOWN, NEXP], F32) if dbg else None
    dbg_h1 = dscr("dbg_h1", [NOWN, D], F32) if dbg else None
    dbg_thr = dscr("dbg_thr", [1, NEXP], F32) if dbg else None

    rKAs, rQAs, rVAs = Res("KAs", True), Res("QAs", True), Res("VAs", True)
    rKBs, rQBs, rVBs = Res("KBs", True), Res("QBs", True), Res("VBs", True)
    rGs, rOAs, rOBs = Res("Gs", True), Res("OAs", True), Res("OBs", True)

    with ExitStack() as top:
        def sem(name):
            return top.enter_context(nc.semaphore(name))

        for e in Sched.ENGS:
            S.sem[e] = sem("sem_" + e)

        def dsem(name):
            d = DSem(sem(name))
            S.all_dsems.append(d)
            return d

        def sb(es, name, shape, dt):
            return es.enter_context(nc.sbuf_tensor("s_" + name, list(shape), dt))

        def psum(es, name, shape, dt):
            return es.enter_context(nc.psum_tensor("p_" + name, list(shape), dt))

        block = top.enter_context(nc.Block())

        def V(fn, r=(), w=()):
            S.op("dve", fn, r, w)

        def A(fn, r=(), w=()):
            S.op("act", fn, r, w)

        def P(fn, r=(), w=(), inc=True):
            S.op("pe", fn, r, w, inc)

        def G(fn, r=(), w=()):
            S.op("pool", fn, r, w)

        def DMA(out, in_, ds, r=(), w=(), q="sp"):
            S.dma(q, (lambda e, o=out, i=in_: e.dma_start(out=o, in_=i)), ds, r, w)

        identb = sb(top, "identb", [128, 128], BF16)
        identf = sb(top, "identf", [128, 128], F32)
        epsT = sb(top, "epsT", [128, 1], F32)
        rC = Res("consts", True)
        dsDbg = dsem("ds_dbg")
        dsC = dsem("ds_const")
        DMA(identb[:, :], identb_d[:, :], dsC, w=[rC])
        DMA(identf[:, :], identf_d[:, :], dsC, w=[rC])
        V(lambda e: e.memset(epsT[:, :], EPS), w=[rC])

        def rstd_from_ss(ss_ap, ln_ap, out_ap, n, rs, ws):
            A(lambda e: e.activation(out=ln_ap, in_=ss_ap, func=AF.Ln,
                                     bias=epsT[:ln_ap.shape[0], 0:1], scale=1.0 / n),
              r=rs + [rC], w=ws)
            A(lambda e: e.activation(out=out_ap, in_=ln_ap, func=AF.Exp, scale=-0.5),
              r=ws, w=ws)

        STS = [(t * 512, 512, 4, 128) for t in range(16)] + [(SEQ, NMETA, 1, NMETA)]

        def is_own(st):
            return st < 4 or st == 16

        def q0_of(st):
            return st * 512 if st < 4 else 2048

        with ExitStack() as p1:
            uT_own = sb(p1, "uT_own", [128, 8, NOWN], BF16)
            rUown = [Res(f"uTown{i}") for i in range(5)]
            gmixT = sb(p1, "gmixT", [128, 8], F32)
            gqb = sb(p1, "gqb", [128, 128], F32)
            gkb = sb(p1, "gkb", [128, 128], F32)
            dsC1 = dsem("ds_c1")
            DMA(gmixT[:, :], gmixT_d[:, :], dsC1, w=[rC])
            DMA(gqb[:, :], gq_d[0:1, :].partition_broadcast(128), dsC1, w=[rC])
            DMA(gkb[:, :], gk_d[0:1, :].partition_broadcast(128), dsC1, w=[rC])

            def norm_rope(raw, H, psub, gb, cs_t, s, out_bf, scr, rs_extra, r_raw, r_scr, r_out):
                sq, ssk, lnk, rk, tmp, yy = scr
                W = H * 128
                V(lambda e: e.tensor_tensor(out=sq[:psub, :W], in0=raw[:psub, :W], in1=raw[:psub, :W], op=ALU.mult),
                  r=[r_raw], w=[r_scr])
                V(lambda e: e.tensor_reduce(out=ssk[:psub, :H],
                                            in_=sq[:psub, :W].rearrange("p (h d) -> p h d", h=H),
                                            axis=AX.X, op=ALU.add), r=[r_scr], w=[r_scr])
                rstd_from_ss(ssk[:psub, :H], lnk[:psub, :H], rk[:psub, :H], 128.0, [r_scr], [r_scr])
                y3 = yy[:psub, :W].rearrange("p (h d) -> p h d", h=H)
                V(lambda e: e.tensor_tensor(out=y3, in0=raw[:psub, :W].rearrange("p (h d) -> p h d", h=H),
                                            in1=rk[:psub, 0:H].unsqueeze(2).to_broadcast([psub, H, 128]),
                                            op=ALU.mult), r=[r_raw, r_scr], w=[r_scr])
                V(lambda e: e.tensor_tensor(out=y3, in0=y3,
                                            in1=gb[:psub, :].unsqueeze(1).to_broadcast([psub, H, 128]),
                                            op=ALU.mult), r=[r_scr, rC], w=[r_scr])
                y5 = yy[:psub, :W].rearrange("p (h a b c) -> p h a b c", h=H, a=2, b=2)
                t5 = tmp[:psub, :W].rearrange("p (h a b c) -> p h a b c", h=H, a=2, b=2)
                sn4 = cs_t[:psub, s, 128:256].rearrange("p (a b c) -> p a b c", a=2, b=2)
                V(lambda e: e.tensor_tensor(out=t5[:, :, :, 0, :], in0=y5[:, :, :, 1, :],
                                            in1=sn4[:, :, 0, :].unsqueeze(1).to_broadcast([psub, H, 2, 32]),
                                            op=ALU.mult), r=[r_scr] + rs_extra, w=[r_scr])
                V(lambda e: e.tensor_tensor(out=t5[:, :, :, 1, :], in0=y5[:, :, :, 0, :],
                                            in1=sn4[:, :, 1, :].unsqueeze(1).to_broadcast([psub, H, 2, 32]),
                                            op=ALU.mult), r=[r_scr] + rs_extra, w=[r_scr])
                V(lambda e: e.tensor_tensor(out=y3, in0=y3,
                                            in1=cs_t[:psub, s, 0:128].unsqueeze(1).to_broadcast([psub, H, 128]),
                                            op=ALU.mult), r=[r_scr] + rs_extra, w=[r_scr])
                V(lambda e: e.tensor_tensor(out=out_bf[:psub, :W], in0=yy[:psub, :W], in1=tmp[:psub, :W],
                                            op=ALU.add), r=[r_scr], w=[r_out])

            with ExitStack() as pa:
                wA = sb(pa, "wA", [128, 8, 2560], BF16)
                rWA = Res("wA", True)
                dsW = dsem("ds_w")
                w_in_v = w_in.rearrange("(c p) f -> p c f", p=128)
                for c in range(8):
                    DMA(wA[:, c, 0:2048], w_in_v[:, c, 1024:3072], dsW, w=[rWA], q="pool")
                    DMA(wA[:, c, 2048:2560], w_in_v[:, c, 4096:4608], dsW, w=[rWA], q="pool")
                xt = [sb(pa, f"xt{i}", [128, 4, D], F32) for i in range(2)]
                rXt = [Res(f"xt{i}", True) for i in range(2)]
                dsXc = [dsem(f"ds_xc{i}") for i in range(2)]
                dsX = [dsem(f"ds_x{i}") for i in range(2)]
                cs = [sb(pa, f"cs{i}", [128, 4, 256], F32) for i in range(2)]
                rCs = [Res(f"cs{i}", True) for i in range(2)]
                xsb = [sb(pa, f"xs{i}", [128, 4, D], BF16) for i in range(2)]
                rXsb = [Res(f"xs{i}") for i in range(2)]
                junk = sb(pa, "junk", [128, D], BF16)
                rJunk = Res("junk")
                ssb = [sb(pa, f"ss{i}", [128, 4], F32) for i in range(2)]
                lnvb = [sb(pa, f"lnv{i}", [128, 4], F32) for i in range(2)]
                rstdb = [sb(pa, f"rstd{i}", [128, 4], F32) for i in range(2)]
                rStb = [Res(f"stats{i}") for i in range(2)]
                uT_tmp = [sb(pa, f"uTt{i}", [128, 8, 512], BF16) for i in range(2)]
                rUt = [Res(f"uTt{i}") for i in range(2)]
                stgK = [sb(pa, f"stgK{i}", [128, 512], BF16) for i in range(3)]
                rStgK = [Res(f"stgK{i}") for i in range(3)]
                dsStgK = [dsem(f"ds_sk{i}") for i in range(3)]
                stgV = [sb(pa, f"stgV{i}", [128, 1024], BF16) for i in range(2)]
                rStgV = [Res(f"stgV{i}") for i in range(2)]
                dsStgV = [dsem(f"ds_sv{i}") for i in range(2)]
                stgVB = [sb(pa, f"stgVB{i}", [128, 256], BF16) for i in range(2)]
                rStgVB = [Res(f"stgVB{i}") for i in range(2)]
                dsStgVB = [dsem(f"ds_svb{i}") for i in range(2)]
                kraw = sb(pa, "kraw", [128, 256], F32)
                rKraw = Res("kraw")
                ksc = (sb(pa, "ksq", [128, 256], F32), sb(pa, "kss", [128, 2], F32),
                       sb(pa, "kln", [128, 2], F32), sb(pa, "krk", [128, 2], F32),
                       sb(pa, "ktmp", [128, 256], F32), sb(pa, "kyy", [128, 256], F32))
                rKsc = Res("ksc")
                krope = sb(pa, "krope", [128, 4, 256], BF16)
                rKropeS = [Res(f"krope{i}") for i in range(4)]
                stgKB = sb(pa, "stgKB", [128, 2, 512], BF16)
                rStgKB = Res("stgKB")
                dsStgKB = dsem("ds_skb")
                tpb = [psum(pa, f"tpb{i}", [128, 1024], BF16) for i in range(4)]
                rTp = [Res(f"tpb{i}", psum=True) for i in range(4)]
                acc = [psum(pa, f"acc{i}", [128, 512], F32) for i in range(3)]
                rAcc = [Res(f"acc{i}", psum=True) for i in range(3)]
                ktp = psum(pa, "ktp", [128, 1024], BF16)
                rKtp = Res("ktp", psum=True)
                acc_i = [0]

                def next_acc():
                    i = acc_i[0] % 3
                    acc_i[0] += 1
                    return acc[i], rAcc[i]

                def load_x(sti):
                    tok0, ntok, nsub, psub = STS[sti]
                    sl = sti % 2
                    DMA(xt[sl][:psub, 0:nsub, :],
                        hx[tok0:tok0 + ntok, :].rearrange("(s p) d -> p s d", p=psub),
                        dsX[sl], w=[rXt[sl]])
                    DMA(cs[sl][:psub, 0:nsub, :],
                        rope_d[tok0:tok0 + ntok, :].rearrange("(s p) d -> p s d", p=psub),
                        dsXc[sl], w=[rCs[sl]])

                load_x(0)

                def prep(sti):
                    tok0, ntok, nsub, psub = STS[sti]
                    sl = sti % 2
                    x_t = xt[sl]
                    ss, lnv, rstd, rSt, xs, rXs = ssb[sl], lnvb[sl], rstdb[sl], rStb[sl], xsb[sl], rXsb[sl]
                    for s in range(nsub):
                        A(lambda e, s=s: e.activation(out=junk[:psub, :], in_=x_t[:psub, s, :],
                                                      func=AF.Square, accum_out=ss[:psub, s:s + 1]),
                          r=[rXt[sl]], w=[rJunk, rSt])
                    rstd_from_ss(ss[:psub, :nsub], lnv[:psub, :nsub], rstd[:psub, :nsub], float(D),
                                 [rSt], [rSt])
                    for s in range(nsub):
                        V(lambda e, s=s: e.tensor_scalar(out=xs[:psub, s, :], in0=x_t[:psub, s, :],
                                                         scalar1=rstd[:psub, s:s + 1], scalar2=None,
                                                         op0=ALU.mult),
                          r=[rXt[sl], rSt], w=[rXs])

                prep(0)
                pend = []
                kcount = [0]
                vcount = [0]
                import os
                DBGL = os.environ.get("KDBG", "")
                for sti, (tok0, ntok, nsub, psub) in enumerate(STS):
                    sl = sti % 2
                    if "one" in DBGL and sti >= 1:
                        break
                    if sti + 1 < len(STS) and "one" not in DBGL:
                        load_x(sti + 1)
                    xs, rXs = xsb[sl], rXsb[sl]
                    own = is_own(sti)
                    if own:
                        q0 = q0_of(sti)
                        uT = uT_own
                        ucol = q0
                        rU = rUown[sti if sti < 4 else 4]
                    else:
                        uT = uT_tmp[sl]
                        ucol = 0
                        rU = rUt[sl]
                    for dc in range(8):
                        bank = dc // 2
                        half = dc % 2
                        for s in range(nsub):
                            P(lambda e, dc=dc, s=s, bank=bank, half=half: e.transpose(
                                out=tpb[bank][:, half * 512 + s * 128: half * 512 + s * 128 + psub],
                                in_=xs[:psub, s, dc * 128:(dc + 1) * 128],
                                identity=identb[:psub, :psub]),
                              r=[rXs, rC], w=[rTp[bank]], inc=(s == nsub - 1))
                        src = tpb[bank][:, half * 512: half * 512 + ntok]
                        dst = uT[:, dc, ucol:ucol + ntok]
                        if bank % 2 == 0:
                            V(lambda e, src=src, dst=dst, dc=dc: e.tensor_scalar(
                                out=dst, in0=src, scalar1=gmixT[:, dc:dc + 1], scalar2=None, op0=ALU.mult),
                              r=[rTp[bank], rC], w=[rU])
                        else:
                            A(lambda e, src=src, dst=dst, dc=dc: e.activation(
                                out=dst, in_=src, func=AF.Copy, scale=gmixT[:, dc:dc + 1]),
                              r=[rTp[bank], rC], w=[rU])
                    if sti + 1 < len(STS) and "one" not in DBGL:
                        prep(sti + 1)
                    if "noproj" in DBGL:
                        continue
                    for h in range(8 if "nokA" not in DBGL else 0):
                        a_t, rA = next_acc()
                        for dc in range(8):
                            P(lambda e, a_t=a_t, dc=dc, h=h: e.matmul(
                                a_t[:, :ntok], lhsT=wA[:, dc, h * 128:(h + 1) * 128],
                                rhs=uT[:, dc, ucol:ucol + ntok], start=(dc == 0), stop=(dc == 7)),
                              r=[rWA, rU], w=[rA], inc=(dc == 7))
                        ks = kcount[0] % 3
                        kcount[0] += 1
                        A(lambda e, a_t=a_t, ks=ks: e.activation(out=stgK[ks][:, :ntok], in_=a_t[:, :ntok],
                                                                 func=AF.Copy),
                          r=[rA], w=[rStgK[ks]])
                        DMA(KAs[h, :, tok0:tok0 + ntok], stgK[ks][:, :ntok], dsStgK[ks],
                            r=[rStgK[ks]], w=[rKAs])
                    if "notok" in DBGL:
                        continue
                    for s in range(nsub):
                        blk = (tok0 // 128) + s
                        vs = vcount[0] % 2
                        vcount[0] += 1
                        for g in range(2):
                            a_t, rA = next_acc()
                            for dc in range(8):
                                P(lambda e, a_t=a_t, dc=dc, g=g, s=s: e.matmul(
                                    a_t[:psub, :], lhsT=uT[:, dc, ucol + s * 128: ucol + s * 128 + psub],
                                    rhs=wA[:, dc, 1024 + g * 512: 1024 + (g + 1) * 512],
                                    start=(dc == 0), stop=(dc == 7)),
                                  r=[rWA, rU], w=[rA], inc=(dc == 7))
                            if g == 0:
                                A(lambda e, a_t=a_t, vs=vs: e.activation(
                                    out=stgV[vs][:psub, 0:512], in_=a_t[:psub, :], func=AF.Copy),
                                  r=[rA], w=[rStgV[vs]])
                            else:
                                V(lambda e, a_t=a_t, vs=vs: e.tensor_copy(
                                    out=stgV[vs][:psub, 512:1024], in_=a_t[:psub, :]),
                                  r=[rA], w=[rStgV[vs]])
                        if "novst" not in DBGL:
                          DMA(VAs[:, 0:psub, blk, :].rearrange("h p e -> p h e"),
                            stgV[vs][:psub, :].rearrange("p (h e) -> p h e", h=8),
                            dsStgV[vs], r=[rStgV[vs]], w=[rVAs])
                        a_t, rA = next_acc()
                        for dc in range(8):
                            P(lambda e, a_t=a_t, dc=dc, s=s: e.matmul(
                                a_t[:psub, :], lhsT=uT[:, dc, ucol + s * 128: ucol + s * 128 + psub],
                                rhs=wA[:, dc, 2048:2560], start=(dc == 0), stop=(dc == 7)),
                              r=[rWA, rU], w=[rA], inc=(dc == 7))
                        A(lambda e, a_t=a_t: e.activation(out=kraw[:psub, :], in_=a_t[:psub, 0:256],
                                                          func=AF.Copy), r=[rA], w=[rKraw])
                        A(lambda e, a_t=a_t, vs=vs: e.activation(out=stgVB[vs][:psub, :],
                                                                 in_=a_t[:psub, 256:512], func=AF.Copy),
                          r=[rA], w=[rStgVB[vs]])
                        if "novst" not in DBGL:
                          DMA(VBs[:, 0:psub, blk, :].rearrange("h p e -> p h e"),
                            stgVB[vs][:psub, :].rearrange("p (h e) -> p h e", h=2),
                            dsStgVB[vs], r=[rStgVB[vs]], w=[rVBs])
                        if "norope" in DBGL:
                            continue
                        norm_rope(kraw, 2, psub, gkb, cs[sl], s, krope[:, s, :], ksc,
                                  [rCs[sl]], rKraw, rKsc, rKropeS[s])
                        old = list(pend)
                        pend.clear()
                        for f_ in old:
                            f_()

                        def tr(s=s, psub=psub):
                            for j in range(2):
                                P(lambda e, j=j: e.transpose(
                                    out=ktp[:, j * 512 + s * 128: j * 512 + s * 128 + psub],
                                    in_=krope[:psub, s, j * 128:(j + 1) * 128],
                                    identity=identb[:psub, :psub]),
                                  r=[rKropeS[s], rC], w=[rKtp], inc=(j == 1))
                        pend.append(tr)
                    if "norope" in DBGL:
                        continue

                    def fin(ntok=ntok, tok0=tok0):
                        for j in range(2):
                            V(lambda e, j=j: e.tensor_copy(out=stgKB[:, j, :ntok],
                                                           in_=ktp[:, j * 512: j * 512 + ntok]),
                              r=[rKtp], w=[rStgKB])
                        for j in range(2):
                            DMA(KBs[j, :, tok0:tok0 + ntok], stgKB[:, j, :ntok], dsStgKB,
                                r=[rStgKB], w=[rKBs])
                    pend.append(fin)
                for f_ in pend:
                    f_()
                pend.clear()
                S.barrier()

            if stop_after >= 1.5:
                with ExitStack() as pb:
                    wB = sb(pb, "wB", [128, 8, 4096], BF16)
                    rWBa, rWBb, rWBg = Res("wBa", True), Res("wBb", True), Res("wBg", True)
                    dsWBa, dsWBb, dsWBg = dsem("ds_wba"), dsem("ds_wbb"), dsem("ds_wbg")
                    w_in_v = w_in.rearrange("(c p) f -> p c f", p=128)
                    for c in range(8):
                        DMA(wB[:, c, 0:1024], w_in_v[:, c, 0:1024], dsWBa, w=[rWBa], q="pool")
                    for c in range(8):
                        DMA(wB[:, c, 1024:2048], w_in_v[:, c, 3072:4096], dsWBb, w=[rWBb], q="pool")
                    for c in range(8):
                        DMA(wB[:, c, 2048:4096], w_in_v[:, c, 4608:6656], dsWBg, w=[rWBg], q="pool")
                    csB = [sb(pb, f"csB{i}", [128, 4, 256], F32) for i in range(2)]
                    rCsB = [Res(f"csB{i}", True) for i in range(2)]
                    dsCsB = [dsem(f"ds_csb{i}") for i in range(2)]
                    stgQ = [sb(pb, f"stgQ{i}", [128, 512], BF16) for i in range(3)]
                    rStgQ = [Res(f"stgQ{i}") for i in range(3)]
                    dsStgQ = [dsem(f"ds_sq{i}") for i in range(3)]
                    qraw = sb(pb, "qraw", [128, 1024], F32)
                    rQraw = Res("qraw")
                    qsc = (sb(pb, "qsq", [128, 1024], F32), sb(pb, "qss", [128, 8], F32),
                           sb(pb, "qln", [128, 8], F32), sb(pb, "qrk", [128, 8], F32),
                           sb(pb, "qtmp", [128, 1024], F32), sb(pb, "qyy", [128, 1024], F32))
                    rQsc = Res("qsc")
                    qrope = sb(pb, "qrope", [128, 1024], BF16)
                    rQrope = Res("qrope")
                    stgQB = [sb(pb, f"stgQB{i}", [128, 8, 128], BF16) for i in range(2)]
                    rStgQB = [Res(f"stgQB{i}") for i in range(2)]
                    dsStgQB = [dsem(f"ds_sqb{i}") for i in range(2)]
                    stgG = [sb(pb, f"stgG{i}", [128, 2048], BF16) for i in range(2)]
                    rStgG = [Res(f"stgG{i}") for i in range(2)]
                    dsStgG = [dsem(f"ds_sg{i}") for i in range(2)]
                    accB = [psum(pb, f"accB{i}", [128, 512], F32) for i in range(6)]
                    rAccB = [Res(f"accB{i}", psum=True) for i in range(6)]
                    qtp = psum(pb, "qtp", [128, 1024], BF16)
                    rQtp = Res("qtp", psum=True)
                    accb_i = [0]

                    def next_accB():
                        i = accb_i[0] % 6
                        accb_i[0] += 1
                        return accB[i], rAccB[i]

                    own_sts = [0, 1, 2, 3, 16]
                    qcount = [0]
                    bcount = [0]
                    qrope2 = [qrope, sb(pb, "qrope1", [128, 1024], BF16)]
                    rQrope2 = [rQrope, Res("qrope1")]
                    for oi, sti in enumerate(own_sts):
                        tok0, ntok, nsub, psub = STS[sti]
                        q0 = q0_of(sti)
                        rU = rUown[oi]
                        for h in range(8):
                            a_t, rA = next_accB()
                            for dc in range(8):
                                P(lambda e, a_t=a_t, dc=dc, h=h: e.matmul(
                                    a_t[:, :ntok], lhsT=wB[:, dc, h * 128:(h + 1) * 128],
                                    rhs=uT_own[:, dc, q0:q0 + ntok], start=(dc == 0), stop=(dc == 7)),
                                  r=[rWBa, rU], w=[rA], inc=(dc == 7))
                            ks = qcount[0] % 3
                            qcount[0] += 1
                            A(lambda e, a_t=a_t, ks=ks: e.activation(out=stgQ[ks][:, :ntok],
                                                                     in_=a_t[:, :ntok], func=AF.Copy),
                              r=[rA], w=[rStgQ[ks]])
                            DMA(QAs[h, :, q0:q0 + ntok], stgQ[ks][:, :ntok], dsStgQ[ks],
                                r=[rStgQ[ks]], w=[rQAs])
                    pendB = []
                    for oi, sti in enumerate(own_sts):
                        tok0, ntok, nsub, psub = STS[sti]
                        q0 = q0_of(sti)
                        rU = rUown[oi]
                        sl = oi % 2
                        DMA(csB[sl][:psub, 0:nsub, :],
                            rope_d[tok0:tok0 + ntok, :].rearrange("(s p) d -> p s d", p=psub),
                            dsCsB[sl], w=[rCsB[sl]])
                        for s in range(nsub):
                            bs = bcount[0] % 2
                            bcount[0] += 1
                            qs = q0 + s * 128
                            for g in range(2):
                                a_t, rA = next_accB()
                                for dc in range(8):
                                    P(lambda e, a_t=a_t, dc=dc, g=g, qs=qs: e.matmul(
                                        a_t[:psub, :], lhsT=uT_own[:, dc, qs:qs + psub],
                                        rhs=wB[:, dc, 1024 + g * 512: 1024 + (g + 1) * 512],
                                        start=(dc == 0), stop=(dc == 7)),
                                      r=[rWBb, rU], w=[rA], inc=(dc == 7))
                                A(lambda e, a_t=a_t, g=g: e.activation(
                                    out=qraw[:psub, g * 512:(g + 1) * 512], in_=a_t[:psub, :], func=AF.Copy),
                                  r=[rA], w=[rQraw])
                            norm_rope(qraw, 8, psub, gqb, csB[sl], s, qrope2[bs], qsc,
                                      [rCsB[sl]], rQraw, rQsc, rQrope2[bs])
                            for g in range(4):
                                a_t, rA = next_accB()
                                for dc in range(8):
                                    P(lambda e, a_t=a_t, dc=dc, g=g, qs=qs: e.matmul(
                                        a_t[:psub, :], lhsT=uT_own[:, dc, qs:qs + psub],
                                        rhs=wB[:, dc, 2048 + g * 512: 2048 + (g + 1) * 512],
                                        start=(dc == 0), stop=(dc == 7)),
                                      r=[rWBg, rU], w=[rA], inc=(dc == 7))
                                A(lambda e, a_t=a_t, g=g, bs=bs: e.activation(
                                    out=stgG[bs][:psub, g * 512:(g + 1) * 512], in_=a_t[:psub, :],
                                    func=AF.Sigmoid), r=[rA], w=[rStgG[bs]])
                            DMA(Gs[qs:qs + psub, :], stgG[bs][:psub, :], dsStgG[bs],
                                r=[rStgG[bs]], w=[rGs])
                            oldB = list(pendB)
                            pendB.clear()
                            for f_ in oldB:
                                f_()

                            def trB(bs=bs, psub=psub, qs=qs):
                                for j in range(8):
                                    P(lambda e, j=j: e.transpose(
                                        out=qtp[:, j * 128: j * 128 + psub],
                                        in_=qrope2[bs][:psub, j * 128:(j + 1) * 128],
                                        identity=identb[:psub, :psub]),
                                      r=[rQrope2[bs], rC], w=[rQtp], inc=(j == 7))
                                V(lambda e: e.tensor_copy(
                                    out=stgQB[bs][:, :, :psub],
                                    in_=qtp[:, :].rearrange("p (g t) -> p g t", g=8)[:, :, :psub]),
                                  r=[rQtp], w=[rStgQB[bs]])
                                DMA(QBs[:, :, qs:qs + psub].rearrange("g p t -> p g t"),
                                    stgQB[bs][:, :, :psub], dsStgQB[bs], r=[rStgQB[bs]], w=[rQBs])
                            pendB.append(trB)
                    for f_ in pendB:
                        f_()
                    pendB.clear()
                    S.barrier()

        if stop_after >= 2:
            with ExitStack() as p2:
                KT = [[sb(p2, f"KT{s}_{m}", [128, NT], BF16) for m in range(2)] for s in range(2)]
                VT = [sb(p2, f"VT{s}", [128, NKB, 129], BF16) for s in range(2)]
                QT = [[sb(p2, f"QT{s}_{m}", [128, NOWN], BF16) for m in range(2)] for s in range(2)]
                rKV = [Res(f"KV{s}", True) for s in range(2)]
                dsC2 = dsem("ds_c2")
                dsSig = [dsem(f"ds_sig{i}") for i in range(2)]
                rSig = [Res(f"sig{i}", True) for i in range(2)]
                dsKV = [dsem(f"ds_kv{s}") for s in range(2)]
                beta = sb(p2, "beta", [128, 8 * 5 * NKB], F32)
                dtab = sb(p2, "dtab", [128, 896], F32)
                gsubb = sb(p2, "gsubb", [128, 128], F32)
                lamb = sb(p2, "lamb", [128, 4, 64], F32)
                lamt = sb(p2, "lamt", [128, 2, 64], F32)
                lame = sb(p2, "lame", [128, 2], F32)
                lamneg = sb(p2, "lamneg", [128, 1], F32)
                rLam = Res("lam")
                DMA(beta[:, :], beta_d[:, :], dsC2, w=[rC])
                DMA(dtab[:, :], dtab_d[:, :], dsC2, w=[rC])
                DMA(gsubb[:, :], gsub_d[0:1, :].partition_broadcast(128), dsC2, w=[rC])
                for i in range(4):
                    DMA(lamb[:, i, :], lam_d[i:i + 1, :].partition_broadcast(128), dsC2, w=[rC])
                for s in range(2):
                    for m in range(2):
                        DMA(KT[s][m][64:68, :], sigk_d[:, :], dsSig[s], w=[rSig[s]])
                    V(lambda e, s=s: e.memset(VT[s][:, :, 128:129], 1.0), w=[rSig[s]])
                V(lambda e: e.tensor_tensor(out=lamt[:, 0, :], in0=lamb[:, 0, :], in1=lamb[:, 1, :], op=ALU.mult),
                  r=[rC], w=[rLam])
                V(lambda e: e.tensor_tensor(out=lamt[:, 1, :], in0=lamb[:, 2, :], in1=lamb[:, 3, :], op=ALU.mult),
                  r=[rC], w=[rLam])
                V(lambda e: e.tensor_reduce(out=lame[:, 0:2], in_=lamt[:, :, :], axis=AX.X, op=ALU.add),
                  r=[rLam], w=[rLam])
                A(lambda e: e.activation(out=lame[:, 0:2], in_=lame[:, 0:2], func=AF.Exp), r=[rLam], w=[rLam])
                V(lambda e: e.tensor_tensor(out=lamneg[:, :], in0=lame[:, 1:2], in1=lame[:, 0:1], op=ALU.subtract),
                  r=[rLam], w=[rLam])
                V(lambda e: e.tensor_scalar(out=lamneg[:, :], in0=lamneg[:, :], scalar1=-LAM_INIT, scalar2=None,
                                            op0=ALU.add), r=[rLam], w=[rLam])
                V(lambda e: e.tensor_scalar(out=gsubb[:, :], in0=gsubb[:, :], scalar1=1.0 - LAM_INIT,
                                            scalar2=None, op0=ALU.mult), r=[rC], w=[rC])

                PT = [sb(p2, f"PT{i}", [128, 1024], BF16) for i in range(3)]
                rPT = [Res(f"PT{i}") for i in range(3)]
                smix = [sb(p2, f"smix{i}", [128, 1024], F32) for i in range(2)]
                rSmix = [Res(f"smix{i}") for i in range(2)]
                Sps = [psum(p2, f"Sps{i}", [128, 1024], F32) for i in range(2)]
                rSps = [Res(f"Sps{i}", psum=True) for i in range(2)]
                Ops = psum(p2, "Ops", [128, 1536], F32)
                rOps = Res("Ops", psum=True)
                ev = {n: sb(p2, "ev_" + n, shp, F32) for n, shp in
                      [("r1", [128, 8]), ("t2", [128, 128]), ("dd", [128, 128]), ("ssd", [128, 1]),
                       ("lnd", [128, 1]), ("rsd", [128, 1]), ("junk", [128, 128])]}
                rEv = Res("ev")
                stgO = [sb(p2, f"stgO{i}", [128, 4, 128], BF16) for i in range(2)]
                rStgO = [Res(f"stgO{i}") for i in range(2)]
                dsStgO = [dsem(f"ds_so{i}") for i in range(2)]

                Osb = [sb(p2, f"Osb{i}", [128, 1536], F32) for i in range(2)]
                rOsb = [Res(f"Osb{i}") for i in range(2)]
                osb_i = [0]

                def oacc(m, s, n=129):
                    i = m * 4 + s
                    off = (i // 3) * 512 + (i % 3) * 129
                    return Ops[:, off:off + n]

                jobs = [("A", h) for h in range(8)] + [("B", pi) for pi in range(4)]

                def load_job(ji):
                    kind, idx = jobs[ji]
                    s = ji % 2
                    if kind == "A":
                        for m in range(2):
                            for half in range(2):
                                c0, c1 = half * 4104, (half + 1) * 4104
                                DMA(KT[s][m][0:64, c0:c1], KAs[idx, m * 64:(m + 1) * 64, c0:c1], dsKV[s],
                                    r=[rKAs], w=[rKV[s]])
                            DMA(QT[s][m][0:64, :], QAs[idx, m * 64:(m + 1) * 64, :], dsKV[s],
                                r=[rQAs], w=[rKV[s]])
                            DMA(QT[s][m][64:68, :], qaug_d[idx, :, :], dsKV[s], w=[rKV[s]])
                        for q4 in range(4):
                            b0, b1 = q4 * 16, (q4 + 1) * 16
                            DMA(VT[s][:, b0:b1, 0:128], VAs[idx, :, b0:b1, :], dsKV[s], r=[rVAs], w=[rKV[s]])
                        DMA(VT[s][0:16, 64:65, 0:128], VAs[idx, 0:16, 64:65, :], dsKV[s], r=[rVAs], w=[rKV[s]])
                    else:
                        kv = idx // 2
                        for half in range(2):
                            c0, c1 = half * 4104, (half + 1) * 4104
                            DMA(KT[s][0][:, c0:c1], KBs[kv, :, c0:c1], dsKV[s], r=[rKBs], w=[rKV[s], rSig[s]])
                        for m in range(2):
                            DMA(QT[s][m][:, :], QBs[2 * idx + m, :, :], dsKV[s], r=[rQBs], w=[rKV[s]])
                        for q4 in range(4):
                            b0, b1 = q4 * 16, (q4 + 1) * 16
                            DMA(VT[s][:, b0:b1, 0:128], VBs[kv, :, b0:b1, :], dsKV[s], r=[rVBs], w=[rKV[s]])
                        DMA(VT[s][0:16, 64:65, 0:128], VBs[kv, 0:16, 64:65, :], dsKV[s], r=[rVBs], w=[rKV[s]])

                CH = [(c * 512, 512, 4, 128) for c in range(4)] + [(2048, NMETA, 1, NMETA)]
                load_job(0)
                pt_i = [0]
                so_i = [0]
                for ji, (kind, idx) in enumerate(jobs):
                    s = ji % 2
                    if ji + 1 < len(jobs):
                        load_job(ji + 1)
                    isA = kind == "A"
                    KR = 68 if isA else 128
                    slope = 2.0 ** (-(idx + 1)) if isA else 0.0
                    scale = 0.125 if isA else 128.0 ** -0.5
                    Kt = [KT[s][0], KT[s][1] if isA else KT[s][0]]
                    Qt = QT[s]
                    for c, (q0, ncq, nsb, psq) in enumerate(CH):

                        started = set()

                        def qk(kb, sp):
                            kp = 128 if kb < 64 else NMETA
                            k0 = kb * 128
                            mixed = isA and c < 4 and (4 * c <= kb <= 4 * c + 3)
                            rows = 64 if mixed else KR
                            for m in range(2):
                                P(lambda e, m=m, sp=sp, kp=kp, k0=k0, rows=rows: e.matmul(
                                    Sps[sp][:kp, m * 512: m * 512 + ncq],
                                    lhsT=Kt[m][0:rows, k0:k0 + kp], rhs=Qt[m][0:rows, q0:q0 + ncq],
                                    start=True, stop=True),
                                  r=[rKV[s], rSig[s]], w=[rSps[sp]], inc=(m == 1))

                        def soft(kb, sp):
                            kp = 128 if kb < 64 else NMETA
                            mixed = isA and c < 4 and (4 * c <= kb <= 4 * c + 3)
                            pi_ = pt_i[0] % 3
                            pt_i[0] += 1
                            if ncq == 512:
                                src = Sps[sp][:kp, :]
                                dst = PT[pi_][:kp, :]
                            else:
                                src = Sps[sp][:kp, :].rearrange("p (m q) -> p m q", m=2)[:, :, 0:ncq]
                                dst = PT[pi_][:kp, :].rearrange("p (m q) -> p m q", m=2)[:, :, 0:ncq]
                            if mixed:
                                mm = kb - 4 * c
                                x0 = 384 - 128 * mm
                                for m in range(2):
                                    V(lambda e, m=m, sp=sp, x0=x0: e.scalar_tensor_tensor(
                                        out=smix[sp][:, m * 512:(m + 1) * 512], in0=dtab[:, x0:x0 + 512],
                                        scalar=-8.0 * slope, in1=Sps[sp][:, m * 512:(m + 1) * 512],
                                        op0=ALU.mult, op1=ALU.add),
                                      r=[rSps[sp], rC], w=[rSmix[sp]])
                                A(lambda e, sp=sp, dst=dst: e.activation(out=dst, in_=smix[sp][:, :], func=AF.Exp,
                                                                         scale=scale),
                                  r=[rSmix[sp]], w=[rPT[pi_]])
                            elif isA:
                                bcol = (idx * 5 + c) * NKB + kb
                                A(lambda e, src=src, dst=dst, bcol=bcol, kp=kp: e.activation(
                                    out=dst, in_=src, func=AF.Exp, bias=beta[:kp, bcol:bcol + 1], scale=scale),
                                  r=[rSps[sp], rC], w=[rPT[pi_]])
                            else:
                                A(lambda e, src=src, dst=dst: e.activation(out=dst, in_=src, func=AF.Exp,
                                                                           scale=scale),
                                  r=[rSps[sp]], w=[rPT[pi_]])
                            return pi_

                        def pv(kb, pi_, is_last, is_first):
                            kp = 128 if kb < 64 else NMETA
                            for m in range(2):
                                for sq in range(nsb):
                                    last = (m == 1 and sq == nsb - 1)
                                    bank_ = (m * 4 + sq) // 3
                                    st_ = is_first and (bank_ not in started)
                                    if is_first:
                                        started.add(bank_)
                                    P(lambda e, m=m, sq=sq, pi_=pi_, kp=kp, kb=kb, st_=st_: e.matmul(
                                        oacc(m, sq)[:psq, :],
                                        lhsT=PT[pi_][:kp, m * 512 + sq * 128: m * 512 + sq * 128 + psq],
                                        rhs=VT[s][:kp, kb, :], start=st_, stop=is_last,
                                        skip_group_check=True),
                                      r=[rPT[pi_], rKV[s], rSig[s]], w=[rOps], inc=last)

                        kbs = []
                        for kb in range(64):
                            if (not isA) or c == 4:
                                kbs.append(kb)
                                continue
                            lo_q, hi_q = 512 * c, 512 * c + 511
                            cands = [(128 * kb, 128 * kb + 127)]
                            if kb >= 16:
                                cands.append((128 * kb - 8192, 128 * kb + 127 - 8192))
                            dmin = min(max(0, ul - hi_q, lo_q - uh) for ul, uh in cands)
                            if slope * dmin <= 100.0:
                                kbs.append(kb)
                        kbs.append(64)
                        if c < 4:
                            qk(kbs[0], 0)
                            if len(kbs) > 1:
                                qk(kbs[1], 1)
                            for i, kb in enumerate(kbs):
                                pi_ = soft(kb, i % 2)
                                if i + 2 < len(kbs):
                                    qk(kbs[i + 2], i % 2)
                                pv(kb, pi_, i == len(kbs) - 1, i == 0)
                        else:
                            groups = [list(range(g * 8, g * 8 + 8)) for g in range(8)] + [[64]]

                            def qk4(grp, sp):
                                kp = 128 if grp[0] < 64 else NMETA
                                n = len(grp) * 2
                                for j, kb in enumerate(grp):
                                    for m in range(2):
                                        P(lambda e, j=j, m=m, kb=kb, kp=kp, sp=sp: e.matmul(
                                            Sps[sp][:kp, j * 32 + m * 16: j * 32 + m * 16 + 16],
                                            lhsT=Kt[m][0:KR, kb * 128: kb * 128 + kp], rhs=Qt[m][0:KR, q0:q0 + ncq],
                                            start=True, stop=True, skip_group_check=True),
                                          r=[rKV[s], rSig[s]], w=[rSps[sp]], inc=(j * 2 + m == n - 1))

                            def soft4(grp, sp):
                                kp = 128 if grp[0] < 64 else NMETA
                                w_ = len(grp) * 32
                                pi_ = pt_i[0] % 3
                                pt_i[0] += 1
                                A(lambda e, sp=sp, pi_=pi_, kp=kp, w_=w_: e.activation(
                                    out=PT[pi_][:kp, 0:w_], in_=Sps[sp][:kp, 0:w_], func=AF.Exp, scale=scale),
                                  r=[rSps[sp]], w=[rPT[pi_]])
                                return pi_

                            def pv4(grp, pi_, is_last, is_first):
                                kp = 128 if grp[0] < 64 else NMETA
                                n = len(grp) * 2
                                for j, kb in enumerate(grp):
                                    for m in range(2):
                                        bank_ = (m * 4) // 3
                                        st_ = is_first and (bank_ not in started)
                                        if is_first:
                                            started.add(bank_)
                                        P(lambda e, j=j, m=m, kb=kb, kp=kp, pi_=pi_, st_=st_: e.matmul(
                                            oacc(m, 0)[:psq, :],
                                            lhsT=PT[pi_][:kp, j * 32 + m * 16: j * 32 + m * 16 + 16],
                                            rhs=VT[s][:kp, kb, :], start=st_,
                                            stop=(is_last and j == len(grp) - 1), skip_group_check=True),
                                          r=[rPT[pi_], rKV[s], rSig[s]], w=[rOps], inc=(j * 2 + m == n - 1))

                            qk4(groups[0], 0)
                            qk4(groups[1], 1)
                            for i, grp in enumerate(groups):
                                pi_ = soft4(grp, i % 2)
                                if i + 2 < len(groups):
                                    qk4(groups[i + 2], i % 2)
                                pv4(grp, pi_, i == len(groups) - 1, i == 0)
                        ob_i = osb_i[0] % 2
                        osb_i[0] += 1
                        rOsbC = rOsb[ob_i]
                        if nsb == 4:
                            V(lambda e, ob_i=ob_i: e.tensor_copy(
                                out=Osb[ob_i][:, 0:1024].rearrange("p (b c) -> p b c", b=2)[:, :, 0:387],
                                in_=Ops[:, 0:1024].rearrange("p (b c) -> p b c", b=2)[:, :, 0:387]),
                              r=[rOps], w=[rOsbC])
                            V(lambda e, ob_i=ob_i: e.tensor_copy(out=Osb[ob_i][:, 1024:1282], in_=Ops[:, 1024:1282]),
                              r=[rOps], w=[rOsbC])
                        else:
                            V(lambda e, ob_i=ob_i: e.tensor_copy(out=Osb[ob_i][:psq, 0:129], in_=Ops[:psq, 0:129]),
                              r=[rOps], w=[rOsbC])
                            V(lambda e, ob_i=ob_i: e.tensor_copy(out=Osb[ob_i][:psq, 641:770], in_=Ops[:psq, 641:770]),
                              r=[rOps], w=[rOsbC])

                        def osbv(m, sq_, n=129, ob_i=ob_i):
                            i = m * 4 + sq_
                            off = (i // 3) * 512 + (i % 3) * 129
                            return Osb[ob_i][:, off:off + n]
                        so = so_i[0] % 2
                        so_i[0] += 1
                        for sq in range(nsb if isA else 0):
                            if isA:
                                O1, O2 = osbv(0, sq), osbv(1, sq)
                                V(lambda e, O1=O1: e.reciprocal(out=ev["r1"][:psq, 0:1], in_=O1[:psq, 128:129]),
                                  r=[rOsbC], w=[rEv])
                                V(lambda e, O2=O2: e.reciprocal(out=ev["r1"][:psq, 1:2], in_=O2[:psq, 128:129]),
                                  r=[rOsbC], w=[rEv])
                                V(lambda e: e.tensor_tensor(out=ev["r1"][:psq, 2:3], in0=ev["r1"][:psq, 1:2],
                                                            in1=lamneg[:psq, 0:1], op=ALU.mult),
                                  r=[rEv, rLam], w=[rEv])
                                V(lambda e, O2=O2: e.tensor_scalar(out=ev["t2"][:psq, :], in0=O2[:psq, 0:128],
                                                                   scalar1=ev["r1"][:psq, 2:3], scalar2=None,
                                                                   op0=ALU.mult), r=[rOsbC, rEv], w=[rEv])
                                V(lambda e, O1=O1: e.scalar_tensor_tensor(
                                    out=ev["dd"][:psq, :], in0=O1[:psq, 0:128], scalar=ev["r1"][:psq, 0:1],
                                    in1=ev["t2"][:psq, :], op0=ALU.mult, op1=ALU.add), r=[rOsbC, rEv], w=[rEv])
                                A(lambda e: e.activation(out=ev["junk"][:psq, :], in_=ev["dd"][:psq, :],
                                                         func=AF.Square, accum_out=ev["ssd"][:psq, 0:1]),
                                  r=[rEv], w=[rEv])
                                rstd_from_ss(ev["ssd"][:psq, 0:1], ev["lnd"][:psq, 0:1], ev["rsd"][:psq, 0:1],
                                             128.0, [rEv], [rEv])
                                V(lambda e, sq=sq, so=so: e.scalar_tensor_tensor(
                                    out=stgO[so][:psq, sq, :], in0=ev["dd"][:psq, :], scalar=ev["rsd"][:psq, 0:1],
                                    in1=gsubb[:psq, :], op0=ALU.mult, op1=ALU.mult),
                                  r=[rEv, rC], w=[rStgO[so]])
                        if isA:
                            dst_d = OAs[q0:q0 + ncq, idx * 128:(idx + 1) * 128].rearrange("(s p) e -> p s e", p=psq)
                            DMA(dst_d, stgO[so][:psq, 0:nsb, :], dsStgO[so], r=[rStgO[so]], w=[rOAs])
                        else:
                            for m in range(2):
                                g = 2 * idx + m
                                so = so_i[0] % 2
                                so_i[0] += 1
                                for sq in range(nsb):
                                    Om = osbv(m, sq)
                                    V(lambda e, Om=Om: e.reciprocal(out=ev["r1"][:psq, 0:1], in_=Om[:psq, 128:129]),
                                      r=[rOsbC], w=[rEv])
                                    V(lambda e, Om=Om, sq=sq, so=so: e.tensor_scalar(
                                        out=stgO[so][:psq, sq, :], in0=Om[:psq, 0:128], scalar1=ev["r1"][:psq, 0:1],
                                        scalar2=None, op0=ALU.mult), r=[rOsbC, rEv], w=[rStgO[so]])
                                dst_d = OBs[q0:q0 + ncq, g * 128:(g + 1) * 128].rearrange("(s p) e -> p s e", p=psq)
                                DMA(dst_d, stgO[so][:psq, 0:nsb, :], dsStgO[so], r=[rStgO[so]], w=[rOBs])
                S.barrier()

        if stop_after >= 3:
            with ExitStack() as p3:
                H = sb(p3, "H", [128, 17, D], F32)
                rH = [Res(f"H{t}") for t in range(17)]
                u2T = sb(p3, "u2T", [128, 8, NOWN], BF16)
                rU2 = [Res(f"u2T{t}") for t in range(17)]
                AFF = sb(p3, "AFF", [128, 17, NEXP], F32)
                rAFF = Res("AFF")
                COEF = sb(p3, "COEF", [128, 17, NEXP], F32)
                rCOEF = Res("COEF")
                rAgin = Res("agin", True)
                gffnb = sb(p3, "gffnb", [128, D], F32)
                dsC3 = dsem("ds_c3")
                DMA(gffnb[:, :], gffn_d[0:1, :].partition_broadcast(128), dsC3, w=[rC])
                BLK = [(t * 128, 128) for t in range(16)] + [(2048, NMETA)]

                with ExitStack() as p3a:
                    Wa = sb(p3a, "Wa", [128, 8, D], BF16)
                    Wb = sb(p3a, "Wb", [128, 8, D], BF16)
                    Wo = sb(p3a, "Wo", [128, 8, D], BF16)
                    wr = sb(p3a, "wr", [128, 8, NEXP], BF16)
                    rW3 = Res("W3", True)
                    dsW3 = dsem("ds_w3")
                    for c in range(8):
                        DMA(Wa[:, c, :], wa_d.rearrange("(c p) f -> p c f", p=128)[:, c, :], dsW3, w=[rW3], q="pool")
                        DMA(Wb[:, c, :], wb_d.rearrange("(c p) f -> p c f", p=128)[:, c, :], dsW3, w=[rW3], q="pool")
                        DMA(Wo[:, c, :], wo_d.rearrange("(c p) f -> p c f", p=128)[:, c, :], dsW3, w=[rW3], q="pool")
                    DMA(wr[:, :, :], wr_d.rearrange("(c p) f -> p c f", p=128), dsW3, w=[rW3], q="pool")
                    oa_t = [sb(p3a, f"oa_t{i}", [128, D], BF16) for i in range(2)]
                    ob_t = [sb(p3a, f"ob_t{i}", [128, D], BF16) for i in range(2)]
                    g_t = [sb(p3a, f"g_t{i}", [128, 2048], BF16) for i in range(2)]
                    x_t3 = [sb(p3a, f"x_t3{i}", [128, D], F32) for i in range(2)]
                    rIn3 = [Res(f"in3_{i}", True) for i in range(2)]
                    dsIn3 = [dsem(f"ds_in3_{i}") for i in range(2)]
                    oaT = sb(p3a, "oaT", [128, 8, 128], BF16)
                    obT = sb(p3a, "obT", [128, 8, 128], BF16)
                    mgT = sb(p3a, "mgT", [128, 8, 128], BF16)
                    rOaT, rObT, rMgT = Res("oaT"), Res("obT"), Res("mgT")
                    m1 = sb(p3a, "m1", [128, D], F32)
                    m2 = sb(p3a, "m2", [128, D], BF16)
                    affS = [sb(p3a, f"affS{i}", [NEXP, 128], F32) for i in range(2)]
                    rAffS = [Res(f"affS{i}") for i in range(2)]
                    dsAffS = [dsem(f"ds_affs{i}") for i in range(2)]
                    mg = sb(p3a, "mg", [128, D], BF16)
                    rM1, rM2, rMg = Res("m1"), Res("m2"), Res("mg")
                    u2 = sb(p3a, "u2", [128, D], BF16)
                    rU2t = Res("u2")
                    st3 = {n: sb(p3a, "st3_" + n, [128, 1], F32) for n in ("ss", "ln", "rs", "se", "rse")}
                    ex3 = sb(p3a, "ex3", [128, NEXP], F32)
                    rSt3 = Res("st3")
                    tp3 = [psum(p3a, f"tp3_{i}", [128, 1024], BF16) for i in range(2)]
                    rTp3 = [Res(f"tp3_{i}", psum=True) for i in range(2)]
                    yps = [psum(p3a, f"yps{i}", [128, 512], F32) for i in range(4)]
                    rYps = [Res(f"yps{i}", psum=True) for i in range(4)]
                    lps = psum(p3a, "lps", [128, 512], F32)
                    rLps = Res("lps", psum=True)
                    tfp = psum(p3a, "tfp", [128, 512], F32)
                    rTfp = Res("tfp", psum=True)

                    def load3(t):
                        q0, pb_ = BLK[t]
                        sl = t % 2
                        tokx = q0 if t < 16 else SEQ
                        DMA(oa_t[sl][:pb_, :], OAs[q0:q0 + pb_, :], dsIn3[sl], r=[rOAs], w=[rIn3[sl]])
                        DMA(ob_t[sl][:pb_, :], OBs[q0:q0 + pb_, :], dsIn3[sl], r=[rOBs], w=[rIn3[sl]])
                        DMA(g_t[sl][:pb_, :], Gs[q0:q0 + pb_, :], dsIn3[sl], r=[rGs], w=[rIn3[sl]])
                        DMA(x_t3[sl][:pb_, :], hx[tokx:tokx + pb_, :], dsIn3[sl], w=[rIn3[sl]])

                    def transpose8(src, rsrc, dstT, rdst, pb_, tpi):
                        for fc in range(8):
                            P(lambda e, fc=fc: e.transpose(out=tp3[tpi][:, fc * 128: fc * 128 + pb_],
                                                           in_=src[:pb_, fc * 128:(fc + 1) * 128],
                                                           identity=identb[:pb_, :pb_]),
                              r=[rsrc, rC], w=[rTp3[tpi]], inc=(fc == 7))

                    load3(0)
                    for t, (q0, pb_) in enumerate(BLK):
                        sl = t % 2
                        if t + 1 < len(BLK):
                            load3(t + 1)
                        transpose8(oa_t[sl], rIn3[sl], oaT, rOaT, pb_, 0)
                        V(lambda e: e.tensor_copy(out=oaT[:, :, :pb_],
                                                  in_=tp3[0][:, :].rearrange("p (c t) -> p c t", c=8)[:, :, :pb_]),
                          r=[rTp3[0]], w=[rOaT])
                        transpose8(ob_t[sl], rIn3[sl], obT, rObT, pb_, 1)
                        A(lambda e: e.activation(out=obT[:, :, :pb_],
                                                 in_=tp3[1][:, :].rearrange("p (c t) -> p c t", c=8)[:, :, :pb_],
                                                 func=AF.Copy),
                          r=[rTp3[1]], w=[rObT])
                        for g in range(2):
                            for fc in range(8):
                                P(lambda e, g=g, fc=fc: e.matmul(yps[g][:pb_, :], lhsT=oaT[:, fc, :pb_],
                                                                 rhs=Wa[:, fc, g * 512:(g + 1) * 512],
                                                                 start=(fc == 0), stop=(fc == 7)),
                                  r=[rOaT, rW3], w=[rYps[g]], inc=(fc == 7))
                        for g in range(2):
                            for fc in range(8):
                                P(lambda e, g=g, fc=fc: e.matmul(yps[2 + g][:pb_, :], lhsT=obT[:, fc, :pb_],
                                                                 rhs=Wb[:, fc, g * 512:(g + 1) * 512],
                                                                 start=(fc == 0), stop=(fc == 7)),
                                  r=[rObT, rW3], w=[rYps[2 + g]], inc=(fc == 7))
                        for g in range(2):
                            V(lambda e, g=g: e.tensor_tensor(out=m1[:pb_, g * 512:(g + 1) * 512], in0=yps[g][:pb_, :],
                                                             in1=g_t[sl][:pb_, g * 512:(g + 1) * 512], op=ALU.mult),
                              r=[rYps[g], rIn3[sl]], w=[rM1])
                            V(lambda e, g=g: e.tensor_tensor(out=m2[:pb_, g * 512:(g + 1) * 512],
                                                             in0=yps[2 + g][:pb_, :],
                                                             in1=g_t[sl][:pb_, 1024 + g * 512:1024 + (g + 1) * 512],
                                                             op=ALU.mult),
                              r=[rYps[2 + g], rIn3[sl]], w=[rM2])
                        V(lambda e: e.tensor_tensor(out=mg[:pb_, :], in0=m1[:pb_, :], in1=m2[:pb_, :], op=ALU.add),
                          r=[rM1, rM2], w=[rMg])
                        transpose8(mg, rMg, mgT, rMgT, pb_, 0)
                        V(lambda e: e.tensor_copy(out=mgT[:, :, :pb_],
                                                  in_=tp3[0][:, :].rearrange("p (c t) -> p c t", c=8)[:, :, :pb_]),
                          r=[rTp3[0]], w=[rMgT])
                        for g in range(2):
                            for fc in range(8):
                                P(lambda e, g=g, fc=fc: e.matmul(yps[g][:pb_, :], lhsT=mgT[:, fc, :pb_],
                                                                 rhs=Wo[:, fc, g * 512:(g + 1) * 512],
                                                                 start=(fc == 0), stop=(fc == 7)),
                                  r=[rMgT, rW3], w=[rYps[g]], inc=(fc == 7))
                        for g in range(2):
                            V(lambda e, g=g, t=t: e.tensor_tensor(out=H[:pb_, t, g * 512:(g + 1) * 512],
                                                                  in0=yps[g][:pb_, :],
                                                                  in1=x_t3[sl][:pb_, g * 512:(g + 1) * 512],
                                                                  op=ALU.add),
                              r=[rYps[g], rIn3[sl]], w=[rH[t]])
                        if dbg:
                            DMA(dbg_h1[q0:q0 + pb_, :], H[:pb_, t, :], dsDbg, r=[rH[t]])
                        A(lambda e, t=t: e.activation(out=u2[:pb_, :], in_=H[:pb_, t, :], func=AF.Square,
                                                      accum_out=st3["ss"][:pb_, 0:1]), r=[rH[t]], w=[rSt3, rU2t])
                        rstd_from_ss(st3["ss"][:pb_, 0:1], st3["ln"][:pb_, 0:1], st3["rs"][:pb_, 0:1], float(D),
                                     [rSt3], [rSt3])
                        V(lambda e, t=t: e.scalar_tensor_tensor(out=u2[:pb_, :], in0=H[:pb_, t, :],
                                                                scalar=st3["rs"][:pb_, 0:1], in1=gffnb[:pb_, :],
                                                                op0=ALU.mult, op1=ALU.mult),
                          r=[rH[t], rSt3, rC], w=[rU2t])
                        transpose8(u2, rU2t, None, None, pb_, 1)
                        A(lambda e, q0=q0: e.activation(
                            out=u2T[:, :, q0:q0 + pb_],
                            in_=tp3[1][:, :].rearrange("p (c t) -> p c t", c=8)[:, :, :pb_], func=AF.Copy),
                          r=[rTp3[1]], w=[rU2[t]])
                        for fc in range(8):
                            P(lambda e, fc=fc, q0=q0: e.matmul(lps[:pb_, 0:NEXP], lhsT=u2T[:, fc, q0:q0 + pb_],
                                                               rhs=wr[:, fc, :], start=(fc == 0), stop=(fc == 7)),
                              r=[rU2[t], rW3], w=[rLps], inc=(fc == 7))
                        A(lambda e: e.activation(out=ex3[:pb_, :], in_=lps[:pb_, 0:NEXP], func=AF.Exp,
                                                 accum_out=st3["se"][:pb_, 0:1]), r=[rLps], w=[rSt3])
                        V(lambda e: e.reciprocal(out=st3["rse"][:pb_, 0:1], in_=st3["se"][:pb_, 0:1]),
                          r=[rSt3], w=[rSt3])
                        V(lambda e, t=t: e.tensor_scalar(out=AFF[:pb_, t, :], in0=ex3[:pb_, :],
                                                         scalar1=st3["rse"][:pb_, 0:1], scalar2=None, op0=ALU.mult),
                          r=[rSt3], w=[rAFF])
                        P(lambda e, t=t: e.transpose(out=tfp[0:NEXP, 0:pb_], in_=AFF[:pb_, t, :],
                                                     identity=identf[:pb_, :pb_]), r=[rAFF, rC], w=[rTfp])
                        V(lambda e, sl=sl: e.tensor_copy(out=affS[sl][:, 0:pb_], in_=tfp[0:NEXP, 0:pb_]),
                          r=[rTfp], w=[rAffS[sl]])
                        DMA(agin.ap()[:, q0:q0 + pb_], affS[sl][:, 0:pb_], dsAffS[sl], r=[rAffS[sl]], w=[rAgin])
                        if dbg:
                            DMA(dbg_aff[q0:q0 + pb_, :], AFF[:pb_, t, :], dsDbg, r=[rAFF])
                    S.barrier()

                if stop_after >= 4:
                    with ExitStack() as p4:
                        AGc = sb(p4, "AGc", [NEXP, SEQ + NMETA], F32)
                        rAG = Res("AGc", True)
                        dsAG = dsem("ds_ag")
                        dsCC = DSem(sem("cc_sem"))
                        rAgout = Res("agout")
                        S.coll(lambda e: e.collective_compute(
                            "AllGather", ALU.bypass, replica_groups=[[0, 1, 2, 3], [4, 5, 6, 7]],
                            ins=[agin.ap().opt()], outs=[agout.ap().opt()]), dsCC, reads=[rAgin], writes=[rAgout])
                        ago = agout.ap()
                        DMA(AGc[:, 0:SEQ].rearrange("e (r t) -> e r t", r=4),
                            ago.rearrange("(r e) t -> e r t", e=NEXP)[:, :, 0:2048], dsAG, r=[rAgout], w=[rAG],
                            q="pool")
                        DMA(AGc[:, SEQ:SEQ + NMETA], ago[0:NEXP, 2048:2048 + NMETA], dsAG, r=[rAgout], w=[rAG],
                            q="pool")
                        lo = sb(p4, "lo", [NEXP, 1], F32)
                        mid = sb(p4, "mid", [NEXP, 1], F32)
                        cnt = sb(p4, "cnt", [NEXP, 1], F32)
                        prd = sb(p4, "prd", [NEXP, 1], F32)
                        cmpj = sb(p4, "cmpj", [NEXP, SEQ + NMETA], BF16)
                        rB = Res("bis")
                        V(lambda e: e.memset(lo[:, :], 0.0), w=[rB])
                        for it in range(NBIS):
                            ck = 2.0 ** (-(it + 1))
                            V(lambda e, ck=ck: e.tensor_scalar(out=mid[:, :], in0=lo[:, :], scalar1=ck, scalar2=None,
                                                               op0=ALU.add), r=[rB], w=[rB])
                            V(lambda e: e.tensor_scalar(out=cmpj[:, :], in0=AGc[:, :], scalar1=mid[:, 0:1],
                                                        scalar2=0.0, op0=ALU.is_ge, op1=ALU.add,
                                                        accum_out=cnt[:, 0:1]), r=[rB, rAG], w=[rB])
                            V(lambda e, ck=ck: e.tensor_scalar(out=prd[:, :], in0=cnt[:, :], scalar1=CAP - 0.5,
                                                               scalar2=ck, op0=ALU.is_ge, op1=ALU.mult),
                              r=[rB], w=[rB])
                            V(lambda e: e.tensor_tensor(out=lo[:, :], in0=lo[:, :], in1=prd[:, :], op=ALU.add),
                              r=[rB], w=[rB])
                        rThrD = Res("thr_d")
                        DMA(thr_d.rearrange("o e -> e o"), lo[:, :], dsAG, r=[rB], w=[rThrD])
                        if dbg:
                            DMA(dbg_thr.rearrange("o e -> e o"), lo[:, :], dsDbg, r=[rB])
                        THR = sb(p4, "THR", [128, NEXP], F32)
                        rTHR = Res("THR")
                        DMA(THR[:, :], thr_d[0:1, :].partition_broadcast(128), dsAG, r=[rThrD], w=[rTHR])
                        for t in range(16):
                            V(lambda e, t=t: e.tensor_tensor(out=COEF[:, t, :], in0=AFF[:, t, :], in1=THR[:, :],
                                                             op=ALU.is_ge), r=[rAFF, rTHR], w=[rCOEF])
                            V(lambda e, t=t: e.tensor_tensor(out=COEF[:, t, :], in0=COEF[:, t, :], in1=AFF[:, t, :],
                                                             op=ALU.mult), r=[rAFF, rCOEF], w=[rCOEF])
                        S.barrier()

                if stop_after >= 5:
                    with ExitStack() as p5:
                        wg = [sb(p5, f"wg{i}", [128, 8, 512], BF16) for i in range(2)]
                        wu = [sb(p5, f"wu{i}", [128, 8, 512], BF16) for i in range(2)]
                        wd = [sb(p5, f"wd{i}", [128, 4, D], BF16) for i in range(2)]
                        rWe = [Res(f"We{i}", True) for i in range(2)]
                        dsWe = [dsem(f"ds_we{i}") for i in range(2)]
                        hT = [sb(p5, f"hT{i}", [128, 4, 512], BF16) for i in range(2)]
                        rHT = [Res(f"hT{i}") for i in range(2)]
                        sg = [sb(p5, f"sg{i}", [128, 512], F32) for i in range(2)]
                        rSg = [Res(f"sg{i}") for i in range(2)]
                        gps = [psum(p5, f"gps{i}", [128, 512], F32) for i in range(2)]
                        ups = [psum(p5, f"ups{i}", [128, 512], F32) for i in range(2)]
                        rGps = [Res(f"gps{i}", psum=True) for i in range(2)]
                        rUps = [Res(f"ups{i}", psum=True) for i in range(2)]
                        ypm = [psum(p5, f"ypm{i}", [128, 512], F32) for i in range(4)]
                        rYpm = [Res(f"ypm{i}", psum=True) for i in range(4)]

                        def load_e(ei):
                            sl = ei % 2
                            for c in range(8):
                                DMA(wg[sl][:, c, :], wg_d[ei, c * 128:(c + 1) * 128, :], dsWe[sl], w=[rWe[sl]], q="pool")
                                DMA(wu[sl][:, c, :], wu_d[ei, c * 128:(c + 1) * 128, :], dsWe[sl], w=[rWe[sl]], q="pool")
                            for c in range(4):
                                DMA(wd[sl][:, c, :], wd_d[ei, c * 128:(c + 1) * 128, :], dsWe[sl], w=[rWe[sl]], q="pool")

                        load_e(0)
                        gi = [0]
                        yi = [0]
                        for ei in range(NEXP):
                            sl = ei % 2
                            if ei + 1 < NEXP:
                                load_e(ei + 1)
                            for tcn in range(4):
                                hs = (ei * 4 + tcn) % 2
                                rUs = [rU2[tcn * 4 + k] for k in range(4)]
                                for fc in range(4):
                                    gs = gi[0] % 2
                                    gi[0] += 1
                                    for dc in range(8):
                                        P(lambda e, gs=gs, dc=dc, fc=fc, tcn=tcn: e.matmul(
                                            gps[gs][:, :], lhsT=wg[sl][:, dc, fc * 128:(fc + 1) * 128],
                                            rhs=u2T[:, dc, tcn * 512:(tcn + 1) * 512], start=(dc == 0), stop=(dc == 7)),
                                          r=[rWe[sl]] + rUs, w=[rGps[gs]], inc=(dc == 7))
                                    for dc in range(8):
                                        P(lambda e, gs=gs, dc=dc, fc=fc, tcn=tcn: e.matmul(
                                            ups[gs][:, :], lhsT=wu[sl][:, dc, fc * 128:(fc + 1) * 128],
                                            rhs=u2T[:, dc, tcn * 512:(tcn + 1) * 512], start=(dc == 0), stop=(dc == 7)),
                                          r=[rWe[sl]] + rUs, w=[rUps[gs]], inc=(dc == 7))
                                    A(lambda e, gs=gs: e.activation(out=sg[gs][:, :], in_=gps[gs][:, :], func=AF.Silu),
                                      r=[rGps[gs]], w=[rSg[gs]])
                                    V(lambda e, gs=gs, fc=fc, hs=hs: e.tensor_tensor(
                                        out=hT[hs][:, fc, :], in0=ups[gs][:, :], in1=sg[gs][:, :], op=ALU.mult),
                                      r=[rUps[gs], rSg[gs]], w=[rHT[hs]])
                                for ts in range(4):
                                    t = tcn * 4 + ts
                                    for dh in range(2):
                                        ys = yi[0] % 4
                                        yi[0] += 1
                                        for fc in range(4):
                                            P(lambda e, ys=ys, fc=fc, ts=ts, dh=dh, hs=hs: e.matmul(
                                                ypm[ys][:, :], lhsT=hT[hs][:, fc, ts * 128:(ts + 1) * 128],
                                                rhs=wd[sl][:, fc, dh * 512:(dh + 1) * 512],
                                                start=(fc == 0), stop=(fc == 3)),
                                              r=[rHT[hs], rWe[sl]], w=[rYpm[ys]], inc=(fc == 3))
                                        V(lambda e, ys=ys, t=t, dh=dh, ei=ei: e.scalar_tensor_tensor(
                                            out=H[:, t, dh * 512:(dh + 1) * 512], in0=ypm[ys][:, :],
                                            scalar=COEF[:, t, ei:ei + 1], in1=H[:, t, dh * 512:(dh + 1) * 512],
                                            op0=ALU.mult, op1=ALU.add),
                                          r=[rYpm[ys], rCOEF, rH[t]], w=[rH[t]])
                        S.barrier()

                with ExitStack() as p6:
                    gfinb = sb(p6, "gfinb", [128, D], F32)
                    dsC6 = dsem("ds_c6")
                    DMA(gfinb[:, :], gfin_d[0:1, :].partition_broadcast(128), dsC6, w=[rC])
                    fo = [sb(p6, f"fo{i}", [128, D], F32) for i in range(2)]
                    rFo = [Res(f"fo{i}") for i in range(2)]
                    dsFo = [dsem(f"ds_fo{i}") for i in range(2)]
                    fj = sb(p6, "fj", [128, D], BF16)
                    fst = {n: sb(p6, "fst_" + n, [128, 1], F32) for n in ("ss", "ln", "rs")}
                    rFst = Res("fst")
                    for t in range(16):
                        sl = t % 2
                        A(lambda e, t=t: e.activation(out=fj[:, :], in_=H[:, t, :], func=AF.Square,
                                                      accum_out=fst["ss"][:, 0:1]), r=[rH[t]], w=[rFst])
                        rstd_from_ss(fst["ss"][:, 0:1], fst["ln"][:, 0:1], fst["rs"][:, 0:1], float(D),
                                     [rFst], [rFst])
                        V(lambda e, t=t, sl=sl: e.scalar_tensor_tensor(
                            out=fo[sl][:, :], in0=H[:, t, :], scalar=fst["rs"][:, 0:1], in1=gfinb[:, :],
                            op0=ALU.mult, op1=ALU.mult), r=[rH[t], rFst, rC], w=[rFo[sl]])
                        DMA(y_out[t * 128:(t + 1) * 128, :], fo[sl][:, :], dsFo[sl], r=[rFo[sl]])
                    S.barrier()
        else:
            with ExitStack() as pz:
                zt = sb(pz, "zt", [128, D], F32)
                rZ = Res("zt")
                dsZ = dsem("ds_z")
                V(lambda e: e.memset(zt[:, :], 0.0), w=[rZ])
                for t in range(16):
                    DMA(y_out[t * 128:(t + 1) * 128, :], zt[:, :], dsZ, r=[rZ])
                S.barrier()

        S.barrier()

        @block.tensor
        def _(eng):
            S.emit("pe", eng)

        @block.scalar
        def _(eng):
            S.emit("act", eng)

        @block.vector
        def _(eng):
            S.emit("dve", eng)

        @block.gpsimd
        def _(eng):
            S.emit("pool", eng)

        @block.sync
        def _(eng):
            S.emit("sp", eng)

    return nc


def _tables(r):
    bf = ml_dtypes.bfloat16
    jp = np.arange(SEQ)
    uj = np.where(jp + 2048 * r < SEQ, jp, jp - SEQ).astype(np.float64)
    sig = np.zeros((4, NT), np.float32)
    beta = np.zeros((128, 8, 5, NKB), np.float32)
    for c in range(4):
        before = uj < 512 * c
        sgn = np.where(before, 1.0, -1.0)
        sig[c, :SEQ] = sgn
        for h in range(8):
            slope = 2.0 ** (-(h + 1))
            b = sgn * slope * (uj - 512 * c - 256)
            beta[:, h, c, :64] = b.reshape(64, 128).T
    qaug = np.zeros((8, 4, NOWN), np.float32)
    a = np.arange(512) - 256
    for h in range(8):
        slope = 2.0 ** (-(h + 1))
        for c in range(4):
            qaug[h, c, c * 512:(c + 1) * 512] = -8.0 * slope * a
    x = np.arange(896)[None, :]
    bb = np.arange(128)[:, None]
    dtab = np.abs(x - bb - 384).astype(np.float32)
    t_true = (jp + 2048 * r) % SEQ
    row_id = (t_true // 64).astype(np.float32)
    col_id = (t_true % 64).astype(np.float32)
    inv_freq = (np.float32(10000.0) ** (-np.arange(0, 64, 2, dtype=np.float32) / np.float32(64))).astype(np.float32)
    ar = (row_id[:, None] * inv_freq[None, :]).astype(np.float32)
    ac = (col_id[:, None] * inv_freq[None, :]).astype(np.float32)
    rope = np.zeros((NT, 256), np.float32)
    rope[:, 0:128] = 1.0
    cr, sr, cc, sc = np.cos(ar), np.sin(ar), np.cos(ac), np.sin(ac)
    rope[:SEQ, 0:32] = cr
    rope[:SEQ, 32:64] = cr
    rope[:SEQ, 64:96] = cc
    rope[:SEQ, 96:128] = cc
    rope[:SEQ, 128:160] = -sr
    rope[:SEQ, 160:192] = sr
    rope[:SEQ, 192:224] = -sc
    rope[:SEQ, 224:256] = sc
    return dict(sigk=sig.astype(bf), qaug=qaug.astype(bf),
                beta=np.ascontiguousarray(beta.reshape(128, -1)), dtab=dtab, rope=rope)


def make_in_maps(inputs):
    bf = ml_dtypes.bfloat16
    x = np.asarray(inputs["x"], np.float32)
    meta = np.asarray(inputs["meta_tokens"], np.float32)
    f = lambda k: np.ascontiguousarray(np.asarray(inputs[k], np.float32))
    common = {
        "w_in": f("w_in")[0],
        "gmixT": np.ascontiguousarray(f("g_mix")[0].reshape(8, 128).T),
        "lamv": np.ascontiguousarray(np.stack([f("lambda_q1")[0], f("lambda_k1")[0],
                                               f("lambda_q2")[0], f("lambda_k2")[0]])),
        "g_subln": f("g_subln"), "g_qnorm": f("g_qnorm"), "g_knorm": f("g_knorm"),
        "w_branch_a": f("w_branch_a")[0], "w_branch_b": f("w_branch_b")[0], "w_out": f("w_out")[0],
        "g_ffn": f("g_ffn"), "w_router": f("w_router")[0],
        "w_gate": f("w_gate")[0], "w_up": f("w_up")[0], "w_down": f("w_down")[0],
        "g_final": f("g_final").reshape(1, D),
        "identb": np.eye(128, dtype=np.float32).astype(bf),
        "identf": np.eye(128, dtype=np.float32),
    }
    tabs = [_tables(r) for r in range(4)]
    maps = []
    for c in range(8):
        b, r = c // 4, c % 4
        hxv = np.concatenate([np.roll(x[b], -2048 * r, axis=0), meta], axis=0)
        m = dict(common)
        m.update(tabs[r])
        m["hx"] = np.ascontiguousarray(hxv)
        maps.append(m)
    return maps


_NC_CACHE = {}


def kernel(**inputs):
    if "nc" not in _NC_CACHE:
        _NC_CACHE["nc"] = build()
    nc = _NC_CACHE["nc"]
    maps = make_in_maps(inputs)
    res = run_bass_kernel_spmd(nc, maps, core_ids=list(range(8)))
    out = np.zeros((2, SEQ, D), np.float32)
    for c in range(8):
        b, r = c // 4, c % 4
        out[b, r * 2048:(r + 1) * 2048, :] = res.results[c]["y"]
    return out
```

```python
import numpy as np
import ml_dtypes
from contextlib import ExitStack
import concourse.bass as bass
import concourse.mybir as mybir
from concourse.bass_utils import run_bass_kernel_spmd

F32 = mybir.dt.float32
BF16 = mybir.dt.bfloat16
AF = mybir.ActivationFunctionType
ALU = mybir.AluOpType
AX = mybir.AxisListType

D = 1024
SEQ = 8192
NMETA = 16
NT = SEQ + NMETA
NOWN = 2048 + NMETA
NKB = 65
EPS = 1e-6
NEXP = 16
CAP = 2 * NT // NEXP
LAM_INIT = 0.2
NBIS = 24


import types


def _freeze(fn):
    if fn is None or fn.__closure__ is None:
        return fn
    cells = []
    for c in fn.__closure__:
        try:
            cells.append(types.CellType(c.cell_contents))
        except ValueError:
            cells.append(c)
    return types.FunctionType(fn.__code__, fn.__globals__, fn.__name__, fn.__defaults__, tuple(cells))


class Res:
    __slots__ = ("name", "w", "rd", "multi", "psum")

    def __init__(self, name, multi=False, psum=False):
        self.name = name
        self.w = {}
        self.rd = {}
        self.multi = multi
        self.psum = psum


class DSem:
    def __init__(self, sem):
        self.sem = sem
        self.count = 0


class Sched:
    ENGS = ("pe", "act", "dve", "pool", "sp")

    def __init__(self):
        self.prog = {e: [] for e in self.ENGS}
        self.cnt = {e: 0 for e in self.ENGS}
        self.sem = {}
        self.known = {e: {} for e in self.ENGS}
        self.all_dsems = []

    def _deps(self, eng, reads, writes):
        deps = {}
        known = self.known[eng]

        def add(tok, kind):
            sem, val, e = tok
            if e == eng and eng in ("pe", "sp"):
                return
            k = id(sem)
            if known.get(k, 0) >= val:
                return
            if k not in deps or deps[k][1] < val:
                deps[k] = (sem, val)

        for r in reads:
            for tok in r.w.values():
                add(tok, "raw")
            if r.psum:
                for tok in r.rd.values():
                    if tok[2] != eng:
                        add(tok, "rar")
        for w in writes:
            if not w.multi:
                for tok in w.w.values():
                    add(tok, "waw")
            for tok in w.rd.values():
                add(tok, "war")
        for k, (sem, val) in deps.items():
            known[k] = val
        return list(deps.values())

    def _record(self, tok, reads, writes):
        k = id(tok[0])
        for r in reads:
            r.rd[k] = tok
        for w in writes:
            if w.multi:
                w.w[k] = tok
            else:
                w.w = {k: tok}
                w.rd = {}

    def op(self, eng, fn, reads=(), writes=(), inc=True):
        deps = self._deps(eng, reads, writes)
        tok = (self.sem[eng], self.cnt[eng] + 1, eng)
        self._record(tok, reads, writes)
        if inc:
            self.cnt[eng] += 1
        self.prog[eng].append((deps, _freeze(fn), self.sem[eng] if inc else None, 1))

    def dma(self, q, fn, ds, reads=(), writes=()):
        deps = self._deps(q, reads, writes)
        ds.count += 16
        tok = (ds.sem, ds.count, None)
        self._record(tok, reads, writes)
        self.prog[q].append((deps, _freeze(fn), ds.sem, 16))

    def coll(self, fn, ds, reads=(), writes=()):
        deps = self._deps("pool", reads, writes)
        ds.count += 1
        tok = (ds.sem, ds.count, None)
        self._record(tok, reads, writes)
        self.prog["pool"].append((deps, _freeze(fn), ds.sem, None))

    def barrier(self):
        toks = []
        for e in self.ENGS:
            if e == "sp":
                continue
            if self.cnt[e] > 0:
                toks.append((self.sem[e], self.cnt[e]))
        for ds in self.all_dsems:
            if ds.count > 0:
                toks.append((ds.sem, ds.count))
        for e in self.ENGS:
            deps = []
            for sem, val in toks:
                if self.known[e].get(id(sem), 0) < val:
                    self.known[e][id(sem)] = val
                    deps.append((sem, val))
            if deps:
                self.prog[e].append((deps, None, None, 0))

    def emit(self, eng, handle):
        for deps, fn, sem, incv in self.prog[eng]:
            for s, v in deps:
                handle.wait_ge(s, v)
            if fn is None:
                continue
            ins = fn(handle)
            if sem is not None:
                if incv is None:
                    ins.then_inc(sem)
                else:
                    ins.then_inc(sem, incv)


def build(dbg=False, stop_after=99):
    nc = bass.Bass("TRN2", target_bir_lowering=False)
    S = Sched()

    def din(name, shape, dt=F32):
        return nc.dram_tensor(name, list(shape), dt, kind="ExternalInput").ap()

    def dscr(name, shape, dt):
        if dbg:
            return nc.dram_tensor(name, list(shape), dt, kind="ExternalOutput").ap()
        return nc.dram_tensor(name, list(shape), dt).ap()

    hx = din("hx", [NT, D])
    w_in = din("w_in", [D, 6656])
    gmixT_d = din("gmixT", [128, 8])
    lam_d = din("lamv", [4, 64])
    gsub_d = din("g_subln", [1, 128])
    gq_d = din("g_qnorm", [1, 128])
    gk_d = din("g_knorm", [1, 128])
    wa_d = din("w_branch_a", [D, D])
    wb_d = din("w_branch_b", [D, D])
    wo_d = din("w_out", [D, D])
    gffn_d = din("g_ffn", [1, D])
    wr_d = din("w_router", [D, NEXP])
    if stop_after >= 5:
        wg_d = din("w_gate", [NEXP, D, 512])
        wu_d = din("w_up", [NEXP, D, 512])
        wd_d = din("w_down", [NEXP, 512, D])
    gfin_d = din("g_final", [1, D])
    sigk_d = din("sigk", [4, NT], BF16)
    qaug_d = din("qaug", [8, 4, NOWN], BF16)
    beta_d = din("beta", [128, 8 * 5 * NKB])
    dtab_d = din("dtab", [128, 896])
    rope_d = din("rope", [NT, 256])
    identb_d = din("identb", [128, 128], BF16)
    identf_d = din("identf", [128, 128])
    y_out = nc.dram_tensor("y", [2048, D], F32, kind="ExternalOutput").ap()

    KAs = dscr("KAs", [8, 128, NT], BF16)
    QAs = dscr("QAs", [8, 128, NOWN], BF16)
    VAs = dscr("VAs", [8, 128, NKB, 128], BF16)
    KBs = dscr("KBs", [2, 128, NT], BF16)
    QBs = dscr("QBs", [8, 128, NOWN], BF16)
    VBs = dscr("VBs", [2, 128, NKB, 128], BF16)
    Gs = dscr("Gs", [NOWN, 2048], BF16)
    OAs = dscr("OAs", [NOWN, D], BF16)
    OBs = dscr("OBs", [NOWN, D], BF16)
    agin = nc.dram_tensor("agin", [NEXP, NOWN], F32)
    agout = nc.dram_tensor("agout", [4 * NEXP, NOWN], F32)
    thr_d = nc.dram_tensor("thr_d", [1, NEXP], F32).ap()
    dbg_aff = dscr("dbg_aff", [NOWN, NEXP], F32) if dbg else None
    dbg_h1 = dscr("dbg_h1", [NOWN, D], F32) if dbg else None
    dbg_thr = dscr("dbg_thr", [1, NEXP], F32) if dbg else None

    rKAs, rQAs, rVAs = Res("KAs", True), Res("QAs", True), Res("VAs", True)
    rKBs, rQBs, rVBs = Res("KBs", True), Res("QBs", True), Res("VBs", True)
    rGs, rOAs, rOBs = Res("Gs", True), Res("OAs", True), Res("OBs", True)

    with ExitStack() as top:
        def sem(name):
            return top.enter_context(nc.semaphore(name))

        for e in Sched.ENGS:
            S.sem[e] = sem("sem_" + e)

        def dsem(name):
            d = DSem(sem(name))
            S.all_dsems.append(d)
            return d

        def sb(es, name, shape, dt):
            return es.enter_context(nc.sbuf_tensor("s_" + name, list(shape), dt))

        def psum(es, name, shape, dt):
            return es.enter_context(nc.psum_tensor("p_" + name, list(shape), dt))

        block = top.enter_context(nc.Block())

        def V(fn, r=(), w=()):
            S.op("dve", fn, r, w)

        def A(fn, r=(), w=()):
            S.op("act", fn, r, w)

        def P(fn, r=(), w=(), inc=True):
            S.op("pe", fn, r, w, inc)

        def G(fn, r=(), w=()):
            S.op("pool", fn, r, w)

        def DMA(out, in_, ds, r=(), w=(), q="sp"):
            S.dma(q, (lambda e, o=out, i=in_: e.dma_start(out=o, in_=i)), ds, r, w)

        identb = sb(top, "identb", [128, 128], BF16)
        identf = sb(top, "identf", [128, 128], F32)
        epsT = sb(top, "epsT", [128, 1], F32)
        rC = Res("consts", True)
        dsDbg = dsem("ds_dbg")
        dsC = dsem("ds_const")
        DMA(identb[:, :], identb_d[:, :], dsC, w=[rC])
        DMA(identf[:, :], identf_d[:, :], dsC, w=[rC])
        V(lambda e: e.memset(epsT[:, :], EPS), w=[rC])

        def rstd_from_ss(ss_ap, ln_ap, out_ap, n, rs, ws):
            A(lambda e: e.activation(out=ln_ap, in_=ss_ap, func=AF.Ln,
                                     bias=epsT[:ln_ap.shape[0], 0:1], scale=1.0 / n),
              r=rs + [rC], w=ws)
            A(lambda e: e.activation(out=out_ap, in_=ln_ap, func=AF.Exp, scale=-0.5),
              r=ws, w=ws)

        STS = [(t * 512, 512, 4, 128) for t in range(16)] + [(SEQ, NMETA, 1, NMETA)]

        def is_own(st):
            return st < 4 or st == 16

        def q0_of(st):
            return st * 512 if st < 4 else 2048

        with ExitStack() as p1:
            uT_own = sb(p1, "uT_own", [128, 8, NOWN], BF16)
            rUown = [Res(f"uTown{i}") for i in range(5)]
            gmixT = sb(p1, "gmixT", [128, 8], F32)
            gqb = sb(p1, "gqb", [128, 128], F32)
            gkb = sb(p1, "gkb", [128, 128], F32)
            dsC1 = dsem("ds_c1")
            DMA(gmixT[:, :], gmixT_d[:, :], dsC1, w=[rC])
            DMA(gqb[:, :], gq_d[0:1, :].partition_broadcast(128), dsC1, w=[rC])
            DMA(gkb[:, :], gk_d[0:1, :].partition_broadcast(128), dsC1, w=[rC])

            def norm_rope(raw, H, psub, gb, cs_t, s, out_bf, scr, rs_extra, r_raw, r_scr, r_out):
                sq, ssk, lnk, rk, tmp, yy = scr
                W = H * 128
                V(lambda e: e.tensor_tensor(out=sq[:psub, :W], in0=raw[:psub, :W], in1=raw[:psub, :W], op=ALU.mult),
                  r=[r_raw], w=[r_scr])
                V(lambda e: e.tensor_reduce(out=ssk[:psub, :H],
                                            in_=sq[:psub, :W].rearrange("p (h d) -> p h d", h=H),
                                            axis=AX.X, op=ALU.add), r=[r_scr], w=[r_scr])
                rstd_from_ss(ssk[:psub, :H], lnk[:psub, :H], rk[:psub, :H], 128.0, [r_scr], [r_scr])
                y3 = yy[:psub, :W].rearrange("p (h d) -> p h d", h=H)
                V(lambda e: e.tensor_tensor(out=y3, in0=raw[:psub, :W].rearrange("p (h d) -> p h d", h=H),
                                            in1=rk[:psub, 0:H].unsqueeze(2).to_broadcast([psub, H, 128]),
                                            op=ALU.mult), r=[r_raw, r_scr], w=[r_scr])
                V(lambda e: e.tensor_tensor(out=y3, in0=y3,
                                            in1=gb[:psub, :].unsqueeze(1).to_broadcast([psub, H, 128]),
                                            op=ALU.mult), r=[r_scr, rC], w=[r_scr])
                y5 = yy[:psub, :W].rearrange("p (h a b c) -> p h a b c", h=H, a=2, b=2)
                t5 = tmp[:psub, :W].rearrange("p (h a b c) -> p h a b c", h=H, a=2, b=2)
                sn4 = cs_t[:psub, s, 128:256].rearrange("p (a b c) -> p a b c", a=2, b=2)
                V(lambda e: e.tensor_tensor(out=t5[:, :, :, 0, :], in0=y5[:, :, :, 1, :],
                                            in1=sn4[:, :, 0, :].unsqueeze(1).to_broadcast([psub, H, 2, 32]),
                                            op=ALU.mult), r=[r_scr] + rs_extra, w=[r_scr])
                V(lambda e: e.tensor_tensor(out=t5[:, :, :, 1, :], in0=y5[:, :, :, 0, :],
                                            in1=sn4[:, :, 1, :].unsqueeze(1).to_broadcast([psub, H, 2, 32]),
                                            op=ALU.mult), r=[r_scr] + rs_extra, w=[r_scr])
                V(lambda e: e.tensor_tensor(out=y3, in0=y3,
                                            in1=cs_t[:psub, s, 0:128].unsqueeze(1).to_broadcast([psub, H, 128]),
                                            op=ALU.mult), r=[r_scr] + rs_extra, w=[r_scr])
                V(lambda e: e.tensor_tensor(out=out_bf[:psub, :W], in0=yy[:psub, :W], in1=tmp[:psub, :W],
                                            op=ALU.add), r=[r_scr], w=[r_out])

            with ExitStack() as pa:
                wA = sb(pa, "wA", [128, 8, 2560], BF16)
                rWA = Res("wA", True)
                dsW = dsem("ds_w")
                w_in_v = w_in.rearrange("(c p) f -> p c f", p=128)
                for c in range(8):
                    DMA(wA[:, c, 0:2048], w_in_v[:, c, 1024:3072], dsW, w=[rWA], q="pool")
                    DMA(wA[:, c, 2048:2560], w_in_v[:, c, 4096:4608], dsW, w=[rWA], q="pool")
                xt = [sb(pa, f"xt{i}", [128, 4, D], F32) for i in range(2)]
                rXt = [Res(f"xt{i}", True) for i in range(2)]
                dsXc = [dsem(f"ds_xc{i}") for i in range(2)]
                dsX = [dsem(f"ds_x{i}") for i in range(2)]
                cs = [sb(pa, f"cs{i}", [128, 4, 256], F32) for i in range(2)]
                rCs = [Res(f"cs{i}", True) for i in range(2)]
                xsb = [sb(pa, f"xs{i}", [128, 4, D], BF16) for i in range(2)]
                rXsb = [Res(f"xs{i}") for i in range(2)]
                junk = sb(pa, "junk", [128, D], BF16)
                rJunk = Res("junk")
                ssb = [sb(pa, f"ss{i}", [128, 4], F32) for i in range(2)]
                lnvb = [sb(pa, f"lnv{i}", [128, 4], F32) for i in range(2)]
                rstdb = [sb(pa, f"rstd{i}", [128, 4], F32) for i in range(2)]
                rStb = [Res(f"stats{i}") for i in range(2)]
                uT_tmp = [sb(pa, f"uTt{i}", [128, 8, 512], BF16) for i in range(2)]
                rUt = [Res(f"uTt{i}") for i in range(2)]
                stgK = [sb(pa, f"stgK{i}", [128, 512], BF16) for i in range(3)]
                rStgK = [Res(f"stgK{i}") for i in range(3)]
                dsStgK = [dsem(f"ds_sk{i}") for i in range(3)]
                stgV = [sb(pa, f"stgV{i}", [128, 1024], BF16) for i in range(2)]
                rStgV = [Res(f"stgV{i}") for i in range(2)]
                dsStgV = [dsem(f"ds_sv{i}") for i in range(2)]
                stgVB = [sb(pa, f"stgVB{i}", [128, 256], BF16) for i in range(2)]
                rStgVB = [Res(f"stgVB{i}") for i in range(2)]
                dsStgVB = [dsem(f"ds_svb{i}") for i in range(2)]
                kraw = sb(pa, "kraw", [128, 256], F32)
                rKraw = Res("kraw")
                ksc = (sb(pa, "ksq", [128, 256], F32), sb(pa, "kss", [128, 2], F32),
                       sb(pa, "kln", [128, 2], F32), sb(pa, "krk", [128, 2], F32),
                       sb(pa, "ktmp", [128, 256], F32), sb(pa, "kyy", [128, 256], F32))
                rKsc = Res("ksc")
                krope = sb(pa, "krope", [128, 4, 256], BF16)
                rKropeS = [Res(f"krope{i}") for i in range(4)]
                stgKB = sb(pa, "stgKB", [128, 2, 512], BF16)
                rStgKB = Res("stgKB")
                dsStgKB = dsem("ds_skb")
                tpb = [psum(pa, f"tpb{i}", [128, 1024], BF16) for i in range(4)]
                rTp = [Res(f"tpb{i}", psum=True) for i in range(4)]
                acc = [psum(pa, f"acc{i}", [128, 512], F32) for i in range(3)]
                rAcc = [Res(f"acc{i}", psum=True) for i in range(3)]
                ktp = psum(pa, "ktp", [128, 1024], BF16)
                rKtp = Res("ktp", psum=True)
                acc_i = [0]

                def next_acc():
                    i = acc_i[0] % 3
                    acc_i[0] += 1
                    return acc[i], rAcc[i]

                def load_x(sti):
                    tok0, ntok, nsub, psub = STS[sti]
                    sl = sti % 2
                    DMA(xt[sl][:psub, 0:nsub, :],
                        hx[tok0:tok0 + ntok, :].rearrange("(s p) d -> p s d", p=psub),
                        dsX[sl], w=[rXt[sl]])
                    DMA(cs[sl][:psub, 0:nsub, :],
                        rope_d[tok0:tok0 + ntok, :].rearrange("(s p) d -> p s d", p=psub),
                        dsXc[sl], w=[rCs[sl]])

                load_x(0)

                def prep(sti):
                    tok0, ntok, nsub, psub = STS[sti]
                    sl = sti % 2
                    x_t = xt[sl]
                    ss, lnv, rstd, rSt, xs, rXs = ssb[sl], lnvb[sl], rstdb[sl], rStb[sl], xsb[sl], rXsb[sl]
                    for s in range(nsub):
                        A(lambda e, s=s: e.activation(out=junk[:psub, :], in_=x_t[:psub, s, :],
                                                      func=AF.Square, accum_out=ss[:psub, s:s + 1]),
                          r=[rXt[sl]], w=[rJunk, rSt])
                    rstd_from_ss(ss[:psub, :nsub], lnv[:psub, :nsub], rstd[:psub, :nsub], float(D),
                                 [rSt], [rSt])
                    for s in range(nsub):
                        V(lambda e, s=s: e.tensor_scalar(out=xs[:psub, s, :], in0=x_t[:psub, s, :],
                                                         scalar1=rstd[:psub, s:s + 1], scalar2=None,
                                                         op0=ALU.mult),
                          r=[rXt[sl], rSt], w=[rXs])

                prep(0)
                pend = []
                kcount = [0]
                vcount = [0]
                import os
                DBGL = os.environ.get("KDBG", "")
                for sti, (tok0, ntok, nsub, psub) in enumerate(STS):
                    sl = sti % 2
                    if "one" in DBGL and sti >= 1:
                        break
                    if sti + 1 < len(STS) and "one" not in DBGL:
                        load_x(sti + 1)
                    xs, rXs = xsb[sl], rXsb[sl]
                    own = is_own(sti)
                    if own:
                        q0 = q0_of(sti)
                        uT = uT_own
                        ucol = q0
                        rU = rUown[sti if sti < 4 else 4]
                    else:
                        uT = uT_tmp[sl]
                        ucol = 0
                        rU = rUt[sl]
                    for dc in range(8):
                        bank = dc // 2
                        half = dc % 2
                        for s in range(nsub):
                            P(lambda e, dc=dc, s=s, bank=bank, half=half: e.transpose(
                                out=tpb[bank][:, half * 512 + s * 128: half * 512 + s * 128 + psub],
                                in_=xs[:psub, s, dc * 128:(dc + 1) * 128],
                                identity=identb[:psub, :psub]),
                              r=[rXs, rC], w=[rTp[bank]], inc=(s == nsub - 1))
                        src = tpb[bank][:, half * 512: half * 512 + ntok]
                        dst = uT[:, dc, ucol:ucol + ntok]
                        if bank % 2 == 0:
                            V(lambda e, src=src, dst=dst, dc=dc: e.tensor_scalar(
                                out=dst, in0=src, scalar1=gmixT[:, dc:dc + 1], scalar2=None, op0=ALU.mult),
                              r=[rTp[bank], rC], w=[rU])
                        else:
                            A(lambda e, src=src, dst=dst, dc=dc: e.activation(
                                out=dst, in_=src, func=AF.Copy, scale=gmixT[:, dc:dc + 1]),
                              r=[rTp[bank], rC], w=[rU])
                    if sti + 1 < len(STS) and "one" not in DBGL:
                        prep(sti + 1)
                    if "noproj" in DBGL:
                        continue
                    for h in range(8 if "nokA" not in DBGL else 0):
                        a_t, rA = next_acc()
                        for dc in range(8):
                            P(lambda e, a_t=a_t, dc=dc, h=h: e.matmul(
                                a_t[:, :ntok], lhsT=wA[:, dc, h * 128:(h + 1) * 128],
                                rhs=uT[:, dc, ucol:ucol + ntok], start=(dc == 0), stop=(dc == 7)),
                              r=[rWA, rU], w=[rA], inc=(dc == 7))
                        ks = kcount[0] % 3
                        kcount[0] += 1
                        A(lambda e, a_t=a_t, ks=ks: e.activation(out=stgK[ks][:, :ntok], in_=a_t[:, :ntok],
                                                                 func=AF.Copy),
                          r=[rA], w=[rStgK[ks]])
                        DMA(KAs[h, :, tok0:tok0 + ntok], stgK[ks][:, :ntok], dsStgK[ks],
                            r=[rStgK[ks]], w=[rKAs])
                    if "notok" in DBGL:
                        continue
                    for s in range(nsub):
                        blk = (tok0 // 128) + s
                        vs = vcount[0] % 2
                        vcount[0] += 1
                        for g in range(2):
                            a_t, rA = next_acc()
                            for dc in range(8):
                                P(lambda e, a_t=a_t, dc=dc, g=g, s=s: e.matmul(
                                    a_t[:psub, :], lhsT=uT[:, dc, ucol + s * 128: ucol + s * 128 + psub],
                                    rhs=wA[:, dc, 1024 + g * 512: 1024 + (g + 1) * 512],
                                    start=(dc == 0), stop=(dc == 7)),
                                  r=[rWA, rU], w=[rA], inc=(dc == 7))
                            if g == 0:
                                A(lambda e, a_t=a_t, vs=vs: e.activation(
                                    out=stgV[vs][:psub, 0:512], in_=a_t[:psub, :], func=AF.Copy),
                                  r=[rA], w=[rStgV[vs]])
                            else:
                                V(lambda e, a_t=a_t, vs=vs: e.tensor_copy(
                                    out=stgV[vs][:psub, 512:1024], in_=a_t[:psub, :]),
                                  r=[rA], w=[rStgV[vs]])
                        if "novst" not in DBGL:
                          DMA(VAs[:, 0:psub, blk, :].rearrange("h p e -> p h e"),
                            stgV[vs][:psub, :].rearrange("p (h e) -> p h e", h=8),
                            dsStgV[vs], r=[rStgV[vs]], w=[rVAs])
                        a_t, rA = next_acc()
                        for dc in range(8):
                            P(lambda e, a_t=a_t, dc=dc, s=s: e.matmul(
                                a_t[:psub, :], lhsT=uT[:, dc, ucol + s * 128: ucol + s * 128 + psub],
                                rhs=wA[:, dc, 2048:2560], start=(dc == 0), stop=(dc == 7)),
                              r=[rWA, rU], w=[rA], inc=(dc == 7))
                        A(lambda e, a_t=a_t: e.activation(out=kraw[:psub, :], in_=a_t[:psub, 0:256],
                                                          func=AF.Copy), r=[rA], w=[rKraw])
                        A(lambda e, a_t=a_t, vs=vs: e.activation(out=stgVB[vs][:psub, :],
                                                                 in_=a_t[:psub, 256:512], func=AF.Copy),
                          r=[rA], w=[rStgVB[vs]])
                        if "novst" not in DBGL:
                          DMA(VBs[:, 0:psub, blk, :].rearrange("h p e -> p h e"),
                            stgVB[vs][:psub, :].rearrange("p (h e) -> p h e", h=2),
                            dsStgVB[vs], r=[rStgVB[vs]], w=[rVBs])
                        if "norope" in DBGL:
                            continue
                        norm_rope(kraw, 2, psub, gkb, cs[sl], s, krope[:, s, :], ksc,
                                  [rCs[sl]], rKraw, rKsc, rKropeS[s])
                        old = list(pend)
                        pend.clear()
                        for f_ in old:
                            f_()

                        def tr(s=s, psub=psub):
                            for j in range(2):
                                P(lambda e, j=j: e.transpose(
                                    out=ktp[:, j * 512 + s * 128: j * 512 + s * 128 + psub],
                                    in_=krope[:psub, s, j * 128:(j + 1) * 128],
                                    identity=identb[:psub, :psub]),
                                  r=[rKropeS[s], rC], w=[rKtp], inc=(j == 1))
                        pend.append(tr)
                    if "norope" in DBGL:
                        continue

                    def fin(ntok=ntok, tok0=tok0):
                        for j in range(2):
                            V(lambda e, j=j: e.tensor_copy(out=stgKB[:, j, :ntok],
                                                           in_=ktp[:, j * 512: j * 512 + ntok]),
                              r=[rKtp], w=[rStgKB])
                        for j in range(2):
                            DMA(KBs[j, :, tok0:tok0 + ntok], stgKB[:, j, :ntok], dsStgKB,
                                r=[rStgKB], w=[rKBs])
                    pend.append(fin)
                for f_ in pend:
                    f_()
                pend.clear()
                S.barrier()

            if stop_after >= 1.5:
                with ExitStack() as pb:
                    wB = sb(pb, "wB", [128, 8, 4096], BF16)
                    rWBa, rWBb, rWBg = Res("wBa", True), Res("wBb", True), Res("wBg", True)
                    dsWBa, dsWBb, dsWBg = dsem("ds_wba"), dsem("ds_wbb"), dsem("ds_wbg")
                    w_in_v = w_in.rearrange("(c p) f -> p c f", p=128)
                    for c in range(8):
                        DMA(wB[:, c, 0:1024], w_in_v[:, c, 0:1024], dsWBa, w=[rWBa], q="pool")
                    for c in range(8):
                        DMA(wB[:, c, 1024:2048], w_in_v[:, c, 3072:4096], dsWBb, w=[rWBb], q="pool")
                    for c in range(8):
                        DMA(wB[:, c, 2048:4096], w_in_v[:, c, 4608:6656], dsWBg, w=[rWBg], q="pool")
                    csB = [sb(pb, f"csB{i}", [128, 4, 256], F32) for i in range(2)]
                    rCsB = [Res(f"csB{i}", True) for i in range(2)]
                    dsCsB = [dsem(f"ds_csb{i}") for i in range(2)]
                    stgQ = [sb(pb, f"stgQ{i}", [128, 512], BF16) for i in range(3)]
                    rStgQ = [Res(f"stgQ{i}") for i in range(3)]
                    dsStgQ = [dsem(f"ds_sq{i}") for i in range(3)]
                    qraw = sb(pb, "qraw", [128, 1024], F32)
                    rQraw = Res("qraw")
                    qsc = (sb(pb, "qsq", [128, 1024], F32), sb(pb, "qss", [128, 8], F32),
                           sb(pb, "qln", [128, 8], F32), sb(pb, "qrk", [128, 8], F32),
                           sb(pb, "qtmp", [128, 1024], F32), sb(pb, "qyy", [128, 1024], F32))
                    rQsc = Res("qsc")
                    qrope = sb(pb, "qrope", [128, 1024], BF16)
                    rQrope = Res("qrope")
                    stgQB = [sb(pb, f"stgQB{i}", [128, 8, 128], BF16) for i in range(2)]
                    rStgQB = [Res(f"stgQB{i}") for i in range(2)]
                    dsStgQB = [dsem(f"ds_sqb{i}") for i in range(2)]
                    stgG = [sb(pb, f"stgG{i}", [128, 2048], BF16) for i in range(2)]
                    rStgG = [Res(f"stgG{i}") for i in range(2)]
                    dsStgG = [dsem(f"ds_sg{i}") for i in range(2)]
                    accB = [psum(pb, f"accB{i}", [128, 512], F32) for i in range(6)]
                    rAccB = [Res(f"accB{i}", psum=True) for i in range(6)]
                    qtp = psum(pb, "qtp", [128, 1024], BF16)
                    rQtp = Res("qtp", psum=True)
                    accb_i = [0]

                    def next_accB():
                        i = accb_i[0] % 6
                        accb_i[0] += 1
                        return accB[i], rAccB[i]

                    own_sts = [0, 1, 2, 3, 16]
                    qcount = [0]
                    bcount = [0]
                    qrope2 = [qrope, sb(pb, "qrope1", [128, 1024], BF16)]
                    rQrope2 = [rQrope, Res("qrope1")]
                    for oi, sti in enumerate(own_sts):
                        tok0, ntok, nsub, psub = STS[sti]
                        q0 = q0_of(sti)
                        rU = rUown[oi]
                        for h in range(8):
                            a_t, rA = next_accB()
                            for dc in range(8):
                                P(lambda e, a_t=a_t, dc=dc, h=h: e.matmul(
                                    a_t[:, :ntok], lhsT=wB[:, dc, h * 128:(h + 1) * 128],
                                    rhs=uT_own[:, dc, q0:q0 + ntok], start=(dc == 0), stop=(dc == 7)),
                                  r=[rWBa, rU], w=[rA], inc=(dc == 7))
                            ks = qcount[0] % 3
                            qcount[0] += 1
                            A(lambda e, a_t=a_t, ks=ks: e.activation(out=stgQ[ks][:, :ntok],
                                                                     in_=a_t[:, :ntok], func=AF.Copy),
                              r=[rA], w=[rStgQ[ks]])
                            DMA(QAs[h, :, q0:q0 + ntok], stgQ[ks][:, :ntok], dsStgQ[ks],
                                r=[rStgQ[ks]], w=[rQAs])
                    pendB = []
                    for oi, sti in enumerate(own_sts):
                        tok0, ntok, nsub, psub = STS[sti]
                        q0 = q0_of(sti)
                        rU = rUown[oi]
                        sl = oi % 2
                        DMA(csB[sl][:psub, 0:nsub, :],
                            rope_d[tok0:tok0 + ntok, :].rearrange("(s p) d -> p s d", p=psub),
                            dsCsB[sl], w=[rCsB[sl]])
                        for s in range(nsub):
                            bs = bcount[0] % 2
                            bcount[0] += 1
                            qs = q0 + s * 128
                            for g in range(2):
                                a_t, rA = next_accB()
                                for dc in range(8):
                                    P(lambda e, a_t=a_t, dc=dc, g=g, qs=qs: e.matmul(
                                        a_t[:psub, :], lhsT=uT_own[:, dc, qs:qs + psub],
                                        rhs=wB[:, dc, 1024 + g * 512: 1024 + (g + 1) * 512],
                                        start=(dc == 0), stop=(dc == 7)),
                                      r=[rWBb, rU], w=[rA], inc=(dc == 7))
                                A(lambda e, a_t=a_t, g=g: e.activation(
                                    out=qraw[:psub, g * 512:(g + 1) * 512], in_=a_t[:psub, :], func=AF.Copy),
                                  r=[rA], w=[rQraw])
                            norm_rope(qraw, 8, psub, gqb, csB[sl], s, qrope2[bs], qsc,
                                      [rCsB[sl]], rQraw, rQsc, rQrope2[bs])
                            for g in range(4):
                                a_t, rA = next_accB()
                                for dc in range(8):
                                    P(lambda e, a_t=a_t, dc=dc, g=g, qs=qs: e.matmul(
                                        a_t[:psub, :], lhsT=uT_own[:, dc, qs:qs + psub],
                                        rhs=wB[:, dc, 2048 + g * 512: 2048 + (g + 1) * 512],
                                        start=(dc == 0), stop=(dc == 7)),
                                      r=[rWBg, rU], w=[rA], inc=(dc == 7))
                                A(lambda e, a_t=a_t, g=g, bs=bs: e.activation(
                                    out=stgG[bs][:psub, g * 512:(g + 1) * 512], in_=a_t[:psub, :],
                                    func=AF.Sigmoid), r=[rA], w=[rStgG[bs]])
                            DMA(Gs[qs:qs + psub, :], stgG[bs][:psub, :], dsStgG[bs],
                                r=[rStgG[bs]], w=[rGs])
                            oldB = list(pendB)
                            pendB.clear()
                            for f_ in oldB:
                                f_()

                            def trB(bs=bs, psub=psub, qs=qs):
                                for j in range(8):
                                    P(lambda e, j=j: e.transpose(
                                        out=qtp[:, j * 128: j * 128 + psub],
                                        in_=qrope2[bs][:psub, j * 128:(j + 1) * 128],
                                        identity=identb[:psub, :psub]),
                                      r=[rQrope2[bs], rC], w=[rQtp], inc=(j == 7))
                                V(lambda e: e.tensor_copy(
                                    out=stgQB[bs][:, :, :psub],
                                    in_=qtp[:, :].rearrange("p (g t) -> p g t", g=8)[:, :, :psub]),
                                  r=[rQtp], w=[rStgQB[bs]])
                                DMA(QBs[:, :, qs:qs + psub].rearrange("g p t -> p g t"),
                                    stgQB[bs][:, :, :psub], dsStgQB[bs], r=[rStgQB[bs]], w=[rQBs])
                            pendB.append(trB)
                    for f_ in pendB:
                        f_()
                    pendB.clear()
                    S.barrier()

        if stop_after >= 2:
            with ExitStack() as p2:
                KT = [[sb(p2, f"KT{s}_{m}", [128, NT], BF16) for m in range(2)] for s in range(2)]
                VT = [sb(p2, f"VT{s}", [128, NKB, 129], BF16) for s in range(2)]
                QT = [[sb(p2, f"QT{s}_{m}", [128, NOWN], BF16) for m in range(2)] for s in range(2)]
                rKV = [Res(f"KV{s}", True) for s in range(2)]
                dsC2 = dsem("ds_c2")
                dsSig = [dsem(f"ds_sig{i}") for i in range(2)]
                rSig = [Res(f"sig{i}", True) for i in range(2)]
                dsKV = [dsem(f"ds_kv{s}") for s in range(2)]
                beta = sb(p2, "beta", [128, 8 * 5 * NKB], F32)
                dtab = sb(p2, "dtab", [128, 896], F32)
                gsubb = sb(p2, "gsubb", [128, 128], F32)
                lamb = sb(p2, "lamb", [128, 4, 64], F32)
                lamt = sb(p2, "lamt", [128, 2, 64], F32)
                lame = sb(p2, "lame", [128, 2], F32)
                lamneg = sb(p2, "lamneg", [128, 1], F32)
                rLam = Res("lam")
                DMA(beta[:, :], beta_d[:, :], dsC2, w=[rC])
                DMA(dtab[:, :], dtab_d[:, :], dsC2, w=[rC])
                DMA(gsubb[:, :], gsub_d[0:1, :].partition_broadcast(128), dsC2, w=[rC])
                for i in range(4):
                    DMA(lamb[:, i, :], lam_d[i:i + 1, :].partition_broadcast(128), dsC2, w=[rC])
                for s in range(2):
                    for m in range(2):
                        DMA(KT[s][m][64:68, :], sigk_d[:, :], dsSig[s], w=[rSig[s]])
                    V(lambda e, s=s: e.memset(VT[s][:, :, 128:129], 1.0), w=[rSig[s]])
                V(lambda e: e.tensor_tensor(out=lamt[:, 0, :], in0=lamb[:, 0, :], in1=lamb[:, 1, :], op=ALU.mult),
                  r=[rC], w=[rLam])
                V(lambda e: e.tensor_tensor(out=lamt[:, 1, :], in0=lamb[:, 2, :], in1=lamb[:, 3, :], op=ALU.mult),
                  r=[rC], w=[rLam])
                V(lambda e: e.tensor_reduce(out=lame[:, 0:2], in_=lamt[:, :, :], axis=AX.X, op=ALU.add),
                  r=[rLam], w=[rLam])
                A(lambda e: e.activation(out=lame[:, 0:2], in_=lame[:, 0:2], func=AF.Exp), r=[rLam], w=[rLam])
                V(lambda e: e.tensor_tensor(out=lamneg[:, :], in0=lame[:, 1:2], in1=lame[:, 0:1], op=ALU.subtract),
                  r=[rLam], w=[rLam])
                V(lambda e: e.tensor_scalar(out=lamneg[:, :], in0=lamneg[:, :], scalar1=-LAM_INIT, scalar2=None,
                                            op0=ALU.add), r=[rLam], w=[rLam])
                V(lambda e: e.tensor_scalar(out=gsubb[:, :], in0=gsubb[:, :], scalar1=1.0 - LAM_INIT,
                                            scalar2=None, op0=ALU.mult), r=[rC], w=[rC])

                PT = [sb(p2, f"PT{i}", [128, 1024], BF16) for i in range(3)]
                rPT = [Res(f"PT{i}") for i in range(3)]
                smix = [sb(p2, f"smix{i}", [128, 1024], F32) for i in range(2)]
                rSmix = [Res(f"smix{i}") for i in range(2)]
                Sps = [psum(p2, f"Sps{i}", [128, 1024], F32) for i in range(2)]
                rSps = [Res(f"Sps{i}", psum=True) for i in range(2)]
                Ops = psum(p2, "Ops", [128, 1536], F32)
                rOps = Res("Ops", psum=True)
                ev = {n: sb(p2, "ev_" + n, shp, F32) for n, shp in
                      [("r1", [128, 8]), ("t2", [128, 128]), ("dd", [128, 128]), ("ssd", [128, 1]),
                       ("lnd", [128, 1]), ("rsd", [128, 1]), ("junk", [128, 128]), ("dd4", [128, 4, 128]),
                       ("ssd4", [128, 4]), ("lnd4", [128, 4]), ("rsd4", [128, 4])]}
                pend_ev = []

                def flush_ev():
                    while pend_ev:
                        pend_ev.pop(0)()
                rEv = Res("ev")
                stgO = [sb(p2, f"stgO{i}", [128, 4, 128], BF16) for i in range(2)]
                rStgO = [Res(f"stgO{i}") for i in range(2)]
                dsStgO = [dsem(f"ds_so{i}") for i in range(2)]

                Osb = [sb(p2, f"Osb{i}", [128, 1536], F32) for i in range(2)]
                rOsb = [Res(f"Osb{i}") for i in range(2)]
                osb_i = [0]

                def oacc(m, s, n=129):
                    i = m * 4 + s
                    off = (i // 3) * 512 + (i % 3) * 129
                    return Ops[:, off:off + n]

                jobs = [("A", h) for h in range(8)] + [("B", pi) for pi in range(4)]

                def load_job(ji):
                    kind, idx = jobs[ji]
                    s = ji % 2
                    if kind == "A":
                        for m in range(2):
                            for half in range(2):
                                c0, c1 = half * 4104, (half + 1) * 4104
                                DMA(KT[s][m][0:64, c0:c1], KAs[idx, m * 64:(m + 1) * 64, c0:c1], dsKV[s],
                                    r=[rKAs], w=[rKV[s]])
                            DMA(QT[s][m][0:64, :], QAs[idx, m * 64:(m + 1) * 64, :], dsKV[s],
                                r=[rQAs], w=[rKV[s]])
                            DMA(QT[s][m][64:68, :], qaug_d[idx, :, :], dsKV[s], w=[rKV[s]])
                        for q4 in range(4):
                            b0, b1 = q4 * 16, (q4 + 1) * 16
                            DMA(VT[s][:, b0:b1, 0:128], VAs[idx, :, b0:b1, :], dsKV[s], r=[rVAs], w=[rKV[s]])
                        DMA(VT[s][0:16, 64:65, 0:128], VAs[idx, 0:16, 64:65, :], dsKV[s], r=[rVAs], w=[rKV[s]])
                    else:
                        kv = idx // 2
                        for half in range(2):
                            c0, c1 = half * 4104, (half + 1) * 4104
                            DMA(KT[s][0][:, c0:c1], KBs[kv, :, c0:c1], dsKV[s], r=[rKBs], w=[rKV[s], rSig[s]])
                        for m in range(2):
                            DMA(QT[s][m][:, :], QBs[2 * idx + m, :, :], dsKV[s], r=[rQBs], w=[rKV[s]])
                        for q4 in range(4):
                            b0, b1 = q4 * 16, (q4 + 1) * 16
                            DMA(VT[s][:, b0:b1, 0:128], VBs[kv, :, b0:b1, :], dsKV[s], r=[rVBs], w=[rKV[s]])
                        DMA(VT[s][0:16, 64:65, 0:128], VBs[kv, 0:16, 64:65, :], dsKV[s], r=[rVBs], w=[rKV[s]])

                CH = [(c * 512, 512, 4, 128) for c in range(4)] + [(2048, NMETA, 1, NMETA)]
                load_job(0)
                pt_i = [0]
                so_i = [0]
                for ji, (kind, idx) in enumerate(jobs):
                    s = ji % 2
                    if ji + 1 < len(jobs):
                        load_job(ji + 1)
                    isA = kind == "A"
                    KR = 68 if isA else 128
                    slope = 2.0 ** (-(idx + 1)) if isA else 0.0
                    scale = 0.125 if isA else 128.0 ** -0.5
                    Kt = [KT[s][0], KT[s][1] if isA else KT[s][0]]
                    Qt = QT[s]
                    for c, (q0, ncq, nsb, psq) in enumerate(CH):

                        started = set()

                        def qk(kb, sp):
                            kp = 128 if kb < 64 else NMETA
                            k0 = kb * 128
                            mixed = isA and c < 4 and (4 * c <= kb <= 4 * c + 3)
                            rows = 64 if mixed else KR
                            for m in range(2):
                                P(lambda e, m=m, sp=sp, kp=kp, k0=k0, rows=rows: e.matmul(
                                    Sps[sp][:kp, m * 512: m * 512 + ncq],
                                    lhsT=Kt[m][0:rows, k0:k0 + kp], rhs=Qt[m][0:rows, q0:q0 + ncq],
                                    start=True, stop=True),
                                  r=[rKV[s], rSig[s]], w=[rSps[sp]], inc=(m == 1))

                        def soft(kb, sp):
                            kp = 128 if kb < 64 else NMETA
                            mixed = isA and c < 4 and (4 * c <= kb <= 4 * c + 3)
                            pi_ = pt_i[0] % 3
                            pt_i[0] += 1
                            if ncq == 512:
                                src = Sps[sp][:kp, :]
                                dst = PT[pi_][:kp, :]
                            else:
                                src = Sps[sp][:kp, :].rearrange("p (m q) -> p m q", m=2)[:, :, 0:ncq]
                                dst = PT[pi_][:kp, :].rearrange("p (m q) -> p m q", m=2)[:, :, 0:ncq]
                            if mixed:
                                mm = kb - 4 * c
                                x0 = 384 - 128 * mm
                                for m in range(2):
                                    V(lambda e, m=m, sp=sp, x0=x0: e.scalar_tensor_tensor(
                                        out=smix[sp][:, m * 512:(m + 1) * 512], in0=dtab[:, x0:x0 + 512],
                                        scalar=-8.0 * slope, in1=Sps[sp][:, m * 512:(m + 1) * 512],
                                        op0=ALU.mult, op1=ALU.add),
                                      r=[rSps[sp], rC], w=[rSmix[sp]])
                                A(lambda e, sp=sp, dst=dst: e.activation(out=dst, in_=smix[sp][:, :], func=AF.Exp,
                                                                         scale=scale),
                                  r=[rSmix[sp]], w=[rPT[pi_]])
                            elif isA:
                                bcol = (idx * 5 + c) * NKB + kb
                                A(lambda e, src=src, dst=dst, bcol=bcol, kp=kp: e.activation(
                                    out=dst, in_=src, func=AF.Exp, bias=beta[:kp, bcol:bcol + 1], scale=scale),
                                  r=[rSps[sp], rC], w=[rPT[pi_]])
                            else:
                                A(lambda e, src=src, dst=dst: e.activation(out=dst, in_=src, func=AF.Exp,
                                                                           scale=scale),
                                  r=[rSps[sp]], w=[rPT[pi_]])
                            return pi_

                        def pv(kb, pi_, is_last, is_first):
                            kp = 128 if kb < 64 else NMETA
                            for m in range(2):
                                for sq in range(nsb):
                                    last = (m == 1 and sq == nsb - 1)
                                    bank_ = (m * 4 + sq) // 3
                                    st_ = is_first and (bank_ not in started)
                                    if is_first:
                                        started.add(bank_)
                                    P(lambda e, m=m, sq=sq, pi_=pi_, kp=kp, kb=kb, st_=st_: e.matmul(
                                        oacc(m, sq)[:psq, :],
                                        lhsT=PT[pi_][:kp, m * 512 + sq * 128: m * 512 + sq * 128 + psq],
                                        rhs=VT[s][:kp, kb, :], start=st_, stop=is_last,
                                        skip_group_check=True),
                                      r=[rPT[pi_], rKV[s], rSig[s]], w=[rOps], inc=last)

                        kbs = []
                        for kb in range(64):
                            if (not isA) or c == 4:
                                kbs.append(kb)
                                continue
                            lo_q, hi_q = 512 * c, 512 * c + 511
                            cands = [(128 * kb, 128 * kb + 127)]
                            if kb >= 16:
                                cands.append((128 * kb - 8192, 128 * kb + 127 - 8192))
                            dmin = min(max(0, ul - hi_q, lo_q - uh) for ul, uh in cands)
                            if slope * dmin <= 100.0:
                                kbs.append(kb)
                        kbs.append(64)
                        if c < 4:
                            qk(kbs[0], 0)
                            if len(kbs) > 1:
                                qk(kbs[1], 1)
                            for i, kb in enumerate(kbs):
                                pi_ = soft(kb, i % 2)
                                if i + 2 < len(kbs):
                                    qk(kbs[i + 2], i % 2)
                                pv(kb, pi_, i == len(kbs) - 1, i == 0)
                                if i == min(2, len(kbs) - 1):
                                    flush_ev()
                        else:
                            groups = [list(range(g * 8, g * 8 + 8)) for g in range(8)] + [[64]]

                            def qk4(grp, sp):
                                kp = 128 if grp[0] < 64 else NMETA
                                n = len(grp) * 2
                                for j, kb in enumerate(grp):
                                    for m in range(2):
                                        P(lambda e, j=j, m=m, kb=kb, kp=kp, sp=sp: e.matmul(
                                            Sps[sp][:kp, j * 32 + m * 16: j * 32 + m * 16 + 16],
                                            lhsT=Kt[m][0:KR, kb * 128: kb * 128 + kp], rhs=Qt[m][0:KR, q0:q0 + ncq],
                                            start=True, stop=True, skip_group_check=True),
                                          r=[rKV[s], rSig[s]], w=[rSps[sp]], inc=(j * 2 + m == n - 1))

                            def soft4(grp, sp):
                                kp = 128 if grp[0] < 64 else NMETA
                                w_ = len(grp) * 32
                                pi_ = pt_i[0] % 3
                                pt_i[0] += 1
                                A(lambda e, sp=sp, pi_=pi_, kp=kp, w_=w_: e.activation(
                                    out=PT[pi_][:kp, 0:w_], in_=Sps[sp][:kp, 0:w_], func=AF.Exp, scale=scale),
                                  r=[rSps[sp]], w=[rPT[pi_]])
                                return pi_

                            def pv4(grp, pi_, is_last, is_first):
                                kp = 128 if grp[0] < 64 else NMETA
                                n = len(grp) * 2
                                for j, kb in enumerate(grp):
                                    for m in range(2):
                                        bank_ = (m * 4) // 3
                                        st_ = is_first and (bank_ not in started)
                                        if is_first:
                                            started.add(bank_)
                                        P(lambda e, j=j, m=m, kb=kb, kp=kp, pi_=pi_, st_=st_: e.matmul(
                                            oacc(m, 0)[:psq, :],
                                            lhsT=PT[pi_][:kp, j * 32 + m * 16: j * 32 + m * 16 + 16],
                                            rhs=VT[s][:kp, kb, :], start=st_,
                                            stop=(is_last and j == len(grp) - 1), skip_group_check=True),
                                          r=[rPT[pi_], rKV[s], rSig[s]], w=[rOps], inc=(j * 2 + m == n - 1))

                            qk4(groups[0], 0)
                            qk4(groups[1], 1)
                            for i, grp in enumerate(groups):
                                pi_ = soft4(grp, i % 2)
                                if i + 2 < len(groups):
                                    qk4(groups[i + 2], i % 2)
                                pv4(grp, pi_, i == len(groups) - 1, i == 0)
                                if i == 2:
                                    flush_ev()
                        ob_i = osb_i[0] % 2
                        osb_i[0] += 1
                        rOsbC = rOsb[ob_i]
                        if nsb == 4:
                            V(lambda e, ob_i=ob_i: e.tensor_copy(
                                out=Osb[ob_i][:, 0:1024].rearrange("p (b c) -> p b c", b=2)[:, :, 0:387],
                                in_=Ops[:, 0:1024].rearrange("p (b c) -> p b c", b=2)[:, :, 0:387]),
                              r=[rOps], w=[rOsbC])
                            V(lambda e, ob_i=ob_i: e.tensor_copy(out=Osb[ob_i][:, 1024:1282], in_=Ops[:, 1024:1282]),
                              r=[rOps], w=[rOsbC])
                        else:
                            V(lambda e, ob_i=ob_i: e.tensor_copy(out=Osb[ob_i][:psq, 0:129], in_=Ops[:psq, 0:129]),
                              r=[rOps], w=[rOsbC])
                            V(lambda e, ob_i=ob_i: e.tensor_copy(out=Osb[ob_i][:psq, 641:770], in_=Ops[:psq, 641:770]),
                              r=[rOps], w=[rOsbC])

                        def osbv(m, sq_, n=129, ob_i=ob_i):
                            i = m * 4 + sq_
                            off = (i // 3) * 512 + (i % 3) * 129
                            return Osb[ob_i][:, off:off + n]
                        def evac_rest(isA=isA, idx=idx, q0=q0, ncq=ncq, nsb=nsb, psq=psq, rOsbC=rOsbC, osbv=osbv):
                            if isA:
                                so = so_i[0] % 2
                                so_i[0] += 1
                                for sq in range(nsb):
                                    O1, O2 = osbv(0, sq), osbv(1, sq)
                                    ddv = ev["dd4"][:psq, sq, :]
                                    V(lambda e, O1=O1: e.reciprocal(out=ev["r1"][:psq, 0:1], in_=O1[:psq, 128:129]),
                                      r=[rOsbC], w=[rEv])
                                    V(lambda e, O2=O2: e.reciprocal(out=ev["r1"][:psq, 1:2], in_=O2[:psq, 128:129]),
                                      r=[rOsbC], w=[rEv])
                                    V(lambda e: e.tensor_tensor(out=ev["r1"][:psq, 2:3], in0=ev["r1"][:psq, 1:2],
                                                                in1=lamneg[:psq, 0:1], op=ALU.mult),
                                      r=[rEv, rLam], w=[rEv])
                                    V(lambda e, O2=O2: e.tensor_scalar(out=ev["t2"][:psq, :], in0=O2[:psq, 0:128],
                                                                       scalar1=ev["r1"][:psq, 2:3], scalar2=None,
                                                                       op0=ALU.mult), r=[rOsbC, rEv], w=[rEv])
                                    V(lambda e, O1=O1, ddv=ddv: e.scalar_tensor_tensor(
                                        out=ddv, in0=O1[:psq, 0:128], scalar=ev["r1"][:psq, 0:1],
                                        in1=ev["t2"][:psq, :], op0=ALU.mult, op1=ALU.add), r=[rOsbC, rEv], w=[rEv])
                                    V(lambda e, ddv=ddv: e.tensor_tensor(out=ev["junk"][:psq, :], in0=ddv, in1=ddv,
                                                                         op=ALU.mult), r=[rEv], w=[rEv])
                                    V(lambda e, sq=sq: e.tensor_reduce(out=ev["ssd4"][:psq, sq:sq + 1],
                                                                       in_=ev["junk"][:psq, :], axis=AX.X, op=ALU.add),
                                      r=[rEv], w=[rEv])
                                rstd_from_ss(ev["ssd4"][:psq, 0:nsb], ev["lnd4"][:psq, 0:nsb], ev["rsd4"][:psq, 0:nsb],
                                             128.0, [rEv], [rEv])
                                for sq in range(nsb):
                                    V(lambda e, sq=sq, so=so: e.scalar_tensor_tensor(
                                        out=stgO[so][:psq, sq, :], in0=ev["dd4"][:psq, sq, :],
                                        scalar=ev["rsd4"][:psq, sq:sq + 1],
                                        in1=gsubb[:psq, :], op0=ALU.mult, op1=ALU.mult),
                                      r=[rEv, rC], w=[rStgO[so]])
                                dst_d = OAs[q0:q0 + ncq, idx * 128:(idx + 1) * 128].rearrange("(s p) e -> p s e", p=psq)
                                DMA(dst_d, stgO[so][:psq, 0:nsb, :], dsStgO[so], r=[rStgO[so]], w=[rOAs])
                            else:
                                for m in range(2):
                                    g = 2 * idx + m
                                    so = so_i[0] % 2
                                    so_i[0] += 1
                                    for sq in range(nsb):
                                        Om = osbv(m, sq)
                                        V(lambda e, Om=Om: e.reciprocal(out=ev["r1"][:psq, 0:1], in_=Om[:psq, 128:129]),
                                          r=[rOsbC], w=[rEv])
                                        V(lambda e, Om=Om, sq=sq, so=so: e.tensor_scalar(
                                            out=stgO[so][:psq, sq, :], in0=Om[:psq, 0:128], scalar1=ev["r1"][:psq, 0:1],
                                            scalar2=None, op0=ALU.mult), r=[rOsbC, rEv], w=[rStgO[so]])
                                    dst_d = OBs[q0:q0 + ncq, g * 128:(g + 1) * 128].rearrange("(s p) e -> p s e", p=psq)
                                    DMA(dst_d, stgO[so][:psq, 0:nsb, :], dsStgO[so], r=[rStgO[so]], w=[rOBs])
                        pend_ev.append(evac_rest)
                flush_ev()
                S.barrier()

        if stop_after >= 3:
            with ExitStack() as p3:
                H = sb(p3, "H", [128, 17, D], F32)
                rH = [Res(f"H{t}") for t in range(17)]
                u2T = sb(p3, "u2T", [128, 8, NOWN], BF16)
                rU2 = [Res(f"u2T{t}") for t in range(17)]
                AFF = sb(p3, "AFF", [128, 17, NEXP], F32)
                rAFF = Res("AFF")
                COEF = sb(p3, "COEF", [128, 17, NEXP], F32)
                rCOEF = Res("COEF")
                rAgin = Res("agin", True)
                gffnb = sb(p3, "gffnb", [128, D], F32)
                dsC3 = dsem("ds_c3")
                DMA(gffnb[:, :], gffn_d[0:1, :].partition_broadcast(128), dsC3, w=[rC])
                BLK = [(t * 128, 128) for t in range(16)] + [(2048, NMETA)]

                with ExitStack() as p3a:
                    Wa = sb(p3a, "Wa", [128, 8, D], BF16)
                    Wb = sb(p3a, "Wb", [128, 8, D], BF16)
                    Wo = sb(p3a, "Wo", [128, 8, D], BF16)
                    wr = sb(p3a, "wr", [128, 8, NEXP], BF16)
                    rW3 = Res("W3", True)
                    dsW3 = dsem("ds_w3")
                    for c in range(8):
                        DMA(Wa[:, c, :], wa_d.rearrange("(c p) f -> p c f", p=128)[:, c, :], dsW3, w=[rW3], q="pool")
                        DMA(Wb[:, c, :], wb_d.rearrange("(c p) f -> p c f", p=128)[:, c, :], dsW3, w=[rW3], q="pool")
                        DMA(Wo[:, c, :], wo_d.rearrange("(c p) f -> p c f", p=128)[:, c, :], dsW3, w=[rW3], q="pool")
                    DMA(wr[:, :, :], wr_d.rearrange("(c p) f -> p c f", p=128), dsW3, w=[rW3], q="pool")
                    oa_t = [sb(p3a, f"oa_t{i}", [128, D], BF16) for i in range(2)]
                    ob_t = [sb(p3a, f"ob_t{i}", [128, D], BF16) for i in range(2)]
                    g_t = [sb(p3a, f"g_t{i}", [128, 2048], BF16) for i in range(2)]
                    x_t3 = [sb(p3a, f"x_t3{i}", [128, D], F32) for i in range(2)]
                    rIn3 = [Res(f"in3_{i}", True) for i in range(2)]
                    dsIn3 = [dsem(f"ds_in3_{i}") for i in range(2)]
                    oaT = sb(p3a, "oaT", [128, 8, 128], BF16)
                    obT = sb(p3a, "obT", [128, 8, 128], BF16)
                    mgT = sb(p3a, "mgT", [128, 8, 128], BF16)
                    rOaT, rObT, rMgT = Res("oaT"), Res("obT"), Res("mgT")
                    m1 = sb(p3a, "m1", [128, D], F32)
                    m2 = sb(p3a, "m2", [128, D], BF16)
                    affS = [sb(p3a, f"affS{i}", [NEXP, 128], F32) for i in range(2)]
                    rAffS = [Res(f"affS{i}") for i in range(2)]
                    dsAffS = [dsem(f"ds_affs{i}") for i in range(2)]
                    mg = sb(p3a, "mg", [128, D], BF16)
                    rM1, rM2, rMg = Res("m1"), Res("m2"), Res("mg")
                    u2 = sb(p3a, "u2", [128, D], BF16)
                    rU2t = Res("u2")
                    st3 = {n: sb(p3a, "st3_" + n, [128, 1], F32) for n in ("ss", "ln", "rs", "se", "rse")}
                    ex3 = sb(p3a, "ex3", [128, NEXP], F32)
                    rSt3 = Res("st3")
                    tp3 = [psum(p3a, f"tp3_{i}", [128, 1024], BF16) for i in range(2)]
                    rTp3 = [Res(f"tp3_{i}", psum=True) for i in range(2)]
                    yps = [psum(p3a, f"yps{i}", [128, 512], F32) for i in range(4)]
                    rYps = [Res(f"yps{i}", psum=True) for i in range(4)]
                    lps = psum(p3a, "lps", [128, 512], F32)
                    rLps = Res("lps", psum=True)
                    tfp = psum(p3a, "tfp", [128, 512], F32)
                    rTfp = Res("tfp", psum=True)

                    def load3(t):
                        q0, pb_ = BLK[t]
                        sl = t % 2
                        tokx = q0 if t < 16 else SEQ
                        DMA(oa_t[sl][:pb_, :], OAs[q0:q0 + pb_, :], dsIn3[sl], r=[rOAs], w=[rIn3[sl]])
                        DMA(ob_t[sl][:pb_, :], OBs[q0:q0 + pb_, :], dsIn3[sl], r=[rOBs], w=[rIn3[sl]])
                        DMA(g_t[sl][:pb_, :], Gs[q0:q0 + pb_, :], dsIn3[sl], r=[rGs], w=[rIn3[sl]])
                        DMA(x_t3[sl][:pb_, :], hx[tokx:tokx + pb_, :], dsIn3[sl], w=[rIn3[sl]])

                    def transpose8(src, rsrc, dstT, rdst, pb_, tpi):
                        for fc in range(8):
                            P(lambda e, fc=fc: e.transpose(out=tp3[tpi][:, fc * 128: fc * 128 + pb_],
                                                           in_=src[:pb_, fc * 128:(fc + 1) * 128],
                                                           identity=identb[:pb_, :pb_]),
                              r=[rsrc, rC], w=[rTp3[tpi]], inc=(fc == 7))

                    load3(0)
                    for t, (q0, pb_) in enumerate(BLK):
                        sl = t % 2
                        if t + 1 < len(BLK):
                            load3(t + 1)
                        transpose8(oa_t[sl], rIn3[sl], oaT, rOaT, pb_, 0)
                        V(lambda e: e.tensor_copy(out=oaT[:, :, :pb_],
                                                  in_=tp3[0][:, :].rearrange("p (c t) -> p c t", c=8)[:, :, :pb_]),
                          r=[rTp3[0]], w=[rOaT])
                        transpose8(ob_t[sl], rIn3[sl], obT, rObT, pb_, 1)
                        A(lambda e: e.activation(out=obT[:, :, :pb_],
                                                 in_=tp3[1][:, :].rearrange("p (c t) -> p c t", c=8)[:, :, :pb_],
                                                 func=AF.Copy),
                          r=[rTp3[1]], w=[rObT])
                        for g in range(2):
                            for fc in range(8):
                                P(lambda e, g=g, fc=fc: e.matmul(yps[g][:pb_, :], lhsT=oaT[:, fc, :pb_],
                                                                 rhs=Wa[:, fc, g * 512:(g + 1) * 512],
                                                                 start=(fc == 0), stop=(fc == 7)),
                                  r=[rOaT, rW3], w=[rYps[g]], inc=(fc == 7))
                        for g in range(2):
                            for fc in range(8):
                                P(lambda e, g=g, fc=fc: e.matmul(yps[2 + g][:pb_, :], lhsT=obT[:, fc, :pb_],
                                                                 rhs=Wb[:, fc, g * 512:(g + 1) * 512],
                                                                 start=(fc == 0), stop=(fc == 7)),
                                  r=[rObT, rW3], w=[rYps[2 + g]], inc=(fc == 7))
                        for g in range(2):
                            V(lambda e, g=g: e.tensor_tensor(out=m1[:pb_, g * 512:(g + 1) * 512], in0=yps[g][:pb_, :],
                                                             in1=g_t[sl][:pb_, g * 512:(g + 1) * 512], op=ALU.mult),
                              r=[rYps[g], rIn3[sl]], w=[rM1])
                            V(lambda e, g=g: e.tensor_tensor(out=m2[:pb_, g * 512:(g + 1) * 512],
                                                             in0=yps[2 + g][:pb_, :],
                                                             in1=g_t[sl][:pb_, 1024 + g * 512:1024 + (g + 1) * 512],
                                                             op=ALU.mult),
                              r=[rYps[2 + g], rIn3[sl]], w=[rM2])
                        V(lambda e: e.tensor_tensor(out=mg[:pb_, :], in0=m1[:pb_, :], in1=m2[:pb_, :], op=ALU.add),
                          r=[rM1, rM2], w=[rMg])
                        transpose8(mg, rMg, mgT, rMgT, pb_, 0)
                        V(lambda e: e.tensor_copy(out=mgT[:, :, :pb_],
                                                  in_=tp3[0][:, :].rearrange("p (c t) -> p c t", c=8)[:, :, :pb_]),
                          r=[rTp3[0]], w=[rMgT])
                        for g in range(2):
                            for fc in range(8):
                                P(lambda e, g=g, fc=fc: e.matmul(yps[g][:pb_, :], lhsT=mgT[:, fc, :pb_],
                                                                 rhs=Wo[:, fc, g * 512:(g + 1) * 512],
                                                                 start=(fc == 0), stop=(fc == 7)),
                                  r=[rMgT, rW3], w=[rYps[g]], inc=(fc == 7))
                        for g in range(2):
                            V(lambda e, g=g, t=t: e.tensor_tensor(out=H[:pb_, t, g * 512:(g + 1) * 512],
                                                                  in0=yps[g][:pb_, :],
                                                                  in1=x_t3[sl][:pb_, g * 512:(g + 1) * 512],
                                                                  op=ALU.add),
                              r=[rYps[g], rIn3[sl]], w=[rH[t]])
                        if dbg:
                            DMA(dbg_h1[q0:q0 + pb_, :], H[:pb_, t, :], dsDbg, r=[rH[t]])
                        A(lambda e, t=t: e.activation(out=u2[:pb_, :], in_=H[:pb_, t, :], func=AF.Square,
                                                      accum_out=st3["ss"][:pb_, 0:1]), r=[rH[t]], w=[rSt3, rU2t])
                        rstd_from_ss(st3["ss"][:pb_, 0:1], st3["ln"][:pb_, 0:1], st3["rs"][:pb_, 0:1], float(D),
                                     [rSt3], [rSt3])
                        V(lambda e, t=t: e.scalar_tensor_tensor(out=u2[:pb_, :], in0=H[:pb_, t, :],
                                                                scalar=st3["rs"][:pb_, 0:1], in1=gffnb[:pb_, :],
                                                                op0=ALU.mult, op1=ALU.mult),
                          r=[rH[t], rSt3, rC], w=[rU2t])
                        transpose8(u2, rU2t, None, None, pb_, 1)
                        A(lambda e, q0=q0: e.activation(
                            out=u2T[:, :, q0:q0 + pb_],
                            in_=tp3[1][:, :].rearrange("p (c t) -> p c t", c=8)[:, :, :pb_], func=AF.Copy),
                          r=[rTp3[1]], w=[rU2[t]])
                        for fc in range(8):
                            P(lambda e, fc=fc, q0=q0: e.matmul(lps[:pb_, 0:NEXP], lhsT=u2T[:, fc, q0:q0 + pb_],
                                                               rhs=wr[:, fc, :], start=(fc == 0), stop=(fc == 7)),
                              r=[rU2[t], rW3], w=[rLps], inc=(fc == 7))
                        A(lambda e: e.activation(out=ex3[:pb_, :], in_=lps[:pb_, 0:NEXP], func=AF.Exp,
                                                 accum_out=st3["se"][:pb_, 0:1]), r=[rLps], w=[rSt3])
                        V(lambda e: e.reciprocal(out=st3["rse"][:pb_, 0:1], in_=st3["se"][:pb_, 0:1]),
                          r=[rSt3], w=[rSt3])
                        V(lambda e, t=t: e.tensor_scalar(out=AFF[:pb_, t, :], in0=ex3[:pb_, :],
                                                         scalar1=st3["rse"][:pb_, 0:1], scalar2=None, op0=ALU.mult),
                          r=[rSt3], w=[rAFF])
                        P(lambda e, t=t: e.transpose(out=tfp[0:NEXP, 0:pb_], in_=AFF[:pb_, t, :],
                                                     identity=identf[:pb_, :pb_]), r=[rAFF, rC], w=[rTfp])
                        V(lambda e, sl=sl: e.tensor_copy(out=affS[sl][:, 0:pb_], in_=tfp[0:NEXP, 0:pb_]),
                          r=[rTfp], w=[rAffS[sl]])
                        DMA(agin.ap()[:, q0:q0 + pb_], affS[sl][:, 0:pb_], dsAffS[sl], r=[rAffS[sl]], w=[rAgin])
                        if dbg:
                            DMA(dbg_aff[q0:q0 + pb_, :], AFF[:pb_, t, :], dsDbg, r=[rAFF])
                    S.barrier()

                if stop_after >= 4:
                    with ExitStack() as p4:
                        AGc = sb(p4, "AGc", [NEXP, SEQ + NMETA], F32)
                        rAG = Res("AGc", True)
                        dsAG = dsem("ds_ag")
                        dsCC = DSem(sem("cc_sem"))
                        rAgout = Res("agout")
                        S.coll(lambda e: e.collective_compute(
                            "AllGather", ALU.bypass, replica_groups=[[0, 1, 2, 3], [4, 5, 6, 7]],
                            ins=[agin.ap().opt()], outs=[agout.ap().opt()]), dsCC, reads=[rAgin], writes=[rAgout])
                        ago = agout.ap()
                        DMA(AGc[:, 0:SEQ].rearrange("e (r t) -> e r t", r=4),
                            ago.rearrange("(r e) t -> e r t", e=NEXP)[:, :, 0:2048], dsAG, r=[rAgout], w=[rAG],
                            q="pool")
                        DMA(AGc[:, SEQ:SEQ + NMETA], ago[0:NEXP, 2048:2048 + NMETA], dsAG, r=[rAgout], w=[rAG],
                            q="pool")
                        lo = sb(p4, "lo", [NEXP, 1], F32)
                        mid = sb(p4, "mid", [NEXP, 1], F32)
                        cnt = sb(p4, "cnt", [NEXP, 1], F32)
                        prd = sb(p4, "prd", [NEXP, 1], F32)
                        cmpj = sb(p4, "cmpj", [NEXP, SEQ + NMETA], BF16)
                        rB = Res("bis")
                        V(lambda e: e.memset(lo[:, :], 0.0), w=[rB])
                        for it in range(NBIS):
                            ck = 2.0 ** (-(it + 1))
                            V(lambda e, ck=ck: e.tensor_scalar(out=mid[:, :], in0=lo[:, :], scalar1=ck, scalar2=None,
                                                               op0=ALU.add), r=[rB], w=[rB])
                            V(lambda e: e.tensor_scalar(out=cmpj[:, :], in0=AGc[:, :], scalar1=mid[:, 0:1],
                                                        scalar2=0.0, op0=ALU.is_ge, op1=ALU.add,
                                                        accum_out=cnt[:, 0:1]), r=[rB, rAG], w=[rB])
                            V(lambda e, ck=ck: e.tensor_scalar(out=prd[:, :], in0=cnt[:, :], scalar1=CAP - 0.5,
                                                               scalar2=ck, op0=ALU.is_ge, op1=ALU.mult),
                              r=[rB], w=[rB])
                            V(lambda e: e.tensor_tensor(out=lo[:, :], in0=lo[:, :], in1=prd[:, :], op=ALU.add),
                              r=[rB], w=[rB])
                        rThrD = Res("thr_d")
                        DMA(thr_d.rearrange("o e -> e o"), lo[:, :], dsAG, r=[rB], w=[rThrD])
                        if dbg:
                            DMA(dbg_thr.rearrange("o e -> e o"), lo[:, :], dsDbg, r=[rB])
                        THR = sb(p4, "THR", [128, NEXP], F32)
                        rTHR = Res("THR")
                        DMA(THR[:, :], thr_d[0:1, :].partition_broadcast(128), dsAG, r=[rThrD], w=[rTHR])
                        for t in range(16):
                            V(lambda e, t=t: e.tensor_tensor(out=COEF[:, t, :], in0=AFF[:, t, :], in1=THR[:, :],
                                                             op=ALU.is_ge), r=[rAFF, rTHR], w=[rCOEF])
                            V(lambda e, t=t: e.tensor_tensor(out=COEF[:, t, :], in0=COEF[:, t, :], in1=AFF[:, t, :],
                                                             op=ALU.mult), r=[rAFF, rCOEF], w=[rCOEF])
                        S.barrier()

                if stop_after >= 5:
                    with ExitStack() as p5:
                        wg = [sb(p5, f"wg{i}", [128, 8, 512], BF16) for i in range(2)]
                        wu = [sb(p5, f"wu{i}", [128, 8, 512], BF16) for i in range(2)]
                        wd = [sb(p5, f"wd{i}", [128, 4, D], BF16) for i in range(2)]
                        rWe = [Res(f"We{i}", True) for i in range(2)]
                        dsWe = [dsem(f"ds_we{i}") for i in range(2)]
                        hT = [sb(p5, f"hT{i}", [128, 4, 512], BF16) for i in range(2)]
                        rHT = [Res(f"hT{i}") for i in range(2)]
                        sg = [sb(p5, f"sg{i}", [128, 512], F32) for i in range(2)]
                        rSg = [Res(f"sg{i}") for i in range(2)]
                        gps = [psum(p5, f"gps{i}", [128, 512], F32) for i in range(2)]
                        ups = [psum(p5, f"ups{i}", [128, 512], F32) for i in range(2)]
                        rGps = [Res(f"gps{i}", psum=True) for i in range(2)]
                        rUps = [Res(f"ups{i}", psum=True) for i in range(2)]
                        ypm = [psum(p5, f"ypm{i}", [128, 512], F32) for i in range(4)]
                        rYpm = [Res(f"ypm{i}", psum=True) for i in range(4)]

                        def load_e(ei):
                            sl = ei % 2
                            for c in range(8):
                                DMA(wg[sl][:, c, :], wg_d[ei, c * 128:(c + 1) * 128, :], dsWe[sl], w=[rWe[sl]], q="pool")
                                DMA(wu[sl][:, c, :], wu_d[ei, c * 128:(c + 1) * 128, :], dsWe[sl], w=[rWe[sl]], q="pool")
                            for c in range(4):
                                DMA(wd[sl][:, c, :], wd_d[ei, c * 128:(c + 1) * 128, :], dsWe[sl], w=[rWe[sl]], q="pool")

                        load_e(0)
                        gi = [0]
                        yi = [0]
                        for ei in range(NEXP):
                            sl = ei % 2
                            if ei + 1 < NEXP:
                                load_e(ei + 1)
                            for tcn in range(4):
                                hs = (ei * 4 + tcn) % 2
                                rUs = [rU2[tcn * 4 + k] for k in range(4)]
                                for fc in range(4):
                                    gs = gi[0] % 2
                                    gi[0] += 1
                                    for dc in range(8):
                                        P(lambda e, gs=gs, dc=dc, fc=fc, tcn=tcn: e.matmul(
                                            gps[gs][:, :], lhsT=wg[sl][:, dc, fc * 128:(fc + 1) * 128],
                                            rhs=u2T[:, dc, tcn * 512:(tcn + 1) * 512], start=(dc == 0), stop=(dc == 7)),
                                          r=[rWe[sl]] + rUs, w=[rGps[gs]], inc=(dc == 7))
                                    for dc in range(8):
                                        P(lambda e, gs=gs, dc=dc, fc=fc, tcn=tcn: e.matmul(
                                            ups[gs][:, :], lhsT=wu[sl][:, dc, fc * 128:(fc + 1) * 128],
                                            rhs=u2T[:, dc, tcn * 512:(tcn + 1) * 512], start=(dc == 0), stop=(dc == 7)),
                                          r=[rWe[sl]] + rUs, w=[rUps[gs]], inc=(dc == 7))
                                    A(lambda e, gs=gs: e.activation(out=sg[gs][:, :], in_=gps[gs][:, :], func=AF.Silu),
                                      r=[rGps[gs]], w=[rSg[gs]])
                                    V(lambda e, gs=gs, fc=fc, hs=hs: e.tensor_tensor(
                                        out=hT[hs][:, fc, :], in0=ups[gs][:, :], in1=sg[gs][:, :], op=ALU.mult),
                                      r=[rUps[gs], rSg[gs]], w=[rHT[hs]])
                                for ts in range(4):
                                    t = tcn * 4 + ts
                                    for dh in range(2):
                                        ys = yi[0] % 4
                                        yi[0] += 1
                                        for fc in range(4):
                                            P(lambda e, ys=ys, fc=fc, ts=ts, dh=dh, hs=hs: e.matmul(
                                                ypm[ys][:, :], lhsT=hT[hs][:, fc, ts * 128:(ts + 1) * 128],
                                                rhs=wd[sl][:, fc, dh * 512:(dh + 1) * 512],
                                                start=(fc == 0), stop=(fc == 3)),
                                              r=[rHT[hs], rWe[sl]], w=[rYpm[ys]], inc=(fc == 3))
                                        V(lambda e, ys=ys, t=t, dh=dh, ei=ei: e.scalar_tensor_tensor(
                                            out=H[:, t, dh * 512:(dh + 1) * 512], in0=ypm[ys][:, :],
                                            scalar=COEF[:, t, ei:ei + 1], in1=H[:, t, dh * 512:(dh + 1) * 512],
                                            op0=ALU.mult, op1=ALU.add),
                                          r=[rYpm[ys], rCOEF, rH[t]], w=[rH[t]])
                        S.barrier()

                with ExitStack() as p6:
                    gfinb = sb(p6, "gfinb", [128, D], F32)
                    dsC6 = dsem("ds_c6")
                    DMA(gfinb[:, :], gfin_d[0:1, :].partition_broadcast(128), dsC6, w=[rC])
                    fo = [sb(p6, f"fo{i}", [128, D], F32) for i in range(2)]
                    rFo = [Res(f"fo{i}") for i in range(2)]
                    dsFo = [dsem(f"ds_fo{i}") for i in range(2)]
                    fj = sb(p6, "fj", [128, D], BF16)
                    fst = {n: sb(p6, "fst_" + n, [128, 1], F32) for n in ("ss", "ln", "rs")}
                    rFst = Res("fst")
                    for t in range(16):
                        sl = t % 2
                        A(lambda e, t=t: e.activation(out=fj[:, :], in_=H[:, t, :], func=AF.Square,
                                                      accum_out=fst["ss"][:, 0:1]), r=[rH[t]], w=[rFst])
                        rstd_from_ss(fst["ss"][:, 0:1], fst["ln"][:, 0:1], fst["rs"][:, 0:1], float(D),
                                     [rFst], [rFst])
                        V(lambda e, t=t, sl=sl: e.scalar_tensor_tensor(
                            out=fo[sl][:, :], in0=H[:, t, :], scalar=fst["rs"][:, 0:1], in1=gfinb[:, :],
                            op0=ALU.mult, op1=ALU.mult), r=[rH[t], rFst, rC], w=[rFo[sl]])
                        DMA(y_out[t * 128:(t + 1) * 128, :], fo[sl][:, :], dsFo[sl], r=[rFo[sl]])
                    S.barrier()
        else:
            with ExitStack() as pz:
                zt = sb(pz, "zt", [128, D], F32)
                rZ = Res("zt")
                dsZ = dsem("ds_z")
                V(lambda e: e.memset(zt[:, :], 0.0), w=[rZ])
                for t in range(16):
                    DMA(y_out[t * 128:(t + 1) * 128, :], zt[:, :], dsZ, r=[rZ])
                S.barrier()

        S.barrier()

        @block.tensor
        def _(eng):
            S.emit("pe", eng)

        @block.scalar
        def _(eng):
            S.emit("act", eng)

        @block.vector
        def _(eng):
            S.emit("dve", eng)

        @block.gpsimd
        def _(eng):
            S.emit("pool", eng)

        @block.sync
        def _(eng):
            S.emit("sp", eng)

    return nc


def _tables(r):
    bf = ml_dtypes.bfloat16
    jp = np.arange(SEQ)
    uj = np.where(jp + 2048 * r < SEQ, jp, jp - SEQ).astype(np.float64)
    sig = np.zeros((4, NT), np.float32)
    beta = np.zeros((128, 8, 5, NKB), np.float32)
    for c in range(4):
        before = uj < 512 * c
        sgn = np.where(before, 1.0, -1.0)
        sig[c, :SEQ] = sgn
        for h in range(8):
            slope = 2.0 ** (-(h + 1))
            b = sgn * slope * (uj - 512 * c - 256)
            beta[:, h, c, :64] = b.reshape(64, 128).T
    qaug = np.zeros((8, 4, NOWN), np.float32)
    a = np.arange(512) - 256
    for h in range(8):
        slope = 2.0 ** (-(h + 1))
        for c in range(4):
            qaug[h, c, c * 512:(c + 1) * 512] = -8.0 * slope * a
    x = np.arange(896)[None, :]
    bb = np.arange(128)[:, None]
    dtab = np.abs(x - bb - 384).astype(np.float32)
    t_true = (jp + 2048 * r) % SEQ
    row_id = (t_true // 64).astype(np.float32)
    col_id = (t_true % 64).astype(np.float32)
    inv_freq = (np.float32(10000.0) ** (-np.arange(0, 64, 2, dtype=np.float32) / np.float32(64))).astype(np.float32)
    ar = (row_id[:, None] * inv_freq[None, :]).astype(np.float32)
    ac = (col_id[:, None] * inv_freq[None, :]).astype(np.float32)
    rope = np.zeros((NT, 256), np.float32)
    rope[:, 0:128] = 1.0
    cr, sr, cc, sc = np.cos(ar), np.sin(ar), np.cos(ac), np.sin(ac)
    rope[:SEQ, 0:32] = cr
    rope[:SEQ, 32:64] = cr
    rope[:SEQ, 64:96] = cc
    rope[:SEQ, 96:128] = cc
    rope[:SEQ, 128:160] = -sr
    rope[:SEQ, 160:192] = sr
    rope[:SEQ, 192:224] = -sc
    rope[:SEQ, 224:256] = sc
    return dict(sigk=sig.astype(bf), qaug=qaug.astype(bf),
                beta=np.ascontiguousarray(beta.reshape(128, -1)), dtab=dtab, rope=rope)


def make_in_maps(inputs):
    bf = ml_dtypes.bfloat16
    x = np.asarray(inputs["x"], np.float32)
    meta = np.asarray(inputs["meta_tokens"], np.float32)
    f = lambda k: np.ascontiguousarray(np.asarray(inputs[k], np.float32))
    common = {
        "w_in": f("w_in")[0],
        "gmixT": np.ascontiguousarray(f("g_mix")[0].reshape(8, 128).T),
        "lamv": np.ascontiguousarray(np.stack([f("lambda_q1")[0], f("lambda_k1")[0],
                                               f("lambda_q2")[0], f("lambda_k2")[0]])),
        "g_subln": f("g_subln"), "g_qnorm": f("g_qnorm"), "g_knorm": f("g_knorm"),
        "w_branch_a": f("w_branch_a")[0], "w_branch_b": f("w_branch_b")[0], "w_out": f("w_out")[0],
        "g_ffn": f("g_ffn"), "w_router": f("w_router")[0],
        "w_gate": f("w_gate")[0], "w_up": f("w_up")[0], "w_down": f("w_down")[0],
        "g_final": f("g_final").reshape(1, D),
        "identb": np.eye(128, dtype=np.float32).astype(bf),
        "identf": np.eye(128, dtype=np.float32),
    }
    tabs = [_tables(r) for r in range(4)]
    maps = []
    for c in range(8):
        b, r = c // 4, c % 4
        hxv = np.concatenate([np.roll(x[b], -2048 * r, axis=0), meta], axis=0)
        m = dict(common)
        m.update(tabs[r])
        m["hx"] = np.ascontiguousarray(hxv)
        maps.append(m)
    return maps


_NC_CACHE = {}


def kernel(**inputs):
    if "nc" not in _NC_CACHE:
        _NC_CACHE["nc"] = build()
    nc = _NC_CACHE["nc"]
    maps = make_in_maps(inputs)
    res = run_bass_kernel_spmd(nc, maps, core_ids=list(range(8)))
    out = np.zeros((2, SEQ, D), np.float32)
    for c in range(8):
        b, r = c // 4, c % 4
        out[b, r * 2048:(r + 1) * 2048, :] = res.results[c]["y"]
    return out
```
